# Optimizing a Trainium2 kernel written in Bass

```python
import jax, jax.numpy as jnp
from jax import lax
import numpy as np

D_MODEL = 2048
BATCH = 8
SEQ = 4096
DEPTH = 1

CHUNK = 64
EPS = 1e-6
D_MIX = D_MODEL
GMLP_BLOCK = 128
GMLP_WIDTH = D_MIX // 2
GMLP_GROUPS = 8
GMLP_GROUP_DIM = GMLP_WIDTH // GMLP_GROUPS
ATT_WIDTH = D_MIX - GMLP_WIDTH
N_HEADS = 16
HEAD_DIM = ATT_WIDTH // N_HEADS
KV_LATENT = 256
IDX_HEADS = 8
IDX_DIM = 64
TOPK_MAX = 256
Q_BLOCK = 128
ROPE_THETA = 10000.0
IN_SPLIT_SIZES = (GMLP_WIDTH, GMLP_WIDTH, ATT_WIDTH, KV_LATENT, IDX_HEADS * IDX_DIM, IDX_DIM, IDX_HEADS)
IN_COLS = sum(IN_SPLIT_SIZES)
N_EXPERT_GROUPS = 4
EXPERTS_PER_GROUP = 8
N_EXPERTS = N_EXPERT_GROUPS * EXPERTS_PER_GROUP
TOP_K_IN_GROUP = 2
D_EXPERT = 1024
MOE_BLOCK = 128

kernel_name = "hybrid_gmlp_dsa_hmoe_block"


def rms_norm(x, g):
    xf = x.astype(jnp.float32)
    y = xf * lax.rsqrt(jnp.mean(xf * xf, axis=-1, keepdims=True) + EPS)
    return (y * g).astype(x.dtype)


def layer_norm(x, g, b):
    xf = x.astype(jnp.float32)
    mu = jnp.mean(xf, axis=-1, keepdims=True)
    var = jnp.mean(jnp.square(xf - mu), axis=-1, keepdims=True)
    return ((xf - mu) * lax.rsqrt(var + EPS) * g + b).astype(x.dtype)


def rope(x, pos):
    half = x.shape[-1] // 2
    freqs = ROPE_THETA ** (-jnp.arange(half, dtype=jnp.float32) / half)
    ang = pos.astype(jnp.float32)[..., None] * freqs
    cos = jnp.cos(ang)[:, :, None, :]
    sin = jnp.sin(ang)[:, :, None, :]
    x1, x2 = x[..., :half], x[..., half:]
    return jnp.concatenate([x1 * cos - x2 * sin, x1 * sin + x2 * cos], axis=-1).astype(x.dtype)


def gmlp_spatial_gate(u, v, v_norm_g, v_norm_b, w_sp, b_sp):
    B, S, _ = u.shape
    nb = S // GMLP_BLOCK
    v = v.reshape(B, nb, GMLP_BLOCK, GMLP_GROUPS, GMLP_GROUP_DIM)
    v = layer_norm(v, v_norm_g, v_norm_b)
    pos = jnp.arange(GMLP_BLOCK)
    mask = (pos[:, None] // CHUNK) >= (pos[None, :] // CHUNK)
    w = jnp.where(mask[None], w_sp, jnp.zeros_like(w_sp))
    z = jnp.einsum('gij,bnjgc->bnigc', w, v) + b_sp.T[None, None, :, :, None]
    return u * z.reshape(B, S, GMLP_WIDTH)


def dsa_attention(q, c_kv, q_idx, k_idx, w_idx, positions, kv_norm_g, w_uk, w_uv, kidx_norm_g):
    B, S = q.shape[0], q.shape[1]
    top_k = min(TOPK_MAX, S // 4)
    c_kv = rms_norm(c_kv, kv_norm_g)
    k = rope((c_kv @ w_uk)[:, :, None, :], positions)[:, :, 0, :]
    v = c_kv @ w_uv
    q = rope(q, positions)
    q_idx = rope(q_idx, positions)
    k_idx = rope(rms_norm(k_idx, kidx_norm_g)[:, :, None, :], positions)[:, :, 0, :]
    w_idx = w_idx.astype(jnp.float32) * (IDX_HEADS ** -0.5) * (IDX_DIM ** -0.5)
    key_chunk = jnp.arange(S) // CHUNK

    def query_block(qb):
        start = qb * Q_BLOCK
        q_blk = lax.dynamic_slice_in_dim(q, start, Q_BLOCK, axis=1)
        qi_blk = lax.dynamic_slice_in_dim(q_idx, start, Q_BLOCK, axis=1)
        wi_blk = lax.dynamic_slice_in_dim(w_idx, start, Q_BLOCK, axis=1)
        q_chunk = (start + jnp.arange(Q_BLOCK)) // CHUNK
        admissible = key_chunk[None, :] <= q_chunk[:, None]
        idx_logits = jnp.einsum('bqhd,bsd->bqhs', qi_blk, k_idx).astype(jnp.float32)
        score = jnp.einsum('bqh,bqhs->bqs', wi_blk, jax.nn.relu(idx_logits))
        score = jnp.where(admissible[None], score, -jnp.inf)
        top_val, top_idx = lax.top_k(score, top_k)
        valid = jnp.isfinite(top_val)
        k_sel = jax.vmap(lambda kb, ib: kb[ib])(k, top_idx)
        v_sel = jax.vmap(lambda vb, ib: vb[ib])(v, top_idx)
        s = jnp.einsum('bqhd,bqkd->bqhk', q_blk, k_sel).astype(jnp.float32) * (HEAD_DIM ** -0.5)
        s = jnp.where(valid[:, :, None, :], s, -jnp.inf)
        p = jax.nn.softmax(s, axis=-1).astype(v.dtype)
        return jnp.einsum('bqhk,bqkd->bqhd', p, v_sel)

    out = lax.map(query_block, jnp.arange(S // Q_BLOCK))
    return out.transpose(1, 0, 2, 3, 4).reshape(B, S, ATT_WIDTH)


def hierarchical_moe(h, w_group, b_group, w_expert, b_expert, w1, w3, w2):
    T, D = h.shape
    hf = h.astype(jnp.float32)
    g_prob = jax.nn.softmax(hf @ w_group + b_group, axis=-1)
    g_p, g_idx = lax.top_k(g_prob, 1)
    e_logits = (hf @ w_expert + b_expert).reshape(T, N_EXPERT_GROUPS, EXPERTS_PER_GROUP)
    e_logits = jnp.take_along_axis(e_logits, g_idx[:, :, None], axis=1)[:, 0]
    e_prob = jax.nn.softmax(e_logits, axis=-1)
    e_p, e_idx = lax.top_k(e_prob, TOP_K_IN_GROUP)
    e_p = e_p / jnp.sum(e_p, axis=-1, keepdims=True)
    weights = g_p * e_p
    experts = g_idx * EXPERTS_PER_GROUP + e_idx

    M = T * TOP_K_IN_GROUP
    e_flat = experts.reshape(M)
    w_flat = weights.reshape(M).astype(h.dtype)
    tok_flat = jnp.arange(M, dtype=jnp.int32) // TOP_K_IN_GROUP
    order = jnp.argsort(e_flat)
    e_sorted = e_flat[order]
    counts = jnp.zeros((N_EXPERTS,), jnp.int32).at[e_flat].add(1)
    starts = jnp.cumsum(counts) - counts
    padded = (counts + MOE_BLOCK - 1) // MOE_BLOCK * MOE_BLOCK
    pends = jnp.cumsum(padded)
    pstarts = pends - padded
    dest = pstarts[e_sorted] + (jnp.arange(M, dtype=jnp.int32) - starts[e_sorted])
    n_blocks = M // MOE_BLOCK + N_EXPERTS
    n_slots = n_blocks * MOE_BLOCK
    slot_tok = jnp.zeros((n_slots,), jnp.int32).at[dest].set(tok_flat[order])
    slot_w = jnp.zeros((n_slots,), h.dtype).at[dest].set(w_flat[order])
    block_start = jnp.arange(n_blocks, dtype=jnp.int32) * MOE_BLOCK
    block_expert = jnp.minimum(jnp.searchsorted(pends, block_start, side='right'), N_EXPERTS - 1)

    def expert_block(args):
        e, toks, wts = args
        xb = h[toks]
        y = (jax.nn.silu(xb @ w1[e]) * (xb @ w3[e])) @ w2[e]
        return y * wts[:, None]

    ys = lax.map(expert_block, (block_expert,
                                slot_tok.reshape(n_blocks, MOE_BLOCK),
                                slot_w.reshape(n_blocks, MOE_BLOCK)))
    return jnp.zeros_like(h).at[slot_tok].add(ys.reshape(n_slots, D))


def setup_inputs(seed: int = 0) -> dict:
    key = jax.random.key(seed)
    ks = jax.random.split(key, 32)

    def nrm(k, shape, scale):
        return jax.random.normal(k, shape, jnp.float32) * scale

    L = DEPTH
    offsets = jax.random.randint(ks[2], (BATCH, 1), 0, 64, dtype=jnp.int32) * CHUNK
    return {
        "x": nrm(ks[0], (BATCH, SEQ, D_MODEL), 1.0),
        "c": nrm(ks[1], (BATCH, D_MODEL), 1.0),
        "positions": offsets + jnp.arange(SEQ, dtype=jnp.int32)[None, :],
        "w_ada": nrm(ks[3], (L, D_MODEL, 6 * D_MODEL), 0.5 * D_MODEL ** -0.5),
        "b_ada": nrm(ks[4], (L, 6 * D_MODEL), 0.02),
        "norm1_g": 1.0 + nrm(ks[5], (L, D_MODEL), 0.02),
        "w_in": nrm(ks[6], (L, D_MODEL, IN_COLS), D_MODEL ** -0.5),
        "v_norm_g": 1.0 + nrm(ks[7], (L, GMLP_GROUPS, GMLP_GROUP_DIM), 0.02),
        "v_norm_b": nrm(ks[8], (L, GMLP_GROUPS, GMLP_GROUP_DIM), 0.02),
        "w_sp": nrm(ks[9], (L, GMLP_GROUPS, GMLP_BLOCK, GMLP_BLOCK), GMLP_BLOCK ** -0.5),
        "b_sp": 1.0 + nrm(ks[10], (L, GMLP_GROUPS, GMLP_BLOCK), 0.02),
        "kv_norm_g": 1.0 + nrm(ks[11], (L, KV_LATENT), 0.02),
        "w_uk": nrm(ks[12], (L, KV_LATENT, HEAD_DIM), KV_LATENT ** -0.5),
        "w_uv": nrm(ks[13], (L, KV_LATENT, HEAD_DIM), KV_LATENT ** -0.5),
        "kidx_norm_g": 1.0 + nrm(ks[14], (L, IDX_DIM), 0.02),
        "gnorm_a_g": 1.0 + nrm(ks[15], (L, GMLP_WIDTH), 0.02),
        "gnorm_b_g": 1.0 + nrm(ks[16], (L, ATT_WIDTH), 0.02),
        "w_out": nrm(ks[17], (L, D_MIX, D_MODEL), D_MIX ** -0.5),
        "norm2_g": 1.0 + nrm(ks[18], (L, D_MODEL), 0.02),
        "w_group": nrm(ks[19], (L, D_MODEL, N_EXPERT_GROUPS), D_MODEL ** -0.5),
        "b_group": nrm(ks[20], (L, N_EXPERT_GROUPS), 0.01),
        "w_expert": nrm(ks[21], (L, D_MODEL, N_EXPERTS), D_MODEL ** -0.5),
        "b_expert": nrm(ks[22], (L, N_EXPERTS), 0.01),
        "w1": nrm(ks[23], (L, N_EXPERTS, D_MODEL, D_EXPERT), D_MODEL ** -0.5),
        "w3": nrm(ks[24], (L, N_EXPERTS, D_MODEL, D_EXPERT), D_MODEL ** -0.5),
        "w2": nrm(ks[25], (L, N_EXPERTS, D_EXPERT, D_MODEL), D_EXPERT ** -0.5),
        "final_g": 1.0 + nrm(ks[26], (D_MODEL,), 0.02),
    }


def reference(x, c, positions, w_ada, b_ada, norm1_g, w_in, v_norm_g, v_norm_b, w_sp, b_sp,
              kv_norm_g, w_uk, w_uv, kidx_norm_g, gnorm_a_g, gnorm_b_g, w_out, norm2_g,
              w_group, b_group, w_expert, b_expert, w1, w3, w2, final_g):
    B, S, D = x.shape
    split_points = [int(p) for p in np.cumsum(IN_SPLIT_SIZES)[:-1]]
    for l in range(DEPTH):
        mod = jax.nn.silu(c) @ w_ada[l] + b_ada[l]
        shift1, scale1, gate1, shift2, scale2, gate2 = [m[:, None, :] for m in jnp.split(mod, 6, axis=-1)]

        h = rms_norm(x, norm1_g[l]) * (1.0 + scale1) + shift1
        proj = h @ w_in[l]
        u, v, q, c_kv, q_idx, k_idx, w_idx = jnp.split(proj, split_points, axis=-1)
        y_a = gmlp_spatial_gate(jax.nn.gelu(u), jax.nn.gelu(v), v_norm_g[l], v_norm_b[l], w_sp[l], b_sp[l])
        y_b = dsa_attention(q.reshape(B, S, N_HEADS, HEAD_DIM), c_kv,
                            q_idx.reshape(B, S, IDX_HEADS, IDX_DIM), k_idx, w_idx, positions,
                            kv_norm_g[l], w_uk[l], w_uv[l], kidx_norm_g[l])
        y = jnp.concatenate([rms_norm(y_a, gnorm_a_g[l]), rms_norm(y_b, gnorm_b_g[l])], axis=-1)
        x = x + gate1 * (y @ w_out[l])

        h2 = rms_norm(x, norm2_g[l]) * (1.0 + scale2) + shift2
        moe = hierarchical_moe(h2.reshape(B * S, D), w_group[l], b_group[l], w_expert[l], b_expert[l],
                               w1[l], w3[l], w2[l])
        x = x + gate2 * moe.reshape(B, S, D)
    return rms_norm(x, final_g)
```

```python
import contextlib
import math
import numpy as np
import concourse.bass as bass
import concourse.mybir as mybir
from concourse.bass_utils import run_bass_kernel_spmd

F32 = mybir.dt.float32
BF16 = mybir.dt.bfloat16
I32 = mybir.dt.int32
U32 = mybir.dt.uint32
AF = mybir.ActivationFunctionType
ALU = mybir.AluOpType
AX = mybir.AxisListType

FLAGS = {"hT": 1, "p3": 1, "yT": 1, "reorder": 1}
EPOCH = 12000
ENGS = ["tensor", "vector", "scalar", "gpsimd", "sync"]
D = 2048
NEXP = 32
EPS = 1e-6


class Buf:
    __slots__ = ("name", "w", "r", "dsem", "dcum", "t", "multi", "sub")

    def __init__(self, name, t=None):
        self.name = name
        self.multi = False
        self.w = {}
        self.r = {}
        self.dsem = None
        self.dcum = 0
        self.t = t

    def __getitem__(self, k):
        return self.t[k]


class Sched:
    def __init__(self, nc, semstack):
        self.nc = nc
        self.semstack = semstack
        self.stack = semstack
        self.ops = {e: [] for e in ENGS}
        self.cnt = {e: 0 for e in ENGS}
        self.sems = {}
        self.waited = {e: {} for e in ENGS}
        self.cur_cum = {}
        self.nsem = 0
        self.bufs = []

    def _newsem(self, name):
        self.nsem += 1
        return self.semstack.enter_context(self.nc.semaphore(f"{name}_{self.nsem}"))

    def sb(self, name, shape, dtype):
        t = self.stack.enter_context(self.nc.sbuf_tensor(name, list(shape), dtype))
        b = Buf(name, t)
        self.bufs.append(b)
        return b

    def ps(self, name, shape, dtype):
        t = self.stack.enter_context(self.nc.psum_tensor(name, list(shape), dtype))
        b = Buf(name, t)
        self.bufs.append(b)
        return b

    def subs(self, buf, n, flag=None):
        buf.sub = []
        if flag is not None and not FLAGS.get(flag, 1):
            buf.sub = [buf] * n
            return buf
        for i in range(n):
            b = Buf(f"{buf.name}_s{i}", buf.t)
            self.bufs.append(b)
            buf.sub.append(b)
        return buf

    def view(self, name):
        b = Buf(name, None)
        b.multi = True
        self.bufs.append(b)
        return b

    def _engkey(self, eng):
        idx = self.cnt[eng]
        ep = idx // EPOCH
        key = ("E", eng, ep)
        if key not in self.sems:
            self.sems[key] = self._newsem(f"e_{eng}_{ep}")
        return key, (idx % EPOCH) + 1

    def _collect(self, eng, reads, writes):
        deps = {}

        def add(d):
            for k, v in d.items():
                if k[0] == "D":
                    v = max(v, self.cur_cum.get(k, v))
                if deps.get(k, 0) < v:
                    deps[k] = v
        for b in reads:
            add(b.w)
        for b in writes:
            if not b.multi:
                add(b.w)
            add(b.r)
        out = []
        wd = self.waited[eng]
        for k, v in deps.items():
            if k[0] == "E" and k[1] == eng and eng in ("tensor", "sync"):
                continue
            if wd.get(k, 0) >= v:
                continue
            wd[k] = v
            out.append((k, v))
        return out

    def _record(self, me, reads, writes):
        k, v = me
        for b in writes:
            if b.multi:
                if b.w.get(k, 0) < v:
                    b.w[k] = v
            else:
                b.w = {k: v}
                b.r = {}
        for b in reads:
            if b.r.get(k, 0) < v:
                b.r[k] = v

    def op(self, eng, fn, reads=(), writes=()):
        waits = self._collect(eng, reads, writes)
        key, val = self._engkey(eng)
        self.ops[eng].append((waits, fn, key, 1))
        self.cnt[eng] += 1
        self._record((key, val), reads, writes)

    def dma(self, queue, fn, reads=(), writes=(), owner=None):
        waits = self._collect(queue, reads, writes)
        if owner is None:
            owner = (list(writes) + list(reads))[0]
        if owner.dsem is None or owner.dcum + 16 > EPOCH * 2:
            key = ("D", id(owner), self.nsem)
            self.sems[key] = self._newsem("d_" + owner.name)
            owner.dsem = key
            owner.dcum = 0
        owner.dcum += 16
        key = owner.dsem
        self.cur_cum[key] = owner.dcum
        self.ops[queue].append((waits, fn, key, 16))
        self._record((key, owner.dcum), reads, writes)

    def raw(self, eng, fn, reads=()):
        waits = self._collect(eng, reads, [])
        self.ops[eng].append((waits, fn, "RAW", 0))

    def barrier(self):
        allk = {}
        for e in ENGS:
            if self.cnt[e] > 0:
                idx = self.cnt[e] - 1
                allk[("E", e, idx // EPOCH)] = (idx % EPOCH) + 1
        for k, v in self.cur_cum.items():
            allk[k] = v
        for e in ENGS:
            wd = self.waited[e]
            ws = []
            for k, v in allk.items():
                if wd.get(k, 0) >= v:
                    continue
                wd[k] = v
                if k[0] == "E" and k[1] == e:
                    continue
                ws.append((k, v))
            if ws:
                self.ops[e].append((ws, None, None, 0))
        for b in self.bufs:
            b.w = {}
            b.r = {}

    def emit(self):
        nc = self.nc
        with nc.Block() as block:
            for e in ENGS:
                ops = self.ops[e]
                if not ops:
                    continue

                def body(eng, ops=ops):
                    for waits, fn, key, inc in ops:
                        for k, v in waits:
                            eng.wait_ge(self.sems[k], v)
                        if fn is None:
                            continue
                        if key == "RAW":
                            fn(eng)
                        else:
                            fn(eng).then_inc(self.sems[key], inc)
                getattr(block, e)(body)
        self.ops = {e: [] for e in ENGS}


def build(NT=32, NITER=16, dbg=False):
    ST = NT * 128
    C = 512
    NSB = (2 * ST + C - 1) // C + NEXP
    nc = bass.Bass("TRN2", target_bir_lowering=False)

    def din(name, shape, dt=F32):
        return nc.dram_tensor(name, list(shape), dt, kind="ExternalInput")

    def dscr(name, shape, dt=F32):
        if dbg:
            return nc.dram_tensor(name, list(shape), dt, kind="ExternalOutput")
        return nc.dram_tensor(name, list(shape), dt)

    x_d = din("x", [ST, D])
    c_d = din("c", [D])
    pos_d = din("pos", [ST], I32)
    wada_d = din("w_ada", [D, 6 * D])
    bada_d = din("b_ada", [6 * D])
    n1g_d = din("norm1_g", [D])
    win_d = din("w_in", [D, 3912])
    vng_d = din("v_norm_g", [1024])
    vnb_d = din("v_norm_b", [1024])
    wsp_d = din("w_sp", [8, 128, 128])
    bsp_d = din("b_sp", [8, 128])
    kvg_d = din("kv_norm_g", [256])
    wuk_d = din("w_uk", [256, 64])
    wuv_d = din("w_uv", [256, 64])
    kig_d = din("kidx_norm_g", [64])
    gna_d = din("gnorm_a_g", [1024])
    gnb_d = din("gnorm_b_g", [1024])
    wout_d = din("w_out", [D, D])
    n2g_d = din("norm2_g", [D])
    wr_d = din("w_r", [D, 36])
    br_d = din("b_r", [36])
    wexp_d = din("wexp", [NEXP, 12, 128, 4096])
    fg_d = din("final_g", [D])
    out_d = nc.dram_tensor("out", [ST, D], F32, kind="ExternalOutput")

    mod_s = dscr("mod_s", [6 * D])
    ya_s = dscr("ya_s", [ST, 1024], BF16)
    qTe_s = dscr("qTe_s", [NT, 128, 1024], BF16)
    qTo_s = dscr("qTo_s", [NT, 128, 1024], BF16)
    qiTe_s = dscr("qiTe_s", [NT, 128, 512], BF16)
    qiTo_s = dscr("qiTo_s", [NT, 128, 512], BF16)
    xmid_s = dscr("xmid_s", [ST, D])
    xn2_s = dscr("xn2_s", [ST, D], BF16)
    xe_s = dscr("xe_s", [NSB * C, D], BF16)
    ye_s = dscr("ye_s", [NSB * C, D])

    with contextlib.ExitStack() as outer:
        S = Sched(nc, outer)
        B = [S.ps(f"B{i}", [128, 512], F32) for i in range(8)]
        DV = {n: S.view("dv_" + n) for n in
              ["in", "mod", "ya", "qTe", "qTo", "qiTe", "qiTo", "xmid", "xe", "ye", "out", "xn2"]}

        def mm(o, l, r, st, sp, R, W, skip=False):
            S.op("tensor", lambda e: e.matmul(o, l, r, start=st, stop=sp, skip_group_check=skip), R, W)

        def tr(o, i, idn, R, W):
            S.op("tensor", lambda e: e.transpose(o, i, idn), R, W)

        def act(o, i, f, R, W, bias=None, scale=None, acc=None):
            kw = {}
            if bias is not None:
                kw["bias"] = bias
            if scale is not None:
                kw["scale"] = scale
            if acc is not None:
                kw["accum_out"] = acc
            S.op("scalar", lambda e: e.activation(out=o, in_=i, func=f, **kw), R, W)

        def ts(o, i, s1, op0, R, W, s2=None, op1=None, eng="vector", acc=None):
            if acc is not None:
                S.op(eng, lambda e: e.tensor_scalar(o, i, s1, s2, op0, op1, accum_out=acc), R, W)
            elif op1 is None:
                S.op(eng, lambda e: e.tensor_scalar(o, i, s1, None, op0), R, W)
            else:
                S.op(eng, lambda e: e.tensor_scalar(o, i, s1, s2, op0, op1), R, W)

        def tt(o, a, b, op, R, W, eng="vector"):
            S.op(eng, lambda e: e.tensor_tensor(out=o, in0=a, in1=b, op=op), R, W)

        def stt(o, a, s, b, op0, op1, R, W):
            S.op("vector", lambda e: e.scalar_tensor_tensor(out=o, in0=a, scalar=s, in1=b, op0=op0, op1=op1), R, W)

        def cp(o, i, R, W, eng="vector"):
            if eng == "scalar":
                S.op("scalar", lambda e: e.copy(o, i), R, W)
            else:
                S.op(eng, lambda e: e.tensor_copy(o, i), R, W)

        def mset(ap, val, W, eng="vector"):
            S.op(eng, lambda e: e.memset(ap, val), [], W)

        def ld(o, i, R, W, q="sync", owner=None):
            S.dma(q, lambda e: e.dma_start(out=o, in_=i), R, W, owner=owner)

        def rsqrt_col(dst, src, scale, R, W, tmp):
            w_ = src.shape[-1]
            ts(tmp[:, 0:w_], src, scale, ALU.mult, R, [tmp], s2=EPS, op1=ALU.add)
            act(tmp[:, 0:w_], tmp[:, 0:w_], AF.Sqrt, [tmp], [tmp])
            S.op("vector", lambda e: e.reciprocal(dst, tmp[:, 0:w_]), [tmp], W)

        ident = S.sb("ident", [128, 128], F32)
        identb = S.sb("identb", [128, 128], BF16)
        onesf = S.sb("onesf", [128, 128], F32)
        onesb = S.sb("onesb", [128, 128], BF16)
        zerob = S.sb("zerob", [128, 512], BF16)
        LTb = S.sb("LTb", [128, 128], BF16)
        pvec = S.sb("pvec", [128, 64], F32)
        gT = S.sb("gT", [128, 58], F32)
        kT2 = S.sb("kT2", [128, ST], BF16)
        kiT2 = S.sb("kiT2", [128, ST], BF16)
        vaug = S.sb("vaug", [128, NT, 65], BF16)
        sgn_all = S.sb("sgn_all", [128, NT, 8], F32)
        dest_all = S.sb("dest_all", [128, NT, 2], I32)
        eid_all = S.sb("eid_all", [128, NT, 2], F32)
        pos_all = S.sb("pos_all", [128, NT, 2], F32)
        base_bc = S.sb("base_bc", [128, 32], F32)
        widx = S.sb("widx", [128, NSB, 12], I32)
        wgt_all = S.sb("wgt_all", [128, NT, 2], F32)
        junk = S.sb("junk", [128, 2048], BF16)
        col = [S.sb(f"col{i}", [128, 16], F32) for i in range(8)]

        mset(onesf[:], 1.0, [onesf], eng="gpsimd")
        mset(onesb[:], 1.0, [onesb], eng="gpsimd")
        mset(zerob[:], 0.0, [zerob], eng="gpsimd")
        S.op("gpsimd", lambda e: e.affine_select(out=ident[:], in_=onesf[:], pattern=[[1, 128]],
                                                 compare_op=ALU.is_equal, fill=0.0, base=0,
                                                 channel_multiplier=-1), [onesf], [ident])
        S.op("gpsimd", lambda e: e.affine_select(out=identb[:], in_=onesf[:], pattern=[[1, 128]],
                                                 compare_op=ALU.is_equal, fill=0.0, base=0,
                                                 channel_multiplier=-1), [onesf], [identb])
        S.op("gpsimd", lambda e: e.affine_select(out=LTb[:], in_=onesf[:], pattern=[[1, 128]],
                                                 compare_op=ALU.is_ge, fill=0.0, base=-1,
                                                 channel_multiplier=-1), [onesf], [LTb])
        mset(vaug[:, :, 64:65], 1.0, [vaug], eng="gpsimd")

        with contextlib.ExitStack() as ph:
            S.stack = ph
            stA = S.sb("stA", [112, 128], F32)
            stB = S.sb("stB", [58, 128], F32)
            siluc = S.sb("siluc", [128, 16], F32)
            cT = S.sb("cT", [128, 16], F32)
            badaT = S.sb("badaT", [128, 96], F32)
            modT = S.sb("modT", [128, 96], F32)
            modR = S.sb("modR", [96, 128], F32)
            wa = [S.sb(f"wa{i}", [128, 16, 512], F32) for i in range(2)]

            ld(stA[0:16, :], c_d.ap().rearrange("(k p) -> k p", p=128), [DV["in"]], [stA])
            ld(stA[16:112, :], bada_d.ap().rearrange("(k p) -> k p", p=128), [DV["in"]], [stA])
            r0 = 0
            for src, nr in [(n1g_d, 16), (n2g_d, 16), (gna_d, 8), (gnb_d, 8), (kvg_d, 2)]:
                ld(stB[r0:r0 + nr, :], src.ap().rearrange("(k p) -> k p", p=128), [DV["in"]], [stB])
                r0 += nr
            ld(stB[50:58, :], bsp_d.ap(), [DV["in"]], [stB])
            tr(B[0][:, 0:112], stA[0:112, :], ident[0:112, 0:112], [stA, ident], [B[0]])
            tr(B[1][:, 0:58], stB[0:58, :], ident[0:58, 0:58], [stB, ident], [B[1]])
            cp(cT[:], B[0][:, 0:16], [B[0]], [cT])
            act(siluc[:], cT[:], AF.Silu, [cT], [siluc])
            cp(badaT[:], B[0][:, 16:112], [B[0]], [badaT])
            cp(gT[:], B[1][:, 0:58], [B[1]], [gT])
            for cb in range(24):
                w = wa[cb % 2]
                ld(w[:], wada_d.ap().rearrange("(k p) c -> p k c", p=128)[:, :, cb * 512:(cb + 1) * 512],
                   [DV["in"]], [w])
                for cc in range(4):
                    j = cb * 4 + cc
                    for k in range(16):
                        mm(B[2][:, j:j + 1], w[:, k, cc * 128:(cc + 1) * 128], siluc[:, k:k + 1],
                           k == 0, k == 15, [w, siluc], [B[2]])
            tt(modT[:], B[2][:, 0:96], badaT[:], ALU.add, [B[2], badaT], [modT])
            stt(pvec[:, 0:16], modT[:, 16:32], 1.0, gT[:, 0:16], ALU.add, ALU.mult, [modT, gT], [pvec])
            cp(pvec[:, 16:32], modT[:, 0:16], [modT], [pvec])
            stt(pvec[:, 32:48], modT[:, 64:80], 1.0, gT[:, 16:32], ALU.add, ALU.mult, [modT, gT], [pvec])
            cp(pvec[:, 48:64], modT[:, 48:64], [modT], [pvec])
            tr(B[3][0:96, 0:128], modT[:, 0:96], ident[:], [modT, ident], [B[3]])
            cp(modR[:], B[3][0:96, 0:128], [B[3]], [modR])
            ld(mod_s.ap().rearrange("(j p) -> j p", p=128), modR[:], [modR], [DV["mod"]], q="gpsimd")
            S.barrier()
            S.emit()

        def prep_hT(xt, xn, hT, c_ss, c_rs, c_tmp, sc_off, sh_off):
            act(junk[:], xt[:], AF.Square, [xt], [junk, c_ss], acc=c_ss[:, 0:1])
            rsqrt_col(c_rs[:, 0:1], c_ss[:, 0:1], 1.0 / D, [c_ss], [c_rs], c_tmp)
            ts(xn[:], xt[:], c_rs[:, 0:1], ALU.mult, [xt, c_rs], [xn])
            for k in range(16):
                bk = B[k // 8]
                tr(bk[:].bitcast(BF16)[:, (k % 8) * 128:(k % 8 + 1) * 128], xn[:, k * 128:(k + 1) * 128],
                   identb[:], [xn, identb], [bk])
            for k in range(16):
                bk = B[k // 8]
                src = bk[:].bitcast(BF16)[:, (k % 8) * 128:(k % 8 + 1) * 128]
                if k < 8:
                    ts(hT[:, k, :], src, pvec[:, sc_off + k:sc_off + k + 1], ALU.mult, [bk, pvec], [hT.sub[k]],
                       s2=pvec[:, sh_off + k:sh_off + k + 1], op1=ALU.add)
                else:
                    act(hT[:, k, :], src, AF.Identity, [bk, pvec], [hT.sub[k]],
                        bias=pvec[:, sh_off + k:sh_off + k + 1], scale=pvec[:, sc_off + k:sc_off + k + 1])

        def load_w_bf(dst, src_ap_fn, ncols, stg, rowscale=None, colscale=None):
            step = 256
            i = 0
            for c0 in range(0, ncols, step):
                cw = min(step, ncols - c0)
                sg = stg[i % 2]
                ld(sg[:, :, 0:cw], src_ap_fn(c0, cw), [DV["in"]], [sg])
                eng = ["vector", "gpsimd"][i % 2]
                if rowscale is None:
                    if i % 3 == 2:
                        cp(dst[:, :, c0:c0 + cw], sg[:, :, 0:cw], [sg], [dst], eng="scalar")
                    else:
                        cp(dst[:, :, c0:c0 + cw], sg[:, :, 0:cw], [sg], [dst], eng=eng)
                else:
                    for k in range(16):
                        stt(dst[:, k, c0:c0 + cw], sg[:, k, 0:cw], rowscale[:, k:k + 1], colscale[:, c0:c0 + cw],
                            ALU.mult, ALU.mult, [sg] + rowscale_bufs, [dst])
                i += 1

        rowscale_bufs = []

        with contextlib.ExitStack() as ph:
            S.stack = ph
            wbf = S.sb("wbfA", [128, 16, 2048], BF16)
            stg = [S.sb(f"stgA{i}", [128, 16, 256], F32) for i in range(2)]
            xts = [S.sb(f"xtA{i}", [128, D], F32) for i in range(2)]
            xns = [S.sb(f"xnA{i}", [128, D], BF16) for i in range(2)]
            hTs = [S.subs(S.sb(f"hTA{i}", [128, 16, 128], BF16), 16, "hT") for i in range(2)]
            gu = S.sb("gu", [128, 1024], F32)
            gv = S.sb("gv", [128, 1024], F32)
            vn = S.sb("vn", [128, 1024], F32)
            vgb = S.sb("vgb", [128, 1024], BF16)
            Gbc = S.sb("Gbc", [128, 1024], F32)
            Bbc = S.sb("Bbc", [128, 1024], F32)
            wsp = S.sb("wsp", [128, 8, 128], F32)
            WmT = S.sb("WmT", [128, 8, 128], BF16)
            ya = S.sb("ya", [128, 1024], F32)
            yan = S.sb("yan", [128, 1024], BF16)
            stats = S.sb("stats", [128, 8, 6], F32)
            mv = S.sb("mv", [128, 8, 2], F32)

            win_v = win_d.ap().rearrange("(k p) c -> p k c", p=128)
            load_w_bf(wbf, lambda c0, cw: win_v[:, :, c0:c0 + cw], 2048, stg)
            ld(Gbc[:], vng_d.ap().partition_broadcast(128), [DV["in"]], [Gbc])
            ld(Bbc[:], vnb_d.ap().partition_broadcast(128), [DV["in"]], [Bbc])
            ld(wsp[:], wsp_d.ap().rearrange("g i j -> i g j"), [DV["in"]], [wsp])
            for g in range(8):
                bk = B[6 + g // 4]
                tr(bk[:, (g % 4) * 128:(g % 4 + 1) * 128], wsp[:, g, :], ident[:], [wsp, ident], [bk])
            for g in range(8):
                bk = B[6 + g // 4]
                cp(WmT[:, g, :], bk[:, (g % 4) * 128:(g % 4 + 1) * 128], [bk], [WmT])
            mset(WmT[64:128, :, 0:64], 0.0, [WmT])

            def H_a(n):
                xt = xts[n % 2]
                ld(xt[:], x_d.ap()[n * 128:(n + 1) * 128, :], [DV["in"]], [xt])
                prep_hT(xt, xns[n % 2], hTs[n % 2], col[0], col[1], col[2], 0, 16)

            def M_a(n):
                hT = hTs[n % 2]
                for cg in range(4):
                    for k in range(16):
                        mm(B[2 + cg][:], hT[:, k, :], wbf[:, k, cg * 512:(cg + 1) * 512], k == 0, k == 15,
                           [hT.sub[k], wbf], [B[2 + cg]])

            def E_a(n):
                act(gu[:, 0:512], B[2][:], AF.Gelu_apprx_tanh, [B[2]], [gu])
                act(gu[:, 512:1024], B[3][:], AF.Gelu_apprx_tanh, [B[3]], [gu])
                act(gv[:, 0:512], B[4][:], AF.Gelu_apprx_tanh, [B[4]], [gv])
                act(gv[:, 512:1024], B[5][:], AF.Gelu_apprx_tanh, [B[5]], [gv])
                for g in range(8):
                    S.op("vector", (lambda g: lambda e: e.bn_stats(stats[:, g, :], gv[:, g * 128:(g + 1) * 128]))(g),
                         [gv], [stats])
                for g in range(8):
                    S.op("vector", (lambda g: lambda e: e.bn_aggr(mv[:, g, :], stats[:, g, :]))(g), [stats], [mv])
                ts(col[4][:, 0:8], mv[:, :, 1], EPS, ALU.add, [mv], [col[4]])
                act(col[4][:, 0:8], col[4][:, 0:8], AF.Sqrt, [col[4]], [col[4]])
                S.op("vector", lambda e: e.reciprocal(col[3][:, 0:8], col[4][:, 0:8]), [col[4]], [col[3]])
                for g in range(8):
                    ts(vn[:, g * 128:(g + 1) * 128], gv[:, g * 128:(g + 1) * 128], mv[:, g, 0:1], ALU.subtract,
                       [gv, mv, col[3]], [vn], s2=col[3][:, g:g + 1], op1=ALU.mult)
                tt(vn[:], vn[:], Gbc[:], ALU.mult, [vn, Gbc], [vn], eng="gpsimd")
                tt(vgb[:], vn[:], Bbc[:], ALU.add, [vn, Bbc], [vgb])

            def E2_a(n):
                for g in range(8):
                    bk = B[6 + g // 4]
                    mm(bk[:, (g % 4) * 128:(g % 4 + 1) * 128], WmT[:, g, :], vgb[:, g * 128:(g + 1) * 128],
                       True, True, [WmT, vgb], [bk])
                for g in range(8):
                    bk = B[6 + g // 4]
                    stt(ya[:, g * 128:(g + 1) * 128], bk[:, (g % 4) * 128:(g % 4 + 1) * 128], gT[:, 50 + g:51 + g],
                        gu[:, g * 128:(g + 1) * 128], ALU.add, ALU.mult, [bk, gT, gu], [ya])
                act(junk[:, 0:1024], ya[:], AF.Square, [ya], [junk, col[5]], acc=col[5][:, 0:1])
                rsqrt_col(col[6][:, 0:1], col[5][:, 0:1], 1.0 / 1024, [col[5]], [col[6]], col[7])
                ts(yan[:], ya[:], col[6][:, 0:1], ALU.mult, [ya, col[6]], [yan])
                ld(ya_s.ap()[n * 128:(n + 1) * 128, :], yan[:], [yan], [DV["ya"]], q="gpsimd")

            H_a(0)
            for n in range(NT):
                M_a(n)
                if n + 1 < NT:
                    H_a(n + 1)
                if FLAGS.get("reorder", 1):
                    if n >= 1:
                        E2_a(n - 1)
                    E_a(n)
                else:
                    E_a(n)
                    E2_a(n)
            if FLAGS.get("reorder", 1):
                E2_a(NT - 1)
            S.barrier()
            S.emit()

        with contextlib.ExitStack() as ph:
            S.stack = ph
            NCB = 3912 - 2048
            wbf = S.sb("wbfB", [128, 16, NCB], BF16)
            stg = [S.sb(f"stgB{i}", [128, 16, 256], F32) for i in range(2)]
            xts = [S.sb(f"xtB{i}", [128, D], F32) for i in range(2)]
            xns = [S.sb(f"xnB{i}", [128, D], BF16) for i in range(2)]
            hTs = [S.subs(S.sb(f"hTB{i}", [128, 16, 128], BF16), 16, "hT") for i in range(2)]
            posR = S.sb("posR", [NT, 128], I32)
            posF = S.sb("posF", [NT, 128], F32)
            posT = S.sb("posT", [128, NT], F32)
            fr = S.sb("fr", [128, 32], F32)
            ang = S.sb("ang", [128, NT, 32], F32)
            rr = S.sb("rr", [128, NT, 32], F32)
            rq = S.sb("rq", [128, NT, 32], F32)
            rni = S.sb("rni", [128, NT, 32], I32)
            sin_t = S.sb("sin_t", [128, NT, 32], F32)
            cos_t = S.sb("cos_t", [128, NT, 32], F32)
            stkv = S.sb("stkv", [128, 2, 128], F32)
            wukv = S.sb("wukv", [128, 2, 128], BF16)
            kig_bc = S.sb("kig_bc", [128, 64], F32)
            qr = S.sb("qr", [128, 1024], BF16)
            qir = S.sb("qir", [128, 512], BF16)
            t1 = S.sb("t1", [128, 512], F32)
            t2 = S.sb("t2", [128, 512], F32)
            cw_ = S.sb("cw_", [128, 8, 32], F32)
            sw_ = S.sb("sw_", [128, 8, 32], F32)
            wis = S.sb("wis", [128, 8], F32)
            ckvb = S.sb("ckvb", [128, 256], BF16)
            ckvf = S.sb("ckvf", [128, 256], F32)
            kif = S.sb("kif", [128, 64], F32)
            ckvT = S.sb("ckvT", [128, 2, 128], BF16)
            kk = S.sb("kk", [128, 64], F32)
            kk2 = S.sb("kk2", [128, 128], BF16)
            kin = S.sb("kin", [128, 64], F32)
            kir = S.sb("kir", [128, 64], F32)
            kki2 = S.sb("kki2", [128, 128], BF16)
            qTe = S.sb("qTe", [128, 8, 128], BF16)
            qTo = S.sb("qTo", [128, 8, 128], BF16)
            qiTe = S.sb("qiTe", [128, 4, 128], BF16)
            qiTo = S.sb("qiTo", [128, 4, 128], BF16)

            win_v = win_d.ap().rearrange("(k p) c -> p k c", p=128)
            load_w_bf(wbf, lambda c0, cw: win_v[:, :, 2048 + c0:2048 + c0 + cw], NCB, stg)
            ld(posR[:], pos_d.ap().rearrange("(n p) -> n p", p=128), [DV["in"]], [posR])
            cp(posF[:], posR[:], [posR], [posF])
            tr(B[7][:, 0:NT], posF[0:NT, :], ident[0:NT, 0:NT], [posF, ident], [B[7]])
            cp(posT[:], B[7][:, 0:NT], [B[7]], [posT])
            for i in range(32):
                mset(fr[:, i:i + 1], float(np.float32(10000.0) ** np.float32(-i / 32.0)), [fr], eng="gpsimd")
            tt(ang[:], posT[:].unsqueeze(2).broadcast_to([128, NT, 32]),
               fr[:].unsqueeze(1).broadcast_to([128, NT, 32]), ALU.mult, [posT, fr], [ang])
            TWO_PI = 2.0 * math.pi
            C1 = 6.28125
            C2 = TWO_PI - C1
            for (dst, shift) in ((sin_t, 0.0), (cos_t, math.pi / 2)):
                ts(rq[:], ang[:], shift, ALU.add, [ang], [rq])
                ts(rr[:], rq[:], 1.0 / TWO_PI, ALU.mult, [rq], [rr])
                cp(rni[:], rr[:], [rr], [rni])
                cp(rr[:], rni[:], [rni], [rr])
                stt(rq[:], rr[:], -C1, rq[:], ALU.mult, ALU.add, [rr, rq], [rq])
                stt(rq[:], rr[:], -C2, rq[:], ALU.mult, ALU.add, [rr, rq], [rq])
                ts(rr[:], rq[:], math.pi, ALU.is_gt, [rq], [rr])
                stt(rq[:], rr[:], -TWO_PI, rq[:], ALU.mult, ALU.add, [rr, rq], [rq])
                ts(rr[:], rq[:], -math.pi, ALU.is_lt, [rq], [rr])
                stt(rq[:], rr[:], TWO_PI, rq[:], ALU.mult, ALU.add, [rr, rq], [rq])
                ts(rq[:], rq[:], 3.141592, ALU.min, [rq], [rq], s2=-3.141592, op1=ALU.max)
                act(dst[:], rq[:], AF.Sin, [rq], [dst])
            ld(stkv[:, :, 0:64], wuk_d.ap().rearrange("(c p) d -> p c d", p=128), [DV["in"]], [stkv])
            ld(stkv[:, :, 64:128], wuv_d.ap().rearrange("(c p) d -> p c d", p=128), [DV["in"]], [stkv])
            for c2 in range(2):
                ts(wukv[:, c2, :], stkv[:, c2, :], gT[:, 48 + c2:49 + c2], ALU.mult, [stkv, gT], [wukv])
            ld(kig_bc[:], kig_d.ap().partition_broadcast(128), [DV["in"]], [kig_bc])
            mset(qTe[:], 0.0, [qTe], eng="gpsimd")
            mset(qTo[:], 0.0, [qTo], eng="gpsimd")
            mset(qiTe[:], 0.0, [qiTe], eng="gpsimd")
            mset(qiTo[:], 0.0, [qiTo], eng="gpsimd")
            CSC = (8 ** -0.5) * (64 ** -0.5)

            def rope(o_lo, o_hi, x_lo, x_hi, cs, sn, nh, RB):
                a = t1[:, 0:nh * 32].rearrange("p (h d) -> p h d", d=32)
                b = t2[:, 0:nh * 32].rearrange("p (h d) -> p h d", d=32)
                tt(a, x_lo, cs, ALU.mult, RB, [t1])
                tt(b, x_hi, sn, ALU.mult, RB, [t2])
                tt(o_lo, a, b, ALU.subtract, [t1, t2], RB[-1:], eng="gpsimd")
                tt(a, x_lo, sn, ALU.mult, RB + [t1], [t1])
                tt(b, x_hi, cs, ALU.mult, RB + [t2], [t2])
                tt(o_hi, a, b, ALU.add, [t1, t2], RB[-1:], eng="gpsimd")

            zt = S.sb("zt", [128, D], BF16)
            mset(zt[:], 0.0, [zt], eng="gpsimd")
            zf_total = NSB * C // 512
            zf_done = [0]

            def H_b(n):
                xt = xts[n % 2]
                ld(xt[:], x_d.ap()[n * 128:(n + 1) * 128, :], [DV["in"]], [xt])
                want = (zf_total * (n + 1) + NT - 1) // NT
                while zf_done[0] < min(want, zf_total):
                    r = zf_done[0]
                    ld(xe_s.ap()[r * 512:(r + 1) * 512, :].rearrange("(a p) d -> p a d", p=128),
                       zt[:].unsqueeze(1).broadcast_to([128, 4, D]), [zt], [DV["xe"]], q="sync", owner=zt)
                    zf_done[0] += 1
                prep_hT(xt, xns[n % 2], hTs[n % 2], col[0], col[1], col[2], 0, 16)

            def M_b(n):
                hT = hTs[n % 2]
                widths = [512, 512, 512, NCB - 1536]
                for cg in range(4):
                    for k in range(16):
                        mm(B[2 + cg][:, 0:widths[cg]], hT[:, k, :], wbf[:, k, cg * 512:cg * 512 + widths[cg]],
                           k == 0, k == 15, [hT.sub[k], wbf], [B[2 + cg]])

            def E_b(n):
                cosb = cos_t[:, n, :].unsqueeze(1)
                sinb = sin_t[:, n, :].unsqueeze(1)
                for hb in range(2):
                    bk = B[2 + hb]
                    qv = bk[:].rearrange("p (h d) -> p h d", d=64)
                    ov = qr[:, hb * 512:(hb + 1) * 512].rearrange("p (h d) -> p h d", d=64)
                    rope(ov[:, :, 0:32], ov[:, :, 32:64], qv[:, :, 0:32], qv[:, :, 32:64],
                         cosb.broadcast_to([128, 8, 32]), sinb.broadcast_to([128, 8, 32]), 8,
                         [bk, cos_t, sin_t, qr])
                ts(wis[:], B[5][:, 320:328], CSC, ALU.mult, [B[5]], [wis])
                ts(sgn_all[:, n, :], B[5][:, 320:328], 0.0, ALU.is_ge, [B[5]], [sgn_all], s2=2.0, op1=ALU.mult)
                ts(sgn_all[:, n, :], sgn_all[:, n, :], -1.0, ALU.add, [sgn_all], [sgn_all])
                tt(cw_[:], cosb.broadcast_to([128, 8, 32]), wis[:].unsqueeze(2).broadcast_to([128, 8, 32]),
                   ALU.mult, [cos_t, wis], [cw_])
                tt(sw_[:], sinb.broadcast_to([128, 8, 32]), wis[:].unsqueeze(2).broadcast_to([128, 8, 32]),
                   ALU.mult, [sin_t, wis], [sw_])
                for hb in range(2):
                    bk = B[4 + hb]
                    src = bk[:, 256:512] if hb == 0 else bk[:, 0:256]
                    qv = src.rearrange("p (h d) -> p h d", d=64)
                    ov = qir[:, hb * 256:(hb + 1) * 256].rearrange("p (h d) -> p h d", d=64)
                    rope(ov[:, :, 0:32], ov[:, :, 32:64], qv[:, :, 0:32], qv[:, :, 32:64],
                         cw_[:, hb * 4:(hb + 1) * 4, :], sw_[:, hb * 4:(hb + 1) * 4, :], 4,
                         [bk, cw_, sw_, qir])
                cp(ckvf[:], B[4][:, 0:256], [B[4]], [ckvf])
                act(junk[:, 0:256], ckvf[:], AF.Square, [ckvf], [junk, col[3]], acc=col[3][:, 0:1])
                rsqrt_col(col[4][:, 0:1], col[3][:, 0:1], 1.0 / 256, [col[3]], [col[4]], col[5])
                cp(ckvb[:], ckvf[:], [ckvf], [ckvb], eng="scalar")
                for c2 in range(2):
                    tr(B[0][:].bitcast(BF16)[:, c2 * 128:(c2 + 1) * 128], ckvb[:, c2 * 128:(c2 + 1) * 128], identb[:],
                       [ckvb, identb], [B[0]])
                cp(ckvT[:], B[0][:].bitcast(BF16)[:, 0:256].rearrange("p (c t) -> p c t", t=128), [B[0]], [ckvT])
                for c2 in range(2):
                    mm(B[1][:, 0:128], ckvT[:, c2, :], wukv[:, c2, :], c2 == 0, c2 == 1, [ckvT, wukv], [B[1]])
                ts(vaug[:, n, 0:64], B[1][:, 64:128], col[4][:, 0:1], ALU.mult, [B[1], col[4]], [vaug])
                kv3 = B[1][:, 0:64].rearrange("p (h d) -> p h d", d=64)
                kk3 = kk[:].rearrange("p (h d) -> p h d", d=64)
                rope(kk3[:, :, 0:32], kk3[:, :, 32:64], kv3[:, :, 0:32], kv3[:, :, 32:64], cosb, sinb, 1,
                     [B[1], cos_t, sin_t, kk])
                ts(kk2[:, 0:64], kk[:], col[4][:, 0:1], ALU.mult, [kk, col[4]], [kk2])
                ts(kk2[:, 64:128], kk[:], col[4][:, 0:1], ALU.mult, [kk, col[4]], [kk2])
                tr(B[0][:].bitcast(BF16)[:, 256:384], kk2[:], identb[:], [kk2, identb], [B[0]])
                cp(kT2[:, n * 128:(n + 1) * 128], B[0][:].bitcast(BF16)[:, 256:384], [B[0]], [kT2])
                cp(kif[:], B[5][:, 256:320], [B[5]], [kif])
                act(junk[:, 0:64], kif[:], AF.Square, [kif], [junk, col[6]], acc=col[6][:, 0:1])
                rsqrt_col(col[7][:, 0:1], col[6][:, 0:1], 1.0 / 64, [col[6]], [col[7]], col[5])
                stt(kin[:], kif[:], col[7][:, 0:1], kig_bc[:], ALU.mult, ALU.mult,
                    [kif, col[7], kig_bc], [kin])
                ki3 = kin[:].rearrange("p (h d) -> p h d", d=64)
                kr3 = kir[:].rearrange("p (h d) -> p h d", d=64)
                rope(kr3[:, :, 0:32], kr3[:, :, 32:64], ki3[:, :, 0:32], ki3[:, :, 32:64], cosb, sinb, 1,
                     [kin, cos_t, sin_t, kir])
                cp(kki2[:, 0:64], kir[:], [kir], [kki2])
                cp(kki2[:, 64:128], kir[:], [kir], [kki2])
                tr(B[0][:].bitcast(BF16)[:, 384:512], kki2[:], identb[:], [kki2, identb], [B[0]])
                cp(kiT2[:, n * 128:(n + 1) * 128], B[0][:].bitcast(BF16)[:, 384:512], [B[0]], [kiT2])
                b6 = B[6][:].bitcast(BF16)
                for pr in range(8):
                    tr(b6[:, pr * 128:(pr + 1) * 128], qr[:, pr * 128:(pr + 1) * 128], identb[:], [qr, identb], [B[6]])
                b63 = b6.rearrange("p (a t) -> p a t", t=128)
                cp(qTe[0:64, :, :], b63[0:64, :, :], [B[6]], [qTe])
                cp(qTo[64:128, :, :], b63[64:128, :, :], [B[6]], [qTo])
                b7 = B[7][:].bitcast(BF16)
                for pr in range(4):
                    tr(b7[:, pr * 128:(pr + 1) * 128], qir[:, pr * 128:(pr + 1) * 128], identb[:], [qir, identb], [B[7]])
                b73 = b7[:, 0:512].rearrange("p (a t) -> p a t", t=128)
                cp(qiTe[0:64, :, :], b73[0:64, :, :], [B[7]], [qiTe], eng="scalar")
                cp(qiTo[64:128, :, :], b73[64:128, :, :], [B[7]], [qiTo], eng="scalar")
                ld(qTe_s.ap()[n], qTe[:].rearrange("p a t -> p (a t)"), [qTe], [DV["qTe"]], q="gpsimd")
                ld(qTo_s.ap()[n], qTo[:].rearrange("p a t -> p (a t)"), [qTo], [DV["qTo"]], q="gpsimd")
                ld(qiTe_s.ap()[n], qiTe[:].rearrange("p a t -> p (a t)"), [qiTe], [DV["qiTe"]], q="gpsimd")
                ld(qiTo_s.ap()[n], qiTo[:].rearrange("p a t -> p (a t)"), [qiTo], [DV["qiTo"]], q="gpsimd")

            H_b(0)
            for n in range(NT):
                M_b(n)
                if n + 1 < NT:
                    H_b(n + 1)
                E_b(n)
            S.barrier()
            S.emit()

        with contextlib.ExitStack() as ph:
            S.stack = ph
            woutb = S.sb("woutb", [128, 16, D], BF16)
            with contextlib.ExitStack() as ph2:
                S.stack = ph2
                stg = [S.sb(f"stgO{i}", [128, 16, 256], F32) for i in range(2)]
                gate1_bc = S.sb("gate1_bc", [128, D], F32)
                ld(gate1_bc[:], mod_s.ap()[2 * D:3 * D].partition_broadcast(128), [DV["mod"]], [gate1_bc])
                wout_v = wout_d.ap().rearrange("(k p) c -> p k c", p=128)
                rowscale_bufs.clear()
                rowscale_bufs.extend([gT, gate1_bc])
                load_w_bf(woutb, lambda c0, cw: wout_v[:, :, c0:c0 + cw], D, stg, rowscale=gT[:, 32:48],
                          colscale=gate1_bc)
                S.barrier()
                S.emit()
            S.stack = ph
            score = S.sb("score", [128, ST], F32)
            NMs = [S.sb(f"NM{i}", [128, ST], BF16) for i in range(2)]
            qTe = [S.sb(f"qTeL{i}", [128, 1024], BF16) for i in range(2)]
            qTo = [S.sb(f"qToL{i}", [128, 1024], BF16) for i in range(2)]
            qiTe = [S.sb(f"qiTeL{i}", [128, 512], BF16) for i in range(2)]
            qiTo = [S.sb(f"qiToL{i}", [128, 512], BF16) for i in range(2)]
            pT = [S.sb(f"pT{i}", [128, 512], BF16) for i in range(3)]
            xt = S.sb("xtC", [128, D], F32)
            yanl = S.sb("yanl", [128, 1024], BF16)
            yb = S.sb("yb", [128, 1024], F32)
            ybn = S.sb("ybn", [128, 1024], BF16)
            yT = S.subs(S.sb("yT", [128, 16, 128], BF16), 2, "yT")
            xm = S.sb("xm", [128, D], F32)
            xn2b = S.sb("xn2b", [128, D], BF16)
            h2T = S.subs(S.sb("h2T", [128, 16, 128], F32), 16)
            wr = S.sb("wr", [128, 16, 36], F32)
            bias_bc = S.sb("bias_bc", [128, 36], F32)
            lg = S.sb("lg", [128, 36], F32)
            em = S.sb("em", [128, 32], F32)
            m8 = S.sb("m8", [128, 8], F32)
            i8 = S.sb("i8", [128, 8], U32)
            Ab = S.sb("Ab", [128, 32], BF16)
            oh0 = S.sb("oh0", [128, 32], F32)
            oh1 = S.sb("oh1", [128, 32], F32)
            posf = S.sb("posf", [128, 32], F32)
            tmp32 = S.sb("tmp32", [128, 32], F32)
            sm = S.sb("sm", [128, 32], F32)
            lo = S.sb("lo", [128, 1], F32)
            w0c = S.sb("w0c", [128, 1], F32)
            mid = S.sb("mid", [128, 1], F32)
            cnt = S.sb("cnt", [128, 1], F32)
            gei = S.sb("gei", [128, 1], U32)
            thr = S.sb("thr", [128, 1], F32)
            rden = S.sb("rden", [128, 16], F32)
            destf = S.sb("destf", [128, 2], F32)

            ld(wr[:], wr_d.ap().rearrange("(k p) c -> p k c", p=128), [DV["in"]], [wr])
            ld(bias_bc[:], br_d.ap().partition_broadcast(128), [DV["in"]], [bias_bc])
            mset(base_bc[:], 0.0, [base_bc])

            def stage_A(n):
                Sk = (n + 1) * 128
                qe, qo, qie, qio = qTe[n % 2], qTo[n % 2], qiTe[n % 2], qiTo[n % 2]
                NM = NMs[n % 2]
                ld(qie[:], qiTe_s.ap()[n], [DV["qiTe"]], [qie])
                ld(qio[:], qiTo_s.ap()[n], [DV["qiTo"]], [qio])
                ld(qe[:], qTe_s.ap()[n], [DV["qTe"]], [qe])
                ld(qo[:], qTo_s.ap()[n], [DV["qTo"]], [qo])
                bi = 0
                for ks in range(0, Sk, 512):
                    ke = min(Sk, ks + 512)
                    for h in range(8):
                        bk = B[bi % 2]
                        bi += 1
                        src = (qie if h % 2 == 0 else qio)[:, (h // 2) * 128:(h // 2 + 1) * 128]
                        mm(bk[:, 0:ke - ks], src, kiT2[:, ks:ke], True, True, [qie, qio, kiT2], [bk])
                        sg = sgn_all[:, n, h:h + 1]
                        act(bk[:, 0:ke - ks], bk[:, 0:ke - ks], AF.Relu, [bk, sgn_all], [bk], scale=sg)
                        if h == 0:
                            ts(score[:, ks:ke], bk[:, 0:ke - ks], sg, ALU.mult, [bk, sgn_all], [score])
                        else:
                            stt(score[:, ks:ke], bk[:, 0:ke - ks], sg, score[:, ks:ke], ALU.mult, ALU.add,
                                [bk, sgn_all, score], [score])
                mset(score[0:64, Sk - 64:Sk], -1.0e30, [score])
                if n < 2:
                    mset(thr[:], -1.0e29, [thr])
                else:
                    S.op("vector", (lambda Sk: lambda e: e.tensor_reduce(out=lo[:], in_=score[:, 0:Sk - 64], axis=AX.X,
                                                                         op=ALU.min))(Sk), [score], [lo])
                    S.op("vector", (lambda Sk: lambda e: e.tensor_reduce(out=w0c[:], in_=score[:, 0:Sk], axis=AX.X,
                                                                         op=ALU.max))(Sk), [score], [w0c])
                    stt(w0c[:], w0c[:], 1.0e-4, lo[:], ALU.add, ALU.subtract, [w0c, lo], [w0c])
                    for it in range(NITER):
                        stt(mid[:], w0c[:], 2.0 ** -(it + 1), lo[:], ALU.mult, ALU.add, [w0c, lo], [mid])
                        ts(NM[:, 0:Sk], score[:, 0:Sk], mid[:, 0:1], ALU.is_ge, [score, mid], [NM, cnt],
                           s2=0.0, op1=ALU.add, acc=cnt[:, 0:1])
                        ts(gei[:], cnt[:], 255.5, ALU.is_ge, [cnt], [gei])
                        S.op("vector", lambda e: e.copy_predicated(lo[:], gei[:], mid[:]), [gei, mid, lo], [lo])
                    cp(thr[:], lo[:], [lo], [thr])
                ts(NM[:, 0:Sk], score[:, 0:Sk], thr[:, 0:1], ALU.is_lt, [score, thr], [NM], s2=-30000.0, op1=ALU.mult)

            def stage_B(n):
                Sk = (n + 1) * 128
                qe, qo = qTe[n % 2], qTo[n % 2]
                NM = NMs[n % 2]
                ld(xt[:], x_d.ap()[n * 128:(n + 1) * 128, :], [DV["in"]], [xt])
                ld(yanl[:], ya_s.ap()[n * 128:(n + 1) * 128, :], [DV["ya"]], [yanl])
                for b3 in range(3):
                    mm(B[4 + b3][:], zerob[:, 0:128], zerob[:], True, False, [zerob], [B[4 + b3]], skip=True)
                units = [(kb, j) for kb in range(n + 1) for j in range(4)]

                def QK(u):
                    kb, j = units[u]
                    bk = B[2 + u % 2]
                    pt = pT[u % 3]
                    qsrc = (qe if j < 2 else qo)[:, (j % 2) * 512:(j % 2 + 1) * 512]
                    mm(bk[:], kT2[:, kb * 128:(kb + 1) * 128], qsrc, True, False, [kT2, qe, qo], [bk])
                    mm(bk[:], NM[:, kb * 128:(kb + 1) * 128],
                       identb[:].unsqueeze(1).broadcast_to([128, 4, 128]), False, True, [NM, identb], [bk])
                    act(pt[:], bk[:], AF.Exp, [bk], [pt], scale=0.125)

                def PV(u):
                    kb, j = units[u]
                    pt = pT[u % 3]
                    for hh in range(4):
                        pair = (j % 2) * 4 + hh
                        head = pair * 2 + (0 if j < 2 else 1)
                        ob = B[4 + head // 7]
                        off = (head % 7) * 65
                        mm(ob[:, off:off + 65], pt[:, hh * 128:(hh + 1) * 128], vaug[:, kb, :], False,
                           kb == n, [pt, vaug], [ob], skip=True)

                QK(0)
                for u in range(len(units)):
                    if u + 1 < len(units):
                        QK(u + 1)
                    PV(u)
                for b3 in range(3):
                    nh = 7 if b3 < 2 else 2
                    ov = B[4 + b3][:, 0:nh * 65].rearrange("p (h d) -> p h d", d=65)
                    S.op("vector", (lambda ov, b3, nh: lambda e: e.reciprocal(
                        rden[:, b3 * 7:b3 * 7 + nh].unsqueeze(2), ov[:, :, 64:65]))(ov, b3, nh), [B[4 + b3]], [rden])
                    tt(yb[:, b3 * 448:b3 * 448 + nh * 64].rearrange("p (h d) -> p h d", d=64), ov[:, :, 0:64],
                       rden[:, b3 * 7:b3 * 7 + nh].unsqueeze(2).broadcast_to([128, nh, 64]), ALU.mult,
                       [B[4 + b3], rden], [yb])
                act(junk[:, 0:1024], yb[:], AF.Square, [yb], [junk, col[0]], acc=col[0][:, 0:1])
                rsqrt_col(col[1][:, 0:1], col[0][:, 0:1], 1.0 / 1024, [col[0]], [col[1]], col[2])
                ts(ybn[:], yb[:], col[1][:, 0:1], ALU.mult, [yb, col[1]], [ybn])
                for k in range(16):
                    bk = B[2 + k // 8]
                    srcy = yanl[:, k * 128:(k + 1) * 128] if k < 8 else ybn[:, (k - 8) * 128:(k - 7) * 128]
                    tr(bk[:].bitcast(BF16)[:, (k % 8) * 128:(k % 8 + 1) * 128], srcy, identb[:],
                       [yanl, ybn, identb], [bk])
                cp(yT[:, 0:8, :], B[2][:].bitcast(BF16).rearrange("p (a t) -> p a t", t=128), [B[2]], [yT.sub[0]])
                cp(yT[:, 8:16, :], B[3][:].bitcast(BF16).rearrange("p (a t) -> p a t", t=128), [B[3]], [yT.sub[1]],
                   eng="scalar")
                for db in range(4):
                    bk = B[(7, 4, 5, 6)[db]]
                    for k in range(16):
                        mm(bk[:], yT[:, k, :], woutb[:, k, db * 512:(db + 1) * 512], k == 0, k == 15,
                           [yT.sub[k // 8], woutb], [bk])
                    tt(xm[:, db * 512:(db + 1) * 512], bk[:], xt[:, db * 512:(db + 1) * 512], ALU.add, [bk, xt], [xm])
                ld(xmid_s.ap()[n * 128:(n + 1) * 128, :], xm[:], [xm], [DV["xmid"]], q="gpsimd")
                act(junk[:], xm[:], AF.Square, [xm], [junk, col[3]], acc=col[3][:, 0:1])
                rsqrt_col(col[4][:, 0:1], col[3][:, 0:1], 1.0 / D, [col[3]], [col[4]], col[5])
                ts(xt[:], xm[:], col[4][:, 0:1], ALU.mult, [xm, col[4]], [xt])
                act(xn2b[:], xm[:], AF.Copy, [xm, col[4]], [xn2b], scale=col[4][:, 0:1])
                for g4 in range(4):
                    bk = B[2 + g4 % 2]
                    for k in range(g4 * 4, g4 * 4 + 4):
                        tr(bk[:, (k % 4) * 128:(k % 4 + 1) * 128], xt[:, k * 128:(k + 1) * 128], ident[:],
                           [xt, ident], [bk])
                    for k in range(g4 * 4, g4 * 4 + 4):
                        if g4 % 2 == 0:
                            ts(h2T[:, k, :], bk[:, (k % 4) * 128:(k % 4 + 1) * 128], pvec[:, 32 + k:33 + k], ALU.mult,
                               [bk, pvec], [h2T.sub[k]], s2=pvec[:, 48 + k:49 + k], op1=ALU.add)
                        else:
                            act(h2T[:, k, :], bk[:, (k % 4) * 128:(k % 4 + 1) * 128], AF.Identity, [bk, pvec],
                                [h2T.sub[k]], bias=pvec[:, 48 + k:49 + k], scale=pvec[:, 32 + k:33 + k])
                for k in range(16):
                    mm(B[7][:, 0:36], h2T[:, k, :], wr[:, k, :], k == 0, k == 15, [h2T.sub[k], wr], [B[7]])
                tt(lg[:], B[7][:, 0:36], bias_bc[:], ALU.add, [B[7], bias_bc], [lg])
                S.op("vector", lambda e: e.tensor_reduce(out=sm[:, 0:1], in_=lg[:, 0:4], axis=AX.X, op=ALU.max), [lg], [sm])
                ts(sm[:, 1:2], sm[:, 0:1], -1.0, ALU.mult, [sm], [sm])
                act(sm[:, 4:8], lg[:, 0:4], AF.Exp, [lg, sm], [sm, col[6]], bias=sm[:, 1:2], scale=1.0,
                    acc=col[6][:, 0:1])
                S.op("vector", lambda e: e.reciprocal(sm[:, 2:3], col[6][:, 0:1]), [col[6]], [sm])
                ts(sm[:, 8:12], lg[:, 0:4], sm[:, 0:1], ALU.is_ge, [lg, sm], [sm], s2=1.0e9, op1=ALU.mult)
                ts(sm[:, 8:12], sm[:, 8:12], -1.0e9, ALU.add, [sm], [sm])
                tt(em[:].rearrange("p (g j) -> p g j", j=8), lg[:, 4:36].rearrange("p (g j) -> p g j", j=8),
                   sm[:, 8:12].unsqueeze(2).broadcast_to([128, 4, 8]), ALU.add, [lg, sm], [em])
                S.op("vector", lambda e: e.max(m8[:], em[:]), [em], [m8])
                S.op("vector", lambda e: e.max_index(i8[:], m8[:], em[:]), [m8, em], [i8])
                tt(sm[:, 12:13], m8[:, 1:2], m8[:, 0:1], ALU.subtract, [m8], [sm])
                act(sm[:, 13:14], sm[:, 12:13], AF.Exp, [sm], [sm])
                ts(sm[:, 13:14], sm[:, 13:14], 1.0, ALU.add, [sm], [sm])
                S.op("vector", lambda e: e.reciprocal(sm[:, 14:15], sm[:, 13:14]), [sm], [sm])
                tt(wgt_all[:, n, 0:1], sm[:, 14:15], sm[:, 2:3], ALU.mult, [sm], [wgt_all])
                tt(wgt_all[:, n, 1:2], sm[:, 2:3], wgt_all[:, n, 0:1], ALU.subtract, [sm, wgt_all], [wgt_all])
                ts(Ab[:], em[:], m8[:, 1:2], ALU.is_ge, [em, m8], [Ab])
                ts(oh0[:], em[:], m8[:, 0:1], ALU.is_ge, [em, m8], [oh0])
                tt(oh1[:], Ab[:], oh0[:], ALU.subtract, [Ab, oh0], [oh1])
                mm(B[4][:, 0:32], LTb[:], Ab[:], True, True, [LTb, Ab], [B[4]])
                tt(posf[:], B[4][:, 0:32], base_bc[:], ALU.add, [B[4], base_bc], [posf])
                mm(B[4][:, 64:96], onesb[:], Ab[:], True, True, [onesb, Ab], [B[4]])
                tt(base_bc[:], B[4][:, 64:96], base_bc[:], ALU.add, [B[4], base_bc, posf], [base_bc])
                cp(eid_all[:, n, :], i8[:, 0:2], [i8], [eid_all])
                for j, oh in enumerate((oh0, oh1)):
                    tt(tmp32[:], posf[:], oh[:], ALU.mult, [posf, oh], [tmp32])
                    S.op("vector", (lambda n, j: lambda e: e.tensor_reduce(out=pos_all[:, n, j:j + 1], in_=tmp32[:],
                                                                            axis=AX.X, op=ALU.add))(n, j),
                         [tmp32], [pos_all])
                ld(xn2_s.ap()[n * 128:(n + 1) * 128, :], xn2b[:], [xn2b], [DV["xn2"]], q="gpsimd")

            stage_A(0)
            for n in range(NT):
                if n + 1 < NT:
                    stage_A(n + 1)
                stage_B(n)
            S.barrier()
            S.emit()

        with contextlib.ExitStack() as ph:
            S.stack = ph
            pa = S.sb("pa", [128, 32], F32)
            pb = S.sb("pb", [128, 32], F32)
            pi_ = S.sb("pi_", [128, 32], I32)
            padded = S.sb("padded", [128, 32], F32)
            pstart = S.sb("pstart", [128, 32], F32)
            iotI = S.sb("iotI", [128, NSB], I32)
            iotF = S.sb("iotF", [128, NSB], F32)
            cmp3 = S.sb("cmp3", [128, NSB, 32], F32)
            bef = S.sb("bef", [128, NSB], F32)
            iw12 = S.sb("iw12", [128, 12], I32)
            iw12f = S.sb("iw12f", [128, 12], F32)
            widxf = S.sb("widxf", [128, NSB, 12], F32)
            ohd = S.sb("ohd", [128, 32], F32)
            dcol = S.sb("dcol", [128, 2], F32)
            xr2 = [S.sb(f"xr2_{i}", [128, D], BF16) for i in range(2)]
            ts(pa[:], base_bc[:], float(C - 1), ALU.add, [base_bc], [pa], s2=1.0 / C, op1=ALU.mult)
            cp(pi_[:], pa[:], [pa], [pi_])
            cp(pb[:], pi_[:], [pi_], [pb])
            tt(pa[:], pb[:], pa[:], ALU.is_gt, [pb, pa], [pa])
            tt(pb[:], pb[:], pa[:], ALU.subtract, [pb, pa], [pb])
            ts(padded[:], pb[:], float(C), ALU.mult, [pb], [padded])
            cp(pa[:], padded[:], [padded], [pa])
            src_, dst_ = pa, pb
            for sh in (1, 2, 4, 8, 16):
                cp(dst_[:, 0:sh], src_[:, 0:sh], [src_], [dst_])
                tt(dst_[:, sh:32], src_[:, sh:32], src_[:, 0:32 - sh], ALU.add, [src_], [dst_])
                src_, dst_ = dst_, src_
            pend = src_
            tt(pstart[:], pend[:], padded[:], ALU.subtract, [pend, padded], [pstart])
            S.op("gpsimd", lambda e: e.iota(iotI[:], [[C, NSB]], base=0, channel_multiplier=0), [], [iotI])
            cp(iotF[:], iotI[:], [iotI], [iotF])
            tt(cmp3[:], pend[:].unsqueeze(1).broadcast_to([128, NSB, 32]),
               iotF[:].unsqueeze(2).broadcast_to([128, NSB, 32]), ALU.is_le, [pend, iotF], [cmp3])
            S.op("vector", lambda e: e.tensor_reduce(out=bef[:], in_=cmp3[:], axis=AX.X, op=ALU.add), [cmp3], [bef])
            ts(bef[:], bef[:], 31.0, ALU.min, [bef], [bef], s2=1536.0, op1=ALU.mult)
            S.op("gpsimd", lambda e: e.iota(iw12[:], [[128, 12]], base=0, channel_multiplier=1), [], [iw12])
            cp(iw12f[:], iw12[:], [iw12], [iw12f])
            tt(widxf[:], bef[:].unsqueeze(2).broadcast_to([128, NSB, 12]),
               iw12f[:].unsqueeze(1).broadcast_to([128, NSB, 12]), ALU.add, [bef, iw12f], [widxf])
            cp(widx[:], widxf[:], [widxf], [widx])
            S.op("gpsimd", lambda e: e.iota(pi_[:], [[1, 32]], base=0, channel_multiplier=0), [pi_], [pi_])
            cp(pa[:], pi_[:], [pi_], [pa])
            for n in range(NT):
                for j in range(2):
                    ts(ohd[:], pa[:], eid_all[:, n, j:j + 1], ALU.is_equal, [pa, eid_all], [ohd])
                    tt(ohd[:], ohd[:], pstart[:], ALU.mult, [ohd, pstart], [ohd])
                    S.op("vector", (lambda j: lambda e: e.tensor_reduce(out=dcol[:, j:j + 1], in_=ohd[:], axis=AX.X,
                                                                         op=ALU.add))(j), [ohd], [dcol])
                tt(dcol[:], dcol[:], pos_all[:, n, :], ALU.add, [dcol, pos_all], [dcol])
                cp(dest_all[:, n, :], dcol[:], [dcol], [dest_all])
                xr = xr2[n % 2]
                ld(xr[:], xn2_s.ap()[n * 128:(n + 1) * 128, :], [DV["xn2"]], [xr])
                for j in range(2):
                    S.dma("gpsimd", (lambda n, j, xr: lambda e: e.indirect_dma_start(
                        out=xe_s.ap(), out_offset=bass.IndirectOffsetOnAxis(ap=dest_all[:, n, j:j + 1], axis=0),
                        in_=xr[:], in_offset=None, bounds_check=None, oob_is_err=False))(n, j, xr),
                        [xr, dest_all, DV["xe"]], [DV["xe"]], owner=xr)
            S.barrier()
            S.emit()

        with contextlib.ExitStack() as ph:
            S.stack = ph
            NB = C // 128
            stg = [S.sb(f"stgE{i}", [128, 4096], F32) for i in range(4)]
            wpb = [S.subs(S.sb(f"wpb{i}", [128, 4096], BF16), 2, "p3") for i in range(3)]
            xer = [[S.sb(f"xer{i}_{b}", [128, D], BF16) for b in range(NB)] for i in range(2)]
            xeT = S.subs(S.sb("xeT", [128, 16, C], BF16), 16, "p3")
            gTt = S.sb("gTt", [128, 8, C], BF16)
            sa = [S.sb(f"sa{i}", [128, C], F32) for i in range(2)]
            yo = [S.sb(f"yo{i}", [128, 512], F32) for i in range(6)]
            wexp_rows = wexp_d.ap().rearrange("e q p c -> (e q p) c")
            pieces = [(j, p) for j in range(NSB) for p in range(12)]
            rot = [0]

            def nbank():
                bk = B[2 + rot[0] % 6]
                rot[0] += 1
                return bk

            def emit_gather(i):
                j, piece = pieces[i]
                sg = stg[i % 4]
                S.dma("gpsimd", (lambda sg, j, piece: lambda e: e.indirect_dma_start(
                    out=sg[:], out_offset=None, in_=wexp_rows,
                    in_offset=bass.IndirectOffsetOnAxis(ap=widx[:, j, piece:piece + 1], axis=0),
                    bounds_check=None, oob_is_err=False))(sg, j, piece), [DV["in"], widx], [sg], owner=sg)

            def emit_cast(i):
                sg = stg[i % 4]
                wb = wpb[i % 3]
                cp(wb[:, 0:2048], sg[:, 0:2048], [sg], [wb.sub[0]], eng="vector")
                cp(wb[:, 2048:4096], sg[:, 2048:4096], [sg], [wb.sub[1]], eng="scalar")

            def load_xe(j):
                for blk in range(NB):
                    xr = xer[j % 2][blk]
                    ld(xr[:], xe_s.ap()[j * C + blk * 128:j * C + (blk + 1) * 128, :], [DV["xe"]], [xr])

            def prologue_part(j, part):
                for kp in (2 * part, 2 * part + 1):
                    bk = B[kp % 2]
                    bv = bk[:].bitcast(BF16)
                    for kk in range(2):
                        k = kp * 2 + kk
                        for blk in range(NB):
                            xr = xer[j % 2][blk]
                            tr(bv[:, (kk * NB + blk) * 128:(kk * NB + blk + 1) * 128], xr[:, k * 128:(k + 1) * 128],
                               identb[:], [xr, identb], [bk])
                    for kk in range(2):
                        k = kp * 2 + kk
                        src = bv[:, kk * C:(kk + 1) * C]
                        if kp % 2 == 0:
                            ts(xeT[:, k, :], src, pvec[:, 32 + k:33 + k], ALU.mult, [bk, pvec], [xeT.sub[k]],
                               s2=pvec[:, 48 + k:49 + k], op1=ALU.add)
                        else:
                            act(xeT[:, k, :], src, AF.Identity, [bk, pvec], [xeT.sub[k]],
                                bias=pvec[:, 48 + k:49 + k], scale=pvec[:, 32 + k:33 + k])

            yoi = [0]

            def compute(i):
                j, piece = pieces[i]
                wb = wpb[i % 3]
                if piece < 8:
                    f = piece
                    w13 = wb[:].rearrange("p (t k c) -> p t k c", t=2, k=16)
                    ba, bb = nbank(), nbank()
                    for k in range(16):
                        mm(ba[:, 0:C], w13[:, 0, k, :], xeT[:, k, :], k == 0, k == 15, [wb.sub[0], xeT.sub[k]], [ba])
                    for k in range(16):
                        mm(bb[:, 0:C], w13[:, 1, k, :], xeT[:, k, :], k == 0, k == 15, [wb.sub[1], xeT.sub[k]], [bb])
                    s_ = sa[f % 2]
                    act(s_[:], ba[:, 0:C], AF.Silu, [ba], [s_])
                    tt(gTt[:, f, :], bb[:, 0:C], s_[:], ALU.mult, [bb, s_], [gTt])
                else:
                    db = piece - 8
                    w2v = wb[:].rearrange("p (k c) -> p k c", k=8)
                    for blk in range(NB):
                        bk = nbank()
                        for fc in range(8):
                            mm(bk[:], gTt[:, fc, blk * 128:(blk + 1) * 128], w2v[:, fc, :], fc == 0, fc == 7,
                               [gTt, wb.sub[fc // 4]], [bk])
                        y_ = yo[yoi[0] % 6]
                        yoi[0] += 1
                        if blk % 2 == 0:
                            cp(y_[:], bk[:], [bk], [y_])
                        else:
                            cp(y_[:], bk[:], [bk], [y_], eng="scalar")
                        ld(ye_s.ap()[j * C + blk * 128:j * C + (blk + 1) * 128, db * 512:(db + 1) * 512], y_[:],
                           [y_], [DV["ye"]], q="sync")

            load_xe(0)
            emit_gather(0)
            emit_gather(1)
            emit_gather(2)
            emit_cast(0)
            for part in range(4):
                prologue_part(0, part)
            if NSB > 1:
                load_xe(1)
            for i, (j, piece) in enumerate(pieces):
                if i + 3 < len(pieces):
                    emit_gather(i + 3)
                if i + 1 < len(pieces):
                    emit_cast(i + 1)
                compute(i)
                if piece >= 8 and j + 1 < NSB:
                    prologue_part(j + 1, piece - 8)
                if piece == 11 and j + 2 < NSB:
                    load_xe(j + 2)
            S.barrier()
            S.emit()

        with contextlib.ExitStack() as ph:
            S.stack = ph
            gate2_bc = S.sb("gate2_bc", [128, D], F32)
            fg_bc = S.sb("fg_bc", [128, D], F32)
            y0 = [S.sb(f"y0_{i}", [128, D], F32) for i in range(2)]
            y1 = [S.sb(f"y1_{i}", [128, D], F32) for i in range(2)]
            xmt = [S.sb(f"xmt{i}", [128, D], F32) for i in range(2)]
            acc = S.sb("acc", [128, D], F32)
            ot = [S.sb(f"ot{i}", [128, D], F32) for i in range(2)]
            ld(gate2_bc[:], mod_s.ap()[5 * D:6 * D].partition_broadcast(128), [DV["mod"]], [gate2_bc])
            ld(fg_bc[:], fg_d.ap().partition_broadcast(128), [DV["in"]], [fg_bc])
            for n in range(NT):
                a0, a1, xq, o_ = y0[n % 2], y1[n % 2], xmt[n % 2], ot[n % 2]
                for j, dst in enumerate((a0, a1)):
                    S.dma("gpsimd", (lambda n, j, dst: lambda e: e.indirect_dma_start(
                        out=dst[:], out_offset=None, in_=ye_s.ap(),
                        in_offset=bass.IndirectOffsetOnAxis(ap=dest_all[:, n, j:j + 1], axis=0),
                        bounds_check=None, oob_is_err=False))(n, j, dst),
                        [DV["ye"], dest_all], [dst], owner=dst)
                ld(xq[:], xmid_s.ap()[n * 128:(n + 1) * 128, :], [DV["xmid"]], [xq])
                act(acc[:], a0[:], AF.Copy, [a0, wgt_all], [acc], scale=wgt_all[:, n, 0:1])
                stt(acc[:], a1[:], wgt_all[:, n, 1:2], acc[:], ALU.mult, ALU.add, [a1, wgt_all, acc], [acc])
                tt(acc[:], acc[:], gate2_bc[:], ALU.mult, [acc, gate2_bc], [acc], eng="gpsimd")
                tt(acc[:], acc[:], xq[:], ALU.add, [acc, xq], [acc])
                act(junk[:], acc[:], AF.Square, [acc], [junk, col[0]], acc=col[0][:, 0:1])
                rsqrt_col(col[1][:, 0:1], col[0][:, 0:1], 1.0 / D, [col[0]], [col[1]], col[2])
                stt(o_[:], acc[:], col[1][:, 0:1], fg_bc[:], ALU.mult, ALU.mult, [acc, col[1], fg_bc], [o_])
                ld(out_d.ap()[n * 128:(n + 1) * 128, :], o_[:], [o_], [DV["out"]], q="sync")
            S.barrier()
            S.emit()
    return nc


_CACHE = {}


def _prep_weights(inp):
    f = lambda a: np.ascontiguousarray(np.asarray(a, dtype=np.float32))
    w1 = np.asarray(inp["w1"], dtype=np.float32)[0]
    w3 = np.asarray(inp["w3"], dtype=np.float32)[0]
    w2 = np.asarray(inp["w2"], dtype=np.float32)[0]
    wexp = np.empty((NEXP, 12, 128, 4096), dtype=np.float32)
    a1 = w1.reshape(NEXP, 16, 128, 8, 128).transpose(0, 3, 2, 1, 4)
    a3 = w3.reshape(NEXP, 16, 128, 8, 128).transpose(0, 3, 2, 1, 4)
    v = wexp[:, 0:8].reshape(NEXP, 8, 128, 2, 16, 128)
    v[:, :, :, 0] = a1
    v[:, :, :, 1] = a3
    a2 = w2.reshape(NEXP, 8, 128, 4, 512).transpose(0, 3, 2, 1, 4)
    wexp[:, 8:12] = a2.reshape(NEXP, 4, 128, 4096)
    shared = {
        "w_ada": f(inp["w_ada"][0]), "b_ada": f(inp["b_ada"][0]), "norm1_g": f(inp["norm1_g"][0]),
        "w_in": f(inp["w_in"][0]), "v_norm_g": f(inp["v_norm_g"][0]).reshape(-1),
        "v_norm_b": f(inp["v_norm_b"][0]).reshape(-1), "w_sp": f(inp["w_sp"][0]), "b_sp": f(inp["b_sp"][0]),
        "kv_norm_g": f(inp["kv_norm_g"][0]), "w_uk": f(inp["w_uk"][0]), "w_uv": f(inp["w_uv"][0]),
        "kidx_norm_g": f(inp["kidx_norm_g"][0]), "gnorm_a_g": f(inp["gnorm_a_g"][0]),
        "gnorm_b_g": f(inp["gnorm_b_g"][0]), "w_out": f(inp["w_out"][0]), "norm2_g": f(inp["norm2_g"][0]),
        "w_r": np.ascontiguousarray(np.concatenate([np.asarray(inp["w_group"][0]), np.asarray(inp["w_expert"][0])],
                                                   axis=1).astype(np.float32)),
        "b_r": np.ascontiguousarray(np.concatenate([np.asarray(inp["b_group"][0]), np.asarray(inp["b_expert"][0])],
                                                   axis=0).astype(np.float32)),
        "wexp": wexp, "final_g": f(inp["final_g"]),
    }
    return shared


def kernel(**inputs):
    x = np.asarray(inputs["x"], dtype=np.float32)
    c = np.asarray(inputs["c"], dtype=np.float32)
    pos = np.asarray(inputs["positions"], dtype=np.int32)
    nb, seq, _ = x.shape
    NT = seq // 128
    key = (NT,)
    if key not in _CACHE:
        _CACHE[key] = build(NT=NT)
    nc = _CACHE[key]
    shared = _prep_weights(inputs)
    in_maps = []
    for b in range(nb):
        m = dict(shared)
        m["x"] = np.ascontiguousarray(x[b])
        m["c"] = np.ascontiguousarray(c[b])
        m["pos"] = np.ascontiguousarray(pos[b])
        in_maps.append(m)
    res = run_bass_kernel_spmd(nc, in_maps, core_ids=list(range(nb)))
    return np.stack([np.asarray(r["out"], dtype=np.float32) for r in res.results], axis=0)
```

```python
import contextlib
import math
import numpy as np
import concourse.bass as bass
import concourse.mybir as mybir
from concourse.bass_utils import run_bass_kernel_spmd

F32 = mybir.dt.float32
BF16 = mybir.dt.bfloat16
I32 = mybir.dt.int32
U32 = mybir.dt.uint32
AF = mybir.ActivationFunctionType
ALU = mybir.AluOpType
AX = mybir.AxisListType

FLAGS = {"hT": 1, "p3": 1, "yT": 1, "reorder": 1}
EPOCH = 12000
ENGS = ["tensor", "vector", "scalar", "gpsimd", "sync"]
D = 2048
NEXP = 32
EPS = 1e-6


class Buf:
    __slots__ = ("name", "w", "r", "dsem", "dcum", "t", "multi", "sub")

    def __init__(self, name, t=None):
        self.name = name
        self.multi = False
        self.w = {}
        self.r = {}
        self.dsem = None
        self.dcum = 0
        self.t = t

    def __getitem__(self, k):
        return self.t[k]


class Sched:
    def __init__(self, nc, semstack):
        self.nc = nc
        self.semstack = semstack
        self.stack = semstack
        self.ops = {e: [] for e in ENGS}
        self.cnt = {e: 0 for e in ENGS}
        self.sems = {}
        self.waited = {e: {} for e in ENGS}
        self.cur_cum = {}
        self.nsem = 0
        self.bufs = []

    def _newsem(self, name):
        self.nsem += 1
        return self.semstack.enter_context(self.nc.semaphore(f"{name}_{self.nsem}"))

    def sb(self, name, shape, dtype):
        t = self.stack.enter_context(self.nc.sbuf_tensor(name, list(shape), dtype))
        b = Buf(name, t)
        self.bufs.append(b)
        return b

    def ps(self, name, shape, dtype):
        t = self.stack.enter_context(self.nc.psum_tensor(name, list(shape), dtype))
        b = Buf(name, t)
        self.bufs.append(b)
        return b

    def subs(self, buf, n, flag=None):
        buf.sub = []
        if flag is not None and not FLAGS.get(flag, 1):
            buf.sub = [buf] * n
            return buf
        for i in range(n):
            b = Buf(f"{buf.name}_s{i}", buf.t)
            self.bufs.append(b)
            buf.sub.append(b)
        return buf

    def view(self, name):
        b = Buf(name, None)
        b.multi = True
        self.bufs.append(b)
        return b

    def _engkey(self, eng):
        idx = self.cnt[eng]
        ep = idx // EPOCH
        key = ("E", eng, ep)
        if key not in self.sems:
            self.sems[key] = self._newsem(f"e_{eng}_{ep}")
        return key, (idx % EPOCH) + 1

    def _collect(self, eng, reads, writes):
        deps = {}

        def add(d):
            for k, v in d.items():
                if k[0] == "D":
                    v = max(v, self.cur_cum.get(k, v))
                if deps.get(k, 0) < v:
                    deps[k] = v
        for b in reads:
            add(b.w)
        for b in writes:
            if not b.multi:
                add(b.w)
            add(b.r)
        out = []
        wd = self.waited[eng]
        for k, v in deps.items():
            if k[0] == "E" and k[1] == eng and eng in ("tensor", "sync"):
                continue
            if wd.get(k, 0) >= v:
                continue
            wd[k] = v
            out.append((k, v))
        return out

    def _record(self, me, reads, writes):
        k, v = me
        for b in writes:
            if b.multi:
                if b.w.get(k, 0) < v:
                    b.w[k] = v
            else:
                b.w = {k: v}
                b.r = {}
        for b in reads:
            if b.r.get(k, 0) < v:
                b.r[k] = v

    def op(self, eng, fn, reads=(), writes=()):
        waits = self._collect(eng, reads, writes)
        key, val = self._engkey(eng)
        self.ops[eng].append((waits, fn, key, 1))
        self.cnt[eng] += 1
        self._record((key, val), reads, writes)

    def dma(self, queue, fn, reads=(), writes=(), owner=None):
        waits = self._collect(queue, reads, writes)
        if owner is None:
            owner = (list(writes) + list(reads))[0]
        if owner.dsem is None or owner.dcum + 16 > EPOCH * 2:
            key = ("D", id(owner), self.nsem)
            self.sems[key] = self._newsem("d_" + owner.name)
            owner.dsem = key
            owner.dcum = 0
        owner.dcum += 16
        key = owner.dsem
        self.cur_cum[key] = owner.dcum
        self.ops[queue].append((waits, fn, key, 16))
        self._record((key, owner.dcum), reads, writes)

    def raw(self, eng, fn, reads=()):
        waits = self._collect(eng, reads, [])
        self.ops[eng].append((waits, fn, "RAW", 0))

    def barrier(self):
        allk = {}
        for e in ENGS:
            if self.cnt[e] > 0:
                idx = self.cnt[e] - 1
                allk[("E", e, idx // EPOCH)] = (idx % EPOCH) + 1
        for k, v in self.cur_cum.items():
            allk[k] = v
        for e in ENGS:
            wd = self.waited[e]
            ws = []
            for k, v in allk.items():
                if wd.get(k, 0) >= v:
                    continue
                wd[k] = v
                if k[0] == "E" and k[1] == e:
                    continue
                ws.append((k, v))
            if ws:
                self.ops[e].append((ws, None, None, 0))
        for b in self.bufs:
            b.w = {}
            b.r = {}

    def emit(self):
        nc = self.nc
        with nc.Block() as block:
            for e in ENGS:
                ops = self.ops[e]
                if not ops:
                    continue

                def body(eng, ops=ops):
                    for waits, fn, key, inc in ops:
                        for k, v in waits:
                            eng.wait_ge(self.sems[k], v)
                        if fn is None:
                            continue
                        if key == "RAW":
                            fn(eng)
                        else:
                            fn(eng).then_inc(self.sems[key], inc)
                getattr(block, e)(body)
        self.ops = {e: [] for e in ENGS}


def build(NT=32, NITER=16, dbg=False):
    ST = NT * 128
    C = 512
    NSB = (2 * ST + C - 1) // C + NEXP
    nc = bass.Bass("TRN2", target_bir_lowering=False)

    def din(name, shape, dt=F32):
        return nc.dram_tensor(name, list(shape), dt, kind="ExternalInput")

    def dscr(name, shape, dt=F32):
        if dbg:
            return nc.dram_tensor(name, list(shape), dt, kind="ExternalOutput")
        return nc.dram_tensor(name, list(shape), dt)

    x_d = din("x", [ST, D])
    c_d = din("c", [D])
    pos_d = din("pos", [ST], I32)
    wada_d = din("w_ada", [D, 6 * D])
    bada_d = din("b_ada", [6 * D])
    n1g_d = din("norm1_g", [D])
    win_d = din("w_in", [D, 3912])
    vng_d = din("v_norm_g", [1024])
    vnb_d = din("v_norm_b", [1024])
    wsp_d = din("w_sp", [8, 128, 128])
    bsp_d = din("b_sp", [8, 128])
    kvg_d = din("kv_norm_g", [256])
    wuk_d = din("w_uk", [256, 64])
    wuv_d = din("w_uv", [256, 64])
    kig_d = din("kidx_norm_g", [64])
    gna_d = din("gnorm_a_g", [1024])
    gnb_d = din("gnorm_b_g", [1024])
    wout_d = din("w_out", [D, D])
    n2g_d = din("norm2_g", [D])
    wr_d = din("w_r", [D, 36])
    br_d = din("b_r", [36])
    wexp_d = din("wexp", [NEXP, 12, 128, 4096])
    fg_d = din("final_g", [D])
    out_d = nc.dram_tensor("out", [ST, D], F32, kind="ExternalOutput")

    mod_s = dscr("mod_s", [6 * D])
    ya_s = dscr("ya_s", [ST, 1024], BF16)
    qTe_s = dscr("qTe_s", [NT, 128, 1024], BF16)
    qTo_s = dscr("qTo_s", [NT, 128, 1024], BF16)
    qiTe_s = dscr("qiTe_s", [NT, 128, 512], BF16)
    qiTo_s = dscr("qiTo_s", [NT, 128, 512], BF16)
    xmid_s = dscr("xmid_s", [ST, D])
    xn2_s = dscr("xn2_s", [ST, D], BF16)
    xe_s = dscr("xe_s", [NSB * C, D], BF16)
    ye_s = dscr("ye_s", [NSB * C, D])

    with contextlib.ExitStack() as outer:
        S = Sched(nc, outer)
        B = [S.ps(f"B{i}", [128, 512], F32) for i in range(8)]
        DV = {n: S.view("dv_" + n) for n in
              ["in", "mod", "ya", "qTe", "qTo", "qiTe", "qiTo", "xmid", "xe", "ye", "out", "xn2"]}

        def mm(o, l, r, st, sp, R, W, skip=False):
            S.op("tensor", lambda e: e.matmul(o, l, r, start=st, stop=sp, skip_group_check=skip), R, W)

        def tr(o, i, idn, R, W):
            S.op("tensor", lambda e: e.transpose(o, i, idn), R, W)

        def act(o, i, f, R, W, bias=None, scale=None, acc=None):
            kw = {}
            if bias is not None:
                kw["bias"] = bias
            if scale is not None:
                kw["scale"] = scale
            if acc is not None:
                kw["accum_out"] = acc
            S.op("scalar", lambda e: e.activation(out=o, in_=i, func=f, **kw), R, W)

        def ts(o, i, s1, op0, R, W, s2=None, op1=None, eng="vector", acc=None):
            if acc is not None:
                S.op(eng, lambda e: e.tensor_scalar(o, i, s1, s2, op0, op1, accum_out=acc), R, W)
            elif op1 is None:
                S.op(eng, lambda e: e.tensor_scalar(o, i, s1, None, op0), R, W)
            else:
                S.op(eng, lambda e: e.tensor_scalar(o, i, s1, s2, op0, op1), R, W)

        def tt(o, a, b, op, R, W, eng="vector"):
            S.op(eng, lambda e: e.tensor_tensor(out=o, in0=a, in1=b, op=op), R, W)

        def stt(o, a, s, b, op0, op1, R, W):
            S.op("vector", lambda e: e.scalar_tensor_tensor(out=o, in0=a, scalar=s, in1=b, op0=op0, op1=op1), R, W)

        def cp(o, i, R, W, eng="vector"):
            if eng == "scalar":
                S.op("scalar", lambda e: e.copy(o, i), R, W)
            else:
                S.op(eng, lambda e: e.tensor_copy(o, i), R, W)

        def mset(ap, val, W, eng="vector"):
            S.op(eng, lambda e: e.memset(ap, val), [], W)

        def ld(o, i, R, W, q="sync", owner=None):
            S.dma(q, lambda e: e.dma_start(out=o, in_=i), R, W, owner=owner)

        def rsqrt_col(dst, src, scale, R, W, tmp):
            w_ = src.shape[-1]
            ts(tmp[:, 0:w_], src, scale, ALU.mult, R, [tmp], s2=EPS, op1=ALU.add)
            act(tmp[:, 0:w_], tmp[:, 0:w_], AF.Sqrt, [tmp], [tmp])
            S.op("vector", lambda e: e.reciprocal(dst, tmp[:, 0:w_]), [tmp], W)

        ident = S.sb("ident", [128, 128], F32)
        identb = S.sb("identb", [128, 128], BF16)
        onesf = S.sb("onesf", [128, 128], F32)
        onesb = S.sb("onesb", [128, 128], BF16)
        zerob = S.sb("zerob", [128, 512], BF16)
        LTb = S.sb("LTb", [128, 128], BF16)
        pvec = S.sb("pvec", [128, 64], F32)
        gT = S.sb("gT", [128, 58], F32)
        kT2 = S.sb("kT2", [128, ST], BF16)
        kiT2 = S.sb("kiT2", [128, ST], BF16)
        vaug = S.sb("vaug", [128, NT, 65], BF16)
        sgn_all = S.sb("sgn_all", [128, NT, 8], F32)
        dest_all = S.sb("dest_all", [128, NT, 2], I32)
        eid_all = S.sb("eid_all", [128, NT, 2], F32)
        pos_all = S.sb("pos_all", [128, NT, 2], F32)
        base_bc = S.sb("base_bc", [128, 32], F32)
        widx = S.sb("widx", [128, NSB, 12], I32)
        wgt_all = S.sb("wgt_all", [128, NT, 2], F32)
        junk = S.sb("junk", [128, 2048], BF16)
        col = [S.sb(f"col{i}", [128, 16], F32) for i in range(8)]

        mset(onesf[:], 1.0, [onesf], eng="gpsimd")
        mset(onesb[:], 1.0, [onesb], eng="gpsimd")
        mset(zerob[:], 0.0, [zerob], eng="gpsimd")
        S.op("gpsimd", lambda e: e.affine_select(out=ident[:], in_=onesf[:], pattern=[[1, 128]],
                                                 compare_op=ALU.is_equal, fill=0.0, base=0,
                                                 channel_multiplier=-1), [onesf], [ident])
        S.op("gpsimd", lambda e: e.affine_select(out=identb[:], in_=onesf[:], pattern=[[1, 128]],
                                                 compare_op=ALU.is_equal, fill=0.0, base=0,
                                                 channel_multiplier=-1), [onesf], [identb])
        S.op("gpsimd", lambda e: e.affine_select(out=LTb[:], in_=onesf[:], pattern=[[1, 128]],
                                                 compare_op=ALU.is_ge, fill=0.0, base=-1,
                                                 channel_multiplier=-1), [onesf], [LTb])
        mset(vaug[:, :, 64:65], 1.0, [vaug], eng="gpsimd")

        with contextlib.ExitStack() as ph:
            S.stack = ph
            stA = S.sb("stA", [112, 128], F32)
            stB = S.sb("stB", [58, 128], F32)
            siluc = S.sb("siluc", [128, 16], F32)
            cT = S.sb("cT", [128, 16], F32)
            badaT = S.sb("badaT", [128, 96], F32)
            modT = S.sb("modT", [128, 96], F32)
            modR = S.sb("modR", [96, 128], F32)
            wa = [S.sb(f"wa{i}", [128, 16, 512], F32) for i in range(2)]

            ld(stA[0:16, :], c_d.ap().rearrange("(k p) -> k p", p=128), [DV["in"]], [stA])
            ld(stA[16:112, :], bada_d.ap().rearrange("(k p) -> k p", p=128), [DV["in"]], [stA])
            r0 = 0
            for src, nr in [(n1g_d, 16), (n2g_d, 16), (gna_d, 8), (gnb_d, 8), (kvg_d, 2)]:
                ld(stB[r0:r0 + nr, :], src.ap().rearrange("(k p) -> k p", p=128), [DV["in"]], [stB])
                r0 += nr
            ld(stB[50:58, :], bsp_d.ap(), [DV["in"]], [stB])
            tr(B[0][:, 0:112], stA[0:112, :], ident[0:112, 0:112], [stA, ident], [B[0]])
            tr(B[1][:, 0:58], stB[0:58, :], ident[0:58, 0:58], [stB, ident], [B[1]])
            cp(cT[:], B[0][:, 0:16], [B[0]], [cT])
            act(siluc[:], cT[:], AF.Silu, [cT], [siluc])
            cp(badaT[:], B[0][:, 16:112], [B[0]], [badaT])
            cp(gT[:], B[1][:, 0:58], [B[1]], [gT])
            for cb in range(24):
                w = wa[cb % 2]
                ld(w[:], wada_d.ap().rearrange("(k p) c -> p k c", p=128)[:, :, cb * 512:(cb + 1) * 512],
                   [DV["in"]], [w])
                for cc in range(4):
                    j = cb * 4 + cc
                    for k in range(16):
                        mm(B[2][:, j:j + 1], w[:, k, cc * 128:(cc + 1) * 128], siluc[:, k:k + 1],
                           k == 0, k == 15, [w, siluc], [B[2]])
            tt(modT[:], B[2][:, 0:96], badaT[:], ALU.add, [B[2], badaT], [modT])
            stt(pvec[:, 0:16], modT[:, 16:32], 1.0, gT[:, 0:16], ALU.add, ALU.mult, [modT, gT], [pvec])
            cp(pvec[:, 16:32], modT[:, 0:16], [modT], [pvec])
            stt(pvec[:, 32:48], modT[:, 64:80], 1.0, gT[:, 16:32], ALU.add, ALU.mult, [modT, gT], [pvec])
            cp(pvec[:, 48:64], modT[:, 48:64], [modT], [pvec])
            tr(B[3][0:96, 0:128], modT[:, 0:96], ident[:], [modT, ident], [B[3]])
            cp(modR[:], B[3][0:96, 0:128], [B[3]], [modR])
            ld(mod_s.ap().rearrange("(j p) -> j p", p=128), modR[:], [modR], [DV["mod"]], q="gpsimd")
            S.barrier()
            S.emit()

        def prep_hT(xt, xn, hT, c_ss, c_rs, c_tmp, sc_off, sh_off):
            act(junk[:], xt[:], AF.Square, [xt], [junk, c_ss], acc=c_ss[:, 0:1])
            rsqrt_col(c_rs[:, 0:1], c_ss[:, 0:1], 1.0 / D, [c_ss], [c_rs], c_tmp)
            ts(xn[:], xt[:], c_rs[:, 0:1], ALU.mult, [xt, c_rs], [xn])
            for k in range(16):
                bk = B[k // 8]
                tr(bk[:].bitcast(BF16)[:, (k % 8) * 128:(k % 8 + 1) * 128], xn[:, k * 128:(k + 1) * 128],
                   identb[:], [xn, identb], [bk])
            for k in range(16):
                bk = B[k // 8]
                src = bk[:].bitcast(BF16)[:, (k % 8) * 128:(k % 8 + 1) * 128]
                if k < 8:
                    ts(hT[:, k, :], src, pvec[:, sc_off + k:sc_off + k + 1], ALU.mult, [bk, pvec], [hT.sub[k]],
                       s2=pvec[:, sh_off + k:sh_off + k + 1], op1=ALU.add)
                else:
                    act(hT[:, k, :], src, AF.Identity, [bk, pvec], [hT.sub[k]],
                        bias=pvec[:, sh_off + k:sh_off + k + 1], scale=pvec[:, sc_off + k:sc_off + k + 1])

        def load_w_bf(dst, src_ap_fn, ncols, stg, rowscale=None, colscale=None):
            step = 256
            i = 0
            for c0 in range(0, ncols, step):
                cw = min(step, ncols - c0)
                sg = stg[i % 2]
                ld(sg[:, :, 0:cw], src_ap_fn(c0, cw), [DV["in"]], [sg])
                eng = ["vector", "gpsimd"][i % 2]
                if rowscale is None:
                    if i % 3 == 2:
                        cp(dst[:, :, c0:c0 + cw], sg[:, :, 0:cw], [sg], [dst], eng="scalar")
                    else:
                        cp(dst[:, :, c0:c0 + cw], sg[:, :, 0:cw], [sg], [dst], eng=eng)
                else:
                    for k in range(16):
                        stt(dst[:, k, c0:c0 + cw], sg[:, k, 0:cw], rowscale[:, k:k + 1], colscale[:, c0:c0 + cw],
                            ALU.mult, ALU.mult, [sg] + rowscale_bufs, [dst])
                i += 1

        rowscale_bufs = []

        with contextlib.ExitStack() as ph:
            S.stack = ph
            wbf = S.sb("wbfA", [128, 16, 2048], BF16)
            stg = [S.sb(f"stgA{i}", [128, 16, 256], F32) for i in range(2)]
            xts = [S.sb(f"xtA{i}", [128, D], F32) for i in range(2)]
            xns = [S.sb(f"xnA{i}", [128, D], BF16) for i in range(2)]
            hTs = [S.subs(S.sb(f"hTA{i}", [128, 16, 128], BF16), 16, "hT") for i in range(2)]
            gu = S.sb("gu", [128, 1024], F32)
            gv = S.sb("gv", [128, 1024], F32)
            vn = S.sb("vn", [128, 1024], F32)
            vgb = S.sb("vgb", [128, 1024], BF16)
            Gbc = S.sb("Gbc", [128, 1024], F32)
            Bbc = S.sb("Bbc", [128, 1024], F32)
            wsp = S.sb("wsp", [128, 8, 128], F32)
            WmT = S.sb("WmT", [128, 8, 128], BF16)
            ya = S.sb("ya", [128, 1024], F32)
            yan = S.sb("yan", [128, 1024], BF16)
            stats = S.sb("stats", [128, 8, 6], F32)
            mv = S.sb("mv", [128, 8, 2], F32)

            win_v = win_d.ap().rearrange("(k p) c -> p k c", p=128)
            load_w_bf(wbf, lambda c0, cw: win_v[:, :, c0:c0 + cw], 2048, stg)
            ld(Gbc[:], vng_d.ap().partition_broadcast(128), [DV["in"]], [Gbc])
            ld(Bbc[:], vnb_d.ap().partition_broadcast(128), [DV["in"]], [Bbc])
            ld(wsp[:], wsp_d.ap().rearrange("g i j -> i g j"), [DV["in"]], [wsp])
            for g in range(8):
                bk = B[6 + g // 4]
                tr(bk[:, (g % 4) * 128:(g % 4 + 1) * 128], wsp[:, g, :], ident[:], [wsp, ident], [bk])
            for g in range(8):
                bk = B[6 + g // 4]
                cp(WmT[:, g, :], bk[:, (g % 4) * 128:(g % 4 + 1) * 128], [bk], [WmT])
            mset(WmT[64:128, :, 0:64], 0.0, [WmT])

            def H_a(n):
                xt = xts[n % 2]
                ld(xt[:], x_d.ap()[n * 128:(n + 1) * 128, :], [DV["in"]], [xt])
                prep_hT(xt, xns[n % 2], hTs[n % 2], col[0], col[1], col[2], 0, 16)

            def M_a(n):
                hT = hTs[n % 2]
                for cg in range(4):
                    for k in range(16):
                        mm(B[2 + cg][:], hT[:, k, :], wbf[:, k, cg * 512:(cg + 1) * 512], k == 0, k == 15,
                           [hT.sub[k], wbf], [B[2 + cg]])

            def E_a(n):
                act(gu[:, 0:512], B[2][:], AF.Gelu_apprx_tanh, [B[2]], [gu])
                act(gu[:, 512:1024], B[3][:], AF.Gelu_apprx_tanh, [B[3]], [gu])
                act(gv[:, 0:512], B[4][:], AF.Gelu_apprx_tanh, [B[4]], [gv])
                act(gv[:, 512:1024], B[5][:], AF.Gelu_apprx_tanh, [B[5]], [gv])
                for g in range(8):
                    S.op("vector", (lambda g: lambda e: e.bn_stats(stats[:, g, :], gv[:, g * 128:(g + 1) * 128]))(g),
                         [gv], [stats])
                for g in range(8):
                    S.op("vector", (lambda g: lambda e: e.bn_aggr(mv[:, g, :], stats[:, g, :]))(g), [stats], [mv])
                ts(col[4][:, 0:8], mv[:, :, 1], EPS, ALU.add, [mv], [col[4]])
                act(col[4][:, 0:8], col[4][:, 0:8], AF.Sqrt, [col[4]], [col[4]])
                S.op("vector", lambda e: e.reciprocal(col[3][:, 0:8], col[4][:, 0:8]), [col[4]], [col[3]])
                for g in range(8):
                    ts(vn[:, g * 128:(g + 1) * 128], gv[:, g * 128:(g + 1) * 128], mv[:, g, 0:1], ALU.subtract,
                       [gv, mv, col[3]], [vn], s2=col[3][:, g:g + 1], op1=ALU.mult)
                tt(vn[:], vn[:], Gbc[:], ALU.mult, [vn, Gbc], [vn], eng="gpsimd")
                tt(vgb[:], vn[:], Bbc[:], ALU.add, [vn, Bbc], [vgb])

            def E2_a(n):
                for g in range(8):
                    bk = B[6 + g // 4]
                    mm(bk[:, (g % 4) * 128:(g % 4 + 1) * 128], WmT[:, g, :], vgb[:, g * 128:(g + 1) * 128],
                       True, True, [WmT, vgb], [bk])
                for g in range(8):
                    bk = B[6 + g // 4]
                    stt(ya[:, g * 128:(g + 1) * 128], bk[:, (g % 4) * 128:(g % 4 + 1) * 128], gT[:, 50 + g:51 + g],
                        gu[:, g * 128:(g + 1) * 128], ALU.add, ALU.mult, [bk, gT, gu], [ya])
                act(junk[:, 0:1024], ya[:], AF.Square, [ya], [junk, col[5]], acc=col[5][:, 0:1])
                rsqrt_col(col[6][:, 0:1], col[5][:, 0:1], 1.0 / 1024, [col[5]], [col[6]], col[7])
                ts(yan[:], ya[:], col[6][:, 0:1], ALU.mult, [ya, col[6]], [yan])
                ld(ya_s.ap()[n * 128:(n + 1) * 128, :], yan[:], [yan], [DV["ya"]], q="gpsimd")

            H_a(0)
            for n in range(NT):
                M_a(n)
                if n + 1 < NT:
                    H_a(n + 1)
                if FLAGS.get("reorder", 1):
                    if n >= 1:
                        E2_a(n - 1)
                    E_a(n)
                else:
                    E_a(n)
                    E2_a(n)
            if FLAGS.get("reorder", 1):
                E2_a(NT - 1)
            S.barrier()
            S.emit()

        with contextlib.ExitStack() as ph:
            S.stack = ph
            NCB = 3912 - 2048
            wbf = S.sb("wbfB", [128, 16, NCB], BF16)
            stg = [S.sb(f"stgB{i}", [128, 16, 256], F32) for i in range(2)]
            xts = [S.sb(f"xtB{i}", [128, D], F32) for i in range(2)]
            xns = [S.sb(f"xnB{i}", [128, D], BF16) for i in range(2)]
            hTs = [S.subs(S.sb(f"hTB{i}", [128, 16, 128], BF16), 16, "hT") for i in range(2)]
            posR = S.sb("posR", [NT, 128], I32)
            posF = S.sb("posF", [NT, 128], F32)
            posT = S.sb("posT", [128, NT], F32)
            fr = S.sb("fr", [128, 32], F32)
            ang = S.sb("ang", [128, NT, 32], F32)
            rr = S.sb("rr", [128, NT, 32], F32)
            rq = S.sb("rq", [128, NT, 32], F32)
            rni = S.sb("rni", [128, NT, 32], I32)
            sin_t = S.sb("sin_t", [128, NT, 32], F32)
            cos_t = S.sb("cos_t", [128, NT, 32], F32)
            stkv = S.sb("stkv", [128, 2, 128], F32)
            wukv = S.sb("wukv", [128, 2, 128], BF16)
            kig_bc = S.sb("kig_bc", [128, 64], F32)
            qr = S.sb("qr", [128, 1024], BF16)
            qir = S.sb("qir", [128, 512], BF16)
            t1 = S.sb("t1", [128, 512], F32)
            t2 = S.sb("t2", [128, 512], F32)
            cw_ = S.sb("cw_", [128, 8, 32], F32)
            sw_ = S.sb("sw_", [128, 8, 32], F32)
            wis = S.sb("wis", [128, 8], F32)
            ckvb = S.sb("ckvb", [128, 256], BF16)
            ckvf = S.sb("ckvf", [128, 256], F32)
            kif = S.sb("kif", [128, 64], F32)
            ckvT = S.sb("ckvT", [128, 2, 128], BF16)
            kk = S.sb("kk", [128, 64], F32)
            kk2 = S.sb("kk2", [128, 128], BF16)
            kin = S.sb("kin", [128, 64], F32)
            kir = S.sb("kir", [128, 64], F32)
            kki2 = S.sb("kki2", [128, 128], BF16)
            qTe = S.sb("qTe", [128, 8, 128], BF16)
            qTo = S.sb("qTo", [128, 8, 128], BF16)
            qiTe = S.sb("qiTe", [128, 4, 128], BF16)
            qiTo = S.sb("qiTo", [128, 4, 128], BF16)

            win_v = win_d.ap().rearrange("(k p) c -> p k c", p=128)
            load_w_bf(wbf, lambda c0, cw: win_v[:, :, 2048 + c0:2048 + c0 + cw], NCB, stg)
            ld(posR[:], pos_d.ap().rearrange("(n p) -> n p", p=128), [DV["in"]], [posR])
            cp(posF[:], posR[:], [posR], [posF])
            tr(B[7][:, 0:NT], posF[0:NT, :], ident[0:NT, 0:NT], [posF, ident], [B[7]])
            cp(posT[:], B[7][:, 0:NT], [B[7]], [posT])
            for i in range(32):
                mset(fr[:, i:i + 1], float(np.float32(10000.0) ** np.float32(-i / 32.0)), [fr], eng="gpsimd")
            tt(ang[:], posT[:].unsqueeze(2).broadcast_to([128, NT, 32]),
               fr[:].unsqueeze(1).broadcast_to([128, NT, 32]), ALU.mult, [posT, fr], [ang])
            TWO_PI = 2.0 * math.pi
            C1 = 6.28125
            C2 = TWO_PI - C1
            for (dst, shift) in ((sin_t, 0.0), (cos_t, math.pi / 2)):
                ts(rq[:], ang[:], shift, ALU.add, [ang], [rq])
                ts(rr[:], rq[:], 1.0 / TWO_PI, ALU.mult, [rq], [rr])
                cp(rni[:], rr[:], [rr], [rni])
                cp(rr[:], rni[:], [rni], [rr])
                stt(rq[:], rr[:], -C1, rq[:], ALU.mult, ALU.add, [rr, rq], [rq])
                stt(rq[:], rr[:], -C2, rq[:], ALU.mult, ALU.add, [rr, rq], [rq])
                ts(rr[:], rq[:], math.pi, ALU.is_gt, [rq], [rr])
                stt(rq[:], rr[:], -TWO_PI, rq[:], ALU.mult, ALU.add, [rr, rq], [rq])
                ts(rr[:], rq[:], -math.pi, ALU.is_lt, [rq], [rr])
                stt(rq[:], rr[:], TWO_PI, rq[:], ALU.mult, ALU.add, [rr, rq], [rq])
                ts(rq[:], rq[:], 3.141592, ALU.min, [rq], [rq], s2=-3.141592, op1=ALU.max)
                act(dst[:], rq[:], AF.Sin, [rq], [dst])
            ld(stkv[:, :, 0:64], wuk_d.ap().rearrange("(c p) d -> p c d", p=128), [DV["in"]], [stkv])
            ld(stkv[:, :, 64:128], wuv_d.ap().rearrange("(c p) d -> p c d", p=128), [DV["in"]], [stkv])
            for c2 in range(2):
                ts(wukv[:, c2, :], stkv[:, c2, :], gT[:, 48 + c2:49 + c2], ALU.mult, [stkv, gT], [wukv])
            ld(kig_bc[:], kig_d.ap().partition_broadcast(128), [DV["in"]], [kig_bc])
            mset(qTe[:], 0.0, [qTe], eng="gpsimd")
            mset(qTo[:], 0.0, [qTo], eng="gpsimd")
            mset(qiTe[:], 0.0, [qiTe], eng="gpsimd")
            mset(qiTo[:], 0.0, [qiTo], eng="gpsimd")
            CSC = (8 ** -0.5) * (64 ** -0.5)

            def rope(o_lo, o_hi, x_lo, x_hi, cs, sn, nh, RB):
                a = t1[:, 0:nh * 32].rearrange("p (h d) -> p h d", d=32)
                b = t2[:, 0:nh * 32].rearrange("p (h d) -> p h d", d=32)
                tt(a, x_lo, cs, ALU.mult, RB, [t1])
                tt(b, x_hi, sn, ALU.mult, RB, [t2])
                tt(o_lo, a, b, ALU.subtract, [t1, t2], RB[-1:], eng="gpsimd")
                tt(a, x_lo, sn, ALU.mult, RB + [t1], [t1])
                tt(b, x_hi, cs, ALU.mult, RB + [t2], [t2])
                tt(o_hi, a, b, ALU.add, [t1, t2], RB[-1:], eng="gpsimd")

            zt = S.sb("zt", [128, D], BF16)
            mset(zt[:], 0.0, [zt], eng="gpsimd")
            zf_total = NSB * C // 512
            zf_done = [0]

            def H_b(n):
                xt = xts[n % 2]
                ld(xt[:], x_d.ap()[n * 128:(n + 1) * 128, :], [DV["in"]], [xt])
                want = (zf_total * (n + 1) + NT - 1) // NT
                while zf_done[0] < min(want, zf_total):
                    r = zf_done[0]
                    ld(xe_s.ap()[r * 512:(r + 1) * 512, :].rearrange("(a p) d -> p a d", p=128),
                       zt[:].unsqueeze(1).broadcast_to([128, 4, D]), [zt], [DV["xe"]], q="sync", owner=zt)
                    zf_done[0] += 1
                prep_hT(xt, xns[n % 2], hTs[n % 2], col[0], col[1], col[2], 0, 16)

            def M_b(n):
                hT = hTs[n % 2]
                widths = [512, 512, 512, NCB - 1536]
                for cg in range(4):
                    for k in range(16):
                        mm(B[2 + cg][:, 0:widths[cg]], hT[:, k, :], wbf[:, k, cg * 512:cg * 512 + widths[cg]],
                           k == 0, k == 15, [hT.sub[k], wbf], [B[2 + cg]])

            def E_b(n):
                cosb = cos_t[:, n, :].unsqueeze(1)
                sinb = sin_t[:, n, :].unsqueeze(1)
                for hb in range(2):
                    bk = B[2 + hb]
                    qv = bk[:].rearrange("p (h d) -> p h d", d=64)
                    ov = qr[:, hb * 512:(hb + 1) * 512].rearrange("p (h d) -> p h d", d=64)
                    rope(ov[:, :, 0:32], ov[:, :, 32:64], qv[:, :, 0:32], qv[:, :, 32:64],
                         cosb.broadcast_to([128, 8, 32]), sinb.broadcast_to([128, 8, 32]), 8,
                         [bk, cos_t, sin_t, qr])
                ts(wis[:], B[5][:, 320:328], CSC, ALU.mult, [B[5]], [wis])
                ts(sgn_all[:, n, :], B[5][:, 320:328], 0.0, ALU.is_ge, [B[5]], [sgn_all], s2=2.0, op1=ALU.mult)
                ts(sgn_all[:, n, :], sgn_all[:, n, :], -1.0, ALU.add, [sgn_all], [sgn_all])
                tt(cw_[:], cosb.broadcast_to([128, 8, 32]), wis[:].unsqueeze(2).broadcast_to([128, 8, 32]),
                   ALU.mult, [cos_t, wis], [cw_])
                tt(sw_[:], sinb.broadcast_to([128, 8, 32]), wis[:].unsqueeze(2).broadcast_to([128, 8, 32]),
                   ALU.mult, [sin_t, wis], [sw_])
                for hb in range(2):
                    bk = B[4 + hb]
                    src = bk[:, 256:512] if hb == 0 else bk[:, 0:256]
                    qv = src.rearrange("p (h d) -> p h d", d=64)
                    ov = qir[:, hb * 256:(hb + 1) * 256].rearrange("p (h d) -> p h d", d=64)
                    rope(ov[:, :, 0:32], ov[:, :, 32:64], qv[:, :, 0:32], qv[:, :, 32:64],
                         cw_[:, hb * 4:(hb + 1) * 4, :], sw_[:, hb * 4:(hb + 1) * 4, :], 4,
                         [bk, cw_, sw_, qir])
                cp(ckvf[:], B[4][:, 0:256], [B[4]], [ckvf])
                act(junk[:, 0:256], ckvf[:], AF.Square, [ckvf], [junk, col[3]], acc=col[3][:, 0:1])
                rsqrt_col(col[4][:, 0:1], col[3][:, 0:1], 1.0 / 256, [col[3]], [col[4]], col[5])
                cp(ckvb[:], ckvf[:], [ckvf], [ckvb], eng="scalar")
                for c2 in range(2):
                    tr(B[0][:].bitcast(BF16)[:, c2 * 128:(c2 + 1) * 128], ckvb[:, c2 * 128:(c2 + 1) * 128], identb[:],
                       [ckvb, identb], [B[0]])
                cp(ckvT[:], B[0][:].bitcast(BF16)[:, 0:256].rearrange("p (c t) -> p c t", t=128), [B[0]], [ckvT])
                for c2 in range(2):
                    mm(B[1][:, 0:128], ckvT[:, c2, :], wukv[:, c2, :], c2 == 0, c2 == 1, [ckvT, wukv], [B[1]])
                ts(vaug[:, n, 0:64], B[1][:, 64:128], col[4][:, 0:1], ALU.mult, [B[1], col[4]], [vaug])
                kv3 = B[1][:, 0:64].rearrange("p (h d) -> p h d", d=64)
                kk3 = kk[:].rearrange("p (h d) -> p h d", d=64)
                rope(kk3[:, :, 0:32], kk3[:, :, 32:64], kv3[:, :, 0:32], kv3[:, :, 32:64], cosb, sinb, 1,
                     [B[1], cos_t, sin_t, kk])
                ts(kk2[:, 0:64], kk[:], col[4][:, 0:1], ALU.mult, [kk, col[4]], [kk2])
                ts(kk2[:, 64:128], kk[:], col[4][:, 0:1], ALU.mult, [kk, col[4]], [kk2])
                tr(B[0][:].bitcast(BF16)[:, 256:384], kk2[:], identb[:], [kk2, identb], [B[0]])
                cp(kT2[:, n * 128:(n + 1) * 128], B[0][:].bitcast(BF16)[:, 256:384], [B[0]], [kT2])
                cp(kif[:], B[5][:, 256:320], [B[5]], [kif])
                act(junk[:, 0:64], kif[:], AF.Square, [kif], [junk, col[6]], acc=col[6][:, 0:1])
                rsqrt_col(col[7][:, 0:1], col[6][:, 0:1], 1.0 / 64, [col[6]], [col[7]], col[5])
                stt(kin[:], kif[:], col[7][:, 0:1], kig_bc[:], ALU.mult, ALU.mult,
                    [kif, col[7], kig_bc], [kin])
                ki3 = kin[:].rearrange("p (h d) -> p h d", d=64)
                kr3 = kir[:].rearrange("p (h d) -> p h d", d=64)
                rope(kr3[:, :, 0:32], kr3[:, :, 32:64], ki3[:, :, 0:32], ki3[:, :, 32:64], cosb, sinb, 1,
                     [kin, cos_t, sin_t, kir])
                cp(kki2[:, 0:64], kir[:], [kir], [kki2])
                cp(kki2[:, 64:128], kir[:], [kir], [kki2])
                tr(B[0][:].bitcast(BF16)[:, 384:512], kki2[:], identb[:], [kki2, identb], [B[0]])
                cp(kiT2[:, n * 128:(n + 1) * 128], B[0][:].bitcast(BF16)[:, 384:512], [B[0]], [kiT2])
                b6 = B[6][:].bitcast(BF16)
                for pr in range(8):
                    tr(b6[:, pr * 128:(pr + 1) * 128], qr[:, pr * 128:(pr + 1) * 128], identb[:], [qr, identb], [B[6]])
                b63 = b6.rearrange("p (a t) -> p a t", t=128)
                cp(qTe[0:64, :, :], b63[0:64, :, :], [B[6]], [qTe])
                cp(qTo[64:128, :, :], b63[64:128, :, :], [B[6]], [qTo])
                b7 = B[7][:].bitcast(BF16)
                for pr in range(4):
                    tr(b7[:, pr * 128:(pr + 1) * 128], qir[:, pr * 128:(pr + 1) * 128], identb[:], [qir, identb], [B[7]])
                b73 = b7[:, 0:512].rearrange("p (a t) -> p a t", t=128)
                cp(qiTe[0:64, :, :], b73[0:64, :, :], [B[7]], [qiTe], eng="scalar")
                cp(qiTo[64:128, :, :], b73[64:128, :, :], [B[7]], [qiTo], eng="scalar")
                ld(qTe_s.ap()[n], qTe[:].rearrange("p a t -> p (a t)"), [qTe], [DV["qTe"]], q="gpsimd")
                ld(qTo_s.ap()[n], qTo[:].rearrange("p a t -> p (a t)"), [qTo], [DV["qTo"]], q="gpsimd")
                ld(qiTe_s.ap()[n], qiTe[:].rearrange("p a t -> p (a t)"), [qiTe], [DV["qiTe"]], q="gpsimd")
                ld(qiTo_s.ap()[n], qiTo[:].rearrange("p a t -> p (a t)"), [qiTo], [DV["qiTo"]], q="gpsimd")

            H_b(0)
            for n in range(NT):
                M_b(n)
                if n + 1 < NT:
                    H_b(n + 1)
                E_b(n)
            S.barrier()
            S.emit()

        with contextlib.ExitStack() as ph:
            S.stack = ph
            woutb = S.sb("woutb", [128, 16, D], BF16)
            with contextlib.ExitStack() as ph2:
                S.stack = ph2
                stg = [S.sb(f"stgO{i}", [128, 16, 256], F32) for i in range(2)]
                gate1_bc = S.sb("gate1_bc", [128, D], F32)
                ld(gate1_bc[:], mod_s.ap()[2 * D:3 * D].partition_broadcast(128), [DV["mod"]], [gate1_bc])
                wout_v = wout_d.ap().rearrange("(k p) c -> p k c", p=128)
                rowscale_bufs.clear()
                rowscale_bufs.extend([gT, gate1_bc])
                load_w_bf(woutb, lambda c0, cw: wout_v[:, :, c0:c0 + cw], D, stg, rowscale=gT[:, 32:48],
                          colscale=gate1_bc)
                S.barrier()
                S.emit()
            S.stack = ph
            score = S.sb("score", [128, ST], F32)
            NMs = [S.sb(f"NM{i}", [128, ST], BF16) for i in range(2)]
            qTe = [S.sb(f"qTeL{i}", [128, 1024], BF16) for i in range(2)]
            qTo = [S.sb(f"qToL{i}", [128, 1024], BF16) for i in range(2)]
            qiTe = [S.sb(f"qiTeL{i}", [128, 512], BF16) for i in range(2)]
            qiTo = [S.sb(f"qiToL{i}", [128, 512], BF16) for i in range(2)]
            pT = [S.sb(f"pT{i}", [128, 512], BF16) for i in range(3)]
            xt = S.sb("xtC", [128, D], F32)
            yanl = S.sb("yanl", [128, 1024], BF16)
            yb = S.sb("yb", [128, 1024], F32)
            ybn = S.sb("ybn", [128, 1024], BF16)
            yT = S.subs(S.sb("yT", [128, 16, 128], BF16), 2, "yT")
            xm = S.sb("xm", [128, D], F32)
            xn2b = S.sb("xn2b", [128, D], BF16)
            h2T = S.subs(S.sb("h2T", [128, 16, 128], F32), 16)
            wr = S.sb("wr", [128, 16, 36], F32)
            bias_bc = S.sb("bias_bc", [128, 36], F32)
            lg = S.sb("lg", [128, 36], F32)
            em = S.sb("em", [128, 32], F32)
            m8 = S.sb("m8", [128, 8], F32)
            i8 = S.sb("i8", [128, 8], U32)
            Ab = S.sb("Ab", [128, 32], BF16)
            oh0 = S.sb("oh0", [128, 32], F32)
            oh1 = S.sb("oh1", [128, 32], F32)
            posf = S.sb("posf", [128, 32], F32)
            tmp32 = S.sb("tmp32", [128, 32], F32)
            sm = S.sb("sm", [128, 32], F32)
            lo = S.sb("lo", [128, 1], F32)
            w0c = S.sb("w0c", [128, 1], F32)
            mid = S.sb("mid", [128, 1], F32)
            cnt = S.sb("cnt", [128, 1], F32)
            gei = S.sb("gei", [128, 1], U32)
            thr = S.sb("thr", [128, 1], F32)
            rden = S.sb("rden", [128, 16], F32)
            destf = S.sb("destf", [128, 2], F32)

            ld(wr[:], wr_d.ap().rearrange("(k p) c -> p k c", p=128), [DV["in"]], [wr])
            ld(bias_bc[:], br_d.ap().partition_broadcast(128), [DV["in"]], [bias_bc])
            mset(base_bc[:], 0.0, [base_bc])

            def stage_A(n):
                Sk = (n + 1) * 128
                qe, qo, qie, qio = qTe[n % 2], qTo[n % 2], qiTe[n % 2], qiTo[n % 2]
                NM = NMs[n % 2]
                ld(qie[:], qiTe_s.ap()[n], [DV["qiTe"]], [qie])
                ld(qio[:], qiTo_s.ap()[n], [DV["qiTo"]], [qio])
                ld(qe[:], qTe_s.ap()[n], [DV["qTe"]], [qe])
                ld(qo[:], qTo_s.ap()[n], [DV["qTo"]], [qo])
                bi = 0
                for ks in range(0, Sk, 512):
                    ke = min(Sk, ks + 512)
                    for h in range(8):
                        bk = B[bi % 2]
                        bi += 1
                        src = (qie if h % 2 == 0 else qio)[:, (h // 2) * 128:(h // 2 + 1) * 128]
                        mm(bk[:, 0:ke - ks], src, kiT2[:, ks:ke], True, True, [qie, qio, kiT2], [bk])
                        sg = sgn_all[:, n, h:h + 1]
                        act(bk[:, 0:ke - ks], bk[:, 0:ke - ks], AF.Relu, [bk, sgn_all], [bk], scale=sg)
                        if h == 0:
                            ts(score[:, ks:ke], bk[:, 0:ke - ks], sg, ALU.mult, [bk, sgn_all], [score])
                        else:
                            stt(score[:, ks:ke], bk[:, 0:ke - ks], sg, score[:, ks:ke], ALU.mult, ALU.add,
                                [bk, sgn_all, score], [score])
                mset(score[0:64, Sk - 64:Sk], -1.0e30, [score])
                if n < 2:
                    mset(thr[:], -1.0e29, [thr])
                else:
                    S.op("vector", (lambda Sk: lambda e: e.tensor_reduce(out=lo[:], in_=score[:, 0:Sk - 64], axis=AX.X,
                                                                         op=ALU.min))(Sk), [score], [lo])
                    S.op("vector", (lambda Sk: lambda e: e.tensor_reduce(out=w0c[:], in_=score[:, 0:Sk], axis=AX.X,
                                                                         op=ALU.max))(Sk), [score], [w0c])
                    stt(w0c[:], w0c[:], 1.0e-4, lo[:], ALU.add, ALU.subtract, [w0c, lo], [w0c])
                    for it in range(NITER):
                        stt(mid[:], w0c[:], 2.0 ** -(it + 1), lo[:], ALU.mult, ALU.add, [w0c, lo], [mid])
                        ts(NM[:, 0:Sk], score[:, 0:Sk], mid[:, 0:1], ALU.is_ge, [score, mid], [NM, cnt],
                           s2=0.0, op1=ALU.add, acc=cnt[:, 0:1])
                        ts(gei[:], cnt[:], 255.5, ALU.is_ge, [cnt], [gei])
                        S.op("vector", lambda e: e.copy_predicated(lo[:], gei[:], mid[:]), [gei, mid, lo], [lo])
                    cp(thr[:], lo[:], [lo], [thr])
                ts(NM[:, 0:Sk], score[:, 0:Sk], thr[:, 0:1], ALU.is_lt, [score, thr], [NM], s2=-30000.0, op1=ALU.mult)

            def stage_B(n):
                Sk = (n + 1) * 128
                qe, qo = qTe[n % 2], qTo[n % 2]
                NM = NMs[n % 2]
                ld(xt[:], x_d.ap()[n * 128:(n + 1) * 128, :], [DV["in"]], [xt])
                ld(yanl[:], ya_s.ap()[n * 128:(n + 1) * 128, :], [DV["ya"]], [yanl])
                for b3 in range(3):
                    mm(B[4 + b3][:], zerob[:, 0:128], zerob[:], True, False, [zerob], [B[4 + b3]], skip=True)
                units = [(kb, j) for kb in range(n + 1) for j in range(4)]

                def QK(u):
                    kb, j = units[u]
                    bk = B[2 + u % 2]
                    pt = pT[u % 3]
                    qsrc = (qe if j < 2 else qo)[:, (j % 2) * 512:(j % 2 + 1) * 512]
                    mm(bk[:], kT2[:, kb * 128:(kb + 1) * 128], qsrc, True, False, [kT2, qe, qo], [bk])
                    mm(bk[:], NM[:, kb * 128:(kb + 1) * 128],
                       identb[:].unsqueeze(1).broadcast_to([128, 4, 128]), False, True, [NM, identb], [bk])
                    act(pt[:], bk[:], AF.Exp, [bk], [pt], scale=0.125)

                def PV(u):
                    kb, j = units[u]
                    pt = pT[u % 3]
                    for hh in range(4):
                        pair = (j % 2) * 4 + hh
                        head = pair * 2 + (0 if j < 2 else 1)
                        ob = B[4 + head // 7]
                        off = (head % 7) * 65
                        mm(ob[:, off:off + 65], pt[:, hh * 128:(hh + 1) * 128], vaug[:, kb, :], False,
                           kb == n, [pt, vaug], [ob], skip=True)

                QK(0)
                for u in range(len(units)):
                    if u + 1 < len(units):
                        QK(u + 1)
                    PV(u)
                for b3 in range(3):
                    nh = 7 if b3 < 2 else 2
                    ov = B[4 + b3][:, 0:nh * 65].rearrange("p (h d) -> p h d", d=65)
                    S.op("vector", (lambda ov, b3, nh: lambda e: e.reciprocal(
                        rden[:, b3 * 7:b3 * 7 + nh].unsqueeze(2), ov[:, :, 64:65]))(ov, b3, nh), [B[4 + b3]], [rden])
                    tt(yb[:, b3 * 448:b3 * 448 + nh * 64].rearrange("p (h d) -> p h d", d=64), ov[:, :, 0:64],
                       rden[:, b3 * 7:b3 * 7 + nh].unsqueeze(2).broadcast_to([128, nh, 64]), ALU.mult,
                       [B[4 + b3], rden], [yb])
                act(junk[:, 0:1024], yb[:], AF.Square, [yb], [junk, col[0]], acc=col[0][:, 0:1])
                rsqrt_col(col[1][:, 0:1], col[0][:, 0:1], 1.0 / 1024, [col[0]], [col[1]], col[2])
                ts(ybn[:], yb[:], col[1][:, 0:1], ALU.mult, [yb, col[1]], [ybn])
                for k in range(16):
                    bk = B[2 + k // 8]
                    srcy = yanl[:, k * 128:(k + 1) * 128] if k < 8 else ybn[:, (k - 8) * 128:(k - 7) * 128]
                    tr(bk[:].bitcast(BF16)[:, (k % 8) * 128:(k % 8 + 1) * 128], srcy, identb[:],
                       [yanl, ybn, identb], [bk])
                cp(yT[:, 0:8, :], B[2][:].bitcast(BF16).rearrange("p (a t) -> p a t", t=128), [B[2]], [yT.sub[0]])
                cp(yT[:, 8:16, :], B[3][:].bitcast(BF16).rearrange("p (a t) -> p a t", t=128), [B[3]], [yT.sub[1]],
                   eng="scalar")
                for db in range(4):
                    bk = B[(7, 4, 5, 6)[db]]
                    for k in range(16):
                        mm(bk[:], yT[:, k, :], woutb[:, k, db * 512:(db + 1) * 512], k == 0, k == 15,
                           [yT.sub[k // 8], woutb], [bk])
                    tt(xm[:, db * 512:(db + 1) * 512], bk[:], xt[:, db * 512:(db + 1) * 512], ALU.add, [bk, xt], [xm])
                ld(xmid_s.ap()[n * 128:(n + 1) * 128, :], xm[:], [xm], [DV["xmid"]], q="gpsimd")
                act(junk[:], xm[:], AF.Square, [xm], [junk, col[3]], acc=col[3][:, 0:1])
                rsqrt_col(col[4][:, 0:1], col[3][:, 0:1], 1.0 / D, [col[3]], [col[4]], col[5])
                ts(xt[:], xm[:], col[4][:, 0:1], ALU.mult, [xm, col[4]], [xt])
                act(xn2b[:], xm[:], AF.Copy, [xm, col[4]], [xn2b], scale=col[4][:, 0:1])
                for g4 in range(4):
                    bk = B[2 + g4 % 2]
                    for k in range(g4 * 4, g4 * 4 + 4):
                        tr(bk[:, (k % 4) * 128:(k % 4 + 1) * 128], xt[:, k * 128:(k + 1) * 128], ident[:],
                           [xt, ident], [bk])
                    for k in range(g4 * 4, g4 * 4 + 4):
                        if g4 % 2 == 0:
                            ts(h2T[:, k, :], bk[:, (k % 4) * 128:(k % 4 + 1) * 128], pvec[:, 32 + k:33 + k], ALU.mult,
                               [bk, pvec], [h2T.sub[k]], s2=pvec[:, 48 + k:49 + k], op1=ALU.add)
                        else:
                            act(h2T[:, k, :], bk[:, (k % 4) * 128:(k % 4 + 1) * 128], AF.Identity, [bk, pvec],
                                [h2T.sub[k]], bias=pvec[:, 48 + k:49 + k], scale=pvec[:, 32 + k:33 + k])
                for k in range(16):
                    mm(B[7][:, 0:36], h2T[:, k, :], wr[:, k, :], k == 0, k == 15, [h2T.sub[k], wr], [B[7]])
                tt(lg[:], B[7][:, 0:36], bias_bc[:], ALU.add, [B[7], bias_bc], [lg])
                S.op("vector", lambda e: e.tensor_reduce(out=sm[:, 0:1], in_=lg[:, 0:4], axis=AX.X, op=ALU.max), [lg], [sm])
                ts(sm[:, 1:2], sm[:, 0:1], -1.0, ALU.mult, [sm], [sm])
                act(sm[:, 4:8], lg[:, 0:4], AF.Exp, [lg, sm], [sm, col[6]], bias=sm[:, 1:2], scale=1.0,
                    acc=col[6][:, 0:1])
                S.op("vector", lambda e: e.reciprocal(sm[:, 2:3], col[6][:, 0:1]), [col[6]], [sm])
                ts(sm[:, 8:12], lg[:, 0:4], sm[:, 0:1], ALU.is_ge, [lg, sm], [sm], s2=1.0e9, op1=ALU.mult)
                ts(sm[:, 8:12], sm[:, 8:12], -1.0e9, ALU.add, [sm], [sm])
                tt(em[:].rearrange("p (g j) -> p g j", j=8), lg[:, 4:36].rearrange("p (g j) -> p g j", j=8),
                   sm[:, 8:12].unsqueeze(2).broadcast_to([128, 4, 8]), ALU.add, [lg, sm], [em])
                S.op("vector", lambda e: e.max(m8[:], em[:]), [em], [m8])
                S.op("vector", lambda e: e.max_index(i8[:], m8[:], em[:]), [m8, em], [i8])
                tt(sm[:, 12:13], m8[:, 1:2], m8[:, 0:1], ALU.subtract, [m8], [sm])
                act(sm[:, 13:14], sm[:, 12:13], AF.Exp, [sm], [sm])
                ts(sm[:, 13:14], sm[:, 13:14], 1.0, ALU.add, [sm], [sm])
                S.op("vector", lambda e: e.reciprocal(sm[:, 14:15], sm[:, 13:14]), [sm], [sm])
                tt(wgt_all[:, n, 0:1], sm[:, 14:15], sm[:, 2:3], ALU.mult, [sm], [wgt_all])
                tt(wgt_all[:, n, 1:2], sm[:, 2:3], wgt_all[:, n, 0:1], ALU.subtract, [sm, wgt_all], [wgt_all])
                ts(Ab[:], em[:], m8[:, 1:2], ALU.is_ge, [em, m8], [Ab])
                ts(oh0[:], em[:], m8[:, 0:1], ALU.is_ge, [em, m8], [oh0])
                tt(oh1[:], Ab[:], oh0[:], ALU.subtract, [Ab, oh0], [oh1])
                mm(B[4][:, 0:32], LTb[:], Ab[:], True, True, [LTb, Ab], [B[4]])
                tt(posf[:], B[4][:, 0:32], base_bc[:], ALU.add, [B[4], base_bc], [posf])
                mm(B[4][:, 64:96], onesb[:], Ab[:], True, True, [onesb, Ab], [B[4]])
                tt(base_bc[:], B[4][:, 64:96], base_bc[:], ALU.add, [B[4], base_bc, posf], [base_bc])
                cp(eid_all[:, n, :], i8[:, 0:2], [i8], [eid_all])
                for j, oh in enumerate((oh0, oh1)):
                    tt(tmp32[:], posf[:], oh[:], ALU.mult, [posf, oh], [tmp32])
                    S.op("vector", (lambda n, j: lambda e: e.tensor_reduce(out=pos_all[:, n, j:j + 1], in_=tmp32[:],
                                                                            axis=AX.X, op=ALU.add))(n, j),
                         [tmp32], [pos_all])
                ld(xn2_s.ap()[n * 128:(n + 1) * 128, :], xn2b[:], [xn2b], [DV["xn2"]], q="gpsimd")

            stage_A(0)
            for n in range(NT):
                if n + 1 < NT:
                    stage_A(n + 1)
                stage_B(n)
            S.barrier()
            S.emit()

        with contextlib.ExitStack() as ph:
            S.stack = ph
            pa = S.sb("pa", [128, 32], F32)
            pb = S.sb("pb", [128, 32], F32)
            pi_ = S.sb("pi_", [128, 32], I32)
            padded = S.sb("padded", [128, 32], F32)
            pstart = S.sb("pstart", [128, 32], F32)
            iotI = S.sb("iotI", [128, NSB], I32)
            iotF = S.sb("iotF", [128, NSB], F32)
            cmp3 = S.sb("cmp3", [128, NSB, 32], F32)
            bef = S.sb("bef", [128, NSB], F32)
            iw12 = S.sb("iw12", [128, 12], I32)
            iw12f = S.sb("iw12f", [128, 12], F32)
            widxf = S.sb("widxf", [128, NSB, 12], F32)
            ohd = S.sb("ohd", [128, 32], F32)
            dcol = S.sb("dcol", [128, 2], F32)
            xr2 = [S.sb(f"xr2_{i}", [128, D], BF16) for i in range(2)]
            ts(pa[:], base_bc[:], float(C - 1), ALU.add, [base_bc], [pa], s2=1.0 / C, op1=ALU.mult)
            cp(pi_[:], pa[:], [pa], [pi_])
            cp(pb[:], pi_[:], [pi_], [pb])
            tt(pa[:], pb[:], pa[:], ALU.is_gt, [pb, pa], [pa])
            tt(pb[:], pb[:], pa[:], ALU.subtract, [pb, pa], [pb])
            ts(padded[:], pb[:], float(C), ALU.mult, [pb], [padded])
            cp(pa[:], padded[:], [padded], [pa])
            src_, dst_ = pa, pb
            for sh in (1, 2, 4, 8, 16):
                cp(dst_[:, 0:sh], src_[:, 0:sh], [src_], [dst_])
                tt(dst_[:, sh:32], src_[:, sh:32], src_[:, 0:32 - sh], ALU.add, [src_], [dst_])
                src_, dst_ = dst_, src_
            pend = src_
            tt(pstart[:], pend[:], padded[:], ALU.subtract, [pend, padded], [pstart])
            S.op("gpsimd", lambda e: e.iota(iotI[:], [[C, NSB]], base=0, channel_multiplier=0), [], [iotI])
            cp(iotF[:], iotI[:], [iotI], [iotF])
            tt(cmp3[:], pend[:].unsqueeze(1).broadcast_to([128, NSB, 32]),
               iotF[:].unsqueeze(2).broadcast_to([128, NSB, 32]), ALU.is_le, [pend, iotF], [cmp3])
            S.op("vector", lambda e: e.tensor_reduce(out=bef[:], in_=cmp3[:], axis=AX.X, op=ALU.add), [cmp3], [bef])
            ts(bef[:], bef[:], 31.0, ALU.min, [bef], [bef], s2=1536.0, op1=ALU.mult)
            S.op("gpsimd", lambda e: e.iota(iw12[:], [[128, 12]], base=0, channel_multiplier=1), [], [iw12])
            cp(iw12f[:], iw12[:], [iw12], [iw12f])
            tt(widxf[:], bef[:].unsqueeze(2).broadcast_to([128, NSB, 12]),
               iw12f[:].unsqueeze(1).broadcast_to([128, NSB, 12]), ALU.add, [bef, iw12f], [widxf])
            cp(widx[:], widxf[:], [widxf], [widx])
            S.op("gpsimd", lambda e: e.iota(pi_[:], [[1, 32]], base=0, channel_multiplier=0), [pi_], [pi_])
            cp(pa[:], pi_[:], [pi_], [pa])
            for n in range(NT):
                for j in range(2):
                    ts(ohd[:], pa[:], eid_all[:, n, j:j + 1], ALU.is_equal, [pa, eid_all], [ohd])
                    tt(ohd[:], ohd[:], pstart[:], ALU.mult, [ohd, pstart], [ohd])
                    S.op("vector", (lambda j: lambda e: e.tensor_reduce(out=dcol[:, j:j + 1], in_=ohd[:], axis=AX.X,
                                                                         op=ALU.add))(j), [ohd], [dcol])
                tt(dcol[:], dcol[:], pos_all[:, n, :], ALU.add, [dcol, pos_all], [dcol])
                cp(dest_all[:, n, :], dcol[:], [dcol], [dest_all])
                xr = xr2[n % 2]
                ld(xr[:], xn2_s.ap()[n * 128:(n + 1) * 128, :], [DV["xn2"]], [xr])
                for j in range(2):
                    S.dma("gpsimd", (lambda n, j, xr: lambda e: e.indirect_dma_start(
                        out=xe_s.ap(), out_offset=bass.IndirectOffsetOnAxis(ap=dest_all[:, n, j:j + 1], axis=0),
                        in_=xr[:], in_offset=None, bounds_check=None, oob_is_err=False))(n, j, xr),
                        [xr, dest_all, DV["xe"]], [DV["xe"]], owner=xr)
            S.barrier()
            S.emit()

        with contextlib.ExitStack() as ph:
            S.stack = ph
            NB = C // 128
            stg = [S.sb(f"stgE{i}", [128, 4096], F32) for i in range(4)]
            wpb = [S.subs(S.sb(f"wpb{i}", [128, 4096], BF16), 2, "p3") for i in range(3)]
            xer1 = [S.sb(f"xer_{b}", [128, D], BF16) for b in range(NB)]
            xer = [xer1, xer1]
            xeT = S.subs(S.sb("xeT", [128, 16, C], BF16), 16, "p3")
            gTt = S.sb("gTt", [128, 8, C], BF16)
            sa = [S.sb(f"sa{i}", [128, C], F32) for i in range(2)]
            yo = [S.subs(S.sb(f"yo{i}", [128, D], F32), 4) for i in range(NB)]
            wexp_rows = wexp_d.ap().rearrange("e q p c -> (e q p) c")
            pieces = [(j, p) for j in range(NSB) for p in range(12)]
            rot = [0]

            def nbank():
                bk = B[2 + rot[0] % 6]
                rot[0] += 1
                return bk

            def emit_gather(i):
                j, piece = pieces[i]
                sg = stg[i % 4]
                S.dma("gpsimd", (lambda sg, j, piece: lambda e: e.indirect_dma_start(
                    out=sg[:], out_offset=None, in_=wexp_rows,
                    in_offset=bass.IndirectOffsetOnAxis(ap=widx[:, j, piece:piece + 1], axis=0),
                    bounds_check=None, oob_is_err=False))(sg, j, piece), [DV["in"], widx], [sg], owner=sg)

            def emit_cast(i):
                sg = stg[i % 4]
                wb = wpb[i % 3]
                cp(wb[:, 0:2048], sg[:, 0:2048], [sg], [wb.sub[0]], eng="vector")
                cp(wb[:, 2048:4096], sg[:, 2048:4096], [sg], [wb.sub[1]], eng="scalar")

            def load_xe(j):
                for blk in range(NB):
                    xr = xer[j % 2][blk]
                    ld(xr[:], xe_s.ap()[j * C + blk * 128:j * C + (blk + 1) * 128, :], [DV["xe"]], [xr])

            def prologue_part(j, part):
                for kp in (2 * part, 2 * part + 1):
                    bk = B[kp % 2]
                    bv = bk[:].bitcast(BF16)
                    for kk in range(2):
                        k = kp * 2 + kk
                        for blk in range(NB):
                            xr = xer[j % 2][blk]
                            tr(bv[:, (kk * NB + blk) * 128:(kk * NB + blk + 1) * 128], xr[:, k * 128:(k + 1) * 128],
                               identb[:], [xr, identb], [bk])
                    for kk in range(2):
                        k = kp * 2 + kk
                        src = bv[:, kk * C:(kk + 1) * C]
                        if kp % 2 == 0:
                            ts(xeT[:, k, :], src, pvec[:, 32 + k:33 + k], ALU.mult, [bk, pvec], [xeT.sub[k]],
                               s2=pvec[:, 48 + k:49 + k], op1=ALU.add)
                        else:
                            act(xeT[:, k, :], src, AF.Identity, [bk, pvec], [xeT.sub[k]],
                                bias=pvec[:, 48 + k:49 + k], scale=pvec[:, 32 + k:33 + k])

            yoi = [0]

            def compute(i):
                j, piece = pieces[i]
                wb = wpb[i % 3]
                if piece < 8:
                    f = piece
                    w13 = wb[:].rearrange("p (t k c) -> p t k c", t=2, k=16)
                    ba, bb = nbank(), nbank()
                    for k in range(16):
                        mm(ba[:, 0:C], w13[:, 0, k, :], xeT[:, k, :], k == 0, k == 15, [wb.sub[0], xeT.sub[k]], [ba])
                    for k in range(16):
                        mm(bb[:, 0:C], w13[:, 1, k, :], xeT[:, k, :], k == 0, k == 15, [wb.sub[1], xeT.sub[k]], [bb])
                    s_ = sa[f % 2]
                    act(s_[:], ba[:, 0:C], AF.Silu, [ba], [s_])
                    tt(gTt[:, f, :], bb[:, 0:C], s_[:], ALU.mult, [bb, s_], [gTt])
                else:
                    db = piece - 8
                    w2v = wb[:].rearrange("p (k c) -> p k c", k=8)
                    for blk in range(NB):
                        bk = nbank()
                        for fc in range(8):
                            mm(bk[:], gTt[:, fc, blk * 128:(blk + 1) * 128], w2v[:, fc, :], fc == 0, fc == 7,
                               [gTt, wb.sub[fc // 4]], [bk])
                        y_ = yo[blk]
                        if blk % 2 == 0:
                            cp(y_[:, db * 512:(db + 1) * 512], bk[:], [bk], [y_.sub[db]])
                        else:
                            cp(y_[:, db * 512:(db + 1) * 512], bk[:], [bk], [y_.sub[db]], eng="scalar")
                        if db == 3:
                            ld(ye_s.ap()[j * C + blk * 128:j * C + (blk + 1) * 128, :], y_[:],
                               list(y_.sub), [DV["ye"]], q="sync", owner=y_)

            load_xe(0)
            emit_gather(0)
            emit_gather(1)
            emit_gather(2)
            emit_cast(0)
            for part in range(4):
                prologue_part(0, part)
            if NSB > 1:
                load_xe(1)
            for i, (j, piece) in enumerate(pieces):
                if piece == 0 and j >= 1 and j + 1 < NSB:
                    load_xe(j + 1)
                if i + 3 < len(pieces):
                    emit_gather(i + 3)
                if i + 1 < len(pieces):
                    emit_cast(i + 1)
                compute(i)
                if piece >= 8 and j + 1 < NSB:
                    prologue_part(j + 1, piece - 8)
            S.barrier()
            S.emit()

        with contextlib.ExitStack() as ph:
            S.stack = ph
            gate2_bc = S.sb("gate2_bc", [128, D], F32)
            fg_bc = S.sb("fg_bc", [128, D], F32)
            y0 = [S.sb(f"y0_{i}", [128, D], F32) for i in range(2)]
            y1 = [S.sb(f"y1_{i}", [128, D], F32) for i in range(2)]
            xmt = [S.sb(f"xmt{i}", [128, D], F32) for i in range(2)]
            acc = S.sb("acc", [128, D], F32)
            ot = [S.sb(f"ot{i}", [128, D], F32) for i in range(2)]
            ld(gate2_bc[:], mod_s.ap()[5 * D:6 * D].partition_broadcast(128), [DV["mod"]], [gate2_bc])
            ld(fg_bc[:], fg_d.ap().partition_broadcast(128), [DV["in"]], [fg_bc])
            for n in range(NT):
                a0, a1, xq, o_ = y0[n % 2], y1[n % 2], xmt[n % 2], ot[n % 2]
                for j, dst in enumerate((a0, a1)):
                    S.dma("gpsimd", (lambda n, j, dst: lambda e: e.indirect_dma_start(
                        out=dst[:], out_offset=None, in_=ye_s.ap(),
                        in_offset=bass.IndirectOffsetOnAxis(ap=dest_all[:, n, j:j + 1], axis=0),
                        bounds_check=None, oob_is_err=False))(n, j, dst),
                        [DV["ye"], dest_all], [dst], owner=dst)
                ld(xq[:], xmid_s.ap()[n * 128:(n + 1) * 128, :], [DV["xmid"]], [xq])
                act(acc[:], a0[:], AF.Copy, [a0, wgt_all], [acc], scale=wgt_all[:, n, 0:1])
                stt(acc[:], a1[:], wgt_all[:, n, 1:2], acc[:], ALU.mult, ALU.add, [a1, wgt_all, acc], [acc])
                tt(acc[:], acc[:], gate2_bc[:], ALU.mult, [acc, gate2_bc], [acc], eng="gpsimd")
                tt(acc[:], acc[:], xq[:], ALU.add, [acc, xq], [acc])
                act(junk[:], acc[:], AF.Square, [acc], [junk, col[0]], acc=col[0][:, 0:1])
                rsqrt_col(col[1][:, 0:1], col[0][:, 0:1], 1.0 / D, [col[0]], [col[1]], col[2])
                stt(o_[:], acc[:], col[1][:, 0:1], fg_bc[:], ALU.mult, ALU.mult, [acc, col[1], fg_bc], [o_])
                ld(out_d.ap()[n * 128:(n + 1) * 128, :], o_[:], [o_], [DV["out"]], q="sync")
            S.barrier()
            S.emit()
    return nc


_CACHE = {}


def _prep_weights(inp):
    f = lambda a: np.ascontiguousarray(np.asarray(a, dtype=np.float32))
    w1 = np.asarray(inp["w1"], dtype=np.float32)[0]
    w3 = np.asarray(inp["w3"], dtype=np.float32)[0]
    w2 = np.asarray(inp["w2"], dtype=np.float32)[0]
    wexp = np.empty((NEXP, 12, 128, 4096), dtype=np.float32)
    a1 = w1.reshape(NEXP, 16, 128, 8, 128).transpose(0, 3, 2, 1, 4)
    a3 = w3.reshape(NEXP, 16, 128, 8, 128).transpose(0, 3, 2, 1, 4)
    v = wexp[:, 0:8].reshape(NEXP, 8, 128, 2, 16, 128)
    v[:, :, :, 0] = a1
    v[:, :, :, 1] = a3
    a2 = w2.reshape(NEXP, 8, 128, 4, 512).transpose(0, 3, 2, 1, 4)
    wexp[:, 8:12] = a2.reshape(NEXP, 4, 128, 4096)
    shared = {
        "w_ada": f(inp["w_ada"][0]), "b_ada": f(inp["b_ada"][0]), "norm1_g": f(inp["norm1_g"][0]),
        "w_in": f(inp["w_in"][0]), "v_norm_g": f(inp["v_norm_g"][0]).reshape(-1),
        "v_norm_b": f(inp["v_norm_b"][0]).reshape(-1), "w_sp": f(inp["w_sp"][0]), "b_sp": f(inp["b_sp"][0]),
        "kv_norm_g": f(inp["kv_norm_g"][0]), "w_uk": f(inp["w_uk"][0]), "w_uv": f(inp["w_uv"][0]),
        "kidx_norm_g": f(inp["kidx_norm_g"][0]), "gnorm_a_g": f(inp["gnorm_a_g"][0]),
        "gnorm_b_g": f(inp["gnorm_b_g"][0]), "w_out": f(inp["w_out"][0]), "norm2_g": f(inp["norm2_g"][0]),
        "w_r": np.ascontiguousarray(np.concatenate([np.asarray(inp["w_group"][0]), np.asarray(inp["w_expert"][0])],
                                                   axis=1).astype(np.float32)),
        "b_r": np.ascontiguousarray(np.concatenate([np.asarray(inp["b_group"][0]), np.asarray(inp["b_expert"][0])],
                                                   axis=0).astype(np.float32)),
        "wexp": wexp, "final_g": f(inp["final_g"]),
    }
    return shared


def kernel(**inputs):
    x = np.asarray(inputs["x"], dtype=np.float32)
    c = np.asarray(inputs["c"], dtype=np.float32)
    pos = np.asarray(inputs["positions"], dtype=np.int32)
    nb, seq, _ = x.shape
    NT = seq // 128
    key = (NT,)
    if key not in _CACHE:
        _CACHE[key] = build(NT=NT)
    nc = _CACHE[key]
    shared = _prep_weights(inputs)
    in_maps = []
    for b in range(nb):
        m = dict(shared)
        m["x"] = np.ascontiguousarray(x[b])
        m["c"] = np.ascontiguousarray(c[b])
        m["pos"] = np.ascontiguousarray(pos[b])
        in_maps.append(m)
    res = run_bass_kernel_spmd(nc, in_maps, core_ids=list(range(nb)))
    return np.stack([np.asarray(r["out"], dtype=np.float32) for r in res.results], axis=0)
```

```python
import contextlib
import math
import numpy as np
import concourse.bass as bass
import concourse.mybir as mybir
from concourse.bass_utils import run_bass_kernel_spmd

F32 = mybir.dt.float32
BF16 = mybir.dt.bfloat16
I32 = mybir.dt.int32
U32 = mybir.dt.uint32
AF = mybir.ActivationFunctionType
ALU = mybir.AluOpType
AX = mybir.AxisListType

FLAGS = {"hT": 1, "p3": 1, "yT": 1, "reorder": 1}
EPOCH = 12000
ENGS = ["tensor", "vector", "scalar", "gpsimd", "sync"]
D = 2048
NEXP = 32
EPS = 1e-6


class Buf:
    __slots__ = ("name", "w", "r", "dsem", "dcum", "t", "multi", "sub")

    def __init__(self, name, t=None):
        self.name = name
        self.multi = False
        self.w = {}
        self.r = {}
        self.dsem = None
        self.dcum = 0
        self.t = t

    def __getitem__(self, k):
        return self.t[k]


class Sched:
    def __init__(self, nc, semstack):
        self.nc = nc
        self.semstack = semstack
        self.stack = semstack
        self.ops = {e: [] for e in ENGS}
        self.cnt = {e: 0 for e in ENGS}
        self.sems = {}
        self.waited = {e: {} for e in ENGS}
        self.cur_cum = {}
        self.nsem = 0
        self.bufs = []

    def _newsem(self, name):
        self.nsem += 1
        return self.semstack.enter_context(self.nc.semaphore(f"{name}_{self.nsem}"))

    def sb(self, name, shape, dtype):
        t = self.stack.enter_context(self.nc.sbuf_tensor(name, list(shape), dtype))
        b = Buf(name, t)
        self.bufs.append(b)
        return b

    def ps(self, name, shape, dtype):
        t = self.stack.enter_context(self.nc.psum_tensor(name, list(shape), dtype))
        b = Buf(name, t)
        self.bufs.append(b)
        return b

    def subs(self, buf, n, flag=None):
        buf.sub = []
        if flag is not None and not FLAGS.get(flag, 1):
            buf.sub = [buf] * n
            return buf
        for i in range(n):
            b = Buf(f"{buf.name}_s{i}", buf.t)
            self.bufs.append(b)
            buf.sub.append(b)
        return buf

    def view(self, name):
        b = Buf(name, None)
        b.multi = True
        self.bufs.append(b)
        return b

    def _engkey(self, eng):
        idx = self.cnt[eng]
        ep = idx // EPOCH
        key = ("E", eng, ep)
        if key not in self.sems:
            self.sems[key] = self._newsem(f"e_{eng}_{ep}")
        return key, (idx % EPOCH) + 1

    def _collect(self, eng, reads, writes):
        deps = {}

        def add(d):
            for k, v in d.items():
                if k[0] == "D":
                    v = max(v, self.cur_cum.get(k, v))
                if deps.get(k, 0) < v:
                    deps[k] = v
        for b in reads:
            add(b.w)
        for b in writes:
            if not b.multi:
                add(b.w)
            add(b.r)
        out = []
        wd = self.waited[eng]
        for k, v in deps.items():
            if k[0] == "E" and k[1] == eng and eng in ("tensor", "sync"):
                continue
            if wd.get(k, 0) >= v:
                continue
            wd[k] = v
            out.append((k, v))
        return out

    def _record(self, me, reads, writes):
        k, v = me
        for b in writes:
            if b.multi:
                if b.w.get(k, 0) < v:
                    b.w[k] = v
            else:
                b.w = {k: v}
                b.r = {}
        for b in reads:
            if b.r.get(k, 0) < v:
                b.r[k] = v

    def op(self, eng, fn, reads=(), writes=()):
        waits = self._collect(eng, reads, writes)
        key, val = self._engkey(eng)
        self.ops[eng].append((waits, fn, key, 1))
        self.cnt[eng] += 1
        self._record((key, val), reads, writes)

    def dma(self, queue, fn, reads=(), writes=(), owner=None):
        waits = self._collect(queue, reads, writes)
        if owner is None:
            owner = (list(writes) + list(reads))[0]
        if owner.dsem is None or owner.dcum + 16 > EPOCH * 2:
            key = ("D", id(owner), self.nsem)
            self.sems[key] = self._newsem("d_" + owner.name)
            owner.dsem = key
            owner.dcum = 0
        owner.dcum += 16
        key = owner.dsem
        self.cur_cum[key] = owner.dcum
        self.ops[queue].append((waits, fn, key, 16))
        self._record((key, owner.dcum), reads, writes)

    def raw(self, eng, fn, reads=()):
        waits = self._collect(eng, reads, [])
        self.ops[eng].append((waits, fn, "RAW", 0))

    def barrier(self):
        allk = {}
        for e in ENGS:
            if self.cnt[e] > 0:
                idx = self.cnt[e] - 1
                allk[("E", e, idx // EPOCH)] = (idx % EPOCH) + 1
        for k, v in self.cur_cum.items():
            allk[k] = v
        for e in ENGS:
            wd = self.waited[e]
            ws = []
            for k, v in allk.items():
                if wd.get(k, 0) >= v:
                    continue
                wd[k] = v
                if k[0] == "E" and k[1] == e:
                    continue
                ws.append((k, v))
            if ws:
                self.ops[e].append((ws, None, None, 0))
        for b in self.bufs:
            b.w = {}
            b.r = {}

    def emit(self):
        nc = self.nc
        with nc.Block() as block:
            for e in ENGS:
                ops = self.ops[e]
                if not ops:
                    continue

                def body(eng, ops=ops):
                    for waits, fn, key, inc in ops:
                        for k, v in waits:
                            eng.wait_ge(self.sems[k], v)
                        if fn is None:
                            continue
                        if key == "RAW":
                            fn(eng)
                        else:
                            fn(eng).then_inc(self.sems[key], inc)
                getattr(block, e)(body)
        self.ops = {e: [] for e in ENGS}


def build(NT=32, NITER=16, dbg=False):
    ST = NT * 128
    C = 512
    NSB = (2 * ST + C - 1) // C + NEXP
    nc = bass.Bass("TRN2", target_bir_lowering=False)

    def din(name, shape, dt=F32):
        return nc.dram_tensor(name, list(shape), dt, kind="ExternalInput")

    def dscr(name, shape, dt=F32):
        if dbg:
            return nc.dram_tensor(name, list(shape), dt, kind="ExternalOutput")
        return nc.dram_tensor(name, list(shape), dt)

    x_d = din("x", [ST, D])
    c_d = din("c", [D])
    pos_d = din("pos", [ST], I32)
    wada_d = din("w_ada", [D, 6 * D])
    bada_d = din("b_ada", [6 * D])
    n1g_d = din("norm1_g", [D])
    win_d = din("w_in", [D, 3912])
    vng_d = din("v_norm_g", [1024])
    vnb_d = din("v_norm_b", [1024])
    wsp_d = din("w_sp", [8, 128, 128])
    bsp_d = din("b_sp", [8, 128])
    kvg_d = din("kv_norm_g", [256])
    wuk_d = din("w_uk", [256, 64])
    wuv_d = din("w_uv", [256, 64])
    kig_d = din("kidx_norm_g", [64])
    gna_d = din("gnorm_a_g", [1024])
    gnb_d = din("gnorm_b_g", [1024])
    wout_d = din("w_out", [D, D])
    n2g_d = din("norm2_g", [D])
    wr_d = din("w_r", [D, 36])
    br_d = din("b_r", [36])
    wexp_d = din("wexp", [NEXP, 12, 128, 4096])
    fg_d = din("final_g", [D])
    out_d = nc.dram_tensor("out", [ST, D], F32, kind="ExternalOutput")

    mod_s = dscr("mod_s", [6 * D])
    ya_s = dscr("ya_s", [ST, 1024], BF16)
    qTe_s = dscr("qTe_s", [NT, 128, 1024], BF16)
    qTo_s = dscr("qTo_s", [NT, 128, 1024], BF16)
    qiTe_s = dscr("qiTe_s", [NT, 128, 512], BF16)
    qiTo_s = dscr("qiTo_s", [NT, 128, 512], BF16)
    xmid_s = dscr("xmid_s", [ST, D])
    xn2_s = dscr("xn2_s", [ST, D], BF16)
    xe_s = dscr("xe_s", [NSB * C, D], BF16)
    ye_s = dscr("ye_s", [NSB * C, D])

    with contextlib.ExitStack() as outer:
        S = Sched(nc, outer)
        B = [S.ps(f"B{i}", [128, 512], F32) for i in range(8)]
        DV = {n: S.view("dv_" + n) for n in
              ["in", "mod", "ya", "qTe", "qTo", "qiTe", "qiTo", "xmid", "xe", "ye", "out", "xn2"]}

        def mm(o, l, r, st, sp, R, W, skip=False):
            S.op("tensor", lambda e: e.matmul(o, l, r, start=st, stop=sp, skip_group_check=skip), R, W)

        def tr(o, i, idn, R, W):
            S.op("tensor", lambda e: e.transpose(o, i, idn), R, W)

        def act(o, i, f, R, W, bias=None, scale=None, acc=None):
            kw = {}
            if bias is not None:
                kw["bias"] = bias
            if scale is not None:
                kw["scale"] = scale
            if acc is not None:
                kw["accum_out"] = acc
            S.op("scalar", lambda e: e.activation(out=o, in_=i, func=f, **kw), R, W)

        def ts(o, i, s1, op0, R, W, s2=None, op1=None, eng="vector", acc=None):
            if acc is not None:
                S.op(eng, lambda e: e.tensor_scalar(o, i, s1, s2, op0, op1, accum_out=acc), R, W)
            elif op1 is None:
                S.op(eng, lambda e: e.tensor_scalar(o, i, s1, None, op0), R, W)
            else:
                S.op(eng, lambda e: e.tensor_scalar(o, i, s1, s2, op0, op1), R, W)

        def tt(o, a, b, op, R, W, eng="vector"):
            S.op(eng, lambda e: e.tensor_tensor(out=o, in0=a, in1=b, op=op), R, W)

        def stt(o, a, s, b, op0, op1, R, W):
            S.op("vector", lambda e: e.scalar_tensor_tensor(out=o, in0=a, scalar=s, in1=b, op0=op0, op1=op1), R, W)

        def cp(o, i, R, W, eng="vector"):
            if eng == "scalar":
                S.op("scalar", lambda e: e.copy(o, i), R, W)
            else:
                S.op(eng, lambda e: e.tensor_copy(o, i), R, W)

        def mset(ap, val, W, eng="vector"):
            S.op(eng, lambda e: e.memset(ap, val), [], W)

        def ld(o, i, R, W, q="sync", owner=None):
            S.dma(q, lambda e: e.dma_start(out=o, in_=i), R, W, owner=owner)

        def rsqrt_col(dst, src, scale, R, W, tmp):
            w_ = src.shape[-1]
            ts(tmp[:, 0:w_], src, scale, ALU.mult, R, [tmp], s2=EPS, op1=ALU.add)
            act(tmp[:, 0:w_], tmp[:, 0:w_], AF.Sqrt, [tmp], [tmp])
            S.op("vector", lambda e: e.reciprocal(dst, tmp[:, 0:w_]), [tmp], W)

        ident = S.sb("ident", [128, 128], F32)
        identb = S.sb("identb", [128, 128], BF16)
        onesf = S.sb("onesf", [128, 128], F32)
        onesb = S.sb("onesb", [128, 128], BF16)
        zerob = S.sb("zerob", [128, 512], BF16)
        LTb = S.sb("LTb", [128, 128], BF16)
        pvec = S.sb("pvec", [128, 64], F32)
        gT = S.sb("gT", [128, 58], F32)
        kT2 = S.sb("kT2", [128, ST], BF16)
        kiT2 = S.sb("kiT2", [128, ST], BF16)
        vaug = S.sb("vaug", [128, NT, 65], BF16)
        sgn_all = S.sb("sgn_all", [128, NT, 8], F32)
        dest_all = S.sb("dest_all", [128, NT, 2], I32)
        eid_all = S.sb("eid_all", [128, NT, 2], F32)
        pos_all = S.sb("pos_all", [128, NT, 2], F32)
        base_bc = S.sb("base_bc", [128, 32], F32)
        widx = S.sb("widx", [128, NSB, 12], I32)
        wgt_all = S.sb("wgt_all", [128, NT, 2], F32)
        junk = S.sb("junk", [128, 2048], BF16)
        col = [S.sb(f"col{i}", [128, 16], F32) for i in range(8)]

        mset(onesf[:], 1.0, [onesf], eng="gpsimd")
        mset(onesb[:], 1.0, [onesb], eng="gpsimd")
        mset(zerob[:], 0.0, [zerob], eng="gpsimd")
        S.op("gpsimd", lambda e: e.affine_select(out=ident[:], in_=onesf[:], pattern=[[1, 128]],
                                                 compare_op=ALU.is_equal, fill=0.0, base=0,
                                                 channel_multiplier=-1), [onesf], [ident])
        S.op("gpsimd", lambda e: e.affine_select(out=identb[:], in_=onesf[:], pattern=[[1, 128]],
                                                 compare_op=ALU.is_equal, fill=0.0, base=0,
                                                 channel_multiplier=-1), [onesf], [identb])
        S.op("gpsimd", lambda e: e.affine_select(out=LTb[:], in_=onesf[:], pattern=[[1, 128]],
                                                 compare_op=ALU.is_ge, fill=0.0, base=-1,
                                                 channel_multiplier=-1), [onesf], [LTb])
        mset(vaug[:, :, 64:65], 1.0, [vaug], eng="gpsimd")

        with contextlib.ExitStack() as ph:
            S.stack = ph
            stA = S.sb("stA", [112, 128], F32)
            stB = S.sb("stB", [58, 128], F32)
            siluc = S.sb("siluc", [128, 16], F32)
            cT = S.sb("cT", [128, 16], F32)
            badaT = S.sb("badaT", [128, 96], F32)
            modT = S.sb("modT", [128, 96], F32)
            modR = S.sb("modR", [96, 128], F32)
            wa = [S.sb(f"wa{i}", [128, 16, 512], F32) for i in range(2)]

            ld(stA[0:16, :], c_d.ap().rearrange("(k p) -> k p", p=128), [DV["in"]], [stA])
            ld(stA[16:112, :], bada_d.ap().rearrange("(k p) -> k p", p=128), [DV["in"]], [stA])
            r0 = 0
            for src, nr in [(n1g_d, 16), (n2g_d, 16), (gna_d, 8), (gnb_d, 8), (kvg_d, 2)]:
                ld(stB[r0:r0 + nr, :], src.ap().rearrange("(k p) -> k p", p=128), [DV["in"]], [stB])
                r0 += nr
            ld(stB[50:58, :], bsp_d.ap(), [DV["in"]], [stB])
            tr(B[0][:, 0:112], stA[0:112, :], ident[0:112, 0:112], [stA, ident], [B[0]])
            tr(B[1][:, 0:58], stB[0:58, :], ident[0:58, 0:58], [stB, ident], [B[1]])
            cp(cT[:], B[0][:, 0:16], [B[0]], [cT])
            act(siluc[:], cT[:], AF.Silu, [cT], [siluc])
            cp(badaT[:], B[0][:, 16:112], [B[0]], [badaT])
            cp(gT[:], B[1][:, 0:58], [B[1]], [gT])
            for cb in range(24):
                w = wa[cb % 2]
                ld(w[:], wada_d.ap().rearrange("(k p) c -> p k c", p=128)[:, :, cb * 512:(cb + 1) * 512],
                   [DV["in"]], [w])
                for cc in range(4):
                    j = cb * 4 + cc
                    for k in range(16):
                        mm(B[2][:, j:j + 1], w[:, k, cc * 128:(cc + 1) * 128], siluc[:, k:k + 1],
                           k == 0, k == 15, [w, siluc], [B[2]])
            tt(modT[:], B[2][:, 0:96], badaT[:], ALU.add, [B[2], badaT], [modT])
            stt(pvec[:, 0:16], modT[:, 16:32], 1.0, gT[:, 0:16], ALU.add, ALU.mult, [modT, gT], [pvec])
            cp(pvec[:, 16:32], modT[:, 0:16], [modT], [pvec])
            stt(pvec[:, 32:48], modT[:, 64:80], 1.0, gT[:, 16:32], ALU.add, ALU.mult, [modT, gT], [pvec])
            cp(pvec[:, 48:64], modT[:, 48:64], [modT], [pvec])
            tr(B[3][0:96, 0:128], modT[:, 0:96], ident[:], [modT, ident], [B[3]])
            cp(modR[:], B[3][0:96, 0:128], [B[3]], [modR])
            ld(mod_s.ap().rearrange("(j p) -> j p", p=128), modR[:], [modR], [DV["mod"]], q="gpsimd")
            S.barrier()
            S.emit()

        def prep_hT(xt, xn, hT, c_ss, c_rs, c_tmp, sc_off, sh_off):
            act(junk[:], xt[:], AF.Square, [xt], [junk, c_ss], acc=c_ss[:, 0:1])
            rsqrt_col(c_rs[:, 0:1], c_ss[:, 0:1], 1.0 / D, [c_ss], [c_rs], c_tmp)
            ts(xn[:], xt[:], c_rs[:, 0:1], ALU.mult, [xt, c_rs], [xn])
            for k in range(16):
                bk = B[k // 8]
                tr(bk[:].bitcast(BF16)[:, (k % 8) * 128:(k % 8 + 1) * 128], xn[:, k * 128:(k + 1) * 128],
                   identb[:], [xn, identb], [bk])
            for k in range(16):
                bk = B[k // 8]
                src = bk[:].bitcast(BF16)[:, (k % 8) * 128:(k % 8 + 1) * 128]
                if k < 8:
                    ts(hT[:, k, :], src, pvec[:, sc_off + k:sc_off + k + 1], ALU.mult, [bk, pvec], [hT.sub[k]],
                       s2=pvec[:, sh_off + k:sh_off + k + 1], op1=ALU.add)
                else:
                    act(hT[:, k, :], src, AF.Identity, [bk, pvec], [hT.sub[k]],
                        bias=pvec[:, sh_off + k:sh_off + k + 1], scale=pvec[:, sc_off + k:sc_off + k + 1])

        def load_w_bf(dst, src_ap_fn, ncols, stg, rowscale=None, colscale=None):
            step = 256
            i = 0
            for c0 in range(0, ncols, step):
                cw = min(step, ncols - c0)
                sg = stg[i % 2]
                ld(sg[:, :, 0:cw], src_ap_fn(c0, cw), [DV["in"]], [sg])
                eng = ["vector", "gpsimd"][i % 2]
                if rowscale is None:
                    if i % 3 == 2:
                        cp(dst[:, :, c0:c0 + cw], sg[:, :, 0:cw], [sg], [dst], eng="scalar")
                    else:
                        cp(dst[:, :, c0:c0 + cw], sg[:, :, 0:cw], [sg], [dst], eng=eng)
                else:
                    for k in range(16):
                        stt(dst[:, k, c0:c0 + cw], sg[:, k, 0:cw], rowscale[:, k:k + 1], colscale[:, c0:c0 + cw],
                            ALU.mult, ALU.mult, [sg] + rowscale_bufs, [dst])
                i += 1

        rowscale_bufs = []

        with contextlib.ExitStack() as ph:
            S.stack = ph
            wbf = S.sb("wbfA", [128, 16, 2048], BF16)
            stg = [S.sb(f"stgA{i}", [128, 16, 256], F32) for i in range(2)]
            xts = [S.sb(f"xtA{i}", [128, D], F32) for i in range(2)]
            xns = [S.sb(f"xnA{i}", [128, D], BF16) for i in range(2)]
            hTs = [S.subs(S.sb(f"hTA{i}", [128, 16, 128], BF16), 16, "hT") for i in range(2)]
            gu = S.sb("gu", [128, 1024], F32)
            gv = S.sb("gv", [128, 1024], F32)
            vn = S.sb("vn", [128, 1024], F32)
            vgb = S.sb("vgb", [128, 1024], BF16)
            Gbc = S.sb("Gbc", [128, 1024], F32)
            Bbc = S.sb("Bbc", [128, 1024], F32)
            wsp = S.sb("wsp", [128, 8, 128], F32)
            WmT = S.sb("WmT", [128, 8, 128], BF16)
            ya = S.sb("ya", [128, 1024], F32)
            yan = S.sb("yan", [128, 1024], BF16)
            stats = S.sb("stats", [128, 8, 6], F32)
            mv = S.sb("mv", [128, 8, 2], F32)

            win_v = win_d.ap().rearrange("(k p) c -> p k c", p=128)
            load_w_bf(wbf, lambda c0, cw: win_v[:, :, c0:c0 + cw], 2048, stg)
            ld(Gbc[:], vng_d.ap().partition_broadcast(128), [DV["in"]], [Gbc])
            ld(Bbc[:], vnb_d.ap().partition_broadcast(128), [DV["in"]], [Bbc])
            ld(wsp[:], wsp_d.ap().rearrange("g i j -> i g j"), [DV["in"]], [wsp])
            for g in range(8):
                bk = B[6 + g // 4]
                tr(bk[:, (g % 4) * 128:(g % 4 + 1) * 128], wsp[:, g, :], ident[:], [wsp, ident], [bk])
            for g in range(8):
                bk = B[6 + g // 4]
                cp(WmT[:, g, :], bk[:, (g % 4) * 128:(g % 4 + 1) * 128], [bk], [WmT])
            mset(WmT[64:128, :, 0:64], 0.0, [WmT])

            def H_a(n):
                xt = xts[n % 2]
                ld(xt[:], x_d.ap()[n * 128:(n + 1) * 128, :], [DV["in"]], [xt])
                prep_hT(xt, xns[n % 2], hTs[n % 2], col[0], col[1], col[2], 0, 16)

            def M_a(n):
                hT = hTs[n % 2]
                for cg in range(4):
                    for k in range(16):
                        mm(B[2 + cg][:], hT[:, k, :], wbf[:, k, cg * 512:(cg + 1) * 512], k == 0, k == 15,
                           [hT.sub[k], wbf], [B[2 + cg]])

            def E_a(n):
                act(gu[:, 0:512], B[2][:], AF.Gelu_apprx_tanh, [B[2]], [gu])
                act(gu[:, 512:1024], B[3][:], AF.Gelu_apprx_tanh, [B[3]], [gu])
                act(gv[:, 0:512], B[4][:], AF.Gelu_apprx_tanh, [B[4]], [gv])
                act(gv[:, 512:1024], B[5][:], AF.Gelu_apprx_tanh, [B[5]], [gv])
                for g in range(8):
                    S.op("vector", (lambda g: lambda e: e.bn_stats(stats[:, g, :], gv[:, g * 128:(g + 1) * 128]))(g),
                         [gv], [stats])
                for g in range(8):
                    S.op("vector", (lambda g: lambda e: e.bn_aggr(mv[:, g, :], stats[:, g, :]))(g), [stats], [mv])
                ts(col[4][:, 0:8], mv[:, :, 1], EPS, ALU.add, [mv], [col[4]])
                act(col[4][:, 0:8], col[4][:, 0:8], AF.Sqrt, [col[4]], [col[4]])
                S.op("vector", lambda e: e.reciprocal(col[3][:, 0:8], col[4][:, 0:8]), [col[4]], [col[3]])
                for g in range(8):
                    ts(vn[:, g * 128:(g + 1) * 128], gv[:, g * 128:(g + 1) * 128], mv[:, g, 0:1], ALU.subtract,
                       [gv, mv, col[3]], [vn], s2=col[3][:, g:g + 1], op1=ALU.mult)
                tt(vn[:], vn[:], Gbc[:], ALU.mult, [vn, Gbc], [vn], eng="gpsimd")
                tt(vgb[:], vn[:], Bbc[:], ALU.add, [vn, Bbc], [vgb])

            def E2_a(n):
                for g in range(8):
                    bk = B[6 + g // 4]
                    mm(bk[:, (g % 4) * 128:(g % 4 + 1) * 128], WmT[:, g, :], vgb[:, g * 128:(g + 1) * 128],
                       True, True, [WmT, vgb], [bk])
                for g in range(8):
                    bk = B[6 + g // 4]
                    stt(ya[:, g * 128:(g + 1) * 128], bk[:, (g % 4) * 128:(g % 4 + 1) * 128], gT[:, 50 + g:51 + g],
                        gu[:, g * 128:(g + 1) * 128], ALU.add, ALU.mult, [bk, gT, gu], [ya])
                act(junk[:, 0:1024], ya[:], AF.Square, [ya], [junk, col[5]], acc=col[5][:, 0:1])
                rsqrt_col(col[6][:, 0:1], col[5][:, 0:1], 1.0 / 1024, [col[5]], [col[6]], col[7])
                ts(yan[:], ya[:], col[6][:, 0:1], ALU.mult, [ya, col[6]], [yan])
                ld(ya_s.ap()[n * 128:(n + 1) * 128, :], yan[:], [yan], [DV["ya"]], q="gpsimd")

            H_a(0)
            for n in range(NT):
                M_a(n)
                if n + 1 < NT:
                    H_a(n + 1)
                if FLAGS.get("reorder", 1):
                    if n >= 1:
                        E2_a(n - 1)
                    E_a(n)
                else:
                    E_a(n)
                    E2_a(n)
            if FLAGS.get("reorder", 1):
                E2_a(NT - 1)
            S.barrier()
            S.emit()

        with contextlib.ExitStack() as ph:
            S.stack = ph
            NCB = 3912 - 2048
            wbf = S.sb("wbfB", [128, 16, NCB], BF16)
            posT = S.sb("posT", [128, NT], F32)
            sin_t = S.sb("sin_t", [128, NT, 32], F32)
            cos_t = S.sb("cos_t", [128, NT, 32], F32)
            wukv = S.sb("wukv", [128, 2, 128], BF16)
            kig_bc = S.sb("kig_bc", [128, 64], F32)
            ph2 = contextlib.ExitStack()
            S.stack = ph2
            stg = [S.sb(f"stgB{i}", [128, 16, 256], F32) for i in range(2)]
            posR = S.sb("posR", [NT, 128], I32)
            posF = S.sb("posF", [NT, 128], F32)
            fr = S.sb("fr", [128, 32], F32)
            ang = S.sb("ang", [128, NT, 32], F32)
            rr = S.sb("rr", [128, NT, 32], F32)
            rq = S.sb("rq", [128, NT, 32], F32)
            rni = S.sb("rni", [128, NT, 32], I32)
            stkv = S.sb("stkv", [128, 2, 128], F32)
            win_v = win_d.ap().rearrange("(k p) c -> p k c", p=128)
            load_w_bf(wbf, lambda c0, cw: win_v[:, :, 2048 + c0:2048 + c0 + cw], NCB, stg)
            ld(posR[:], pos_d.ap().rearrange("(n p) -> n p", p=128), [DV["in"]], [posR])
            cp(posF[:], posR[:], [posR], [posF])
            tr(B[7][:, 0:NT], posF[0:NT, :], ident[0:NT, 0:NT], [posF, ident], [B[7]])
            cp(posT[:], B[7][:, 0:NT], [B[7]], [posT])
            for i in range(32):
                mset(fr[:, i:i + 1], float(np.float32(10000.0) ** np.float32(-i / 32.0)), [fr], eng="gpsimd")
            tt(ang[:], posT[:].unsqueeze(2).broadcast_to([128, NT, 32]),
               fr[:].unsqueeze(1).broadcast_to([128, NT, 32]), ALU.mult, [posT, fr], [ang])
            TWO_PI = 2.0 * math.pi
            C1 = 6.28125
            C2 = TWO_PI - C1
            for (dst, shift) in ((sin_t, 0.0), (cos_t, math.pi / 2)):
                ts(rq[:], ang[:], shift, ALU.add, [ang], [rq])
                ts(rr[:], rq[:], 1.0 / TWO_PI, ALU.mult, [rq], [rr])
                cp(rni[:], rr[:], [rr], [rni])
                cp(rr[:], rni[:], [rni], [rr])
                stt(rq[:], rr[:], -C1, rq[:], ALU.mult, ALU.add, [rr, rq], [rq])
                stt(rq[:], rr[:], -C2, rq[:], ALU.mult, ALU.add, [rr, rq], [rq])
                ts(rr[:], rq[:], math.pi, ALU.is_gt, [rq], [rr])
                stt(rq[:], rr[:], -TWO_PI, rq[:], ALU.mult, ALU.add, [rr, rq], [rq])
                ts(rr[:], rq[:], -math.pi, ALU.is_lt, [rq], [rr])
                stt(rq[:], rr[:], TWO_PI, rq[:], ALU.mult, ALU.add, [rr, rq], [rq])
                ts(rq[:], rq[:], 3.141592, ALU.min, [rq], [rq], s2=-3.141592, op1=ALU.max)
                act(dst[:], rq[:], AF.Sin, [rq], [dst])
            ld(stkv[:, :, 0:64], wuk_d.ap().rearrange("(c p) d -> p c d", p=128), [DV["in"]], [stkv])
            ld(stkv[:, :, 64:128], wuv_d.ap().rearrange("(c p) d -> p c d", p=128), [DV["in"]], [stkv])
            for c2 in range(2):
                ts(wukv[:, c2, :], stkv[:, c2, :], gT[:, 48 + c2:49 + c2], ALU.mult, [stkv, gT], [wukv])
            ld(kig_bc[:], kig_d.ap().partition_broadcast(128), [DV["in"]], [kig_bc])
            S.barrier()
            S.emit()
            ph2.close()
            S.stack = ph
            xts = [S.sb(f"xtB{i}", [128, D], F32) for i in range(2)]
            xns = [S.sb(f"xnB{i}", [128, D], BF16) for i in range(2)]
            hTs = [S.subs(S.sb(f"hTB{i}", [128, 16, 128], BF16), 16, "hT") for i in range(2)]
            qrs = [S.sb(f"qr{i}", [128, 1024], BF16) for i in range(2)]
            qirs = [S.sb(f"qir{i}", [128, 512], BF16) for i in range(2)]
            qf = S.subs(S.sb("qf", [128, 1024], F32), 2)
            c4f = S.sb("c4f", [128, 512], F32)
            c5f = S.sb("c5f", [128, 328], F32)
            t1 = S.sb("t1", [128, 512], F32)
            t2 = S.sb("t2", [128, 512], F32)
            cw_ = S.sb("cw_", [128, 8, 32], F32)
            sw_ = S.sb("sw_", [128, 8, 32], F32)
            wis = S.sb("wis", [128, 8], F32)
            ckvb = S.sb("ckvb", [128, 256], BF16)
            ckvf = S.sb("ckvf", [128, 256], F32)
            kif = S.sb("kif", [128, 64], F32)
            ckvT = S.sb("ckvT", [128, 2, 128], BF16)
            kk = S.sb("kk", [128, 64], F32)
            kk2 = S.sb("kk2", [128, 128], BF16)
            kin = S.sb("kin", [128, 64], F32)
            kir = S.sb("kir", [128, 64], F32)
            kki2 = S.sb("kki2", [128, 128], BF16)
            qTe = S.sb("qTe", [128, 8, 128], BF16)
            qTo = S.sb("qTo", [128, 8, 128], BF16)
            qiTe = S.sb("qiTe", [128, 4, 128], BF16)
            qiTo = S.sb("qiTo", [128, 4, 128], BF16)
            mset(qTe[:], 0.0, [qTe], eng="gpsimd")
            mset(qTo[:], 0.0, [qTo], eng="gpsimd")
            mset(qiTe[:], 0.0, [qiTe], eng="gpsimd")
            mset(qiTo[:], 0.0, [qiTo], eng="gpsimd")
            CSC = (8 ** -0.5) * (64 ** -0.5)

            def rope(o_lo, o_hi, x_lo, x_hi, cs, sn, nh, RB):
                a = t1[:, 0:nh * 32].rearrange("p (h d) -> p h d", d=32)
                b = t2[:, 0:nh * 32].rearrange("p (h d) -> p h d", d=32)
                tt(a, x_lo, cs, ALU.mult, RB, [t1])
                tt(b, x_hi, sn, ALU.mult, RB, [t2])
                tt(o_lo, a, b, ALU.subtract, [t1, t2], RB[-1:], eng="gpsimd")
                tt(a, x_lo, sn, ALU.mult, RB + [t1], [t1])
                tt(b, x_hi, cs, ALU.mult, RB + [t2], [t2])
                tt(o_hi, a, b, ALU.add, [t1, t2], RB[-1:], eng="gpsimd")

            zt = S.sb("zt", [128, D], BF16)
            mset(zt[:], 0.0, [zt], eng="gpsimd")
            zf_total = NSB * C // 512
            zf_done = [0]

            def H_b(n):
                xt = xts[n % 2]
                ld(xt[:], x_d.ap()[n * 128:(n + 1) * 128, :], [DV["in"]], [xt])
                want = (zf_total * (n + 1) + NT - 1) // NT
                while zf_done[0] < min(want, zf_total):
                    r = zf_done[0]
                    ld(xe_s.ap()[r * 512:(r + 1) * 512, :].rearrange("(a p) d -> p a d", p=128),
                       zt[:].unsqueeze(1).broadcast_to([128, 4, D]), [zt], [DV["xe"]], q="sync", owner=zt)
                    zf_done[0] += 1
                prep_hT(xt, xns[n % 2], hTs[n % 2], col[0], col[1], col[2], 0, 16)

            def M_b(n):
                hT = hTs[n % 2]
                widths = [512, 512, 512, NCB - 1536]
                for cg in range(4):
                    for k in range(16):
                        mm(B[2 + cg][:, 0:widths[cg]], hT[:, k, :], wbf[:, k, cg * 512:cg * 512 + widths[cg]],
                           k == 0, k == 15, [hT.sub[k], wbf], [B[2 + cg]])

            def E1_b(n):
                cosb = cos_t[:, n, :].unsqueeze(1)
                sinb = sin_t[:, n, :].unsqueeze(1)
                qr, qir = qrs[n % 2], qirs[n % 2]
                cp(c4f[:], B[4][:], [B[4]], [c4f], eng="scalar")
                cp(c5f[:, 0:328], B[5][:, 0:328], [B[5]], [c5f], eng="scalar")
                cp(qf[:, 0:512], B[2][:], [B[2]], [qf.sub[0]], eng="scalar")
                cp(qf[:, 512:1024], B[3][:], [B[3]], [qf.sub[1]], eng="scalar")
                act(junk[:, 0:256], c4f[:, 0:256], AF.Square, [c4f], [junk, col[3]], acc=col[3][:, 0:1])
                rsqrt_col(col[4][:, 0:1], col[3][:, 0:1], 1.0 / 256, [col[3]], [col[4]], col[5])
                cp(ckvb[:], c4f[:, 0:256], [c4f], [ckvb])
                for c2 in range(2):
                    tr(B[0][:].bitcast(BF16)[:, c2 * 128:(c2 + 1) * 128], ckvb[:, c2 * 128:(c2 + 1) * 128], identb[:],
                       [ckvb, identb], [B[0]])
                cp(ckvT[:], B[0][:].bitcast(BF16)[:, 0:256].rearrange("p (c t) -> p c t", t=128), [B[0]], [ckvT])
                for c2 in range(2):
                    mm(B[1][:, 0:128], ckvT[:, c2, :], wukv[:, c2, :], c2 == 0, c2 == 1, [ckvT, wukv], [B[1]])
                act(junk[:, 0:64], c5f[:, 256:320], AF.Square, [c5f], [junk, col[6]], acc=col[6][:, 0:1])
                rsqrt_col(col[7][:, 0:1], col[6][:, 0:1], 1.0 / 64, [col[6]], [col[7]], col[5])
                stt(kin[:], c5f[:, 256:320], col[7][:, 0:1], kig_bc[:], ALU.mult, ALU.mult,
                    [c5f, col[7], kig_bc], [kin])
                ki3 = kin[:].rearrange("p (h d) -> p h d", d=64)
                kr3 = kir[:].rearrange("p (h d) -> p h d", d=64)
                rope(kr3[:, :, 0:32], kr3[:, :, 32:64], ki3[:, :, 0:32], ki3[:, :, 32:64], cosb, sinb, 1,
                     [kin, cos_t, sin_t, kir])
                cp(kki2[:, 0:64], kir[:], [kir], [kki2])
                cp(kki2[:, 64:128], kir[:], [kir], [kki2])
                tr(B[0][:].bitcast(BF16)[:, 384:512], kki2[:], identb[:], [kki2, identb], [B[0]])
                cp(kiT2[:, n * 128:(n + 1) * 128], B[0][:].bitcast(BF16)[:, 384:512], [B[0]], [kiT2])
                ts(vaug[:, n, 0:64], B[1][:, 64:128], col[4][:, 0:1], ALU.mult, [B[1], col[4]], [vaug])
                kv3 = B[1][:, 0:64].rearrange("p (h d) -> p h d", d=64)
                kk3 = kk[:].rearrange("p (h d) -> p h d", d=64)
                rope(kk3[:, :, 0:32], kk3[:, :, 32:64], kv3[:, :, 0:32], kv3[:, :, 32:64], cosb, sinb, 1,
                     [B[1], cos_t, sin_t, kk])
                ts(kk2[:, 0:64], kk[:], col[4][:, 0:1], ALU.mult, [kk, col[4]], [kk2])
                ts(kk2[:, 64:128], kk[:], col[4][:, 0:1], ALU.mult, [kk, col[4]], [kk2])
                tr(B[0][:].bitcast(BF16)[:, 256:384], kk2[:], identb[:], [kk2, identb], [B[0]])
                cp(kT2[:, n * 128:(n + 1) * 128], B[0][:].bitcast(BF16)[:, 256:384], [B[0]], [kT2])
                ts(wis[:], c5f[:, 320:328], CSC, ALU.mult, [c5f], [wis])
                ts(sgn_all[:, n, :], c5f[:, 320:328], 0.0, ALU.is_ge, [c5f], [sgn_all], s2=2.0, op1=ALU.mult)
                ts(sgn_all[:, n, :], sgn_all[:, n, :], -1.0, ALU.add, [sgn_all], [sgn_all])
                tt(cw_[:], cosb.broadcast_to([128, 8, 32]), wis[:].unsqueeze(2).broadcast_to([128, 8, 32]),
                   ALU.mult, [cos_t, wis], [cw_])
                tt(sw_[:], sinb.broadcast_to([128, 8, 32]), wis[:].unsqueeze(2).broadcast_to([128, 8, 32]),
                   ALU.mult, [sin_t, wis], [sw_])
                for hb in range(2):
                    qv = qf[:, hb * 512:(hb + 1) * 512].rearrange("p (h d) -> p h d", d=64)
                    ov = qr[:, hb * 512:(hb + 1) * 512].rearrange("p (h d) -> p h d", d=64)
                    rope(ov[:, :, 0:32], ov[:, :, 32:64], qv[:, :, 0:32], qv[:, :, 32:64],
                         cosb.broadcast_to([128, 8, 32]), sinb.broadcast_to([128, 8, 32]), 8,
                         [qf.sub[hb], cos_t, sin_t, qr])
                for hb in range(2):
                    src = c4f[:, 256:512] if hb == 0 else c5f[:, 0:256]
                    qv = src.rearrange("p (h d) -> p h d", d=64)
                    ov = qir[:, hb * 256:(hb + 1) * 256].rearrange("p (h d) -> p h d", d=64)
                    rope(ov[:, :, 0:32], ov[:, :, 32:64], qv[:, :, 0:32], qv[:, :, 32:64],
                         cw_[:, hb * 4:(hb + 1) * 4, :], sw_[:, hb * 4:(hb + 1) * 4, :], 4,
                         [c4f if hb == 0 else c5f, cw_, sw_, qir])

            def E2_b(n):
                qr, qir = qrs[n % 2], qirs[n % 2]
                b6 = B[6][:].bitcast(BF16)
                for pr in range(8):
                    tr(b6[:, pr * 128:(pr + 1) * 128], qr[:, pr * 128:(pr + 1) * 128], identb[:], [qr, identb], [B[6]])
                b63 = b6.rearrange("p (a t) -> p a t", t=128)
                cp(qTe[0:64, :, :], b63[0:64, :, :], [B[6]], [qTe])
                cp(qTo[64:128, :, :], b63[64:128, :, :], [B[6]], [qTo])
                b7 = B[7][:].bitcast(BF16)
                for pr in range(4):
                    tr(b7[:, pr * 128:(pr + 1) * 128], qir[:, pr * 128:(pr + 1) * 128], identb[:], [qir, identb], [B[7]])
                b73 = b7[:, 0:512].rearrange("p (a t) -> p a t", t=128)
                cp(qiTe[0:64, :, :], b73[0:64, :, :], [B[7]], [qiTe], eng="scalar")
                cp(qiTo[64:128, :, :], b73[64:128, :, :], [B[7]], [qiTo], eng="scalar")
                ld(qTe_s.ap()[n], qTe[:].rearrange("p a t -> p (a t)"), [qTe], [DV["qTe"]], q="gpsimd")
                ld(qTo_s.ap()[n], qTo[:].rearrange("p a t -> p (a t)"), [qTo], [DV["qTo"]], q="gpsimd")
                ld(qiTe_s.ap()[n], qiTe[:].rearrange("p a t -> p (a t)"), [qiTe], [DV["qiTe"]], q="gpsimd")
                ld(qiTo_s.ap()[n], qiTo[:].rearrange("p a t -> p (a t)"), [qiTo], [DV["qiTo"]], q="gpsimd")

            H_b(0)
            for n in range(NT):
                M_b(n)
                if n + 1 < NT:
                    H_b(n + 1)
                if n >= 1:
                    E2_b(n - 1)
                E1_b(n)
            E2_b(NT - 1)
            S.barrier()
            S.emit()

        with contextlib.ExitStack() as ph:
            S.stack = ph
            woutb = S.sb("woutb", [128, 16, D], BF16)
            with contextlib.ExitStack() as ph2:
                S.stack = ph2
                stg = [S.sb(f"stgO{i}", [128, 16, 256], F32) for i in range(2)]
                gate1_bc = S.sb("gate1_bc", [128, D], F32)
                ld(gate1_bc[:], mod_s.ap()[2 * D:3 * D].partition_broadcast(128), [DV["mod"]], [gate1_bc])
                wout_v = wout_d.ap().rearrange("(k p) c -> p k c", p=128)
                rowscale_bufs.clear()
                rowscale_bufs.extend([gT, gate1_bc])
                load_w_bf(woutb, lambda c0, cw: wout_v[:, :, c0:c0 + cw], D, stg, rowscale=gT[:, 32:48],
                          colscale=gate1_bc)
                S.barrier()
                S.emit()
            S.stack = ph
            score = S.sb("score", [128, ST], F32)
            NMs = [S.sb(f"NM{i}", [128, ST], BF16) for i in range(2)]
            qTe = [S.sb(f"qTeL{i}", [128, 1024], BF16) for i in range(2)]
            qTo = [S.sb(f"qToL{i}", [128, 1024], BF16) for i in range(2)]
            qiTe = [S.sb(f"qiTeL{i}", [128, 512], BF16) for i in range(2)]
            qiTo = [S.sb(f"qiToL{i}", [128, 512], BF16) for i in range(2)]
            pT = [S.sb(f"pT{i}", [128, 512], BF16) for i in range(3)]
            xt = S.sb("xtC", [128, D], F32)
            yanl = S.sb("yanl", [128, 1024], BF16)
            yb = S.sb("yb", [128, 1024], F32)
            ybn = S.sb("ybn", [128, 1024], BF16)
            yT = S.subs(S.sb("yT", [128, 16, 128], BF16), 2, "yT")
            xm = S.sb("xm", [128, D], F32)
            xn2b = S.sb("xn2b", [128, D], BF16)
            h2T = S.subs(S.sb("h2T", [128, 16, 128], F32), 16)
            wr = S.sb("wr", [128, 16, 36], F32)
            bias_bc = S.sb("bias_bc", [128, 36], F32)
            lg = S.sb("lg", [128, 36], F32)
            em = S.sb("em", [128, 32], F32)
            m8 = S.sb("m8", [128, 8], F32)
            i8 = S.sb("i8", [128, 8], U32)
            Ab = S.sb("Ab", [128, 32], BF16)
            oh0 = S.sb("oh0", [128, 32], F32)
            oh1 = S.sb("oh1", [128, 32], F32)
            posf = S.sb("posf", [128, 32], F32)
            tmp32 = S.sb("tmp32", [128, 32], F32)
            sm = S.sb("sm", [128, 32], F32)
            lo = S.sb("lo", [128, 1], F32)
            w0c = S.sb("w0c", [128, 1], F32)
            mid = S.sb("mid", [128, 1], F32)
            cnt = S.sb("cnt", [128, 1], F32)
            gei = S.sb("gei", [128, 1], U32)
            thr = S.sb("thr", [128, 1], F32)
            rden = S.sb("rden", [128, 16], F32)
            destf = S.sb("destf", [128, 2], F32)

            ld(wr[:], wr_d.ap().rearrange("(k p) c -> p k c", p=128), [DV["in"]], [wr])
            ld(bias_bc[:], br_d.ap().partition_broadcast(128), [DV["in"]], [bias_bc])
            mset(base_bc[:], 0.0, [base_bc])

            def stage_A(n):
                Sk = (n + 1) * 128
                qe, qo, qie, qio = qTe[n % 2], qTo[n % 2], qiTe[n % 2], qiTo[n % 2]
                NM = NMs[n % 2]
                ld(qie[:], qiTe_s.ap()[n], [DV["qiTe"]], [qie])
                ld(qio[:], qiTo_s.ap()[n], [DV["qiTo"]], [qio])
                ld(qe[:], qTe_s.ap()[n], [DV["qTe"]], [qe])
                ld(qo[:], qTo_s.ap()[n], [DV["qTo"]], [qo])
                bi = 0
                for ks in range(0, Sk, 512):
                    ke = min(Sk, ks + 512)
                    for h in range(8):
                        bk = B[bi % 2]
                        bi += 1
                        src = (qie if h % 2 == 0 else qio)[:, (h // 2) * 128:(h // 2 + 1) * 128]
                        mm(bk[:, 0:ke - ks], src, kiT2[:, ks:ke], True, True, [qie, qio, kiT2], [bk])
                        sg = sgn_all[:, n, h:h + 1]
                        act(bk[:, 0:ke - ks], bk[:, 0:ke - ks], AF.Relu, [bk, sgn_all], [bk], scale=sg)
                        if h == 0:
                            ts(score[:, ks:ke], bk[:, 0:ke - ks], sg, ALU.mult, [bk, sgn_all], [score])
                        else:
                            stt(score[:, ks:ke], bk[:, 0:ke - ks], sg, score[:, ks:ke], ALU.mult, ALU.add,
                                [bk, sgn_all, score], [score])
                mset(score[0:64, Sk - 64:Sk], -1.0e30, [score])
                if n < 2:
                    mset(thr[:], -1.0e29, [thr])
                else:
                    S.op("vector", (lambda Sk: lambda e: e.tensor_reduce(out=lo[:], in_=score[:, 0:Sk - 64], axis=AX.X,
                                                                         op=ALU.min))(Sk), [score], [lo])
                    S.op("vector", (lambda Sk: lambda e: e.tensor_reduce(out=w0c[:], in_=score[:, 0:Sk], axis=AX.X,
                                                                         op=ALU.max))(Sk), [score], [w0c])
                    stt(w0c[:], w0c[:], 1.0e-4, lo[:], ALU.add, ALU.subtract, [w0c, lo], [w0c])
                    for it in range(NITER):
                        stt(mid[:], w0c[:], 2.0 ** -(it + 1), lo[:], ALU.mult, ALU.add, [w0c, lo], [mid])
                        ts(NM[:, 0:Sk], score[:, 0:Sk], mid[:, 0:1], ALU.is_ge, [score, mid], [NM, cnt],
                           s2=0.0, op1=ALU.add, acc=cnt[:, 0:1])
                        ts(gei[:], cnt[:], 255.5, ALU.is_ge, [cnt], [gei])
                        S.op("vector", lambda e: e.copy_predicated(lo[:], gei[:], mid[:]), [gei, mid, lo], [lo])
                    cp(thr[:], lo[:], [lo], [thr])
                ts(NM[:, 0:Sk], score[:, 0:Sk], thr[:, 0:1], ALU.is_lt, [score, thr], [NM], s2=-30000.0, op1=ALU.mult)

            def stage_B(n):
                Sk = (n + 1) * 128
                qe, qo = qTe[n % 2], qTo[n % 2]
                NM = NMs[n % 2]
                ld(xt[:], x_d.ap()[n * 128:(n + 1) * 128, :], [DV["in"]], [xt])
                ld(yanl[:], ya_s.ap()[n * 128:(n + 1) * 128, :], [DV["ya"]], [yanl])
                for b3 in range(3):
                    mm(B[4 + b3][:], zerob[:, 0:128], zerob[:], True, False, [zerob], [B[4 + b3]], skip=True)
                units = [(kb, j) for kb in range(n + 1) for j in range(4)]

                def QK(u):
                    kb, j = units[u]
                    bk = B[2 + u % 2]
                    pt = pT[u % 3]
                    qsrc = (qe if j < 2 else qo)[:, (j % 2) * 512:(j % 2 + 1) * 512]
                    mm(bk[:], kT2[:, kb * 128:(kb + 1) * 128], qsrc, True, False, [kT2, qe, qo], [bk])
                    mm(bk[:], NM[:, kb * 128:(kb + 1) * 128],
                       identb[:].unsqueeze(1).broadcast_to([128, 4, 128]), False, True, [NM, identb], [bk])
                    act(pt[:], bk[:], AF.Exp, [bk], [pt], scale=0.125)

                def PV(u):
                    kb, j = units[u]
                    pt = pT[u % 3]
                    for hh in range(4):
                        pair = (j % 2) * 4 + hh
                        head = pair * 2 + (0 if j < 2 else 1)
                        ob = B[4 + head // 7]
                        off = (head % 7) * 65
                        mm(ob[:, off:off + 65], pt[:, hh * 128:(hh + 1) * 128], vaug[:, kb, :], False,
                           kb == n, [pt, vaug], [ob], skip=True)

                QK(0)
                for u in range(len(units)):
                    if u + 1 < len(units):
                        QK(u + 1)
                    PV(u)
                for b3 in range(3):
                    nh = 7 if b3 < 2 else 2
                    ov = B[4 + b3][:, 0:nh * 65].rearrange("p (h d) -> p h d", d=65)
                    S.op("vector", (lambda ov, b3, nh: lambda e: e.reciprocal(
                        rden[:, b3 * 7:b3 * 7 + nh].unsqueeze(2), ov[:, :, 64:65]))(ov, b3, nh), [B[4 + b3]], [rden])
                    tt(yb[:, b3 * 448:b3 * 448 + nh * 64].rearrange("p (h d) -> p h d", d=64), ov[:, :, 0:64],
                       rden[:, b3 * 7:b3 * 7 + nh].unsqueeze(2).broadcast_to([128, nh, 64]), ALU.mult,
                       [B[4 + b3], rden], [yb])
                act(junk[:, 0:1024], yb[:], AF.Square, [yb], [junk, col[0]], acc=col[0][:, 0:1])
                rsqrt_col(col[1][:, 0:1], col[0][:, 0:1], 1.0 / 1024, [col[0]], [col[1]], col[2])
                ts(ybn[:], yb[:], col[1][:, 0:1], ALU.mult, [yb, col[1]], [ybn])
                for k in range(16):
                    bk = B[2 + k // 8]
                    srcy = yanl[:, k * 128:(k + 1) * 128] if k < 8 else ybn[:, (k - 8) * 128:(k - 7) * 128]
                    tr(bk[:].bitcast(BF16)[:, (k % 8) * 128:(k % 8 + 1) * 128], srcy, identb[:],
                       [yanl, ybn, identb], [bk])
                cp(yT[:, 0:8, :], B[2][:].bitcast(BF16).rearrange("p (a t) -> p a t", t=128), [B[2]], [yT.sub[0]])
                cp(yT[:, 8:16, :], B[3][:].bitcast(BF16).rearrange("p (a t) -> p a t", t=128), [B[3]], [yT.sub[1]],
                   eng="scalar")
                for db in range(4):
                    bk = B[(7, 4, 5, 6)[db]]
                    for k in range(16):
                        mm(bk[:], yT[:, k, :], woutb[:, k, db * 512:(db + 1) * 512], k == 0, k == 15,
                           [yT.sub[k // 8], woutb], [bk])
                    tt(xm[:, db * 512:(db + 1) * 512], bk[:], xt[:, db * 512:(db + 1) * 512], ALU.add, [bk, xt], [xm])
                ld(xmid_s.ap()[n * 128:(n + 1) * 128, :], xm[:], [xm], [DV["xmid"]], q="gpsimd")
                act(junk[:], xm[:], AF.Square, [xm], [junk, col[3]], acc=col[3][:, 0:1])
                rsqrt_col(col[4][:, 0:1], col[3][:, 0:1], 1.0 / D, [col[3]], [col[4]], col[5])
                ts(xt[:], xm[:], col[4][:, 0:1], ALU.mult, [xm, col[4]], [xt])
                act(xn2b[:], xm[:], AF.Copy, [xm, col[4]], [xn2b], scale=col[4][:, 0:1])
                for g4 in range(4):
                    bk = B[2 + g4 % 2]
                    for k in range(g4 * 4, g4 * 4 + 4):
                        tr(bk[:, (k % 4) * 128:(k % 4 + 1) * 128], xt[:, k * 128:(k + 1) * 128], ident[:],
                           [xt, ident], [bk])
                    for k in range(g4 * 4, g4 * 4 + 4):
                        if g4 % 2 == 0:
                            ts(h2T[:, k, :], bk[:, (k % 4) * 128:(k % 4 + 1) * 128], pvec[:, 32 + k:33 + k], ALU.mult,
                               [bk, pvec], [h2T.sub[k]], s2=pvec[:, 48 + k:49 + k], op1=ALU.add)
                        else:
                            act(h2T[:, k, :], bk[:, (k % 4) * 128:(k % 4 + 1) * 128], AF.Identity, [bk, pvec],
                                [h2T.sub[k]], bias=pvec[:, 48 + k:49 + k], scale=pvec[:, 32 + k:33 + k])
                for k in range(16):
                    mm(B[7][:, 0:36], h2T[:, k, :], wr[:, k, :], k == 0, k == 15, [h2T.sub[k], wr], [B[7]])
                tt(lg[:], B[7][:, 0:36], bias_bc[:], ALU.add, [B[7], bias_bc], [lg])
                S.op("vector", lambda e: e.tensor_reduce(out=sm[:, 0:1], in_=lg[:, 0:4], axis=AX.X, op=ALU.max), [lg], [sm])
                ts(sm[:, 1:2], sm[:, 0:1], -1.0, ALU.mult, [sm], [sm])
                act(sm[:, 4:8], lg[:, 0:4], AF.Exp, [lg, sm], [sm, col[6]], bias=sm[:, 1:2], scale=1.0,
                    acc=col[6][:, 0:1])
                S.op("vector", lambda e: e.reciprocal(sm[:, 2:3], col[6][:, 0:1]), [col[6]], [sm])
                ts(sm[:, 8:12], lg[:, 0:4], sm[:, 0:1], ALU.is_ge, [lg, sm], [sm], s2=1.0e9, op1=ALU.mult)
                ts(sm[:, 8:12], sm[:, 8:12], -1.0e9, ALU.add, [sm], [sm])
                tt(em[:].rearrange("p (g j) -> p g j", j=8), lg[:, 4:36].rearrange("p (g j) -> p g j", j=8),
                   sm[:, 8:12].unsqueeze(2).broadcast_to([128, 4, 8]), ALU.add, [lg, sm], [em])
                S.op("vector", lambda e: e.max(m8[:], em[:]), [em], [m8])
                S.op("vector", lambda e: e.max_index(i8[:], m8[:], em[:]), [m8, em], [i8])
                tt(sm[:, 12:13], m8[:, 1:2], m8[:, 0:1], ALU.subtract, [m8], [sm])
                act(sm[:, 13:14], sm[:, 12:13], AF.Exp, [sm], [sm])
                ts(sm[:, 13:14], sm[:, 13:14], 1.0, ALU.add, [sm], [sm])
                S.op("vector", lambda e: e.reciprocal(sm[:, 14:15], sm[:, 13:14]), [sm], [sm])
                tt(wgt_all[:, n, 0:1], sm[:, 14:15], sm[:, 2:3], ALU.mult, [sm], [wgt_all])
                tt(wgt_all[:, n, 1:2], sm[:, 2:3], wgt_all[:, n, 0:1], ALU.subtract, [sm, wgt_all], [wgt_all])
                ts(Ab[:], em[:], m8[:, 1:2], ALU.is_ge, [em, m8], [Ab])
                ts(oh0[:], em[:], m8[:, 0:1], ALU.is_ge, [em, m8], [oh0])
                tt(oh1[:], Ab[:], oh0[:], ALU.subtract, [Ab, oh0], [oh1])
                mm(B[4][:, 0:32], LTb[:], Ab[:], True, True, [LTb, Ab], [B[4]])
                tt(posf[:], B[4][:, 0:32], base_bc[:], ALU.add, [B[4], base_bc], [posf])
                mm(B[4][:, 64:96], onesb[:], Ab[:], True, True, [onesb, Ab], [B[4]])
                tt(base_bc[:], B[4][:, 64:96], base_bc[:], ALU.add, [B[4], base_bc, posf], [base_bc])
                cp(eid_all[:, n, :], i8[:, 0:2], [i8], [eid_all])
                for j, oh in enumerate((oh0, oh1)):
                    tt(tmp32[:], posf[:], oh[:], ALU.mult, [posf, oh], [tmp32])
                    S.op("vector", (lambda n, j: lambda e: e.tensor_reduce(out=pos_all[:, n, j:j + 1], in_=tmp32[:],
                                                                            axis=AX.X, op=ALU.add))(n, j),
                         [tmp32], [pos_all])
                ld(xn2_s.ap()[n * 128:(n + 1) * 128, :], xn2b[:], [xn2b], [DV["xn2"]], q="gpsimd")

            stage_A(0)
            for n in range(NT):
                if n + 1 < NT:
                    stage_A(n + 1)
                stage_B(n)
            S.barrier()
            S.emit()

        with contextlib.ExitStack() as ph:
            S.stack = ph
            pa = S.sb("pa", [128, 32], F32)
            pb = S.sb("pb", [128, 32], F32)
            pi_ = S.sb("pi_", [128, 32], I32)
            padded = S.sb("padded", [128, 32], F32)
            pstart = S.sb("pstart", [128, 32], F32)
            iotI = S.sb("iotI", [128, NSB], I32)
            iotF = S.sb("iotF", [128, NSB], F32)
            cmp3 = S.sb("cmp3", [128, NSB, 32], F32)
            bef = S.sb("bef", [128, NSB], F32)
            iw12 = S.sb("iw12", [128, 12], I32)
            iw12f = S.sb("iw12f", [128, 12], F32)
            widxf = S.sb("widxf", [128, NSB, 12], F32)
            ohd = S.sb("ohd", [128, 32], F32)
            dcol = S.sb("dcol", [128, 2], F32)
            xr2 = [S.sb(f"xr2_{i}", [128, D], BF16) for i in range(2)]
            ts(pa[:], base_bc[:], float(C - 1), ALU.add, [base_bc], [pa], s2=1.0 / C, op1=ALU.mult)
            cp(pi_[:], pa[:], [pa], [pi_])
            cp(pb[:], pi_[:], [pi_], [pb])
            tt(pa[:], pb[:], pa[:], ALU.is_gt, [pb, pa], [pa])
            tt(pb[:], pb[:], pa[:], ALU.subtract, [pb, pa], [pb])
            ts(padded[:], pb[:], float(C), ALU.mult, [pb], [padded])
            cp(pa[:], padded[:], [padded], [pa])
            src_, dst_ = pa, pb
            for sh in (1, 2, 4, 8, 16):
                cp(dst_[:, 0:sh], src_[:, 0:sh], [src_], [dst_])
                tt(dst_[:, sh:32], src_[:, sh:32], src_[:, 0:32 - sh], ALU.add, [src_], [dst_])
                src_, dst_ = dst_, src_
            pend = src_
            tt(pstart[:], pend[:], padded[:], ALU.subtract, [pend, padded], [pstart])
            S.op("gpsimd", lambda e: e.iota(iotI[:], [[C, NSB]], base=0, channel_multiplier=0), [], [iotI])
            cp(iotF[:], iotI[:], [iotI], [iotF])
            tt(cmp3[:], pend[:].unsqueeze(1).broadcast_to([128, NSB, 32]),
               iotF[:].unsqueeze(2).broadcast_to([128, NSB, 32]), ALU.is_le, [pend, iotF], [cmp3])
            S.op("vector", lambda e: e.tensor_reduce(out=bef[:], in_=cmp3[:], axis=AX.X, op=ALU.add), [cmp3], [bef])
            ts(bef[:], bef[:], 31.0, ALU.min, [bef], [bef], s2=1536.0, op1=ALU.mult)
            S.op("gpsimd", lambda e: e.iota(iw12[:], [[128, 12]], base=0, channel_multiplier=1), [], [iw12])
            cp(iw12f[:], iw12[:], [iw12], [iw12f])
            tt(widxf[:], bef[:].unsqueeze(2).broadcast_to([128, NSB, 12]),
               iw12f[:].unsqueeze(1).broadcast_to([128, NSB, 12]), ALU.add, [bef, iw12f], [widxf])
            cp(widx[:], widxf[:], [widxf], [widx])
            S.op("gpsimd", lambda e: e.iota(pi_[:], [[1, 32]], base=0, channel_multiplier=0), [pi_], [pi_])
            cp(pa[:], pi_[:], [pi_], [pa])
            for n in range(NT):
                for j in range(2):
                    ts(ohd[:], pa[:], eid_all[:, n, j:j + 1], ALU.is_equal, [pa, eid_all], [ohd])
                    tt(ohd[:], ohd[:], pstart[:], ALU.mult, [ohd, pstart], [ohd])
                    S.op("vector", (lambda j: lambda e: e.tensor_reduce(out=dcol[:, j:j + 1], in_=ohd[:], axis=AX.X,
                                                                         op=ALU.add))(j), [ohd], [dcol])
                tt(dcol[:], dcol[:], pos_all[:, n, :], ALU.add, [dcol, pos_all], [dcol])
                cp(dest_all[:, n, :], dcol[:], [dcol], [dest_all])
                xr = xr2[n % 2]
                ld(xr[:], xn2_s.ap()[n * 128:(n + 1) * 128, :], [DV["xn2"]], [xr])
                for j in range(2):
                    S.dma("gpsimd", (lambda n, j, xr: lambda e: e.indirect_dma_start(
                        out=xe_s.ap(), out_offset=bass.IndirectOffsetOnAxis(ap=dest_all[:, n, j:j + 1], axis=0),
                        in_=xr[:], in_offset=None, bounds_check=None, oob_is_err=False))(n, j, xr),
                        [xr, dest_all, DV["xe"]], [DV["xe"]], owner=xr)
            S.barrier()
            S.emit()

        with contextlib.ExitStack() as ph:
            S.stack = ph
            NB = C // 128
            stg = [S.sb(f"stgE{i}", [128, 4096], F32) for i in range(4)]
            wpb = [S.subs(S.sb(f"wpb{i}", [128, 4096], BF16), 2, "p3") for i in range(3)]
            xer1 = [S.sb(f"xer_{b}", [128, D], BF16) for b in range(NB)]
            xer = [xer1, xer1]
            xeT = S.subs(S.sb("xeT", [128, 16, C], BF16), 16, "p3")
            gTt = S.sb("gTt", [128, 8, C], BF16)
            sa = [S.sb(f"sa{i}", [128, C], F32) for i in range(2)]
            yo = [S.subs(S.sb(f"yo{i}", [128, D], F32), 4) for i in range(NB)]
            wexp_rows = wexp_d.ap().rearrange("e q p c -> (e q p) c")
            pieces = [(j, p) for j in range(NSB) for p in range(12)]
            rot = [0]

            def nbank():
                bk = B[2 + rot[0] % 6]
                rot[0] += 1
                return bk

            def emit_gather(i):
                j, piece = pieces[i]
                sg = stg[i % 4]
                S.dma("gpsimd", (lambda sg, j, piece: lambda e: e.indirect_dma_start(
                    out=sg[:], out_offset=None, in_=wexp_rows,
                    in_offset=bass.IndirectOffsetOnAxis(ap=widx[:, j, piece:piece + 1], axis=0),
                    bounds_check=None, oob_is_err=False))(sg, j, piece), [DV["in"], widx], [sg], owner=sg)

            def emit_cast(i):
                sg = stg[i % 4]
                wb = wpb[i % 3]
                cp(wb[:, 0:2048], sg[:, 0:2048], [sg], [wb.sub[0]], eng="vector")
                cp(wb[:, 2048:4096], sg[:, 2048:4096], [sg], [wb.sub[1]], eng="scalar")

            def load_xe(j):
                for blk in range(NB):
                    xr = xer[j % 2][blk]
                    ld(xr[:], xe_s.ap()[j * C + blk * 128:j * C + (blk + 1) * 128, :], [DV["xe"]], [xr])

            def prologue_part(j, part):
                for kp in (2 * part, 2 * part + 1):
                    bk = B[kp % 2]
                    bv = bk[:].bitcast(BF16)
                    for kk in range(2):
                        k = kp * 2 + kk
                        for blk in range(NB):
                            xr = xer[j % 2][blk]
                            tr(bv[:, (kk * NB + blk) * 128:(kk * NB + blk + 1) * 128], xr[:, k * 128:(k + 1) * 128],
                               identb[:], [xr, identb], [bk])
                    for kk in range(2):
                        k = kp * 2 + kk
                        src = bv[:, kk * C:(kk + 1) * C]
                        if kp % 2 == 0:
                            ts(xeT[:, k, :], src, pvec[:, 32 + k:33 + k], ALU.mult, [bk, pvec], [xeT.sub[k]],
                               s2=pvec[:, 48 + k:49 + k], op1=ALU.add)
                        else:
                            act(xeT[:, k, :], src, AF.Identity, [bk, pvec], [xeT.sub[k]],
                                bias=pvec[:, 48 + k:49 + k], scale=pvec[:, 32 + k:33 + k])

            yoi = [0]

            def compute(i):
                j, piece = pieces[i]
                wb = wpb[i % 3]
                if piece < 8:
                    f = piece
                    w13 = wb[:].rearrange("p (t k c) -> p t k c", t=2, k=16)
                    ba, bb = nbank(), nbank()
                    for k in range(16):
                        mm(ba[:, 0:C], w13[:, 0, k, :], xeT[:, k, :], k == 0, k == 15, [wb.sub[0], xeT.sub[k]], [ba])
                    for k in range(16):
                        mm(bb[:, 0:C], w13[:, 1, k, :], xeT[:, k, :], k == 0, k == 15, [wb.sub[1], xeT.sub[k]], [bb])
                    s_ = sa[f % 2]
                    act(s_[:], ba[:, 0:C], AF.Silu, [ba], [s_])
                    tt(gTt[:, f, :], bb[:, 0:C], s_[:], ALU.mult, [bb, s_], [gTt])
                else:
                    db = piece - 8
                    w2v = wb[:].rearrange("p (k c) -> p k c", k=8)
                    for blk in range(NB):
                        bk = nbank()
                        for fc in range(8):
                            mm(bk[:], gTt[:, fc, blk * 128:(blk + 1) * 128], w2v[:, fc, :], fc == 0, fc == 7,
                               [gTt, wb.sub[fc // 4]], [bk])
                        y_ = yo[blk]
                        if blk % 2 == 0:
                            cp(y_[:, db * 512:(db + 1) * 512], bk[:], [bk], [y_.sub[db]])
                        else:
                            cp(y_[:, db * 512:(db + 1) * 512], bk[:], [bk], [y_.sub[db]], eng="scalar")
                        if db == 3:
                            ld(ye_s.ap()[j * C + blk * 128:j * C + (blk + 1) * 128, :], y_[:],
                               list(y_.sub), [DV["ye"]], q="sync", owner=y_)

            load_xe(0)
            emit_gather(0)
            emit_gather(1)
            emit_gather(2)
            emit_cast(0)
            for part in range(4):
                prologue_part(0, part)
            if NSB > 1:
                load_xe(1)
            for i, (j, piece) in enumerate(pieces):
                if piece == 0 and j >= 1 and j + 1 < NSB:
                    load_xe(j + 1)
                if i + 3 < len(pieces):
                    emit_gather(i + 3)
                if i + 1 < len(pieces):
                    emit_cast(i + 1)
                compute(i)
                if piece >= 8 and j + 1 < NSB:
                    prologue_part(j + 1, piece - 8)
            S.barrier()
            S.emit()

        with contextlib.ExitStack() as ph:
            S.stack = ph
            gate2_bc = S.sb("gate2_bc", [128, D], F32)
            fg_bc = S.sb("fg_bc", [128, D], F32)
            y0 = [S.sb(f"y0_{i}", [128, D], F32) for i in range(2)]
            y1 = [S.sb(f"y1_{i}", [128, D], F32) for i in range(2)]
            xmt = [S.sb(f"xmt{i}", [128, D], F32) for i in range(2)]
            acc = S.sb("acc", [128, D], F32)
            ot = [S.sb(f"ot{i}", [128, D], F32) for i in range(2)]
            ld(gate2_bc[:], mod_s.ap()[5 * D:6 * D].partition_broadcast(128), [DV["mod"]], [gate2_bc])
            ld(fg_bc[:], fg_d.ap().partition_broadcast(128), [DV["in"]], [fg_bc])
            for n in range(NT):
                a0, a1, xq, o_ = y0[n % 2], y1[n % 2], xmt[n % 2], ot[n % 2]
                for j, dst in enumerate((a0, a1)):
                    S.dma("gpsimd", (lambda n, j, dst: lambda e: e.indirect_dma_start(
                        out=dst[:], out_offset=None, in_=ye_s.ap(),
                        in_offset=bass.IndirectOffsetOnAxis(ap=dest_all[:, n, j:j + 1], axis=0),
                        bounds_check=None, oob_is_err=False))(n, j, dst),
                        [DV["ye"], dest_all], [dst], owner=dst)
                ld(xq[:], xmid_s.ap()[n * 128:(n + 1) * 128, :], [DV["xmid"]], [xq])
                act(acc[:], a0[:], AF.Copy, [a0, wgt_all], [acc], scale=wgt_all[:, n, 0:1])
                stt(acc[:], a1[:], wgt_all[:, n, 1:2], acc[:], ALU.mult, ALU.add, [a1, wgt_all, acc], [acc])
                tt(acc[:], acc[:], gate2_bc[:], ALU.mult, [acc, gate2_bc], [acc], eng="gpsimd")
                tt(acc[:], acc[:], xq[:], ALU.add, [acc, xq], [acc])
                act(junk[:], acc[:], AF.Square, [acc], [junk, col[0]], acc=col[0][:, 0:1])
                rsqrt_col(col[1][:, 0:1], col[0][:, 0:1], 1.0 / D, [col[0]], [col[1]], col[2])
                stt(o_[:], acc[:], col[1][:, 0:1], fg_bc[:], ALU.mult, ALU.mult, [acc, col[1], fg_bc], [o_])
                ld(out_d.ap()[n * 128:(n + 1) * 128, :], o_[:], [o_], [DV["out"]], q="sync")
            S.barrier()
            S.emit()
    return nc


_CACHE = {}


def _prep_weights(inp):
    f = lambda a: np.ascontiguousarray(np.asarray(a, dtype=np.float32))
    w1 = np.asarray(inp["w1"], dtype=np.float32)[0]
    w3 = np.asarray(inp["w3"], dtype=np.float32)[0]
    w2 = np.asarray(inp["w2"], dtype=np.float32)[0]
    wexp = np.empty((NEXP, 12, 128, 4096), dtype=np.float32)
    a1 = w1.reshape(NEXP, 16, 128, 8, 128).transpose(0, 3, 2, 1, 4)
    a3 = w3.reshape(NEXP, 16, 128, 8, 128).transpose(0, 3, 2, 1, 4)
    v = wexp[:, 0:8].reshape(NEXP, 8, 128, 2, 16, 128)
    v[:, :, :, 0] = a1
    v[:, :, :, 1] = a3
    a2 = w2.reshape(NEXP, 8, 128, 4, 512).transpose(0, 3, 2, 1, 4)
    wexp[:, 8:12] = a2.reshape(NEXP, 4, 128, 4096)
    shared = {
        "w_ada": f(inp["w_ada"][0]), "b_ada": f(inp["b_ada"][0]), "norm1_g": f(inp["norm1_g"][0]),
        "w_in": f(inp["w_in"][0]), "v_norm_g": f(inp["v_norm_g"][0]).reshape(-1),
        "v_norm_b": f(inp["v_norm_b"][0]).reshape(-1), "w_sp": f(inp["w_sp"][0]), "b_sp": f(inp["b_sp"][0]),
        "kv_norm_g": f(inp["kv_norm_g"][0]), "w_uk": f(inp["w_uk"][0]), "w_uv": f(inp["w_uv"][0]),
        "kidx_norm_g": f(inp["kidx_norm_g"][0]), "gnorm_a_g": f(inp["gnorm_a_g"][0]),
        "gnorm_b_g": f(inp["gnorm_b_g"][0]), "w_out": f(inp["w_out"][0]), "norm2_g": f(inp["norm2_g"][0]),
        "w_r": np.ascontiguousarray(np.concatenate([np.asarray(inp["w_group"][0]), np.asarray(inp["w_expert"][0])],
                                                   axis=1).astype(np.float32)),
        "b_r": np.ascontiguousarray(np.concatenate([np.asarray(inp["b_group"][0]), np.asarray(inp["b_expert"][0])],
                                                   axis=0).astype(np.float32)),
        "wexp": wexp, "final_g": f(inp["final_g"]),
    }
    return shared


def kernel(**inputs):
    x = np.asarray(inputs["x"], dtype=np.float32)
    c = np.asarray(inputs["c"], dtype=np.float32)
    pos = np.asarray(inputs["positions"], dtype=np.int32)
    nb, seq, _ = x.shape
    NT = seq // 128
    key = (NT,)
    if key not in _CACHE:
        _CACHE[key] = build(NT=NT)
    nc = _CACHE[key]
    shared = _prep_weights(inputs)
    in_maps = []
    for b in range(nb):
        m = dict(shared)
        m["x"] = np.ascontiguousarray(x[b])
        m["c"] = np.ascontiguousarray(c[b])
        m["pos"] = np.ascontiguousarray(pos[b])
        in_maps.append(m)
    res = run_bass_kernel_spmd(nc, in_maps, core_ids=list(range(nb)))
    return np.stack([np.asarray(r["out"], dtype=np.float32) for r in res.results], axis=0)
```

```python
import contextlib
import math
import numpy as np
import concourse.bass as bass
import concourse.mybir as mybir
from concourse.bass_utils import run_bass_kernel_spmd

F32 = mybir.dt.float32
BF16 = mybir.dt.bfloat16
I32 = mybir.dt.int32
U32 = mybir.dt.uint32
AF = mybir.ActivationFunctionType
ALU = mybir.AluOpType
AX = mybir.AxisListType

FLAGS = {"hT": 1, "p3": 1, "yT": 1, "reorder": 1}
EPOCH = 12000
ENGS = ["tensor", "vector", "scalar", "gpsimd", "sync"]
D = 2048
NEXP = 32
EPS = 1e-6


class Buf:
    __slots__ = ("name", "w", "r", "dsem", "dcum", "t", "multi", "sub")

    def __init__(self, name, t=None):
        self.name = name
        self.multi = False
        self.w = {}
        self.r = {}
        self.dsem = None
        self.dcum = 0
        self.t = t

    def __getitem__(self, k):
        return self.t[k]


class Sched:
    def __init__(self, nc, semstack):
        self.nc = nc
        self.semstack = semstack
        self.stack = semstack
        self.ops = {e: [] for e in ENGS}
        self.cnt = {e: 0 for e in ENGS}
        self.sems = {}
        self.waited = {e: {} for e in ENGS}
        self.cur_cum = {}
        self.nsem = 0
        self.bufs = []

    def _newsem(self, name):
        self.nsem += 1
        return self.semstack.enter_context(self.nc.semaphore(f"{name}_{self.nsem}"))

    def sb(self, name, shape, dtype):
        t = self.stack.enter_context(self.nc.sbuf_tensor(name, list(shape), dtype))
        b = Buf(name, t)
        self.bufs.append(b)
        return b

    def ps(self, name, shape, dtype):
        t = self.stack.enter_context(self.nc.psum_tensor(name, list(shape), dtype))
        b = Buf(name, t)
        self.bufs.append(b)
        return b

    def subs(self, buf, n, flag=None):
        buf.sub = []
        if flag is not None and not FLAGS.get(flag, 1):
            buf.sub = [buf] * n
            return buf
        for i in range(n):
            b = Buf(f"{buf.name}_s{i}", buf.t)
            self.bufs.append(b)
            buf.sub.append(b)
        return buf

    def view(self, name):
        b = Buf(name, None)
        b.multi = True
        self.bufs.append(b)
        return b

    def _engkey(self, eng):
        idx = self.cnt[eng]
        ep = idx // EPOCH
        key = ("E", eng, ep)
        if key not in self.sems:
            self.sems[key] = self._newsem(f"e_{eng}_{ep}")
        return key, (idx % EPOCH) + 1

    def _collect(self, eng, reads, writes):
        deps = {}

        def add(d):
            for k, v in d.items():
                if k[0] == "D":
                    v = max(v, self.cur_cum.get(k, v))
                if deps.get(k, 0) < v:
                    deps[k] = v
        for b in reads:
            add(b.w)
        for b in writes:
            if not b.multi:
                add(b.w)
            add(b.r)
        out = []
        wd = self.waited[eng]
        for k, v in deps.items():
            if k[0] == "E" and k[1] == eng and eng in ("tensor", "sync"):
                continue
            if wd.get(k, 0) >= v:
                continue
            wd[k] = v
            out.append((k, v))
        return out

    def _record(self, me, reads, writes):
        k, v = me
        for b in writes:
            if b.multi:
                if b.w.get(k, 0) < v:
                    b.w[k] = v
            else:
                b.w = {k: v}
                b.r = {}
        for b in reads:
            if b.r.get(k, 0) < v:
                b.r[k] = v

    def op(self, eng, fn, reads=(), writes=()):
        waits = self._collect(eng, reads, writes)
        key, val = self._engkey(eng)
        self.ops[eng].append((waits, fn, key, 1))
        self.cnt[eng] += 1
        self._record((key, val), reads, writes)

    def dma(self, queue, fn, reads=(), writes=(), owner=None):
        waits = self._collect(queue, reads, writes)
        if owner is None:
            owner = (list(writes) + list(reads))[0]
        if owner.dsem is None or owner.dcum + 16 > EPOCH * 2:
            key = ("D", id(owner), self.nsem)
            self.sems[key] = self._newsem("d_" + owner.name)
            owner.dsem = key
            owner.dcum = 0
        owner.dcum += 16
        key = owner.dsem
        self.cur_cum[key] = owner.dcum
        self.ops[queue].append((waits, fn, key, 16))
        self._record((key, owner.dcum), reads, writes)

    def raw(self, eng, fn, reads=()):
        waits = self._collect(eng, reads, [])
        self.ops[eng].append((waits, fn, "RAW", 0))

    def barrier(self):
        allk = {}
        for e in ENGS:
            if self.cnt[e] > 0:
                idx = self.cnt[e] - 1
                allk[("E", e, idx // EPOCH)] = (idx % EPOCH) + 1
        for k, v in self.cur_cum.items():
            allk[k] = v
        for e in ENGS:
            wd = self.waited[e]
            ws = []
            for k, v in allk.items():
                if wd.get(k, 0) >= v:
                    continue
                wd[k] = v
                if k[0] == "E" and k[1] == e:
                    continue
                ws.append((k, v))
            if ws:
                self.ops[e].append((ws, None, None, 0))
        for b in self.bufs:
            b.w = {}
            b.r = {}

    def emit(self):
        nc = self.nc
        with nc.Block() as block:
            for e in ENGS:
                ops = self.ops[e]
                if not ops:
                    continue

                def body(eng, ops=ops):
                    for waits, fn, key, inc in ops:
                        for k, v in waits:
                            eng.wait_ge(self.sems[k], v)
                        if fn is None:
                            continue
                        if key == "RAW":
                            fn(eng)
                        else:
                            fn(eng).then_inc(self.sems[key], inc)
                getattr(block, e)(body)
        self.ops = {e: [] for e in ENGS}


def build(NT=32, NITER=16, dbg=False):
    ST = NT * 128
    C = 512
    NSB = (2 * ST + C - 1) // C + NEXP
    nc = bass.Bass("TRN2", target_bir_lowering=False)

    def din(name, shape, dt=F32):
        return nc.dram_tensor(name, list(shape), dt, kind="ExternalInput")

    def dscr(name, shape, dt=F32):
        if dbg:
            return nc.dram_tensor(name, list(shape), dt, kind="ExternalOutput")
        return nc.dram_tensor(name, list(shape), dt)

    x_d = din("x", [ST, D])
    c_d = din("c", [D])
    pos_d = din("pos", [ST], I32)
    wada_d = din("w_ada", [D, 6 * D])
    bada_d = din("b_ada", [6 * D])
    n1g_d = din("norm1_g", [D])
    win_d = din("w_in", [D, 3912])
    vng_d = din("v_norm_g", [1024])
    vnb_d = din("v_norm_b", [1024])
    wsp_d = din("w_sp", [8, 128, 128])
    bsp_d = din("b_sp", [8, 128])
    kvg_d = din("kv_norm_g", [256])
    wuk_d = din("w_uk", [256, 64])
    wuv_d = din("w_uv", [256, 64])
    kig_d = din("kidx_norm_g", [64])
    gna_d = din("gnorm_a_g", [1024])
    gnb_d = din("gnorm_b_g", [1024])
    wout_d = din("w_out", [D, D])
    n2g_d = din("norm2_g", [D])
    wr_d = din("w_r", [D, 36])
    br_d = din("b_r", [36])
    wexp_d = din("wexp", [NEXP, 12, 128, 4096])
    fg_d = din("final_g", [D])
    out_d = nc.dram_tensor("out", [ST, D], F32, kind="ExternalOutput")

    mod_s = dscr("mod_s", [6 * D])
    ya_s = dscr("ya_s", [ST, 1024], BF16)
    qTe_s = dscr("qTe_s", [NT, 128, 1024], BF16)
    qTo_s = dscr("qTo_s", [NT, 128, 1024], BF16)
    qiTe_s = dscr("qiTe_s", [NT, 128, 512], BF16)
    qiTo_s = dscr("qiTo_s", [NT, 128, 512], BF16)
    xmid_s = dscr("xmid_s", [ST, D])
    xn2_s = dscr("xn2_s", [ST, D], BF16)
    xe_s = dscr("xe_s", [NSB * C, D], BF16)
    ye_s = dscr("ye_s", [NSB * C, D])

    with contextlib.ExitStack() as outer:
        S = Sched(nc, outer)
        B = [S.ps(f"B{i}", [128, 512], F32) for i in range(8)]
        DV = {n: S.view("dv_" + n) for n in
              ["in", "mod", "ya", "qTe", "qTo", "qiTe", "qiTo", "xmid", "xe", "ye", "out", "xn2"]}

        def mm(o, l, r, st, sp, R, W, skip=False):
            S.op("tensor", lambda e: e.matmul(o, l, r, start=st, stop=sp, skip_group_check=skip), R, W)

        def tr(o, i, idn, R, W):
            S.op("tensor", lambda e: e.transpose(o, i, idn), R, W)

        def act(o, i, f, R, W, bias=None, scale=None, acc=None):
            kw = {}
            if bias is not None:
                kw["bias"] = bias
            if scale is not None:
                kw["scale"] = scale
            if acc is not None:
                kw["accum_out"] = acc
            S.op("scalar", lambda e: e.activation(out=o, in_=i, func=f, **kw), R, W)

        def ts(o, i, s1, op0, R, W, s2=None, op1=None, eng="vector", acc=None):
            if acc is not None:
                S.op(eng, lambda e: e.tensor_scalar(o, i, s1, s2, op0, op1, accum_out=acc), R, W)
            elif op1 is None:
                S.op(eng, lambda e: e.tensor_scalar(o, i, s1, None, op0), R, W)
            else:
                S.op(eng, lambda e: e.tensor_scalar(o, i, s1, s2, op0, op1), R, W)

        def tt(o, a, b, op, R, W, eng="vector"):
            S.op(eng, lambda e: e.tensor_tensor(out=o, in0=a, in1=b, op=op), R, W)

        def stt(o, a, s, b, op0, op1, R, W):
            S.op("vector", lambda e: e.scalar_tensor_tensor(out=o, in0=a, scalar=s, in1=b, op0=op0, op1=op1), R, W)

        def cp(o, i, R, W, eng="vector"):
            if eng == "scalar":
                S.op("scalar", lambda e: e.copy(o, i), R, W)
            else:
                S.op(eng, lambda e: e.tensor_copy(o, i), R, W)

        def mset(ap, val, W, eng="vector"):
            S.op(eng, lambda e: e.memset(ap, val), [], W)

        def ld(o, i, R, W, q="sync", owner=None):
            S.dma(q, lambda e: e.dma_start(out=o, in_=i), R, W, owner=owner)

        def rsqrt_col(dst, src, scale, R, W, tmp):
            w_ = src.shape[-1]
            ts(tmp[:, 0:w_], src, scale, ALU.mult, R, [tmp], s2=EPS, op1=ALU.add)
            act(tmp[:, 0:w_], tmp[:, 0:w_], AF.Sqrt, [tmp], [tmp])
            S.op("vector", lambda e: e.reciprocal(dst, tmp[:, 0:w_]), [tmp], W)

        ident = S.sb("ident", [128, 128], F32)
        identb = S.sb("identb", [128, 128], BF16)
        onesf = S.sb("onesf", [128, 128], F32)
        onesb = S.sb("onesb", [128, 128], BF16)
        zerob = S.sb("zerob", [128, 512], BF16)
        LTb = S.sb("LTb", [128, 128], BF16)
        pvec = S.sb("pvec", [128, 64], F32)
        gT = S.sb("gT", [128, 58], F32)
        kT2 = S.sb("kT2", [128, ST], BF16)
        kiT2 = S.sb("kiT2", [128, ST], BF16)
        vaug = S.sb("vaug", [128, NT, 65], BF16)
        sgn_all = S.sb("sgn_all", [128, NT, 8], F32)
        dest_all = S.sb("dest_all", [128, NT, 2], I32)
        eid_all = S.sb("eid_all", [128, NT, 2], F32)
        pos_all = S.sb("pos_all", [128, NT, 2], F32)
        base_bc = S.sb("base_bc", [128, 32], F32)
        widx = S.sb("widx", [128, NSB, 12], I32)
        wgt_all = S.sb("wgt_all", [128, NT, 2], F32)
        junk = S.sb("junk", [128, 2048], BF16)
        col = [S.sb(f"col{i}", [128, 16], F32) for i in range(8)]

        mset(onesf[:], 1.0, [onesf], eng="gpsimd")
        mset(onesb[:], 1.0, [onesb], eng="gpsimd")
        mset(zerob[:], 0.0, [zerob], eng="gpsimd")
        S.op("gpsimd", lambda e: e.affine_select(out=ident[:], in_=onesf[:], pattern=[[1, 128]],
                                                 compare_op=ALU.is_equal, fill=0.0, base=0,
                                                 channel_multiplier=-1), [onesf], [ident])
        S.op("gpsimd", lambda e: e.affine_select(out=identb[:], in_=onesf[:], pattern=[[1, 128]],
                                                 compare_op=ALU.is_equal, fill=0.0, base=0,
                                                 channel_multiplier=-1), [onesf], [identb])
        S.op("gpsimd", lambda e: e.affine_select(out=LTb[:], in_=onesf[:], pattern=[[1, 128]],
                                                 compare_op=ALU.is_ge, fill=0.0, base=-1,
                                                 channel_multiplier=-1), [onesf], [LTb])
        mset(vaug[:, :, 64:65], 1.0, [vaug], eng="gpsimd")

        with contextlib.ExitStack() as ph:
            S.stack = ph
            stA = S.sb("stA", [112, 128], F32)
            stB = S.sb("stB", [58, 128], F32)
            siluc = S.sb("siluc", [128, 16], F32)
            cT = S.sb("cT", [128, 16], F32)
            badaT = S.sb("badaT", [128, 96], F32)
            modT = S.sb("modT", [128, 96], F32)
            modR = S.sb("modR", [96, 128], F32)
            wa = [S.sb(f"wa{i}", [128, 16, 512], F32) for i in range(2)]
            modrow = S.sb("modrow", [1, 6 * D], F32)

            ld(stA[0:16, :], c_d.ap().rearrange("(k p) -> k p", p=128), [DV["in"]], [stA])
            ld(stA[16:112, :], bada_d.ap().rearrange("(k p) -> k p", p=128), [DV["in"]], [stA])
            r0 = 0
            for src, nr in [(n1g_d, 16), (n2g_d, 16), (gna_d, 8), (gnb_d, 8), (kvg_d, 2)]:
                ld(stB[r0:r0 + nr, :], src.ap().rearrange("(k p) -> k p", p=128), [DV["in"]], [stB])
                r0 += nr
            ld(stB[50:58, :], bsp_d.ap(), [DV["in"]], [stB])
            tr(B[0][:, 0:112], stA[0:112, :], ident[0:112, 0:112], [stA, ident], [B[0]])
            tr(B[1][:, 0:58], stB[0:58, :], ident[0:58, 0:58], [stB, ident], [B[1]])
            cp(cT[:], B[0][:, 0:16], [B[0]], [cT])
            act(siluc[:], cT[:], AF.Silu, [cT], [siluc])
            cp(badaT[:], B[0][:, 16:112], [B[0]], [badaT])
            cp(gT[:], B[1][:, 0:58], [B[1]], [gT])
            for cb in range(24):
                w = wa[cb % 2]
                ld(w[:], wada_d.ap().rearrange("(k p) c -> p k c", p=128)[:, :, cb * 512:(cb + 1) * 512],
                   [DV["in"]], [w])
                rb = B[2 + cb % 2]
                for k in range(16):
                    mm(rb[0:1, :], siluc[:, k:k + 1], w[:, k, :], k == 0, k == 15, [w, siluc], [rb])
                cp(modrow[0:1, cb * 512:(cb + 1) * 512], rb[0:1, :], [rb], [modrow])
            for j in range(96):
                mm(B[4][:, j:j + 1], modrow[0:1, j * 128:(j + 1) * 128], onesf[0:1, 0:1], True, True,
                   [modrow, onesf], [B[4]])
            tt(modT[:], B[4][:, 0:96], badaT[:], ALU.add, [B[4], badaT], [modT])
            stt(pvec[:, 0:16], modT[:, 16:32], 1.0, gT[:, 0:16], ALU.add, ALU.mult, [modT, gT], [pvec])
            cp(pvec[:, 16:32], modT[:, 0:16], [modT], [pvec])
            stt(pvec[:, 32:48], modT[:, 64:80], 1.0, gT[:, 16:32], ALU.add, ALU.mult, [modT, gT], [pvec])
            cp(pvec[:, 48:64], modT[:, 48:64], [modT], [pvec])
            tr(B[3][0:96, 0:128], modT[:, 0:96], ident[:], [modT, ident], [B[3]])
            cp(modR[:], B[3][0:96, 0:128], [B[3]], [modR])
            ld(mod_s.ap().rearrange("(j p) -> j p", p=128), modR[:], [modR], [DV["mod"]], q="gpsimd")
            S.barrier()
            S.emit()

        def prep_hT(xt, xn, hT, c_ss, c_rs, c_tmp, sc_off, sh_off):
            act(junk[:], xt[:], AF.Square, [xt], [junk, c_ss], acc=c_ss[:, 0:1])
            rsqrt_col(c_rs[:, 0:1], c_ss[:, 0:1], 1.0 / D, [c_ss], [c_rs], c_tmp)
            ts(xn[:], xt[:], c_rs[:, 0:1], ALU.mult, [xt, c_rs], [xn])
            for k in range(16):
                bk = B[k // 8]
                tr(bk[:].bitcast(BF16)[:, (k % 8) * 128:(k % 8 + 1) * 128], xn[:, k * 128:(k + 1) * 128],
                   identb[:], [xn, identb], [bk])
            for k in range(16):
                bk = B[k // 8]
                src = bk[:].bitcast(BF16)[:, (k % 8) * 128:(k % 8 + 1) * 128]
                if k < 8:
                    ts(hT[:, k, :], src, pvec[:, sc_off + k:sc_off + k + 1], ALU.mult, [bk, pvec], [hT.sub[k]],
                       s2=pvec[:, sh_off + k:sh_off + k + 1], op1=ALU.add)
                else:
                    act(hT[:, k, :], src, AF.Identity, [bk, pvec], [hT.sub[k]],
                        bias=pvec[:, sh_off + k:sh_off + k + 1], scale=pvec[:, sc_off + k:sc_off + k + 1])

        def load_w_bf(dst, src_ap_fn, ncols, stg, rowscale=None, colscale=None):
            step = 256
            i = 0
            for c0 in range(0, ncols, step):
                cw = min(step, ncols - c0)
                sg = stg[i % 2]
                ld(sg[:, :, 0:cw], src_ap_fn(c0, cw), [DV["in"]], [sg])
                eng = ["vector", "gpsimd"][i % 2]
                if rowscale is None:
                    if i % 3 == 2:
                        cp(dst[:, :, c0:c0 + cw], sg[:, :, 0:cw], [sg], [dst], eng="scalar")
                    else:
                        cp(dst[:, :, c0:c0 + cw], sg[:, :, 0:cw], [sg], [dst], eng=eng)
                else:
                    for k in range(16):
                        stt(dst[:, k, c0:c0 + cw], sg[:, k, 0:cw], rowscale[:, k:k + 1], colscale[:, c0:c0 + cw],
                            ALU.mult, ALU.mult, [sg] + rowscale_bufs, [dst])
                i += 1

        rowscale_bufs = []

        with contextlib.ExitStack() as ph:
            S.stack = ph
            wbf = S.sb("wbfA", [128, 16, 2048], BF16)
            stg = [S.sb(f"stgA{i}", [128, 16, 256], F32) for i in range(2)]
            xts = [S.sb(f"xtA{i}", [128, D], F32) for i in range(2)]
            xns = [S.sb(f"xnA{i}", [128, D], BF16) for i in range(2)]
            hTs = [S.subs(S.sb(f"hTA{i}", [128, 16, 128], BF16), 16, "hT") for i in range(2)]
            gu = S.sb("gu", [128, 1024], F32)
            gv = S.sb("gv", [128, 1024], F32)
            vn = S.sb("vn", [128, 1024], F32)
            vgb = S.sb("vgb", [128, 1024], BF16)
            Gbc = S.sb("Gbc", [128, 1024], F32)
            Bbc = S.sb("Bbc", [128, 1024], F32)
            wsp = S.sb("wsp", [128, 8, 128], F32)
            WmT = S.sb("WmT", [128, 8, 128], BF16)
            ya = S.sb("ya", [128, 1024], F32)
            yan = S.sb("yan", [128, 1024], BF16)
            stats = S.sb("stats", [128, 8, 6], F32)
            mv = S.sb("mv", [128, 8, 2], F32)

            win_v = win_d.ap().rearrange("(k p) c -> p k c", p=128)
            load_w_bf(wbf, lambda c0, cw: win_v[:, :, c0:c0 + cw], 2048, stg)
            ld(Gbc[:], vng_d.ap().partition_broadcast(128), [DV["in"]], [Gbc])
            ld(Bbc[:], vnb_d.ap().partition_broadcast(128), [DV["in"]], [Bbc])
            ld(wsp[:], wsp_d.ap().rearrange("g i j -> i g j"), [DV["in"]], [wsp])
            for g in range(8):
                bk = B[6 + g // 4]
                tr(bk[:, (g % 4) * 128:(g % 4 + 1) * 128], wsp[:, g, :], ident[:], [wsp, ident], [bk])
            for g in range(8):
                bk = B[6 + g // 4]
                cp(WmT[:, g, :], bk[:, (g % 4) * 128:(g % 4 + 1) * 128], [bk], [WmT])
            mset(WmT[64:128, :, 0:64], 0.0, [WmT])

            def H_a(n):
                xt = xts[n % 2]
                ld(xt[:], x_d.ap()[n * 128:(n + 1) * 128, :], [DV["in"]], [xt])
                prep_hT(xt, xns[n % 2], hTs[n % 2], col[0], col[1], col[2], 0, 16)

            def M_a(n):
                hT = hTs[n % 2]
                for cg in range(4):
                    for k in range(16):
                        mm(B[2 + cg][:], hT[:, k, :], wbf[:, k, cg * 512:(cg + 1) * 512], k == 0, k == 15,
                           [hT.sub[k], wbf], [B[2 + cg]])

            def E_a(n):
                act(gu[:, 0:512], B[2][:], AF.Gelu_apprx_tanh, [B[2]], [gu])
                act(gu[:, 512:1024], B[3][:], AF.Gelu_apprx_tanh, [B[3]], [gu])
                act(gv[:, 0:512], B[4][:], AF.Gelu_apprx_tanh, [B[4]], [gv])
                act(gv[:, 512:1024], B[5][:], AF.Gelu_apprx_tanh, [B[5]], [gv])
                for g in range(8):
                    S.op("vector", (lambda g: lambda e: e.bn_stats(stats[:, g, :], gv[:, g * 128:(g + 1) * 128]))(g),
                         [gv], [stats])
                for g in range(8):
                    S.op("vector", (lambda g: lambda e: e.bn_aggr(mv[:, g, :], stats[:, g, :]))(g), [stats], [mv])
                ts(col[4][:, 0:8], mv[:, :, 1], EPS, ALU.add, [mv], [col[4]])
                act(col[4][:, 0:8], col[4][:, 0:8], AF.Sqrt, [col[4]], [col[4]])
                S.op("vector", lambda e: e.reciprocal(col[3][:, 0:8], col[4][:, 0:8]), [col[4]], [col[3]])
                for g in range(8):
                    ts(vn[:, g * 128:(g + 1) * 128], gv[:, g * 128:(g + 1) * 128], mv[:, g, 0:1], ALU.subtract,
                       [gv, mv, col[3]], [vn], s2=col[3][:, g:g + 1], op1=ALU.mult)
                tt(vn[:], vn[:], Gbc[:], ALU.mult, [vn, Gbc], [vn], eng="gpsimd")
                tt(vgb[:], vn[:], Bbc[:], ALU.add, [vn, Bbc], [vgb])

            def E2_a(n):
                for g in range(8):
                    bk = B[6 + g // 4]
                    mm(bk[:, (g % 4) * 128:(g % 4 + 1) * 128], WmT[:, g, :], vgb[:, g * 128:(g + 1) * 128],
                       True, True, [WmT, vgb], [bk])
                for g in range(8):
                    bk = B[6 + g // 4]
                    stt(ya[:, g * 128:(g + 1) * 128], bk[:, (g % 4) * 128:(g % 4 + 1) * 128], gT[:, 50 + g:51 + g],
                        gu[:, g * 128:(g + 1) * 128], ALU.add, ALU.mult, [bk, gT, gu], [ya])
                act(junk[:, 0:1024], ya[:], AF.Square, [ya], [junk, col[5]], acc=col[5][:, 0:1])
                rsqrt_col(col[6][:, 0:1], col[5][:, 0:1], 1.0 / 1024, [col[5]], [col[6]], col[7])
                ts(yan[:], ya[:], col[6][:, 0:1], ALU.mult, [ya, col[6]], [yan])
                ld(ya_s.ap()[n * 128:(n + 1) * 128, :], yan[:], [yan], [DV["ya"]], q="gpsimd")

            H_a(0)
            for n in range(NT):
                M_a(n)
                if n + 1 < NT:
                    H_a(n + 1)
                if FLAGS.get("reorder", 1):
                    if n >= 1:
                        E2_a(n - 1)
                    E_a(n)
                else:
                    E_a(n)
                    E2_a(n)
            if FLAGS.get("reorder", 1):
                E2_a(NT - 1)
            S.barrier()
            S.emit()

        with contextlib.ExitStack() as ph:
            S.stack = ph
            NCB = 3912 - 2048
            wbf = S.sb("wbfB", [128, 16, NCB], BF16)
            posT = S.sb("posT", [128, NT], F32)
            sin_t = S.sb("sin_t", [128, NT, 32], F32)
            cos_t = S.sb("cos_t", [128, NT, 32], F32)
            wukv = S.sb("wukv", [128, 2, 128], BF16)
            kig_bc = S.sb("kig_bc", [128, 64], F32)
            ph2 = contextlib.ExitStack()
            S.stack = ph2
            stg = [S.sb(f"stgB{i}", [128, 16, 256], F32) for i in range(2)]
            posR = S.sb("posR", [NT, 128], I32)
            posF = S.sb("posF", [NT, 128], F32)
            fr = S.sb("fr", [128, 32], F32)
            ang = S.sb("ang", [128, NT, 32], F32)
            rr = S.sb("rr", [128, NT, 32], F32)
            rq = S.sb("rq", [128, NT, 32], F32)
            rni = S.sb("rni", [128, NT, 32], I32)
            stkv = S.sb("stkv", [128, 2, 128], F32)
            win_v = win_d.ap().rearrange("(k p) c -> p k c", p=128)
            load_w_bf(wbf, lambda c0, cw: win_v[:, :, 2048 + c0:2048 + c0 + cw], NCB, stg)
            ld(posR[:], pos_d.ap().rearrange("(n p) -> n p", p=128), [DV["in"]], [posR])
            cp(posF[:], posR[:], [posR], [posF])
            tr(B[7][:, 0:NT], posF[0:NT, :], ident[0:NT, 0:NT], [posF, ident], [B[7]])
            cp(posT[:], B[7][:, 0:NT], [B[7]], [posT])
            for i in range(32):
                mset(fr[:, i:i + 1], float(np.float32(10000.0) ** np.float32(-i / 32.0)), [fr], eng="gpsimd")
            tt(ang[:], posT[:].unsqueeze(2).broadcast_to([128, NT, 32]),
               fr[:].unsqueeze(1).broadcast_to([128, NT, 32]), ALU.mult, [posT, fr], [ang])
            TWO_PI = 2.0 * math.pi
            C1 = 6.28125
            C2 = TWO_PI - C1
            for (dst, shift) in ((sin_t, 0.0), (cos_t, math.pi / 2)):
                ts(rq[:], ang[:], shift, ALU.add, [ang], [rq])
                ts(rr[:], rq[:], 1.0 / TWO_PI, ALU.mult, [rq], [rr])
                cp(rni[:], rr[:], [rr], [rni])
                cp(rr[:], rni[:], [rni], [rr])
                stt(rq[:], rr[:], -C1, rq[:], ALU.mult, ALU.add, [rr, rq], [rq])
                stt(rq[:], rr[:], -C2, rq[:], ALU.mult, ALU.add, [rr, rq], [rq])
                ts(rr[:], rq[:], math.pi, ALU.is_gt, [rq], [rr])
                stt(rq[:], rr[:], -TWO_PI, rq[:], ALU.mult, ALU.add, [rr, rq], [rq])
                ts(rr[:], rq[:], -math.pi, ALU.is_lt, [rq], [rr])
                stt(rq[:], rr[:], TWO_PI, rq[:], ALU.mult, ALU.add, [rr, rq], [rq])
                ts(rq[:], rq[:], 3.141592, ALU.min, [rq], [rq], s2=-3.141592, op1=ALU.max)
                act(dst[:], rq[:], AF.Sin, [rq], [dst])
            ld(stkv[:, :, 0:64], wuk_d.ap().rearrange("(c p) d -> p c d", p=128), [DV["in"]], [stkv])
            ld(stkv[:, :, 64:128], wuv_d.ap().rearrange("(c p) d -> p c d", p=128), [DV["in"]], [stkv])
            for c2 in range(2):
                ts(wukv[:, c2, :], stkv[:, c2, :], gT[:, 48 + c2:49 + c2], ALU.mult, [stkv, gT], [wukv])
            ld(kig_bc[:], kig_d.ap().partition_broadcast(128), [DV["in"]], [kig_bc])
            S.barrier()
            S.emit()
            ph2.close()
            S.stack = ph
            xts = [S.sb(f"xtB{i}", [128, D], F32) for i in range(2)]
            xns = [S.sb(f"xnB{i}", [128, D], BF16) for i in range(2)]
            hTs = [S.subs(S.sb(f"hTB{i}", [128, 16, 128], BF16), 16, "hT") for i in range(2)]
            qrs = [S.sb(f"qr{i}", [128, 1024], BF16) for i in range(2)]
            qirs = [S.sb(f"qir{i}", [128, 512], BF16) for i in range(2)]
            qf = S.subs(S.sb("qf", [128, 1024], F32), 2)
            c4f = S.sb("c4f", [128, 512], F32)
            c5f = S.sb("c5f", [128, 328], F32)
            t1 = S.sb("t1", [128, 512], F32)
            t2 = S.sb("t2", [128, 512], F32)
            cw_ = S.sb("cw_", [128, 8, 32], F32)
            sw_ = S.sb("sw_", [128, 8, 32], F32)
            wis = S.sb("wis", [128, 8], F32)
            ckvb = S.sb("ckvb", [128, 256], BF16)
            ckvf = S.sb("ckvf", [128, 256], F32)
            kif = S.sb("kif", [128, 64], F32)
            ckvT = S.sb("ckvT", [128, 2, 128], BF16)
            kk = S.sb("kk", [128, 64], F32)
            kk2 = S.sb("kk2", [128, 128], BF16)
            kin = S.sb("kin", [128, 64], F32)
            kir = S.sb("kir", [128, 64], F32)
            kki2 = S.sb("kki2", [128, 128], BF16)
            qTe = S.sb("qTe", [128, 8, 128], BF16)
            qTo = S.sb("qTo", [128, 8, 128], BF16)
            qiTe = S.sb("qiTe", [128, 4, 128], BF16)
            qiTo = S.sb("qiTo", [128, 4, 128], BF16)
            mset(qTe[:], 0.0, [qTe], eng="gpsimd")
            mset(qTo[:], 0.0, [qTo], eng="gpsimd")
            mset(qiTe[:], 0.0, [qiTe], eng="gpsimd")
            mset(qiTo[:], 0.0, [qiTo], eng="gpsimd")
            CSC = (8 ** -0.5) * (64 ** -0.5)

            def rope(o_lo, o_hi, x_lo, x_hi, cs, sn, nh, RB):
                a = t1[:, 0:nh * 32].rearrange("p (h d) -> p h d", d=32)
                b = t2[:, 0:nh * 32].rearrange("p (h d) -> p h d", d=32)
                tt(a, x_lo, cs, ALU.mult, RB, [t1])
                tt(b, x_hi, sn, ALU.mult, RB, [t2])
                tt(o_lo, a, b, ALU.subtract, [t1, t2], RB[-1:], eng="gpsimd")
                tt(a, x_lo, sn, ALU.mult, RB + [t1], [t1])
                tt(b, x_hi, cs, ALU.mult, RB + [t2], [t2])
                tt(o_hi, a, b, ALU.add, [t1, t2], RB[-1:], eng="gpsimd")

            zt = S.sb("zt", [128, D], BF16)
            mset(zt[:], 0.0, [zt], eng="gpsimd")
            zf_total = NSB * C // 512
            zf_done = [0]

            def H_b(n):
                xt = xts[n % 2]
                ld(xt[:], x_d.ap()[n * 128:(n + 1) * 128, :], [DV["in"]], [xt])
                want = (zf_total * (n + 1) + NT - 1) // NT
                while zf_done[0] < min(want, zf_total):
                    r = zf_done[0]
                    ld(xe_s.ap()[r * 512:(r + 1) * 512, :].rearrange("(a p) d -> p a d", p=128),
                       zt[:].unsqueeze(1).broadcast_to([128, 4, D]), [zt], [DV["xe"]], q="sync", owner=zt)
                    zf_done[0] += 1
                prep_hT(xt, xns[n % 2], hTs[n % 2], col[0], col[1], col[2], 0, 16)

            def M_b(n):
                hT = hTs[n % 2]
                widths = [512, 512, 512, NCB - 1536]
                for cg in range(4):
                    for k in range(16):
                        mm(B[2 + cg][:, 0:widths[cg]], hT[:, k, :], wbf[:, k, cg * 512:cg * 512 + widths[cg]],
                           k == 0, k == 15, [hT.sub[k], wbf], [B[2 + cg]])

            def E1_b(n):
                cosb = cos_t[:, n, :].unsqueeze(1)
                sinb = sin_t[:, n, :].unsqueeze(1)
                qr, qir = qrs[n % 2], qirs[n % 2]
                cp(c4f[:], B[4][:], [B[4]], [c4f], eng="scalar")
                cp(c5f[:, 0:328], B[5][:, 0:328], [B[5]], [c5f], eng="scalar")
                cp(qf[:, 0:512], B[2][:], [B[2]], [qf.sub[0]], eng="scalar")
                cp(qf[:, 512:1024], B[3][:], [B[3]], [qf.sub[1]], eng="scalar")
                act(junk[:, 0:256], c4f[:, 0:256], AF.Square, [c4f], [junk, col[3]], acc=col[3][:, 0:1])
                rsqrt_col(col[4][:, 0:1], col[3][:, 0:1], 1.0 / 256, [col[3]], [col[4]], col[5])
                cp(ckvb[:], c4f[:, 0:256], [c4f], [ckvb])
                for c2 in range(2):
                    tr(B[0][:].bitcast(BF16)[:, c2 * 128:(c2 + 1) * 128], ckvb[:, c2 * 128:(c2 + 1) * 128], identb[:],
                       [ckvb, identb], [B[0]])
                cp(ckvT[:], B[0][:].bitcast(BF16)[:, 0:256].rearrange("p (c t) -> p c t", t=128), [B[0]], [ckvT])
                for c2 in range(2):
                    mm(B[1][:, 0:128], ckvT[:, c2, :], wukv[:, c2, :], c2 == 0, c2 == 1, [ckvT, wukv], [B[1]])
                act(junk[:, 0:64], c5f[:, 256:320], AF.Square, [c5f], [junk, col[6]], acc=col[6][:, 0:1])
                rsqrt_col(col[7][:, 0:1], col[6][:, 0:1], 1.0 / 64, [col[6]], [col[7]], col[5])
                stt(kin[:], c5f[:, 256:320], col[7][:, 0:1], kig_bc[:], ALU.mult, ALU.mult,
                    [c5f, col[7], kig_bc], [kin])
                ki3 = kin[:].rearrange("p (h d) -> p h d", d=64)
                kr3 = kir[:].rearrange("p (h d) -> p h d", d=64)
                rope(kr3[:, :, 0:32], kr3[:, :, 32:64], ki3[:, :, 0:32], ki3[:, :, 32:64], cosb, sinb, 1,
                     [kin, cos_t, sin_t, kir])
                cp(kki2[:, 0:64], kir[:], [kir], [kki2])
                cp(kki2[:, 64:128], kir[:], [kir], [kki2])
                tr(B[0][:].bitcast(BF16)[:, 384:512], kki2[:], identb[:], [kki2, identb], [B[0]])
                cp(kiT2[:, n * 128:(n + 1) * 128], B[0][:].bitcast(BF16)[:, 384:512], [B[0]], [kiT2])
                ts(vaug[:, n, 0:64], B[1][:, 64:128], col[4][:, 0:1], ALU.mult, [B[1], col[4]], [vaug])
                kv3 = B[1][:, 0:64].rearrange("p (h d) -> p h d", d=64)
                kk3 = kk[:].rearrange("p (h d) -> p h d", d=64)
                rope(kk3[:, :, 0:32], kk3[:, :, 32:64], kv3[:, :, 0:32], kv3[:, :, 32:64], cosb, sinb, 1,
                     [B[1], cos_t, sin_t, kk])
                ts(kk2[:, 0:64], kk[:], col[4][:, 0:1], ALU.mult, [kk, col[4]], [kk2])
                ts(kk2[:, 64:128], kk[:], col[4][:, 0:1], ALU.mult, [kk, col[4]], [kk2])
                tr(B[0][:].bitcast(BF16)[:, 256:384], kk2[:], identb[:], [kk2, identb], [B[0]])
                cp(kT2[:, n * 128:(n + 1) * 128], B[0][:].bitcast(BF16)[:, 256:384], [B[0]], [kT2])
                ts(wis[:], c5f[:, 320:328], CSC, ALU.mult, [c5f], [wis])
                ts(sgn_all[:, n, :], c5f[:, 320:328], 0.0, ALU.is_ge, [c5f], [sgn_all], s2=2.0, op1=ALU.mult)
                ts(sgn_all[:, n, :], sgn_all[:, n, :], -1.0, ALU.add, [sgn_all], [sgn_all])
                tt(cw_[:], cosb.broadcast_to([128, 8, 32]), wis[:].unsqueeze(2).broadcast_to([128, 8, 32]),
                   ALU.mult, [cos_t, wis], [cw_])
                tt(sw_[:], sinb.broadcast_to([128, 8, 32]), wis[:].unsqueeze(2).broadcast_to([128, 8, 32]),
                   ALU.mult, [sin_t, wis], [sw_])
                for hb in range(2):
                    qv = qf[:, hb * 512:(hb + 1) * 512].rearrange("p (h d) -> p h d", d=64)
                    ov = qr[:, hb * 512:(hb + 1) * 512].rearrange("p (h d) -> p h d", d=64)
                    rope(ov[:, :, 0:32], ov[:, :, 32:64], qv[:, :, 0:32], qv[:, :, 32:64],
                         cosb.broadcast_to([128, 8, 32]), sinb.broadcast_to([128, 8, 32]), 8,
                         [qf.sub[hb], cos_t, sin_t, qr])
                for hb in range(2):
                    src = c4f[:, 256:512] if hb == 0 else c5f[:, 0:256]
                    qv = src.rearrange("p (h d) -> p h d", d=64)
                    ov = qir[:, hb * 256:(hb + 1) * 256].rearrange("p (h d) -> p h d", d=64)
                    rope(ov[:, :, 0:32], ov[:, :, 32:64], qv[:, :, 0:32], qv[:, :, 32:64],
                         cw_[:, hb * 4:(hb + 1) * 4, :], sw_[:, hb * 4:(hb + 1) * 4, :], 4,
                         [c4f if hb == 0 else c5f, cw_, sw_, qir])

            def E2_b(n):
                qr, qir = qrs[n % 2], qirs[n % 2]
                b6 = B[6][:].bitcast(BF16)
                for pr in range(8):
                    tr(b6[:, pr * 128:(pr + 1) * 128], qr[:, pr * 128:(pr + 1) * 128], identb[:], [qr, identb], [B[6]])
                b63 = b6.rearrange("p (a t) -> p a t", t=128)
                cp(qTe[0:64, :, :], b63[0:64, :, :], [B[6]], [qTe])
                cp(qTo[64:128, :, :], b63[64:128, :, :], [B[6]], [qTo])
                b7 = B[7][:].bitcast(BF16)
                for pr in range(4):
                    tr(b7[:, pr * 128:(pr + 1) * 128], qir[:, pr * 128:(pr + 1) * 128], identb[:], [qir, identb], [B[7]])
                b73 = b7[:, 0:512].rearrange("p (a t) -> p a t", t=128)
                cp(qiTe[0:64, :, :], b73[0:64, :, :], [B[7]], [qiTe], eng="scalar")
                cp(qiTo[64:128, :, :], b73[64:128, :, :], [B[7]], [qiTo], eng="scalar")
                ld(qTe_s.ap()[n], qTe[:].rearrange("p a t -> p (a t)"), [qTe], [DV["qTe"]], q="gpsimd")
                ld(qTo_s.ap()[n], qTo[:].rearrange("p a t -> p (a t)"), [qTo], [DV["qTo"]], q="gpsimd")
                ld(qiTe_s.ap()[n], qiTe[:].rearrange("p a t -> p (a t)"), [qiTe], [DV["qiTe"]], q="gpsimd")
                ld(qiTo_s.ap()[n], qiTo[:].rearrange("p a t -> p (a t)"), [qiTo], [DV["qiTo"]], q="gpsimd")

            H_b(0)
            for n in range(NT):
                M_b(n)
                if n + 1 < NT:
                    H_b(n + 1)
                if n >= 1:
                    E2_b(n - 1)
                E1_b(n)
            E2_b(NT - 1)
            S.barrier()
            S.emit()

        with contextlib.ExitStack() as ph:
            S.stack = ph
            woutb = S.sb("woutb", [128, 16, D], BF16)
            with contextlib.ExitStack() as ph2:
                S.stack = ph2
                stg = [S.sb(f"stgO{i}", [128, 16, 256], F32) for i in range(2)]
                gate1_bc = S.sb("gate1_bc", [128, D], F32)
                ld(gate1_bc[:], mod_s.ap()[2 * D:3 * D].partition_broadcast(128), [DV["mod"]], [gate1_bc])
                wout_v = wout_d.ap().rearrange("(k p) c -> p k c", p=128)
                rowscale_bufs.clear()
                rowscale_bufs.extend([gT, gate1_bc])
                load_w_bf(woutb, lambda c0, cw: wout_v[:, :, c0:c0 + cw], D, stg, rowscale=gT[:, 32:48],
                          colscale=gate1_bc)
                S.barrier()
                S.emit()
            S.stack = ph
            score = S.sb("score", [128, ST], F32)
            NMs = [S.sb(f"NM{i}", [128, ST], BF16) for i in range(2)]
            qTe = [S.sb(f"qTeL{i}", [128, 1024], BF16) for i in range(2)]
            qTo = [S.sb(f"qToL{i}", [128, 1024], BF16) for i in range(2)]
            qiTe = [S.sb(f"qiTeL{i}", [128, 512], BF16) for i in range(2)]
            qiTo = [S.sb(f"qiToL{i}", [128, 512], BF16) for i in range(2)]
            pT = [S.sb(f"pT{i}", [128, 512], BF16) for i in range(3)]
            xt = S.sb("xtC", [128, D], F32)
            yanl = S.sb("yanl", [128, 1024], BF16)
            yb = S.sb("yb", [128, 1024], F32)
            ybn = S.sb("ybn", [128, 1024], BF16)
            yT = S.subs(S.sb("yT", [128, 16, 128], BF16), 2, "yT")
            xm = S.sb("xm", [128, D], F32)
            xn2b = S.sb("xn2b", [128, D], BF16)
            h2T = S.subs(S.sb("h2T", [128, 16, 128], F32), 16)
            wr = S.sb("wr", [128, 16, 36], F32)
            bias_bc = S.sb("bias_bc", [128, 36], F32)
            lg = S.sb("lg", [128, 36], F32)
            em = S.sb("em", [128, 32], F32)
            m8 = S.sb("m8", [128, 8], F32)
            i8 = S.sb("i8", [128, 8], U32)
            Ab = S.sb("Ab", [128, 32], BF16)
            oh0 = S.sb("oh0", [128, 32], F32)
            oh1 = S.sb("oh1", [128, 32], F32)
            posf = S.sb("posf", [128, 32], F32)
            tmp32 = S.sb("tmp32", [128, 32], F32)
            sm = S.sb("sm", [128, 32], F32)
            lo = S.sb("lo", [128, 1], F32)
            w0c = S.sb("w0c", [128, 1], F32)
            mid = S.sb("mid", [128, 1], F32)
            cnt = S.sb("cnt", [128, 1], F32)
            gei = S.sb("gei", [128, 1], U32)
            thr = S.sb("thr", [128, 1], F32)
            rden = S.sb("rden", [128, 16], F32)
            destf = S.sb("destf", [128, 2], F32)

            ld(wr[:], wr_d.ap().rearrange("(k p) c -> p k c", p=128), [DV["in"]], [wr])
            ld(bias_bc[:], br_d.ap().partition_broadcast(128), [DV["in"]], [bias_bc])
            mset(base_bc[:], 0.0, [base_bc])

            def stage_A(n):
                Sk = (n + 1) * 128
                qe, qo, qie, qio = qTe[n % 2], qTo[n % 2], qiTe[n % 2], qiTo[n % 2]
                NM = NMs[n % 2]
                ld(qie[:], qiTe_s.ap()[n], [DV["qiTe"]], [qie])
                ld(qio[:], qiTo_s.ap()[n], [DV["qiTo"]], [qio])
                ld(qe[:], qTe_s.ap()[n], [DV["qTe"]], [qe])
                ld(qo[:], qTo_s.ap()[n], [DV["qTo"]], [qo])
                bi = 0
                for ks in range(0, Sk, 512):
                    ke = min(Sk, ks + 512)
                    for h in range(8):
                        bk = B[bi % 2]
                        bi += 1
                        src = (qie if h % 2 == 0 else qio)[:, (h // 2) * 128:(h // 2 + 1) * 128]
                        mm(bk[:, 0:ke - ks], src, kiT2[:, ks:ke], True, True, [qie, qio, kiT2], [bk])
                        sg = sgn_all[:, n, h:h + 1]
                        act(bk[:, 0:ke - ks], bk[:, 0:ke - ks], AF.Relu, [bk, sgn_all], [bk], scale=sg)
                        if h == 0:
                            ts(score[:, ks:ke], bk[:, 0:ke - ks], sg, ALU.mult, [bk, sgn_all], [score])
                        else:
                            stt(score[:, ks:ke], bk[:, 0:ke - ks], sg, score[:, ks:ke], ALU.mult, ALU.add,
                                [bk, sgn_all, score], [score])
                mset(score[0:64, Sk - 64:Sk], -1.0e30, [score])
                if n < 2:
                    mset(thr[:], -1.0e29, [thr])
                else:
                    S.op("vector", (lambda Sk: lambda e: e.tensor_reduce(out=lo[:], in_=score[:, 0:Sk - 64], axis=AX.X,
                                                                         op=ALU.min))(Sk), [score], [lo])
                    S.op("vector", (lambda Sk: lambda e: e.tensor_reduce(out=w0c[:], in_=score[:, 0:Sk], axis=AX.X,
                                                                         op=ALU.max))(Sk), [score], [w0c])
                    stt(w0c[:], w0c[:], 1.0e-4, lo[:], ALU.add, ALU.subtract, [w0c, lo], [w0c])
                    for it in range(NITER):
                        stt(mid[:], w0c[:], 2.0 ** -(it + 1), lo[:], ALU.mult, ALU.add, [w0c, lo], [mid])
                        ts(NM[:, 0:Sk], score[:, 0:Sk], mid[:, 0:1], ALU.is_ge, [score, mid], [NM, cnt],
                           s2=0.0, op1=ALU.add, acc=cnt[:, 0:1])
                        ts(gei[:], cnt[:], 255.5, ALU.is_ge, [cnt], [gei])
                        S.op("vector", lambda e: e.copy_predicated(lo[:], gei[:], mid[:]), [gei, mid, lo], [lo])
                    cp(thr[:], lo[:], [lo], [thr])
                ts(NM[:, 0:Sk], score[:, 0:Sk], thr[:, 0:1], ALU.is_lt, [score, thr], [NM], s2=-30000.0, op1=ALU.mult)

            def stage_B(n):
                Sk = (n + 1) * 128
                qe, qo = qTe[n % 2], qTo[n % 2]
                NM = NMs[n % 2]
                ld(xt[:], x_d.ap()[n * 128:(n + 1) * 128, :], [DV["in"]], [xt])
                ld(yanl[:], ya_s.ap()[n * 128:(n + 1) * 128, :], [DV["ya"]], [yanl])
                for b3 in range(3):
                    mm(B[4 + b3][:], zerob[:, 0:128], zerob[:], True, False, [zerob], [B[4 + b3]], skip=True)
                units = [(kb, j) for kb in range(n + 1) for j in range(4)]

                def QK(u):
                    kb, j = units[u]
                    bk = B[2 + u % 2]
                    pt = pT[u % 3]
                    qsrc = (qe if j < 2 else qo)[:, (j % 2) * 512:(j % 2 + 1) * 512]
                    mm(bk[:], kT2[:, kb * 128:(kb + 1) * 128], qsrc, True, False, [kT2, qe, qo], [bk])
                    mm(bk[:], NM[:, kb * 128:(kb + 1) * 128],
                       identb[:].unsqueeze(1).broadcast_to([128, 4, 128]), False, True, [NM, identb], [bk])
                    act(pt[:], bk[:], AF.Exp, [bk], [pt], scale=0.125)

                def PV(u):
                    kb, j = units[u]
                    pt = pT[u % 3]
                    for hh in range(4):
                        pair = (j % 2) * 4 + hh
                        head = pair * 2 + (0 if j < 2 else 1)
                        ob = B[4 + head // 7]
                        off = (head % 7) * 65
                        mm(ob[:, off:off + 65], pt[:, hh * 128:(hh + 1) * 128], vaug[:, kb, :], False,
                           kb == n, [pt, vaug], [ob], skip=True)

                QK(0)
                for u in range(len(units)):
                    if u + 1 < len(units):
                        QK(u + 1)
                    PV(u)
                for b3 in range(3):
                    nh = 7 if b3 < 2 else 2
                    ov = B[4 + b3][:, 0:nh * 65].rearrange("p (h d) -> p h d", d=65)
                    S.op("vector", (lambda ov, b3, nh: lambda e: e.reciprocal(
                        rden[:, b3 * 7:b3 * 7 + nh].unsqueeze(2), ov[:, :, 64:65]))(ov, b3, nh), [B[4 + b3]], [rden])
                    tt(yb[:, b3 * 448:b3 * 448 + nh * 64].rearrange("p (h d) -> p h d", d=64), ov[:, :, 0:64],
                       rden[:, b3 * 7:b3 * 7 + nh].unsqueeze(2).broadcast_to([128, nh, 64]), ALU.mult,
                       [B[4 + b3], rden], [yb])
                act(junk[:, 0:1024], yb[:], AF.Square, [yb], [junk, col[0]], acc=col[0][:, 0:1])
                rsqrt_col(col[1][:, 0:1], col[0][:, 0:1], 1.0 / 1024, [col[0]], [col[1]], col[2])
                ts(ybn[:], yb[:], col[1][:, 0:1], ALU.mult, [yb, col[1]], [ybn])
                for k in range(16):
                    bk = B[2 + k // 8]
                    srcy = yanl[:, k * 128:(k + 1) * 128] if k < 8 else ybn[:, (k - 8) * 128:(k - 7) * 128]
                    tr(bk[:].bitcast(BF16)[:, (k % 8) * 128:(k % 8 + 1) * 128], srcy, identb[:],
                       [yanl, ybn, identb], [bk])
                cp(yT[:, 0:8, :], B[2][:].bitcast(BF16).rearrange("p (a t) -> p a t", t=128), [B[2]], [yT.sub[0]])
                cp(yT[:, 8:16, :], B[3][:].bitcast(BF16).rearrange("p (a t) -> p a t", t=128), [B[3]], [yT.sub[1]],
                   eng="scalar")
                for db in range(4):
                    bk = B[(7, 4, 5, 6)[db]]
                    for k in range(16):
                        mm(bk[:], yT[:, k, :], woutb[:, k, db * 512:(db + 1) * 512], k == 0, k == 15,
                           [yT.sub[k // 8], woutb], [bk])
                    tt(xm[:, db * 512:(db + 1) * 512], bk[:], xt[:, db * 512:(db + 1) * 512], ALU.add, [bk, xt], [xm])
                ld(xmid_s.ap()[n * 128:(n + 1) * 128, :], xm[:], [xm], [DV["xmid"]], q="gpsimd")
                act(junk[:], xm[:], AF.Square, [xm], [junk, col[3]], acc=col[3][:, 0:1])
                rsqrt_col(col[4][:, 0:1], col[3][:, 0:1], 1.0 / D, [col[3]], [col[4]], col[5])
                ts(xt[:], xm[:], col[4][:, 0:1], ALU.mult, [xm, col[4]], [xt])
                act(xn2b[:], xm[:], AF.Copy, [xm, col[4]], [xn2b], scale=col[4][:, 0:1])
                for g4 in range(4):
                    bk = B[2 + g4 % 2]
                    for k in range(g4 * 4, g4 * 4 + 4):
                        tr(bk[:, (k % 4) * 128:(k % 4 + 1) * 128], xt[:, k * 128:(k + 1) * 128], ident[:],
                           [xt, ident], [bk])
                    for k in range(g4 * 4, g4 * 4 + 4):
                        if g4 % 2 == 0:
                            ts(h2T[:, k, :], bk[:, (k % 4) * 128:(k % 4 + 1) * 128], pvec[:, 32 + k:33 + k], ALU.mult,
                               [bk, pvec], [h2T.sub[k]], s2=pvec[:, 48 + k:49 + k], op1=ALU.add)
                        else:
                            act(h2T[:, k, :], bk[:, (k % 4) * 128:(k % 4 + 1) * 128], AF.Identity, [bk, pvec],
                                [h2T.sub[k]], bias=pvec[:, 48 + k:49 + k], scale=pvec[:, 32 + k:33 + k])
                for k in range(16):
                    mm(B[7][:, 0:36], h2T[:, k, :], wr[:, k, :], k == 0, k == 15, [h2T.sub[k], wr], [B[7]])
                tt(lg[:], B[7][:, 0:36], bias_bc[:], ALU.add, [B[7], bias_bc], [lg])
                S.op("vector", lambda e: e.tensor_reduce(out=sm[:, 0:1], in_=lg[:, 0:4], axis=AX.X, op=ALU.max), [lg], [sm])
                ts(sm[:, 1:2], sm[:, 0:1], -1.0, ALU.mult, [sm], [sm])
                act(sm[:, 4:8], lg[:, 0:4], AF.Exp, [lg, sm], [sm, col[6]], bias=sm[:, 1:2], scale=1.0,
                    acc=col[6][:, 0:1])
                S.op("vector", lambda e: e.reciprocal(sm[:, 2:3], col[6][:, 0:1]), [col[6]], [sm])
                ts(sm[:, 8:12], lg[:, 0:4], sm[:, 0:1], ALU.is_ge, [lg, sm], [sm], s2=1.0e9, op1=ALU.mult)
                ts(sm[:, 8:12], sm[:, 8:12], -1.0e9, ALU.add, [sm], [sm])
                tt(em[:].rearrange("p (g j) -> p g j", j=8), lg[:, 4:36].rearrange("p (g j) -> p g j", j=8),
                   sm[:, 8:12].unsqueeze(2).broadcast_to([128, 4, 8]), ALU.add, [lg, sm], [em])
                S.op("vector", lambda e: e.max(m8[:], em[:]), [em], [m8])
                S.op("vector", lambda e: e.max_index(i8[:], m8[:], em[:]), [m8, em], [i8])
                tt(sm[:, 12:13], m8[:, 1:2], m8[:, 0:1], ALU.subtract, [m8], [sm])
                act(sm[:, 13:14], sm[:, 12:13], AF.Exp, [sm], [sm])
                ts(sm[:, 13:14], sm[:, 13:14], 1.0, ALU.add, [sm], [sm])
                S.op("vector", lambda e: e.reciprocal(sm[:, 14:15], sm[:, 13:14]), [sm], [sm])
                tt(wgt_all[:, n, 0:1], sm[:, 14:15], sm[:, 2:3], ALU.mult, [sm], [wgt_all])
                tt(wgt_all[:, n, 1:2], sm[:, 2:3], wgt_all[:, n, 0:1], ALU.subtract, [sm, wgt_all], [wgt_all])
                ts(Ab[:], em[:], m8[:, 1:2], ALU.is_ge, [em, m8], [Ab])
                ts(oh0[:], em[:], m8[:, 0:1], ALU.is_ge, [em, m8], [oh0])
                tt(oh1[:], Ab[:], oh0[:], ALU.subtract, [Ab, oh0], [oh1])
                mm(B[4][:, 0:32], LTb[:], Ab[:], True, True, [LTb, Ab], [B[4]])
                tt(posf[:], B[4][:, 0:32], base_bc[:], ALU.add, [B[4], base_bc], [posf])
                mm(B[4][:, 64:96], onesb[:], Ab[:], True, True, [onesb, Ab], [B[4]])
                tt(base_bc[:], B[4][:, 64:96], base_bc[:], ALU.add, [B[4], base_bc, posf], [base_bc])
                cp(eid_all[:, n, :], i8[:, 0:2], [i8], [eid_all])
                for j, oh in enumerate((oh0, oh1)):
                    tt(tmp32[:], posf[:], oh[:], ALU.mult, [posf, oh], [tmp32])
                    S.op("vector", (lambda n, j: lambda e: e.tensor_reduce(out=pos_all[:, n, j:j + 1], in_=tmp32[:],
                                                                            axis=AX.X, op=ALU.add))(n, j),
                         [tmp32], [pos_all])
                ld(xn2_s.ap()[n * 128:(n + 1) * 128, :], xn2b[:], [xn2b], [DV["xn2"]], q="gpsimd")

            stage_A(0)
            for n in range(NT):
                if n + 1 < NT:
                    stage_A(n + 1)
                stage_B(n)
            S.barrier()
            S.emit()

        with contextlib.ExitStack() as ph:
            S.stack = ph
            pa = S.sb("pa", [128, 32], F32)
            pb = S.sb("pb", [128, 32], F32)
            pi_ = S.sb("pi_", [128, 32], I32)
            padded = S.sb("padded", [128, 32], F32)
            pstart = S.sb("pstart", [128, 32], F32)
            iotI = S.sb("iotI", [128, NSB], I32)
            iotF = S.sb("iotF", [128, NSB], F32)
            cmp3 = S.sb("cmp3", [128, NSB, 32], F32)
            bef = S.sb("bef", [128, NSB], F32)
            iw12 = S.sb("iw12", [128, 12], I32)
            iw12f = S.sb("iw12f", [128, 12], F32)
            widxf = S.sb("widxf", [128, NSB, 12], F32)
            ohd = S.sb("ohd", [128, 32], F32)
            dcol = S.sb("dcol", [128, 2], F32)
            xr2 = [S.sb(f"xr2_{i}", [128, D], BF16) for i in range(2)]
            ts(pa[:], base_bc[:], float(C - 1), ALU.add, [base_bc], [pa], s2=1.0 / C, op1=ALU.mult)
            cp(pi_[:], pa[:], [pa], [pi_])
            cp(pb[:], pi_[:], [pi_], [pb])
            tt(pa[:], pb[:], pa[:], ALU.is_gt, [pb, pa], [pa])
            tt(pb[:], pb[:], pa[:], ALU.subtract, [pb, pa], [pb])
            ts(padded[:], pb[:], float(C), ALU.mult, [pb], [padded])
            cp(pa[:], padded[:], [padded], [pa])
            src_, dst_ = pa, pb
            for sh in (1, 2, 4, 8, 16):
                cp(dst_[:, 0:sh], src_[:, 0:sh], [src_], [dst_])
                tt(dst_[:, sh:32], src_[:, sh:32], src_[:, 0:32 - sh], ALU.add, [src_], [dst_])
                src_, dst_ = dst_, src_
            pend = src_
            tt(pstart[:], pend[:], padded[:], ALU.subtract, [pend, padded], [pstart])
            S.op("gpsimd", lambda e: e.iota(iotI[:], [[C, NSB]], base=0, channel_multiplier=0), [], [iotI])
            cp(iotF[:], iotI[:], [iotI], [iotF])
            tt(cmp3[:], pend[:].unsqueeze(1).broadcast_to([128, NSB, 32]),
               iotF[:].unsqueeze(2).broadcast_to([128, NSB, 32]), ALU.is_le, [pend, iotF], [cmp3])
            S.op("vector", lambda e: e.tensor_reduce(out=bef[:], in_=cmp3[:], axis=AX.X, op=ALU.add), [cmp3], [bef])
            ts(bef[:], bef[:], 31.0, ALU.min, [bef], [bef], s2=1536.0, op1=ALU.mult)
            S.op("gpsimd", lambda e: e.iota(iw12[:], [[128, 12]], base=0, channel_multiplier=1), [], [iw12])
            cp(iw12f[:], iw12[:], [iw12], [iw12f])
            tt(widxf[:], bef[:].unsqueeze(2).broadcast_to([128, NSB, 12]),
               iw12f[:].unsqueeze(1).broadcast_to([128, NSB, 12]), ALU.add, [bef, iw12f], [widxf])
            cp(widx[:], widxf[:], [widxf], [widx])
            S.op("gpsimd", lambda e: e.iota(pi_[:], [[1, 32]], base=0, channel_multiplier=0), [pi_], [pi_])
            cp(pa[:], pi_[:], [pi_], [pa])
            for n in range(NT):
                for j in range(2):
                    ts(ohd[:], pa[:], eid_all[:, n, j:j + 1], ALU.is_equal, [pa, eid_all], [ohd])
                    tt(ohd[:], ohd[:], pstart[:], ALU.mult, [ohd, pstart], [ohd])
                    S.op("vector", (lambda j: lambda e: e.tensor_reduce(out=dcol[:, j:j + 1], in_=ohd[:], axis=AX.X,
                                                                         op=ALU.add))(j), [ohd], [dcol])
                tt(dcol[:], dcol[:], pos_all[:, n, :], ALU.add, [dcol, pos_all], [dcol])
                cp(dest_all[:, n, :], dcol[:], [dcol], [dest_all])
                xr = xr2[n % 2]
                ld(xr[:], xn2_s.ap()[n * 128:(n + 1) * 128, :], [DV["xn2"]], [xr])
                for j in range(2):
                    S.dma("gpsimd", (lambda n, j, xr: lambda e: e.indirect_dma_start(
                        out=xe_s.ap(), out_offset=bass.IndirectOffsetOnAxis(ap=dest_all[:, n, j:j + 1], axis=0),
                        in_=xr[:], in_offset=None, bounds_check=None, oob_is_err=False))(n, j, xr),
                        [xr, dest_all, DV["xe"]], [DV["xe"]], owner=xr)
            S.barrier()
            S.emit()

        with contextlib.ExitStack() as ph:
            S.stack = ph
            NB = C // 128
            stg = [S.sb(f"stgE{i}", [128, 4096], F32) for i in range(4)]
            wpb = [S.subs(S.sb(f"wpb{i}", [128, 4096], BF16), 2, "p3") for i in range(3)]
            xer1 = [S.sb(f"xer_{b}", [128, D], BF16) for b in range(NB)]
            xer = [xer1, xer1]
            xeT = S.subs(S.sb("xeT", [128, 16, C], BF16), 16, "p3")
            gTt = S.sb("gTt", [128, 8, C], BF16)
            sa = [S.sb(f"sa{i}", [128, C], F32) for i in range(2)]
            yo = [S.subs(S.sb(f"yo{i}", [128, D], F32), 4) for i in range(NB)]
            wexp_rows = wexp_d.ap().rearrange("e q p c -> (e q p) c")
            pieces = [(j, p) for j in range(NSB) for p in range(12)]
            rot = [0]

            def nbank():
                bk = B[2 + rot[0] % 6]
                rot[0] += 1
                return bk

            def emit_gather(i):
                j, piece = pieces[i]
                sg = stg[i % 4]
                S.dma("gpsimd", (lambda sg, j, piece: lambda e: e.indirect_dma_start(
                    out=sg[:], out_offset=None, in_=wexp_rows,
                    in_offset=bass.IndirectOffsetOnAxis(ap=widx[:, j, piece:piece + 1], axis=0),
                    bounds_check=None, oob_is_err=False))(sg, j, piece), [DV["in"], widx], [sg], owner=sg)

            def emit_cast(i):
                sg = stg[i % 4]
                wb = wpb[i % 3]
                cp(wb[:, 0:2048], sg[:, 0:2048], [sg], [wb.sub[0]], eng="vector")
                cp(wb[:, 2048:4096], sg[:, 2048:4096], [sg], [wb.sub[1]], eng="scalar")

            def load_xe(j):
                for blk in range(NB):
                    xr = xer[j % 2][blk]
                    ld(xr[:], xe_s.ap()[j * C + blk * 128:j * C + (blk + 1) * 128, :], [DV["xe"]], [xr])

            def prologue_part(j, part):
                for kp in (2 * part, 2 * part + 1):
                    bk = B[kp % 2]
                    bv = bk[:].bitcast(BF16)
                    for kk in range(2):
                        k = kp * 2 + kk
                        for blk in range(NB):
                            xr = xer[j % 2][blk]
                            tr(bv[:, (kk * NB + blk) * 128:(kk * NB + blk + 1) * 128], xr[:, k * 128:(k + 1) * 128],
                               identb[:], [xr, identb], [bk])
                    for kk in range(2):
                        k = kp * 2 + kk
                        src = bv[:, kk * C:(kk + 1) * C]
                        if kp % 2 == 0:
                            ts(xeT[:, k, :], src, pvec[:, 32 + k:33 + k], ALU.mult, [bk, pvec], [xeT.sub[k]],
                               s2=pvec[:, 48 + k:49 + k], op1=ALU.add)
                        else:
                            act(xeT[:, k, :], src, AF.Identity, [bk, pvec], [xeT.sub[k]],
                                bias=pvec[:, 48 + k:49 + k], scale=pvec[:, 32 + k:33 + k])

            yoi = [0]

            def compute(i):
                j, piece = pieces[i]
                wb = wpb[i % 3]
                if piece < 8:
                    f = piece
                    w13 = wb[:].rearrange("p (t k c) -> p t k c", t=2, k=16)
                    ba, bb = nbank(), nbank()
                    for k in range(16):
                        mm(ba[:, 0:C], w13[:, 0, k, :], xeT[:, k, :], k == 0, k == 15, [wb.sub[0], xeT.sub[k]], [ba])
                    for k in range(16):
                        mm(bb[:, 0:C], w13[:, 1, k, :], xeT[:, k, :], k == 0, k == 15, [wb.sub[1], xeT.sub[k]], [bb])
                    s_ = sa[f % 2]
                    act(s_[:], ba[:, 0:C], AF.Silu, [ba], [s_])
                    tt(gTt[:, f, :], bb[:, 0:C], s_[:], ALU.mult, [bb, s_], [gTt])
                else:
                    db = piece - 8
                    w2v = wb[:].rearrange("p (k c) -> p k c", k=8)
                    for blk in range(NB):
                        bk = nbank()
                        for fc in range(8):
                            mm(bk[:], gTt[:, fc, blk * 128:(blk + 1) * 128], w2v[:, fc, :], fc == 0, fc == 7,
                               [gTt, wb.sub[fc // 4]], [bk])
                        y_ = yo[blk]
                        if blk % 2 == 0:
                            cp(y_[:, db * 512:(db + 1) * 512], bk[:], [bk], [y_.sub[db]])
                        else:
                            cp(y_[:, db * 512:(db + 1) * 512], bk[:], [bk], [y_.sub[db]], eng="scalar")
                        if db == 3:
                            ld(ye_s.ap()[j * C + blk * 128:j * C + (blk + 1) * 128, :], y_[:],
                               list(y_.sub), [DV["ye"]], q="sync", owner=y_)

            load_xe(0)
            emit_gather(0)
            emit_gather(1)
            emit_gather(2)
            emit_cast(0)
            for part in range(4):
                prologue_part(0, part)
            if NSB > 1:
                load_xe(1)
            for i, (j, piece) in enumerate(pieces):
                if piece == 0 and j >= 1 and j + 1 < NSB:
                    load_xe(j + 1)
                if i + 3 < len(pieces):
                    emit_gather(i + 3)
                if i + 1 < len(pieces):
                    emit_cast(i + 1)
                compute(i)
                if piece >= 8 and j + 1 < NSB:
                    prologue_part(j + 1, piece - 8)
            S.barrier()
            S.emit()

        with contextlib.ExitStack() as ph:
            S.stack = ph
            gate2_bc = S.sb("gate2_bc", [128, D], F32)
            fg_bc = S.sb("fg_bc", [128, D], F32)
            y0 = [S.sb(f"y0_{i}", [128, D], F32) for i in range(2)]
            y1 = [S.sb(f"y1_{i}", [128, D], F32) for i in range(2)]
            xmt = [S.sb(f"xmt{i}", [128, D], F32) for i in range(2)]
            acc = S.sb("acc", [128, D], F32)
            ot = [S.sb(f"ot{i}", [128, D], F32) for i in range(2)]
            ld(gate2_bc[:], mod_s.ap()[5 * D:6 * D].partition_broadcast(128), [DV["mod"]], [gate2_bc])
            ld(fg_bc[:], fg_d.ap().partition_broadcast(128), [DV["in"]], [fg_bc])
            for n in range(NT):
                a0, a1, xq, o_ = y0[n % 2], y1[n % 2], xmt[n % 2], ot[n % 2]
                for j, dst in enumerate((a0, a1)):
                    S.dma("gpsimd", (lambda n, j, dst: lambda e: e.indirect_dma_start(
                        out=dst[:], out_offset=None, in_=ye_s.ap(),
                        in_offset=bass.IndirectOffsetOnAxis(ap=dest_all[:, n, j:j + 1], axis=0),
                        bounds_check=None, oob_is_err=False))(n, j, dst),
                        [DV["ye"], dest_all], [dst], owner=dst)
                ld(xq[:], xmid_s.ap()[n * 128:(n + 1) * 128, :], [DV["xmid"]], [xq])
                act(acc[:], a0[:], AF.Copy, [a0, wgt_all], [acc], scale=wgt_all[:, n, 0:1])
                stt(acc[:], a1[:], wgt_all[:, n, 1:2], acc[:], ALU.mult, ALU.add, [a1, wgt_all, acc], [acc])
                tt(acc[:], acc[:], gate2_bc[:], ALU.mult, [acc, gate2_bc], [acc], eng="gpsimd")
                tt(acc[:], acc[:], xq[:], ALU.add, [acc, xq], [acc])
                act(junk[:], acc[:], AF.Square, [acc], [junk, col[0]], acc=col[0][:, 0:1])
                rsqrt_col(col[1][:, 0:1], col[0][:, 0:1], 1.0 / D, [col[0]], [col[1]], col[2])
                stt(o_[:], acc[:], col[1][:, 0:1], fg_bc[:], ALU.mult, ALU.mult, [acc, col[1], fg_bc], [o_])
                ld(out_d.ap()[n * 128:(n + 1) * 128, :], o_[:], [o_], [DV["out"]], q="sync")
            S.barrier()
            S.emit()
    return nc


_CACHE = {}


def _prep_weights(inp):
    f = lambda a: np.ascontiguousarray(np.asarray(a, dtype=np.float32))
    w1 = np.asarray(inp["w1"], dtype=np.float32)[0]
    w3 = np.asarray(inp["w3"], dtype=np.float32)[0]
    w2 = np.asarray(inp["w2"], dtype=np.float32)[0]
    wexp = np.empty((NEXP, 12, 128, 4096), dtype=np.float32)
    a1 = w1.reshape(NEXP, 16, 128, 8, 128).transpose(0, 3, 2, 1, 4)
    a3 = w3.reshape(NEXP, 16, 128, 8, 128).transpose(0, 3, 2, 1, 4)
    v = wexp[:, 0:8].reshape(NEXP, 8, 128, 2, 16, 128)
    v[:, :, :, 0] = a1
    v[:, :, :, 1] = a3
    a2 = w2.reshape(NEXP, 8, 128, 4, 512).transpose(0, 3, 2, 1, 4)
    wexp[:, 8:12] = a2.reshape(NEXP, 4, 128, 4096)
    shared = {
        "w_ada": f(inp["w_ada"][0]), "b_ada": f(inp["b_ada"][0]), "norm1_g": f(inp["norm1_g"][0]),
        "w_in": f(inp["w_in"][0]), "v_norm_g": f(inp["v_norm_g"][0]).reshape(-1),
        "v_norm_b": f(inp["v_norm_b"][0]).reshape(-1), "w_sp": f(inp["w_sp"][0]), "b_sp": f(inp["b_sp"][0]),
        "kv_norm_g": f(inp["kv_norm_g"][0]), "w_uk": f(inp["w_uk"][0]), "w_uv": f(inp["w_uv"][0]),
        "kidx_norm_g": f(inp["kidx_norm_g"][0]), "gnorm_a_g": f(inp["gnorm_a_g"][0]),
        "gnorm_b_g": f(inp["gnorm_b_g"][0]), "w_out": f(inp["w_out"][0]), "norm2_g": f(inp["norm2_g"][0]),
        "w_r": np.ascontiguousarray(np.concatenate([np.asarray(inp["w_group"][0]), np.asarray(inp["w_expert"][0])],
                                                   axis=1).astype(np.float32)),
        "b_r": np.ascontiguousarray(np.concatenate([np.asarray(inp["b_group"][0]), np.asarray(inp["b_expert"][0])],
                                                   axis=0).astype(np.float32)),
        "wexp": wexp, "final_g": f(inp["final_g"]),
    }
    return shared


def kernel(**inputs):
    x = np.asarray(inputs["x"], dtype=np.float32)
    c = np.asarray(inputs["c"], dtype=np.float32)
    pos = np.asarray(inputs["positions"], dtype=np.int32)
    nb, seq, _ = x.shape
    NT = seq // 128
    key = (NT,)
    if key not in _CACHE:
        _CACHE[key] = build(NT=NT)
    nc = _CACHE[key]
    shared = _prep_weights(inputs)
    in_maps = []
    for b in range(nb):
        m = dict(shared)
        m["x"] = np.ascontiguousarray(x[b])
        m["c"] = np.ascontiguousarray(c[b])
        m["pos"] = np.ascontiguousarray(pos[b])
        in_maps.append(m)
    res = run_bass_kernel_spmd(nc, in_maps, core_ids=list(range(nb)))
    return np.stack([np.asarray(r["out"], dtype=np.float32) for r in res.results], axis=0)
```

```python
import contextlib
import math
import numpy as np
import concourse.bass as bass
import concourse.mybir as mybir
from concourse.bass_utils import run_bass_kernel_spmd

F32 = mybir.dt.float32
BF16 = mybir.dt.bfloat16
I32 = mybir.dt.int32
U32 = mybir.dt.uint32
AF = mybir.ActivationFunctionType
ALU = mybir.AluOpType
AX = mybir.AxisListType

FLAGS = {"hT": 1, "p3": 1, "yT": 1, "reorder": 1}
EPOCH = 12000
ENGS = ["tensor", "vector", "scalar", "gpsimd", "sync"]
D = 2048
NEXP = 32
EPS = 1e-6


class Buf:
    __slots__ = ("name", "w", "r", "dsem", "dcum", "t", "multi", "sub")

    def __init__(self, name, t=None):
        self.name = name
        self.multi = False
        self.w = {}
        self.r = {}
        self.dsem = None
        self.dcum = 0
        self.t = t

    def __getitem__(self, k):
        return self.t[k]


class Sched:
    def __init__(self, nc, semstack):
        self.nc = nc
        self.semstack = semstack
        self.stack = semstack
        self.ops = {e: [] for e in ENGS}
        self.cnt = {e: 0 for e in ENGS}
        self.sems = {}
        self.waited = {e: {} for e in ENGS}
        self.cur_cum = {}
        self.nsem = 0
        self.bufs = []

    def _newsem(self, name):
        self.nsem += 1
        return self.semstack.enter_context(self.nc.semaphore(f"{name}_{self.nsem}"))

    def sb(self, name, shape, dtype):
        t = self.stack.enter_context(self.nc.sbuf_tensor(name, list(shape), dtype))
        b = Buf(name, t)
        self.bufs.append(b)
        return b

    def ps(self, name, shape, dtype):
        t = self.stack.enter_context(self.nc.psum_tensor(name, list(shape), dtype))
        b = Buf(name, t)
        self.bufs.append(b)
        return b

    def subs(self, buf, n, flag=None):
        buf.sub = []
        if flag is not None and not FLAGS.get(flag, 1):
            buf.sub = [buf] * n
            return buf
        for i in range(n):
            b = Buf(f"{buf.name}_s{i}", buf.t)
            self.bufs.append(b)
            buf.sub.append(b)
        return buf

    def view(self, name):
        b = Buf(name, None)
        b.multi = True
        self.bufs.append(b)
        return b

    def _engkey(self, eng):
        idx = self.cnt[eng]
        ep = idx // EPOCH
        key = ("E", eng, ep)
        if key not in self.sems:
            self.sems[key] = self._newsem(f"e_{eng}_{ep}")
        return key, (idx % EPOCH) + 1

    def _collect(self, eng, reads, writes):
        deps = {}

        def add(d):
            for k, v in d.items():
                if k[0] == "D":
                    v = max(v, self.cur_cum.get(k, v))
                if deps.get(k, 0) < v:
                    deps[k] = v
        for b in reads:
            add(b.w)
        for b in writes:
            if not b.multi:
                add(b.w)
            add(b.r)
        out = []
        wd = self.waited[eng]
        for k, v in deps.items():
            if k[0] == "E" and k[1] == eng and eng in ("tensor", "sync"):
                continue
            if wd.get(k, 0) >= v:
                continue
            wd[k] = v
            out.append((k, v))
        return out

    def _record(self, me, reads, writes):
        k, v = me
        for b in writes:
            if b.multi:
                if b.w.get(k, 0) < v:
                    b.w[k] = v
            else:
                b.w = {k: v}
                b.r = {}
        for b in reads:
            if b.r.get(k, 0) < v:
                b.r[k] = v

    def op(self, eng, fn, reads=(), writes=()):
        waits = self._collect(eng, reads, writes)
        key, val = self._engkey(eng)
        self.ops[eng].append((waits, fn, key, 1))
        self.cnt[eng] += 1
        self._record((key, val), reads, writes)

    def dma(self, queue, fn, reads=(), writes=(), owner=None):
        waits = self._collect(queue, reads, writes)
        if owner is None:
            owner = (list(writes) + list(reads))[0]
        if owner.dsem is None or owner.dcum + 16 > EPOCH * 2:
            key = ("D", id(owner), self.nsem)
            self.sems[key] = self._newsem("d_" + owner.name)
            owner.dsem = key
            owner.dcum = 0
        owner.dcum += 16
        key = owner.dsem
        self.cur_cum[key] = owner.dcum
        self.ops[queue].append((waits, fn, key, 16))
        self._record((key, owner.dcum), reads, writes)

    def raw(self, eng, fn, reads=()):
        waits = self._collect(eng, reads, [])
        self.ops[eng].append((waits, fn, "RAW", 0))

    def barrier(self):
        allk = {}
        for e in ENGS:
            if self.cnt[e] > 0:
                idx = self.cnt[e] - 1
                allk[("E", e, idx // EPOCH)] = (idx % EPOCH) + 1
        for k, v in self.cur_cum.items():
            allk[k] = v
        for e in ENGS:
            wd = self.waited[e]
            ws = []
            for k, v in allk.items():
                if wd.get(k, 0) >= v:
                    continue
                wd[k] = v
                if k[0] == "E" and k[1] == e:
                    continue
                ws.append((k, v))
            if ws:
                self.ops[e].append((ws, None, None, 0))
        for b in self.bufs:
            b.w = {}
            b.r = {}

    def emit(self):
        nc = self.nc
        with nc.Block() as block:
            for e in ENGS:
                ops = self.ops[e]
                if not ops:
                    continue

                def body(eng, ops=ops):
                    for waits, fn, key, inc in ops:
                        for k, v in waits:
                            eng.wait_ge(self.sems[k], v)
                        if fn is None:
                            continue
                        if key == "RAW":
                            fn(eng)
                        else:
                            fn(eng).then_inc(self.sems[key], inc)
                getattr(block, e)(body)
        self.ops = {e: [] for e in ENGS}


def build(NT=32, NITER=16, dbg=False):
    ST = NT * 128
    C = 512
    NSB = (2 * ST + C - 1) // C + NEXP
    nc = bass.Bass("TRN2", target_bir_lowering=False)

    def din(name, shape, dt=F32):
        return nc.dram_tensor(name, list(shape), dt, kind="ExternalInput")

    def dscr(name, shape, dt=F32):
        if dbg:
            return nc.dram_tensor(name, list(shape), dt, kind="ExternalOutput")
        return nc.dram_tensor(name, list(shape), dt)

    x_d = din("x", [ST, D])
    c_d = din("c", [D])
    pos_d = din("pos", [ST], I32)
    wada_d = din("w_ada", [D, 6 * D])
    bada_d = din("b_ada", [6 * D])
    n1g_d = din("norm1_g", [D])
    win_d = din("w_in", [D, 3912])
    vng_d = din("v_norm_g", [1024])
    vnb_d = din("v_norm_b", [1024])
    wsp_d = din("w_sp", [8, 128, 128])
    bsp_d = din("b_sp", [8, 128])
    kvg_d = din("kv_norm_g", [256])
    wuk_d = din("w_uk", [256, 64])
    wuv_d = din("w_uv", [256, 64])
    kig_d = din("kidx_norm_g", [64])
    gna_d = din("gnorm_a_g", [1024])
    gnb_d = din("gnorm_b_g", [1024])
    wout_d = din("w_out", [D, D])
    n2g_d = din("norm2_g", [D])
    wr_d = din("w_r", [D, 36])
    br_d = din("b_r", [36])
    wexp_d = din("wexp", [NEXP, 12, 128, 4096])
    fg_d = din("final_g", [D])
    out_d = nc.dram_tensor("out", [ST, D], F32, kind="ExternalOutput")

    mod_s = dscr("mod_s", [6 * D])
    ya_s = dscr("ya_s", [ST, 1024], BF16)
    qTe_s = dscr("qTe_s", [NT, 128, 1024], BF16)
    qTo_s = dscr("qTo_s", [NT, 128, 1024], BF16)
    qiTe_s = dscr("qiTe_s", [NT, 128, 512], BF16)
    qiTo_s = dscr("qiTo_s", [NT, 128, 512], BF16)
    xmid_s = dscr("xmid_s", [ST, D])
    xn2_s = dscr("xn2_s", [ST, D], BF16)
    xe_s = dscr("xe_s", [NSB * C, D], BF16)
    ye_s = dscr("ye_s", [NSB * C, D])

    with contextlib.ExitStack() as outer:
        S = Sched(nc, outer)
        B = [S.ps(f"B{i}", [128, 512], F32) for i in range(8)]
        DV = {n: S.view("dv_" + n) for n in
              ["in", "mod", "ya", "qTe", "qTo", "qiTe", "qiTo", "xmid", "xe", "ye", "out", "xn2"]}

        def mm(o, l, r, st, sp, R, W, skip=False):
            S.op("tensor", lambda e: e.matmul(o, l, r, start=st, stop=sp, skip_group_check=skip), R, W)

        def tr(o, i, idn, R, W):
            S.op("tensor", lambda e: e.transpose(o, i, idn), R, W)

        def act(o, i, f, R, W, bias=None, scale=None, acc=None):
            kw = {}
            if bias is not None:
                kw["bias"] = bias
            if scale is not None:
                kw["scale"] = scale
            if acc is not None:
                kw["accum_out"] = acc
            S.op("scalar", lambda e: e.activation(out=o, in_=i, func=f, **kw), R, W)

        def ts(o, i, s1, op0, R, W, s2=None, op1=None, eng="vector", acc=None):
            if acc is not None:
                S.op(eng, lambda e: e.tensor_scalar(o, i, s1, s2, op0, op1, accum_out=acc), R, W)
            elif op1 is None:
                S.op(eng, lambda e: e.tensor_scalar(o, i, s1, None, op0), R, W)
            else:
                S.op(eng, lambda e: e.tensor_scalar(o, i, s1, s2, op0, op1), R, W)

        def tt(o, a, b, op, R, W, eng="vector"):
            S.op(eng, lambda e: e.tensor_tensor(out=o, in0=a, in1=b, op=op), R, W)

        def stt(o, a, s, b, op0, op1, R, W):
            S.op("vector", lambda e: e.scalar_tensor_tensor(out=o, in0=a, scalar=s, in1=b, op0=op0, op1=op1), R, W)

        def cp(o, i, R, W, eng="vector"):
            if eng == "scalar":
                S.op("scalar", lambda e: e.copy(o, i), R, W)
            else:
                S.op(eng, lambda e: e.tensor_copy(o, i), R, W)

        def mset(ap, val, W, eng="vector"):
            S.op(eng, lambda e: e.memset(ap, val), [], W)

        def ld(o, i, R, W, q="sync", owner=None):
            S.dma(q, lambda e: e.dma_start(out=o, in_=i), R, W, owner=owner)

        def rsqrt_col(dst, src, scale, R, W, tmp):
            w_ = src.shape[-1]
            ts(tmp[:, 0:w_], src, scale, ALU.mult, R, [tmp], s2=EPS, op1=ALU.add)
            act(tmp[:, 0:w_], tmp[:, 0:w_], AF.Sqrt, [tmp], [tmp])
            S.op("vector", lambda e: e.reciprocal(dst, tmp[:, 0:w_]), [tmp], W)

        ident = S.sb("ident", [128, 128], F32)
        identb = S.sb("identb", [128, 128], BF16)
        onesf = S.sb("onesf", [128, 128], F32)
        onesb = S.sb("onesb", [128, 128], BF16)
        zerob = S.sb("zerob", [128, 512], BF16)
        LTb = S.sb("LTb", [128, 128], BF16)
        pvec = S.sb("pvec", [128, 64], F32)
        gT = S.sb("gT", [128, 58], F32)
        kT2 = S.sb("kT2", [128, ST], BF16)
        kiT2 = S.sb("kiT2", [128, ST], BF16)
        vaug = S.sb("vaug", [128, NT, 65], BF16)
        sgn_all = S.sb("sgn_all", [128, NT, 8], F32)
        dest_all = S.sb("dest_all", [128, NT, 2], I32)
        eid_all = S.sb("eid_all", [128, NT, 2], F32)
        pos_all = S.sb("pos_all", [128, NT, 2], F32)
        base_bc = S.sb("base_bc", [128, 32], F32)
        widx = S.sb("widx", [128, NSB, 12], I32)
        wgt_all = S.sb("wgt_all", [128, NT, 2], F32)
        junk = S.sb("junk", [128, 2048], BF16)
        col = [S.sb(f"col{i}", [128, 16], F32) for i in range(8)]

        mset(onesf[:], 1.0, [onesf], eng="gpsimd")
        mset(onesb[:], 1.0, [onesb], eng="gpsimd")
        mset(zerob[:], 0.0, [zerob], eng="gpsimd")
        S.op("gpsimd", lambda e: e.affine_select(out=ident[:], in_=onesf[:], pattern=[[1, 128]],
                                                 compare_op=ALU.is_equal, fill=0.0, base=0,
                                                 channel_multiplier=-1), [onesf], [ident])
        S.op("gpsimd", lambda e: e.affine_select(out=identb[:], in_=onesf[:], pattern=[[1, 128]],
                                                 compare_op=ALU.is_equal, fill=0.0, base=0,
                                                 channel_multiplier=-1), [onesf], [identb])
        S.op("gpsimd", lambda e: e.affine_select(out=LTb[:], in_=onesf[:], pattern=[[1, 128]],
                                                 compare_op=ALU.is_ge, fill=0.0, base=-1,
                                                 channel_multiplier=-1), [onesf], [LTb])
        mset(vaug[:, :, 64:65], 1.0, [vaug], eng="gpsimd")
        mset(dest_all[:], 0, [dest_all], eng="gpsimd")
        mset(widx[:], 0, [widx], eng="gpsimd")
        r_slots = outer.enter_context(nc.gpsimd.register("r_slots"))
        r_wrows = outer.enter_context(nc.gpsimd.register("r_wrows"))
        S.raw("gpsimd", lambda e: e.reg_mov(r_slots, NSB * C - 1))
        S.raw("gpsimd", lambda e: e.reg_mov(r_wrows, NEXP * 12 * 128 - 1))

        with contextlib.ExitStack() as ph:
            S.stack = ph
            stA = S.sb("stA", [112, 128], F32)
            stB = S.sb("stB", [58, 128], F32)
            siluc = S.sb("siluc", [128, 16], F32)
            cT = S.sb("cT", [128, 16], F32)
            badaT = S.sb("badaT", [128, 96], F32)
            modT = S.sb("modT", [128, 96], F32)
            modR = S.sb("modR", [96, 128], F32)
            wa = [S.sb(f"wa{i}", [128, 16, 512], F32) for i in range(2)]
            modrow = S.sb("modrow", [1, 6 * D], F32)

            ld(stA[0:16, :], c_d.ap().rearrange("(k p) -> k p", p=128), [DV["in"]], [stA])
            ld(stA[16:112, :], bada_d.ap().rearrange("(k p) -> k p", p=128), [DV["in"]], [stA])
            r0 = 0
            for src, nr in [(n1g_d, 16), (n2g_d, 16), (gna_d, 8), (gnb_d, 8), (kvg_d, 2)]:
                ld(stB[r0:r0 + nr, :], src.ap().rearrange("(k p) -> k p", p=128), [DV["in"]], [stB])
                r0 += nr
            ld(stB[50:58, :], bsp_d.ap(), [DV["in"]], [stB])
            tr(B[0][:, 0:112], stA[0:112, :], ident[0:112, 0:112], [stA, ident], [B[0]])
            tr(B[1][:, 0:58], stB[0:58, :], ident[0:58, 0:58], [stB, ident], [B[1]])
            cp(cT[:], B[0][:, 0:16], [B[0]], [cT])
            act(siluc[:], cT[:], AF.Silu, [cT], [siluc])
            cp(badaT[:], B[0][:, 16:112], [B[0]], [badaT])
            cp(gT[:], B[1][:, 0:58], [B[1]], [gT])
            for cb in range(24):
                w = wa[cb % 2]
                ld(w[:], wada_d.ap().rearrange("(k p) c -> p k c", p=128)[:, :, cb * 512:(cb + 1) * 512],
                   [DV["in"]], [w])
                rb = B[2 + cb % 2]
                for k in range(16):
                    mm(rb[0:1, :], siluc[:, k:k + 1], w[:, k, :], k == 0, k == 15, [w, siluc], [rb])
                cp(modrow[0:1, cb * 512:(cb + 1) * 512], rb[0:1, :], [rb], [modrow])
            for j in range(96):
                mm(B[4][:, j:j + 1], modrow[0:1, j * 128:(j + 1) * 128], onesf[0:1, 0:1], True, True,
                   [modrow, onesf], [B[4]])
            tt(modT[:], B[4][:, 0:96], badaT[:], ALU.add, [B[4], badaT], [modT])
            stt(pvec[:, 0:16], modT[:, 16:32], 1.0, gT[:, 0:16], ALU.add, ALU.mult, [modT, gT], [pvec])
            cp(pvec[:, 16:32], modT[:, 0:16], [modT], [pvec])
            stt(pvec[:, 32:48], modT[:, 64:80], 1.0, gT[:, 16:32], ALU.add, ALU.mult, [modT, gT], [pvec])
            cp(pvec[:, 48:64], modT[:, 48:64], [modT], [pvec])
            tr(B[3][0:96, 0:128], modT[:, 0:96], ident[:], [modT, ident], [B[3]])
            cp(modR[:], B[3][0:96, 0:128], [B[3]], [modR])
            ld(mod_s.ap().rearrange("(j p) -> j p", p=128), modR[:], [modR], [DV["mod"]], q="gpsimd")
            S.barrier()
            S.emit()

        def prep_hT(xt, xn, hT, c_ss, c_rs, c_tmp, sc_off, sh_off):
            act(junk[:], xt[:], AF.Square, [xt], [junk, c_ss], acc=c_ss[:, 0:1])
            rsqrt_col(c_rs[:, 0:1], c_ss[:, 0:1], 1.0 / D, [c_ss], [c_rs], c_tmp)
            ts(xn[:], xt[:], c_rs[:, 0:1], ALU.mult, [xt, c_rs], [xn])
            for k in range(16):
                bk = B[k // 8]
                tr(bk[:].bitcast(BF16)[:, (k % 8) * 128:(k % 8 + 1) * 128], xn[:, k * 128:(k + 1) * 128],
                   identb[:], [xn, identb], [bk])
            for k in range(16):
                bk = B[k // 8]
                src = bk[:].bitcast(BF16)[:, (k % 8) * 128:(k % 8 + 1) * 128]
                if k < 8:
                    ts(hT[:, k, :], src, pvec[:, sc_off + k:sc_off + k + 1], ALU.mult, [bk, pvec], [hT.sub[k]],
                       s2=pvec[:, sh_off + k:sh_off + k + 1], op1=ALU.add)
                else:
                    act(hT[:, k, :], src, AF.Identity, [bk, pvec], [hT.sub[k]],
                        bias=pvec[:, sh_off + k:sh_off + k + 1], scale=pvec[:, sc_off + k:sc_off + k + 1])

        def load_w_bf(dst, src_ap_fn, ncols, stg, rowscale=None, colscale=None):
            step = 256
            i = 0
            for c0 in range(0, ncols, step):
                cw = min(step, ncols - c0)
                sg = stg[i % 2]
                ld(sg[:, :, 0:cw], src_ap_fn(c0, cw), [DV["in"]], [sg])
                eng = ["vector", "gpsimd"][i % 2]
                if rowscale is None:
                    if i % 3 == 2:
                        cp(dst[:, :, c0:c0 + cw], sg[:, :, 0:cw], [sg], [dst], eng="scalar")
                    else:
                        cp(dst[:, :, c0:c0 + cw], sg[:, :, 0:cw], [sg], [dst], eng=eng)
                else:
                    for k in range(16):
                        stt(dst[:, k, c0:c0 + cw], sg[:, k, 0:cw], rowscale[:, k:k + 1], colscale[:, c0:c0 + cw],
                            ALU.mult, ALU.mult, [sg] + rowscale_bufs, [dst])
                i += 1

        rowscale_bufs = []

        with contextlib.ExitStack() as ph:
            S.stack = ph
            wbf = S.sb("wbfA", [128, 16, 2048], BF16)
            stg = [S.sb(f"stgA{i}", [128, 16, 256], F32) for i in range(2)]
            xts = [S.sb(f"xtA{i}", [128, D], F32) for i in range(2)]
            xns = [S.sb(f"xnA{i}", [128, D], BF16) for i in range(2)]
            hTs = [S.subs(S.sb(f"hTA{i}", [128, 16, 128], BF16), 16, "hT") for i in range(2)]
            gu = S.sb("gu", [128, 1024], F32)
            gv = S.sb("gv", [128, 1024], F32)
            vn = S.sb("vn", [128, 1024], F32)
            vgb = S.sb("vgb", [128, 1024], BF16)
            Gbc = S.sb("Gbc", [128, 1024], F32)
            Bbc = S.sb("Bbc", [128, 1024], F32)
            wsp = S.sb("wsp", [128, 8, 128], F32)
            WmT = S.sb("WmT", [128, 8, 128], BF16)
            ya = S.sb("ya", [128, 1024], F32)
            yan = S.sb("yan", [128, 1024], BF16)
            stats = S.sb("stats", [128, 8, 6], F32)
            mv = S.sb("mv", [128, 8, 2], F32)

            win_v = win_d.ap().rearrange("(k p) c -> p k c", p=128)
            load_w_bf(wbf, lambda c0, cw: win_v[:, :, c0:c0 + cw], 2048, stg)
            ld(Gbc[:], vng_d.ap().partition_broadcast(128), [DV["in"]], [Gbc])
            ld(Bbc[:], vnb_d.ap().partition_broadcast(128), [DV["in"]], [Bbc])
            ld(wsp[:], wsp_d.ap().rearrange("g i j -> i g j"), [DV["in"]], [wsp])
            for g in range(8):
                bk = B[6 + g // 4]
                tr(bk[:, (g % 4) * 128:(g % 4 + 1) * 128], wsp[:, g, :], ident[:], [wsp, ident], [bk])
            for g in range(8):
                bk = B[6 + g // 4]
                cp(WmT[:, g, :], bk[:, (g % 4) * 128:(g % 4 + 1) * 128], [bk], [WmT])
            mset(WmT[64:128, :, 0:64], 0.0, [WmT])

            def H_a(n):
                xt = xts[n % 2]
                ld(xt[:], x_d.ap()[n * 128:(n + 1) * 128, :], [DV["in"]], [xt])
                prep_hT(xt, xns[n % 2], hTs[n % 2], col[0], col[1], col[2], 0, 16)

            def M_a(n):
                hT = hTs[n % 2]
                for cg in range(4):
                    for k in range(16):
                        mm(B[2 + cg][:], hT[:, k, :], wbf[:, k, cg * 512:(cg + 1) * 512], k == 0, k == 15,
                           [hT.sub[k], wbf], [B[2 + cg]])

            def E_a(n):
                act(gu[:, 0:512], B[2][:], AF.Gelu_apprx_tanh, [B[2]], [gu])
                act(gu[:, 512:1024], B[3][:], AF.Gelu_apprx_tanh, [B[3]], [gu])
                act(gv[:, 0:512], B[4][:], AF.Gelu_apprx_tanh, [B[4]], [gv])
                act(gv[:, 512:1024], B[5][:], AF.Gelu_apprx_tanh, [B[5]], [gv])
                for g in range(8):
                    S.op("vector", (lambda g: lambda e: e.bn_stats(stats[:, g, :], gv[:, g * 128:(g + 1) * 128]))(g),
                         [gv], [stats])
                for g in range(8):
                    S.op("vector", (lambda g: lambda e: e.bn_aggr(mv[:, g, :], stats[:, g, :]))(g), [stats], [mv])
                ts(col[4][:, 0:8], mv[:, :, 1], EPS, ALU.add, [mv], [col[4]])
                act(col[4][:, 0:8], col[4][:, 0:8], AF.Sqrt, [col[4]], [col[4]])
                S.op("vector", lambda e: e.reciprocal(col[3][:, 0:8], col[4][:, 0:8]), [col[4]], [col[3]])
                for g in range(8):
                    ts(vn[:, g * 128:(g + 1) * 128], gv[:, g * 128:(g + 1) * 128], mv[:, g, 0:1], ALU.subtract,
                       [gv, mv, col[3]], [vn], s2=col[3][:, g:g + 1], op1=ALU.mult)
                tt(vn[:], vn[:], Gbc[:], ALU.mult, [vn, Gbc], [vn], eng="gpsimd")
                tt(vgb[:], vn[:], Bbc[:], ALU.add, [vn, Bbc], [vgb])

            def E2_a(n):
                for g in range(8):
                    bk = B[6 + g // 4]
                    mm(bk[:, (g % 4) * 128:(g % 4 + 1) * 128], WmT[:, g, :], vgb[:, g * 128:(g + 1) * 128],
                       True, True, [WmT, vgb], [bk])
                for g in range(8):
                    bk = B[6 + g // 4]
                    stt(ya[:, g * 128:(g + 1) * 128], bk[:, (g % 4) * 128:(g % 4 + 1) * 128], gT[:, 50 + g:51 + g],
                        gu[:, g * 128:(g + 1) * 128], ALU.add, ALU.mult, [bk, gT, gu], [ya])
                act(junk[:, 0:1024], ya[:], AF.Square, [ya], [junk, col[5]], acc=col[5][:, 0:1])
                rsqrt_col(col[6][:, 0:1], col[5][:, 0:1], 1.0 / 1024, [col[5]], [col[6]], col[7])
                ts(yan[:], ya[:], col[6][:, 0:1], ALU.mult, [ya, col[6]], [yan])
                ld(ya_s.ap()[n * 128:(n + 1) * 128, :], yan[:], [yan], [DV["ya"]], q="gpsimd")

            H_a(0)
            for n in range(NT):
                M_a(n)
                if n + 1 < NT:
                    H_a(n + 1)
                if FLAGS.get("reorder", 1):
                    if n >= 1:
                        E2_a(n - 1)
                    E_a(n)
                else:
                    E_a(n)
                    E2_a(n)
            if FLAGS.get("reorder", 1):
                E2_a(NT - 1)
            S.barrier()
            S.emit()

        with contextlib.ExitStack() as ph:
            S.stack = ph
            NCB = 3912 - 2048
            wbf = S.sb("wbfB", [128, 16, NCB], BF16)
            posT = S.sb("posT", [128, NT], F32)
            sin_t = S.sb("sin_t", [128, NT, 32], F32)
            cos_t = S.sb("cos_t", [128, NT, 32], F32)
            wukv = S.sb("wukv", [128, 2, 128], BF16)
            kig_bc = S.sb("kig_bc", [128, 64], F32)
            ph2 = contextlib.ExitStack()
            S.stack = ph2
            stg = [S.sb(f"stgB{i}", [128, 16, 256], F32) for i in range(2)]
            posR = S.sb("posR", [NT, 128], I32)
            posF = S.sb("posF", [NT, 128], F32)
            fr = S.sb("fr", [128, 32], F32)
            ang = S.sb("ang", [128, NT, 32], F32)
            rr = S.sb("rr", [128, NT, 32], F32)
            rq = S.sb("rq", [128, NT, 32], F32)
            rni = S.sb("rni", [128, NT, 32], I32)
            stkv = S.sb("stkv", [128, 2, 128], F32)
            win_v = win_d.ap().rearrange("(k p) c -> p k c", p=128)
            load_w_bf(wbf, lambda c0, cw: win_v[:, :, 2048 + c0:2048 + c0 + cw], NCB, stg)
            ld(posR[:], pos_d.ap().rearrange("(n p) -> n p", p=128), [DV["in"]], [posR])
            cp(posF[:], posR[:], [posR], [posF])
            tr(B[7][:, 0:NT], posF[0:NT, :], ident[0:NT, 0:NT], [posF, ident], [B[7]])
            cp(posT[:], B[7][:, 0:NT], [B[7]], [posT])
            for i in range(32):
                mset(fr[:, i:i + 1], float(np.float32(10000.0) ** np.float32(-i / 32.0)), [fr], eng="gpsimd")
            tt(ang[:], posT[:].unsqueeze(2).broadcast_to([128, NT, 32]),
               fr[:].unsqueeze(1).broadcast_to([128, NT, 32]), ALU.mult, [posT, fr], [ang])
            TWO_PI = 2.0 * math.pi
            C1 = 6.28125
            C2 = TWO_PI - C1
            for (dst, shift) in ((sin_t, 0.0), (cos_t, math.pi / 2)):
                ts(rq[:], ang[:], shift, ALU.add, [ang], [rq])
                ts(rr[:], rq[:], 1.0 / TWO_PI, ALU.mult, [rq], [rr])
                cp(rni[:], rr[:], [rr], [rni])
                cp(rr[:], rni[:], [rni], [rr])
                stt(rq[:], rr[:], -C1, rq[:], ALU.mult, ALU.add, [rr, rq], [rq])
                stt(rq[:], rr[:], -C2, rq[:], ALU.mult, ALU.add, [rr, rq], [rq])
                ts(rr[:], rq[:], math.pi, ALU.is_gt, [rq], [rr])
                stt(rq[:], rr[:], -TWO_PI, rq[:], ALU.mult, ALU.add, [rr, rq], [rq])
                ts(rr[:], rq[:], -math.pi, ALU.is_lt, [rq], [rr])
                stt(rq[:], rr[:], TWO_PI, rq[:], ALU.mult, ALU.add, [rr, rq], [rq])
                ts(rq[:], rq[:], 3.141592, ALU.min, [rq], [rq], s2=-3.141592, op1=ALU.max)
                act(dst[:], rq[:], AF.Sin, [rq], [dst])
            ld(stkv[:, :, 0:64], wuk_d.ap().rearrange("(c p) d -> p c d", p=128), [DV["in"]], [stkv])
            ld(stkv[:, :, 64:128], wuv_d.ap().rearrange("(c p) d -> p c d", p=128), [DV["in"]], [stkv])
            for c2 in range(2):
                ts(wukv[:, c2, :], stkv[:, c2, :], gT[:, 48 + c2:49 + c2], ALU.mult, [stkv, gT], [wukv])
            ld(kig_bc[:], kig_d.ap().partition_broadcast(128), [DV["in"]], [kig_bc])
            S.barrier()
            S.emit()
            ph2.close()
            S.stack = ph
            xts = [S.sb(f"xtB{i}", [128, D], F32) for i in range(2)]
            xns = [S.sb(f"xnB{i}", [128, D], BF16) for i in range(2)]
            hTs = [S.subs(S.sb(f"hTB{i}", [128, 16, 128], BF16), 16, "hT") for i in range(2)]
            qrs = [S.sb(f"qr{i}", [128, 1024], BF16) for i in range(2)]
            qirs = [S.sb(f"qir{i}", [128, 512], BF16) for i in range(2)]
            qf = S.subs(S.sb("qf", [128, 1024], F32), 2)
            c4f = S.sb("c4f", [128, 512], F32)
            c5f = S.sb("c5f", [128, 328], F32)
            t1 = S.sb("t1", [128, 512], F32)
            t2 = S.sb("t2", [128, 512], F32)
            cw_ = S.sb("cw_", [128, 8, 32], F32)
            sw_ = S.sb("sw_", [128, 8, 32], F32)
            wis = S.sb("wis", [128, 8], F32)
            ckvb = S.sb("ckvb", [128, 256], BF16)
            ckvf = S.sb("ckvf", [128, 256], F32)
            kif = S.sb("kif", [128, 64], F32)
            ckvT = S.sb("ckvT", [128, 2, 128], BF16)
            kk = S.sb("kk", [128, 64], F32)
            kk2 = S.sb("kk2", [128, 128], BF16)
            kin = S.sb("kin", [128, 64], F32)
            kir = S.sb("kir", [128, 64], F32)
            kki2 = S.sb("kki2", [128, 128], BF16)
            qTe = S.sb("qTe", [128, 8, 128], BF16)
            qTo = S.sb("qTo", [128, 8, 128], BF16)
            qiTe = S.sb("qiTe", [128, 4, 128], BF16)
            qiTo = S.sb("qiTo", [128, 4, 128], BF16)
            mset(qTe[:], 0.0, [qTe], eng="gpsimd")
            mset(qTo[:], 0.0, [qTo], eng="gpsimd")
            mset(qiTe[:], 0.0, [qiTe], eng="gpsimd")
            mset(qiTo[:], 0.0, [qiTo], eng="gpsimd")
            CSC = (8 ** -0.5) * (64 ** -0.5)

            def rope(o_lo, o_hi, x_lo, x_hi, cs, sn, nh, RB):
                a = t1[:, 0:nh * 32].rearrange("p (h d) -> p h d", d=32)
                b = t2[:, 0:nh * 32].rearrange("p (h d) -> p h d", d=32)
                tt(a, x_lo, cs, ALU.mult, RB, [t1])
                tt(b, x_hi, sn, ALU.mult, RB, [t2])
                tt(o_lo, a, b, ALU.subtract, [t1, t2], RB[-1:], eng="gpsimd")
                tt(a, x_lo, sn, ALU.mult, RB + [t1], [t1])
                tt(b, x_hi, cs, ALU.mult, RB + [t2], [t2])
                tt(o_hi, a, b, ALU.add, [t1, t2], RB[-1:], eng="gpsimd")

            zt = S.sb("zt", [128, D], BF16)
            mset(zt[:], 0.0, [zt], eng="gpsimd")
            zf_total = NSB * C // 512
            zf_done = [0]

            def H_b(n):
                xt = xts[n % 2]
                ld(xt[:], x_d.ap()[n * 128:(n + 1) * 128, :], [DV["in"]], [xt])
                want = (zf_total * (n + 1) + NT - 1) // NT
                while zf_done[0] < min(want, zf_total):
                    r = zf_done[0]
                    ld(xe_s.ap()[r * 512:(r + 1) * 512, :].rearrange("(a p) d -> p a d", p=128),
                       zt[:].unsqueeze(1).broadcast_to([128, 4, D]), [zt], [DV["xe"]], q="sync", owner=zt)
                    zf_done[0] += 1
                prep_hT(xt, xns[n % 2], hTs[n % 2], col[0], col[1], col[2], 0, 16)

            def M_b(n):
                hT = hTs[n % 2]
                widths = [512, 512, 512, NCB - 1536]
                for cg in range(4):
                    for k in range(16):
                        mm(B[2 + cg][:, 0:widths[cg]], hT[:, k, :], wbf[:, k, cg * 512:cg * 512 + widths[cg]],
                           k == 0, k == 15, [hT.sub[k], wbf], [B[2 + cg]])

            def E1_b(n):
                cosb = cos_t[:, n, :].unsqueeze(1)
                sinb = sin_t[:, n, :].unsqueeze(1)
                qr, qir = qrs[n % 2], qirs[n % 2]
                cp(c4f[:], B[4][:], [B[4]], [c4f], eng="scalar")
                cp(c5f[:, 0:328], B[5][:, 0:328], [B[5]], [c5f], eng="scalar")
                cp(qf[:, 0:512], B[2][:], [B[2]], [qf.sub[0]], eng="scalar")
                cp(qf[:, 512:1024], B[3][:], [B[3]], [qf.sub[1]], eng="scalar")
                act(junk[:, 0:256], c4f[:, 0:256], AF.Square, [c4f], [junk, col[3]], acc=col[3][:, 0:1])
                rsqrt_col(col[4][:, 0:1], col[3][:, 0:1], 1.0 / 256, [col[3]], [col[4]], col[5])
                cp(ckvb[:], c4f[:, 0:256], [c4f], [ckvb])
                for c2 in range(2):
                    tr(B[0][:].bitcast(BF16)[:, c2 * 128:(c2 + 1) * 128], ckvb[:, c2 * 128:(c2 + 1) * 128], identb[:],
                       [ckvb, identb], [B[0]])
                cp(ckvT[:], B[0][:].bitcast(BF16)[:, 0:256].rearrange("p (c t) -> p c t", t=128), [B[0]], [ckvT])
                for c2 in range(2):
                    mm(B[1][:, 0:128], ckvT[:, c2, :], wukv[:, c2, :], c2 == 0, c2 == 1, [ckvT, wukv], [B[1]])
                act(junk[:, 0:64], c5f[:, 256:320], AF.Square, [c5f], [junk, col[6]], acc=col[6][:, 0:1])
                rsqrt_col(col[7][:, 0:1], col[6][:, 0:1], 1.0 / 64, [col[6]], [col[7]], col[5])
                stt(kin[:], c5f[:, 256:320], col[7][:, 0:1], kig_bc[:], ALU.mult, ALU.mult,
                    [c5f, col[7], kig_bc], [kin])
                ki3 = kin[:].rearrange("p (h d) -> p h d", d=64)
                kr3 = kir[:].rearrange("p (h d) -> p h d", d=64)
                rope(kr3[:, :, 0:32], kr3[:, :, 32:64], ki3[:, :, 0:32], ki3[:, :, 32:64], cosb, sinb, 1,
                     [kin, cos_t, sin_t, kir])
                cp(kki2[:, 0:64], kir[:], [kir], [kki2])
                cp(kki2[:, 64:128], kir[:], [kir], [kki2])
                tr(B[0][:].bitcast(BF16)[:, 384:512], kki2[:], identb[:], [kki2, identb], [B[0]])
                cp(kiT2[:, n * 128:(n + 1) * 128], B[0][:].bitcast(BF16)[:, 384:512], [B[0]], [kiT2])
                ts(vaug[:, n, 0:64], B[1][:, 64:128], col[4][:, 0:1], ALU.mult, [B[1], col[4]], [vaug])
                kv3 = B[1][:, 0:64].rearrange("p (h d) -> p h d", d=64)
                kk3 = kk[:].rearrange("p (h d) -> p h d", d=64)
                rope(kk3[:, :, 0:32], kk3[:, :, 32:64], kv3[:, :, 0:32], kv3[:, :, 32:64], cosb, sinb, 1,
                     [B[1], cos_t, sin_t, kk])
                ts(kk2[:, 0:64], kk[:], col[4][:, 0:1], ALU.mult, [kk, col[4]], [kk2])
                ts(kk2[:, 64:128], kk[:], col[4][:, 0:1], ALU.mult, [kk, col[4]], [kk2])
                tr(B[0][:].bitcast(BF16)[:, 256:384], kk2[:], identb[:], [kk2, identb], [B[0]])
                cp(kT2[:, n * 128:(n + 1) * 128], B[0][:].bitcast(BF16)[:, 256:384], [B[0]], [kT2])
                ts(wis[:], c5f[:, 320:328], CSC, ALU.mult, [c5f], [wis])
                ts(sgn_all[:, n, :], c5f[:, 320:328], 0.0, ALU.is_ge, [c5f], [sgn_all], s2=2.0, op1=ALU.mult)
                ts(sgn_all[:, n, :], sgn_all[:, n, :], -1.0, ALU.add, [sgn_all], [sgn_all])
                tt(cw_[:], cosb.broadcast_to([128, 8, 32]), wis[:].unsqueeze(2).broadcast_to([128, 8, 32]),
                   ALU.mult, [cos_t, wis], [cw_])
                tt(sw_[:], sinb.broadcast_to([128, 8, 32]), wis[:].unsqueeze(2).broadcast_to([128, 8, 32]),
                   ALU.mult, [sin_t, wis], [sw_])
                for hb in range(2):
                    qv = qf[:, hb * 512:(hb + 1) * 512].rearrange("p (h d) -> p h d", d=64)
                    ov = qr[:, hb * 512:(hb + 1) * 512].rearrange("p (h d) -> p h d", d=64)
                    rope(ov[:, :, 0:32], ov[:, :, 32:64], qv[:, :, 0:32], qv[:, :, 32:64],
                         cosb.broadcast_to([128, 8, 32]), sinb.broadcast_to([128, 8, 32]), 8,
                         [qf.sub[hb], cos_t, sin_t, qr])
                for hb in range(2):
                    src = c4f[:, 256:512] if hb == 0 else c5f[:, 0:256]
                    qv = src.rearrange("p (h d) -> p h d", d=64)
                    ov = qir[:, hb * 256:(hb + 1) * 256].rearrange("p (h d) -> p h d", d=64)
                    rope(ov[:, :, 0:32], ov[:, :, 32:64], qv[:, :, 0:32], qv[:, :, 32:64],
                         cw_[:, hb * 4:(hb + 1) * 4, :], sw_[:, hb * 4:(hb + 1) * 4, :], 4,
                         [c4f if hb == 0 else c5f, cw_, sw_, qir])

            def E2_b(n):
                qr, qir = qrs[n % 2], qirs[n % 2]
                b6 = B[6][:].bitcast(BF16)
                for pr in range(8):
                    tr(b6[:, pr * 128:(pr + 1) * 128], qr[:, pr * 128:(pr + 1) * 128], identb[:], [qr, identb], [B[6]])
                b63 = b6.rearrange("p (a t) -> p a t", t=128)
                cp(qTe[0:64, :, :], b63[0:64, :, :], [B[6]], [qTe])
                cp(qTo[64:128, :, :], b63[64:128, :, :], [B[6]], [qTo])
                b7 = B[7][:].bitcast(BF16)
                for pr in range(4):
                    tr(b7[:, pr * 128:(pr + 1) * 128], qir[:, pr * 128:(pr + 1) * 128], identb[:], [qir, identb], [B[7]])
                b73 = b7[:, 0:512].rearrange("p (a t) -> p a t", t=128)
                cp(qiTe[0:64, :, :], b73[0:64, :, :], [B[7]], [qiTe], eng="scalar")
                cp(qiTo[64:128, :, :], b73[64:128, :, :], [B[7]], [qiTo], eng="scalar")
                ld(qTe_s.ap()[n], qTe[:].rearrange("p a t -> p (a t)"), [qTe], [DV["qTe"]], q="gpsimd")
                ld(qTo_s.ap()[n], qTo[:].rearrange("p a t -> p (a t)"), [qTo], [DV["qTo"]], q="gpsimd")
                ld(qiTe_s.ap()[n], qiTe[:].rearrange("p a t -> p (a t)"), [qiTe], [DV["qiTe"]], q="gpsimd")
                ld(qiTo_s.ap()[n], qiTo[:].rearrange("p a t -> p (a t)"), [qiTo], [DV["qiTo"]], q="gpsimd")

            H_b(0)
            for n in range(NT):
                M_b(n)
                if n + 1 < NT:
                    H_b(n + 1)
                if n >= 1:
                    E2_b(n - 1)
                E1_b(n)
            E2_b(NT - 1)
            S.barrier()
            S.emit()

        with contextlib.ExitStack() as ph:
            S.stack = ph
            woutb = S.sb("woutb", [128, 16, D], BF16)
            with contextlib.ExitStack() as ph2:
                S.stack = ph2
                stg = [S.sb(f"stgO{i}", [128, 16, 256], F32) for i in range(2)]
                gate1_bc = S.sb("gate1_bc", [128, D], F32)
                ld(gate1_bc[:], mod_s.ap()[2 * D:3 * D].partition_broadcast(128), [DV["mod"]], [gate1_bc])
                wout_v = wout_d.ap().rearrange("(k p) c -> p k c", p=128)
                rowscale_bufs.clear()
                rowscale_bufs.extend([gT, gate1_bc])
                load_w_bf(woutb, lambda c0, cw: wout_v[:, :, c0:c0 + cw], D, stg, rowscale=gT[:, 32:48],
                          colscale=gate1_bc)
                S.barrier()
                S.emit()
            S.stack = ph
            score = S.sb("score", [128, ST], F32)
            NMs = [S.sb(f"NM{i}", [128, ST], BF16) for i in range(2)]
            qTe = [S.sb(f"qTeL{i}", [128, 1024], BF16) for i in range(2)]
            qTo = [S.sb(f"qToL{i}", [128, 1024], BF16) for i in range(2)]
            qiTe = [S.sb(f"qiTeL{i}", [128, 512], BF16) for i in range(2)]
            qiTo = [S.sb(f"qiToL{i}", [128, 512], BF16) for i in range(2)]
            pT = [S.sb(f"pT{i}", [128, 512], BF16) for i in range(3)]
            xt = S.sb("xtC", [128, D], F32)
            yanl = S.sb("yanl", [128, 1024], BF16)
            yb = S.sb("yb", [128, 1024], F32)
            ybn = S.sb("ybn", [128, 1024], BF16)
            yT = S.subs(S.sb("yT", [128, 16, 128], BF16), 2, "yT")
            xm = S.sb("xm", [128, D], F32)
            xn2b = S.sb("xn2b", [128, D], BF16)
            h2T = S.subs(S.sb("h2T", [128, 16, 128], F32), 16)
            wr = S.sb("wr", [128, 16, 36], F32)
            bias_bc = S.sb("bias_bc", [128, 36], F32)
            lg = S.sb("lg", [128, 36], F32)
            em = S.sb("em", [128, 32], F32)
            m8 = S.sb("m8", [128, 8], F32)
            i8 = S.sb("i8", [128, 8], U32)
            Ab = S.sb("Ab", [128, 32], BF16)
            oh0 = S.sb("oh0", [128, 32], F32)
            oh1 = S.sb("oh1", [128, 32], F32)
            posf = S.sb("posf", [128, 32], F32)
            tmp32 = S.sb("tmp32", [128, 32], F32)
            sm = S.sb("sm", [128, 32], F32)
            lo = S.sb("lo", [128, 1], F32)
            w0c = S.sb("w0c", [128, 1], F32)
            mid = S.sb("mid", [128, 1], F32)
            cnt = S.sb("cnt", [128, 1], F32)
            gei = S.sb("gei", [128, 1], U32)
            thr = S.sb("thr", [128, 1], F32)
            rden = S.sb("rden", [128, 16], F32)
            destf = S.sb("destf", [128, 2], F32)

            ld(wr[:], wr_d.ap().rearrange("(k p) c -> p k c", p=128), [DV["in"]], [wr])
            ld(bias_bc[:], br_d.ap().partition_broadcast(128), [DV["in"]], [bias_bc])
            mset(base_bc[:], 0.0, [base_bc])

            def stage_A(n):
                Sk = (n + 1) * 128
                qe, qo, qie, qio = qTe[n % 2], qTo[n % 2], qiTe[n % 2], qiTo[n % 2]
                NM = NMs[n % 2]
                ld(qie[:], qiTe_s.ap()[n], [DV["qiTe"]], [qie])
                ld(qio[:], qiTo_s.ap()[n], [DV["qiTo"]], [qio])
                ld(qe[:], qTe_s.ap()[n], [DV["qTe"]], [qe])
                ld(qo[:], qTo_s.ap()[n], [DV["qTo"]], [qo])
                bi = 0
                for ks in range(0, Sk, 512):
                    ke = min(Sk, ks + 512)
                    for h in range(8):
                        bk = B[bi % 2]
                        bi += 1
                        src = (qie if h % 2 == 0 else qio)[:, (h // 2) * 128:(h // 2 + 1) * 128]
                        mm(bk[:, 0:ke - ks], src, kiT2[:, ks:ke], True, True, [qie, qio, kiT2], [bk])
                        sg = sgn_all[:, n, h:h + 1]
                        act(bk[:, 0:ke - ks], bk[:, 0:ke - ks], AF.Relu, [bk, sgn_all], [bk], scale=sg)
                        if h == 0:
                            ts(score[:, ks:ke], bk[:, 0:ke - ks], sg, ALU.mult, [bk, sgn_all], [score])
                        else:
                            stt(score[:, ks:ke], bk[:, 0:ke - ks], sg, score[:, ks:ke], ALU.mult, ALU.add,
                                [bk, sgn_all, score], [score])
                mset(score[0:64, Sk - 64:Sk], -1.0e30, [score])
                if n < 2:
                    mset(thr[:], -1.0e29, [thr])
                else:
                    S.op("vector", (lambda Sk: lambda e: e.tensor_reduce(out=lo[:], in_=score[:, 0:Sk - 64], axis=AX.X,
                                                                         op=ALU.min))(Sk), [score], [lo])
                    S.op("vector", (lambda Sk: lambda e: e.tensor_reduce(out=w0c[:], in_=score[:, 0:Sk], axis=AX.X,
                                                                         op=ALU.max))(Sk), [score], [w0c])
                    stt(w0c[:], w0c[:], 1.0e-4, lo[:], ALU.add, ALU.subtract, [w0c, lo], [w0c])
                    for it in range(NITER):
                        stt(mid[:], w0c[:], 2.0 ** -(it + 1), lo[:], ALU.mult, ALU.add, [w0c, lo], [mid])
                        ts(NM[:, 0:Sk], score[:, 0:Sk], mid[:, 0:1], ALU.is_ge, [score, mid], [NM, cnt],
                           s2=0.0, op1=ALU.add, acc=cnt[:, 0:1])
                        ts(gei[:], cnt[:], 255.5, ALU.is_ge, [cnt], [gei])
                        S.op("vector", lambda e: e.copy_predicated(lo[:], gei[:], mid[:]), [gei, mid, lo], [lo])
                    cp(thr[:], lo[:], [lo], [thr])
                ts(NM[:, 0:Sk], score[:, 0:Sk], thr[:, 0:1], ALU.is_lt, [score, thr], [NM], s2=-30000.0, op1=ALU.mult)

            def stage_B(n):
                Sk = (n + 1) * 128
                qe, qo = qTe[n % 2], qTo[n % 2]
                NM = NMs[n % 2]
                ld(xt[:], x_d.ap()[n * 128:(n + 1) * 128, :], [DV["in"]], [xt])
                ld(yanl[:], ya_s.ap()[n * 128:(n + 1) * 128, :], [DV["ya"]], [yanl])
                for b3 in range(3):
                    mm(B[4 + b3][:], zerob[:, 0:128], zerob[:], True, False, [zerob], [B[4 + b3]], skip=True)
                units = [(kb, j) for kb in range(n + 1) for j in range(4)]

                def QK(u):
                    kb, j = units[u]
                    bk = B[2 + u % 2]
                    pt = pT[u % 3]
                    qsrc = (qe if j < 2 else qo)[:, (j % 2) * 512:(j % 2 + 1) * 512]
                    mm(bk[:], kT2[:, kb * 128:(kb + 1) * 128], qsrc, True, False, [kT2, qe, qo], [bk])
                    mm(bk[:], NM[:, kb * 128:(kb + 1) * 128],
                       identb[:].unsqueeze(1).broadcast_to([128, 4, 128]), False, True, [NM, identb], [bk])
                    act(pt[:], bk[:], AF.Exp, [bk], [pt], scale=0.125)

                def PV(u):
                    kb, j = units[u]
                    pt = pT[u % 3]
                    for hh in range(4):
                        pair = (j % 2) * 4 + hh
                        head = pair * 2 + (0 if j < 2 else 1)
                        ob = B[4 + head // 7]
                        off = (head % 7) * 65
                        mm(ob[:, off:off + 65], pt[:, hh * 128:(hh + 1) * 128], vaug[:, kb, :], False,
                           kb == n, [pt, vaug], [ob], skip=True)

                QK(0)
                for u in range(len(units)):
                    if u + 1 < len(units):
                        QK(u + 1)
                    PV(u)
                for b3 in range(3):
                    nh = 7 if b3 < 2 else 2
                    ov = B[4 + b3][:, 0:nh * 65].rearrange("p (h d) -> p h d", d=65)
                    S.op("vector", (lambda ov, b3, nh: lambda e: e.reciprocal(
                        rden[:, b3 * 7:b3 * 7 + nh].unsqueeze(2), ov[:, :, 64:65]))(ov, b3, nh), [B[4 + b3]], [rden])
                    tt(yb[:, b3 * 448:b3 * 448 + nh * 64].rearrange("p (h d) -> p h d", d=64), ov[:, :, 0:64],
                       rden[:, b3 * 7:b3 * 7 + nh].unsqueeze(2).broadcast_to([128, nh, 64]), ALU.mult,
                       [B[4 + b3], rden], [yb])
                act(junk[:, 0:1024], yb[:], AF.Square, [yb], [junk, col[0]], acc=col[0][:, 0:1])
                rsqrt_col(col[1][:, 0:1], col[0][:, 0:1], 1.0 / 1024, [col[0]], [col[1]], col[2])
                ts(ybn[:], yb[:], col[1][:, 0:1], ALU.mult, [yb, col[1]], [ybn])
                for k in range(16):
                    bk = B[2 + k // 8]
                    srcy = yanl[:, k * 128:(k + 1) * 128] if k < 8 else ybn[:, (k - 8) * 128:(k - 7) * 128]
                    tr(bk[:].bitcast(BF16)[:, (k % 8) * 128:(k % 8 + 1) * 128], srcy, identb[:],
                       [yanl, ybn, identb], [bk])
                cp(yT[:, 0:8, :], B[2][:].bitcast(BF16).rearrange("p (a t) -> p a t", t=128), [B[2]], [yT.sub[0]])
                cp(yT[:, 8:16, :], B[3][:].bitcast(BF16).rearrange("p (a t) -> p a t", t=128), [B[3]], [yT.sub[1]],
                   eng="scalar")
                for db in range(4):
                    bk = B[(7, 4, 5, 6)[db]]
                    for k in range(16):
                        mm(bk[:], yT[:, k, :], woutb[:, k, db * 512:(db + 1) * 512], k == 0, k == 15,
                           [yT.sub[k // 8], woutb], [bk])
                    tt(xm[:, db * 512:(db + 1) * 512], bk[:], xt[:, db * 512:(db + 1) * 512], ALU.add, [bk, xt], [xm])
                ld(xmid_s.ap()[n * 128:(n + 1) * 128, :], xm[:], [xm], [DV["xmid"]], q="gpsimd")
                act(junk[:], xm[:], AF.Square, [xm], [junk, col[3]], acc=col[3][:, 0:1])
                rsqrt_col(col[4][:, 0:1], col[3][:, 0:1], 1.0 / D, [col[3]], [col[4]], col[5])
                ts(xt[:], xm[:], col[4][:, 0:1], ALU.mult, [xm, col[4]], [xt])
                act(xn2b[:], xm[:], AF.Copy, [xm, col[4]], [xn2b], scale=col[4][:, 0:1])
                for g4 in range(4):
                    bk = B[2 + g4 % 2]
                    for k in range(g4 * 4, g4 * 4 + 4):
                        tr(bk[:, (k % 4) * 128:(k % 4 + 1) * 128], xt[:, k * 128:(k + 1) * 128], ident[:],
                           [xt, ident], [bk])
                    for k in range(g4 * 4, g4 * 4 + 4):
                        if g4 % 2 == 0:
                            ts(h2T[:, k, :], bk[:, (k % 4) * 128:(k % 4 + 1) * 128], pvec[:, 32 + k:33 + k], ALU.mult,
                               [bk, pvec], [h2T.sub[k]], s2=pvec[:, 48 + k:49 + k], op1=ALU.add)
                        else:
                            act(h2T[:, k, :], bk[:, (k % 4) * 128:(k % 4 + 1) * 128], AF.Identity, [bk, pvec],
                                [h2T.sub[k]], bias=pvec[:, 48 + k:49 + k], scale=pvec[:, 32 + k:33 + k])
                for k in range(16):
                    mm(B[7][:, 0:36], h2T[:, k, :], wr[:, k, :], k == 0, k == 15, [h2T.sub[k], wr], [B[7]])
                tt(lg[:], B[7][:, 0:36], bias_bc[:], ALU.add, [B[7], bias_bc], [lg])
                S.op("vector", lambda e: e.tensor_reduce(out=sm[:, 0:1], in_=lg[:, 0:4], axis=AX.X, op=ALU.max), [lg], [sm])
                ts(sm[:, 1:2], sm[:, 0:1], -1.0, ALU.mult, [sm], [sm])
                act(sm[:, 4:8], lg[:, 0:4], AF.Exp, [lg, sm], [sm, col[6]], bias=sm[:, 1:2], scale=1.0,
                    acc=col[6][:, 0:1])
                S.op("vector", lambda e: e.reciprocal(sm[:, 2:3], col[6][:, 0:1]), [col[6]], [sm])
                ts(sm[:, 8:12], lg[:, 0:4], sm[:, 0:1], ALU.is_ge, [lg, sm], [sm], s2=1.0e9, op1=ALU.mult)
                ts(sm[:, 8:12], sm[:, 8:12], -1.0e9, ALU.add, [sm], [sm])
                tt(em[:].rearrange("p (g j) -> p g j", j=8), lg[:, 4:36].rearrange("p (g j) -> p g j", j=8),
                   sm[:, 8:12].unsqueeze(2).broadcast_to([128, 4, 8]), ALU.add, [lg, sm], [em])
                S.op("vector", lambda e: e.max(m8[:], em[:]), [em], [m8])
                S.op("vector", lambda e: e.max_index(i8[:], m8[:], em[:]), [m8, em], [i8])
                tt(sm[:, 12:13], m8[:, 1:2], m8[:, 0:1], ALU.subtract, [m8], [sm])
                act(sm[:, 13:14], sm[:, 12:13], AF.Exp, [sm], [sm])
                ts(sm[:, 13:14], sm[:, 13:14], 1.0, ALU.add, [sm], [sm])
                S.op("vector", lambda e: e.reciprocal(sm[:, 14:15], sm[:, 13:14]), [sm], [sm])
                tt(wgt_all[:, n, 0:1], sm[:, 14:15], sm[:, 2:3], ALU.mult, [sm], [wgt_all])
                tt(wgt_all[:, n, 1:2], sm[:, 2:3], wgt_all[:, n, 0:1], ALU.subtract, [sm, wgt_all], [wgt_all])
                ts(Ab[:], em[:], m8[:, 1:2], ALU.is_ge, [em, m8], [Ab])
                ts(oh0[:], em[:], m8[:, 0:1], ALU.is_ge, [em, m8], [oh0])
                tt(oh1[:], Ab[:], oh0[:], ALU.subtract, [Ab, oh0], [oh1])
                mm(B[4][:, 0:32], LTb[:], Ab[:], True, True, [LTb, Ab], [B[4]])
                tt(posf[:], B[4][:, 0:32], base_bc[:], ALU.add, [B[4], base_bc], [posf])
                mm(B[4][:, 64:96], onesb[:], Ab[:], True, True, [onesb, Ab], [B[4]])
                tt(base_bc[:], B[4][:, 64:96], base_bc[:], ALU.add, [B[4], base_bc, posf], [base_bc])
                cp(eid_all[:, n, :], i8[:, 0:2], [i8], [eid_all])
                for j, oh in enumerate((oh0, oh1)):
                    tt(tmp32[:], posf[:], oh[:], ALU.mult, [posf, oh], [tmp32])
                    S.op("vector", (lambda n, j: lambda e: e.tensor_reduce(out=pos_all[:, n, j:j + 1], in_=tmp32[:],
                                                                            axis=AX.X, op=ALU.add))(n, j),
                         [tmp32], [pos_all])
                ld(xn2_s.ap()[n * 128:(n + 1) * 128, :], xn2b[:], [xn2b], [DV["xn2"]], q="gpsimd")

            stage_A(0)
            for n in range(NT):
                if n + 1 < NT:
                    stage_A(n + 1)
                stage_B(n)
            S.barrier()
            S.emit()

        with contextlib.ExitStack() as ph:
            S.stack = ph
            pa = S.sb("pa", [128, 32], F32)
            pb = S.sb("pb", [128, 32], F32)
            pi_ = S.sb("pi_", [128, 32], I32)
            padded = S.sb("padded", [128, 32], F32)
            pstart = S.sb("pstart", [128, 32], F32)
            iotI = S.sb("iotI", [128, NSB], I32)
            iotF = S.sb("iotF", [128, NSB], F32)
            cmp3 = S.sb("cmp3", [128, NSB, 32], F32)
            bef = S.sb("bef", [128, NSB], F32)
            iw12 = S.sb("iw12", [128, 12], I32)
            iw12f = S.sb("iw12f", [128, 12], F32)
            widxf = S.sb("widxf", [128, NSB, 12], F32)
            ohd = S.sb("ohd", [128, 32], F32)
            dcol = S.sb("dcol", [128, 2], F32)
            xr2 = [S.sb(f"xr2_{i}", [128, D], BF16) for i in range(2)]
            ts(pa[:], base_bc[:], float(C - 1), ALU.add, [base_bc], [pa], s2=1.0 / C, op1=ALU.mult)
            cp(pi_[:], pa[:], [pa], [pi_])
            cp(pb[:], pi_[:], [pi_], [pb])
            tt(pa[:], pb[:], pa[:], ALU.is_gt, [pb, pa], [pa])
            tt(pb[:], pb[:], pa[:], ALU.subtract, [pb, pa], [pb])
            ts(padded[:], pb[:], float(C), ALU.mult, [pb], [padded])
            cp(pa[:], padded[:], [padded], [pa])
            src_, dst_ = pa, pb
            for sh in (1, 2, 4, 8, 16):
                cp(dst_[:, 0:sh], src_[:, 0:sh], [src_], [dst_])
                tt(dst_[:, sh:32], src_[:, sh:32], src_[:, 0:32 - sh], ALU.add, [src_], [dst_])
                src_, dst_ = dst_, src_
            pend = src_
            tt(pstart[:], pend[:], padded[:], ALU.subtract, [pend, padded], [pstart])
            S.op("gpsimd", lambda e: e.iota(iotI[:], [[C, NSB]], base=0, channel_multiplier=0), [], [iotI])
            cp(iotF[:], iotI[:], [iotI], [iotF])
            tt(cmp3[:], pend[:].unsqueeze(1).broadcast_to([128, NSB, 32]),
               iotF[:].unsqueeze(2).broadcast_to([128, NSB, 32]), ALU.is_le, [pend, iotF], [cmp3])
            S.op("vector", lambda e: e.tensor_reduce(out=bef[:], in_=cmp3[:], axis=AX.X, op=ALU.add), [cmp3], [bef])
            ts(bef[:], bef[:], 31.0, ALU.min, [bef], [bef], s2=1536.0, op1=ALU.mult)
            S.op("gpsimd", lambda e: e.iota(iw12[:], [[128, 12]], base=0, channel_multiplier=1), [], [iw12])
            cp(iw12f[:], iw12[:], [iw12], [iw12f])
            tt(widxf[:], bef[:].unsqueeze(2).broadcast_to([128, NSB, 12]),
               iw12f[:].unsqueeze(1).broadcast_to([128, NSB, 12]), ALU.add, [bef, iw12f], [widxf])
            cp(widx[:], widxf[:], [widxf], [widx])
            S.op("gpsimd", lambda e: e.iota(pi_[:], [[1, 32]], base=0, channel_multiplier=0), [pi_], [pi_])
            cp(pa[:], pi_[:], [pi_], [pa])
            for n in range(NT):
                for j in range(2):
                    ts(ohd[:], pa[:], eid_all[:, n, j:j + 1], ALU.is_equal, [pa, eid_all], [ohd])
                    tt(ohd[:], ohd[:], pstart[:], ALU.mult, [ohd, pstart], [ohd])
                    S.op("vector", (lambda j: lambda e: e.tensor_reduce(out=dcol[:, j:j + 1], in_=ohd[:], axis=AX.X,
                                                                         op=ALU.add))(j), [ohd], [dcol])
                tt(dcol[:], dcol[:], pos_all[:, n, :], ALU.add, [dcol, pos_all], [dcol])
                cp(dest_all[:, n, :], dcol[:], [dcol], [dest_all])
                xr = xr2[n % 2]
                ld(xr[:], xn2_s.ap()[n * 128:(n + 1) * 128, :], [DV["xn2"]], [xr])
                for j in range(2):
                    S.dma("gpsimd", (lambda n, j, xr: lambda e: e.indirect_dma_start(
                        out=xe_s.ap(), out_offset=bass.IndirectOffsetOnAxis(ap=dest_all[:, n, j:j + 1], axis=0),
                        in_=xr[:], in_offset=None, bounds_check=r_slots, oob_is_err=False))(n, j, xr),
                        [xr, dest_all, DV["xe"]], [DV["xe"]], owner=xr)
            S.barrier()
            S.emit()

        with contextlib.ExitStack() as ph:
            S.stack = ph
            NB = C // 128
            stg = [S.sb(f"stgE{i}", [128, 4096], F32) for i in range(4)]
            wpb = [S.subs(S.sb(f"wpb{i}", [128, 4096], BF16), 2, "p3") for i in range(3)]
            xer1 = [S.sb(f"xer_{b}", [128, D], BF16) for b in range(NB)]
            xer = [xer1, xer1]
            xeT = S.subs(S.sb("xeT", [128, 16, C], BF16), 16, "p3")
            gTt = S.sb("gTt", [128, 8, C], BF16)
            sa = [S.sb(f"sa{i}", [128, C], F32) for i in range(2)]
            yo = [S.subs(S.sb(f"yo{i}", [128, D], F32), 4) for i in range(NB)]
            wexp_rows = wexp_d.ap().rearrange("e q p c -> (e q p) c")
            pieces = [(j, p) for j in range(NSB) for p in range(12)]
            rot = [0]

            def nbank():
                bk = B[2 + rot[0] % 6]
                rot[0] += 1
                return bk

            def emit_gather(i):
                j, piece = pieces[i]
                sg = stg[i % 4]
                S.dma("gpsimd", (lambda sg, j, piece: lambda e: e.indirect_dma_start(
                    out=sg[:], out_offset=None, in_=wexp_rows,
                    in_offset=bass.IndirectOffsetOnAxis(ap=widx[:, j, piece:piece + 1], axis=0),
                    bounds_check=r_wrows, oob_is_err=False))(sg, j, piece), [DV["in"], widx], [sg], owner=sg)

            def emit_cast(i):
                sg = stg[i % 4]
                wb = wpb[i % 3]
                cp(wb[:, 0:2048], sg[:, 0:2048], [sg], [wb.sub[0]], eng="vector")
                cp(wb[:, 2048:4096], sg[:, 2048:4096], [sg], [wb.sub[1]], eng="scalar")

            def load_xe(j):
                for blk in range(NB):
                    xr = xer[j % 2][blk]
                    ld(xr[:], xe_s.ap()[j * C + blk * 128:j * C + (blk + 1) * 128, :], [DV["xe"]], [xr])

            def prologue_part(j, part):
                for kp in (2 * part, 2 * part + 1):
                    bk = B[kp % 2]
                    bv = bk[:].bitcast(BF16)
                    for kk in range(2):
                        k = kp * 2 + kk
                        for blk in range(NB):
                            xr = xer[j % 2][blk]
                            tr(bv[:, (kk * NB + blk) * 128:(kk * NB + blk + 1) * 128], xr[:, k * 128:(k + 1) * 128],
                               identb[:], [xr, identb], [bk])
                    for kk in range(2):
                        k = kp * 2 + kk
                        src = bv[:, kk * C:(kk + 1) * C]
                        if kp % 2 == 0:
                            ts(xeT[:, k, :], src, pvec[:, 32 + k:33 + k], ALU.mult, [bk, pvec], [xeT.sub[k]],
                               s2=pvec[:, 48 + k:49 + k], op1=ALU.add)
                        else:
                            act(xeT[:, k, :], src, AF.Identity, [bk, pvec], [xeT.sub[k]],
                                bias=pvec[:, 48 + k:49 + k], scale=pvec[:, 32 + k:33 + k])

            yoi = [0]

            def compute(i):
                j, piece = pieces[i]
                wb = wpb[i % 3]
                if piece < 8:
                    f = piece
                    w13 = wb[:].rearrange("p (t k c) -> p t k c", t=2, k=16)
                    ba, bb = nbank(), nbank()
                    for k in range(16):
                        mm(ba[:, 0:C], w13[:, 0, k, :], xeT[:, k, :], k == 0, k == 15, [wb.sub[0], xeT.sub[k]], [ba])
                    for k in range(16):
                        mm(bb[:, 0:C], w13[:, 1, k, :], xeT[:, k, :], k == 0, k == 15, [wb.sub[1], xeT.sub[k]], [bb])
                    s_ = sa[f % 2]
                    act(s_[:], ba[:, 0:C], AF.Silu, [ba], [s_])
                    tt(gTt[:, f, :], bb[:, 0:C], s_[:], ALU.mult, [bb, s_], [gTt])
                else:
                    db = piece - 8
                    w2v = wb[:].rearrange("p (k c) -> p k c", k=8)
                    for blk in range(NB):
                        bk = nbank()
                        for fc in range(8):
                            mm(bk[:], gTt[:, fc, blk * 128:(blk + 1) * 128], w2v[:, fc, :], fc == 0, fc == 7,
                               [gTt, wb.sub[fc // 4]], [bk])
                        y_ = yo[blk]
                        if blk % 2 == 0:
                            cp(y_[:, db * 512:(db + 1) * 512], bk[:], [bk], [y_.sub[db]])
                        else:
                            cp(y_[:, db * 512:(db + 1) * 512], bk[:], [bk], [y_.sub[db]], eng="scalar")
                        if db == 3:
                            ld(ye_s.ap()[j * C + blk * 128:j * C + (blk + 1) * 128, :], y_[:],
                               list(y_.sub), [DV["ye"]], q="sync", owner=y_)

            load_xe(0)
            emit_gather(0)
            emit_gather(1)
            emit_gather(2)
            emit_cast(0)
            for part in range(4):
                prologue_part(0, part)
            if NSB > 1:
                load_xe(1)
            for i, (j, piece) in enumerate(pieces):
                if piece == 0 and j >= 1 and j + 1 < NSB:
                    load_xe(j + 1)
                if i + 3 < len(pieces):
                    emit_gather(i + 3)
                if i + 1 < len(pieces):
                    emit_cast(i + 1)
                compute(i)
                if piece >= 8 and j + 1 < NSB:
                    prologue_part(j + 1, piece - 8)
            S.barrier()
            S.emit()

        with contextlib.ExitStack() as ph:
            S.stack = ph
            gate2_bc = S.sb("gate2_bc", [128, D], F32)
            fg_bc = S.sb("fg_bc", [128, D], F32)
            y0 = [S.sb(f"y0_{i}", [128, D], F32) for i in range(2)]
            y1 = [S.sb(f"y1_{i}", [128, D], F32) for i in range(2)]
            xmt = [S.sb(f"xmt{i}", [128, D], F32) for i in range(2)]
            acc = S.sb("acc", [128, D], F32)
            ot = [S.sb(f"ot{i}", [128, D], F32) for i in range(2)]
            ld(gate2_bc[:], mod_s.ap()[5 * D:6 * D].partition_broadcast(128), [DV["mod"]], [gate2_bc])
            ld(fg_bc[:], fg_d.ap().partition_broadcast(128), [DV["in"]], [fg_bc])
            for n in range(NT):
                a0, a1, xq, o_ = y0[n % 2], y1[n % 2], xmt[n % 2], ot[n % 2]
                for j, dst in enumerate((a0, a1)):
                    S.dma("gpsimd", (lambda n, j, dst: lambda e: e.indirect_dma_start(
                        out=dst[:], out_offset=None, in_=ye_s.ap(),
                        in_offset=bass.IndirectOffsetOnAxis(ap=dest_all[:, n, j:j + 1], axis=0),
                        bounds_check=r_slots, oob_is_err=False))(n, j, dst),
                        [DV["ye"], dest_all], [dst], owner=dst)
                ld(xq[:], xmid_s.ap()[n * 128:(n + 1) * 128, :], [DV["xmid"]], [xq])
                act(acc[:], a0[:], AF.Copy, [a0, wgt_all], [acc], scale=wgt_all[:, n, 0:1])
                stt(acc[:], a1[:], wgt_all[:, n, 1:2], acc[:], ALU.mult, ALU.add, [a1, wgt_all, acc], [acc])
                tt(acc[:], acc[:], gate2_bc[:], ALU.mult, [acc, gate2_bc], [acc], eng="gpsimd")
                tt(acc[:], acc[:], xq[:], ALU.add, [acc, xq], [acc])
                act(junk[:], acc[:], AF.Square, [acc], [junk, col[0]], acc=col[0][:, 0:1])
                rsqrt_col(col[1][:, 0:1], col[0][:, 0:1], 1.0 / D, [col[0]], [col[1]], col[2])
                stt(o_[:], acc[:], col[1][:, 0:1], fg_bc[:], ALU.mult, ALU.mult, [acc, col[1], fg_bc], [o_])
                ld(out_d.ap()[n * 128:(n + 1) * 128, :], o_[:], [o_], [DV["out"]], q="sync")
            S.barrier()
            S.emit()
    return nc


_CACHE = {}


def _prep_weights(inp):
    f = lambda a: np.ascontiguousarray(np.asarray(a, dtype=np.float32))
    w1 = np.asarray(inp["w1"], dtype=np.float32)[0]
    w3 = np.asarray(inp["w3"], dtype=np.float32)[0]
    w2 = np.asarray(inp["w2"], dtype=np.float32)[0]
    wexp = np.empty((NEXP, 12, 128, 4096), dtype=np.float32)
    a1 = w1.reshape(NEXP, 16, 128, 8, 128).transpose(0, 3, 2, 1, 4)
    a3 = w3.reshape(NEXP, 16, 128, 8, 128).transpose(0, 3, 2, 1, 4)
    v = wexp[:, 0:8].reshape(NEXP, 8, 128, 2, 16, 128)
    v[:, :, :, 0] = a1
    v[:, :, :, 1] = a3
    a2 = w2.reshape(NEXP, 8, 128, 4, 512).transpose(0, 3, 2, 1, 4)
    wexp[:, 8:12] = a2.reshape(NEXP, 4, 128, 4096)
    shared = {
        "w_ada": f(inp["w_ada"][0]), "b_ada": f(inp["b_ada"][0]), "norm1_g": f(inp["norm1_g"][0]),
        "w_in": f(inp["w_in"][0]), "v_norm_g": f(inp["v_norm_g"][0]).reshape(-1),
        "v_norm_b": f(inp["v_norm_b"][0]).reshape(-1), "w_sp": f(inp["w_sp"][0]), "b_sp": f(inp["b_sp"][0]),
        "kv_norm_g": f(inp["kv_norm_g"][0]), "w_uk": f(inp["w_uk"][0]), "w_uv": f(inp["w_uv"][0]),
        "kidx_norm_g": f(inp["kidx_norm_g"][0]), "gnorm_a_g": f(inp["gnorm_a_g"][0]),
        "gnorm_b_g": f(inp["gnorm_b_g"][0]), "w_out": f(inp["w_out"][0]), "norm2_g": f(inp["norm2_g"][0]),
        "w_r": np.ascontiguousarray(np.concatenate([np.asarray(inp["w_group"][0]), np.asarray(inp["w_expert"][0])],
                                                   axis=1).astype(np.float32)),
        "b_r": np.ascontiguousarray(np.concatenate([np.asarray(inp["b_group"][0]), np.asarray(inp["b_expert"][0])],
                                                   axis=0).astype(np.float32)),
        "wexp": wexp, "final_g": f(inp["final_g"]),
    }
    return shared


def kernel(**inputs):
    x = np.asarray(inputs["x"], dtype=np.float32)
    c = np.asarray(inputs["c"], dtype=np.float32)
    pos = np.asarray(inputs["positions"], dtype=np.int32)
    nb, seq, _ = x.shape
    NT = seq // 128
    key = (NT,)
    if key not in _CACHE:
        _CACHE[key] = build(NT=NT)
    nc = _CACHE[key]
    shared = _prep_weights(inputs)
    in_maps = []
    for b in range(nb):
        m = dict(shared)
        m["x"] = np.ascontiguousarray(x[b])
        m["c"] = np.ascontiguousarray(c[b])
        m["pos"] = np.ascontiguousarray(pos[b])
        in_maps.append(m)
    res = run_bass_kernel_spmd(nc, in_maps, core_ids=list(range(nb)))
    return np.stack([np.asarray(r["out"], dtype=np.float32) for r in res.results], axis=0)
```

```python
import contextlib
import math
import numpy as np
import concourse.bass as bass
import concourse.mybir as mybir
from concourse.bass_utils import run_bass_kernel_spmd

F32 = mybir.dt.float32
BF16 = mybir.dt.bfloat16
I32 = mybir.dt.int32
U32 = mybir.dt.uint32
AF = mybir.ActivationFunctionType
ALU = mybir.AluOpType
AX = mybir.AxisListType

FLAGS = {"hT": 1, "p3": 1, "yT": 1, "reorder": 1}
EPOCH = 12000
ENGS = ["tensor", "vector", "scalar", "gpsimd", "sync"]
D = 2048
NEXP = 32
EPS = 1e-6


class Buf:
    __slots__ = ("name", "w", "r", "dsem", "dcum", "t", "multi", "sub")

    def __init__(self, name, t=None):
        self.name = name
        self.multi = False
        self.w = {}
        self.r = {}
        self.dsem = None
        self.dcum = 0
        self.t = t

    def __getitem__(self, k):
        return self.t[k]


class Sched:
    def __init__(self, nc, semstack):
        self.nc = nc
        self.semstack = semstack
        self.stack = semstack
        self.ops = {e: [] for e in ENGS}
        self.cnt = {e: 0 for e in ENGS}
        self.sems = {}
        self.waited = {e: {} for e in ENGS}
        self.cur_cum = {}
        self.nsem = 0
        self.bufs = []

    def _newsem(self, name):
        self.nsem += 1
        return self.semstack.enter_context(self.nc.semaphore(f"{name}_{self.nsem}"))

    def sb(self, name, shape, dtype):
        t = self.stack.enter_context(self.nc.sbuf_tensor(name, list(shape), dtype))
        b = Buf(name, t)
        self.bufs.append(b)
        return b

    def ps(self, name, shape, dtype):
        t = self.stack.enter_context(self.nc.psum_tensor(name, list(shape), dtype))
        b = Buf(name, t)
        self.bufs.append(b)
        return b

    def subs(self, buf, n, flag=None):
        buf.sub = []
        if flag is not None and not FLAGS.get(flag, 1):
            buf.sub = [buf] * n
            return buf
        for i in range(n):
            b = Buf(f"{buf.name}_s{i}", buf.t)
            self.bufs.append(b)
            buf.sub.append(b)
        return buf

    def view(self, name):
        b = Buf(name, None)
        b.multi = True
        self.bufs.append(b)
        return b

    def _engkey(self, eng):
        idx = self.cnt[eng]
        ep = idx // EPOCH
        key = ("E", eng, ep)
        if key not in self.sems:
            self.sems[key] = self._newsem(f"e_{eng}_{ep}")
        return key, (idx % EPOCH) + 1

    def _collect(self, eng, reads, writes):
        deps = {}

        def add(d):
            for k, v in d.items():
                if k[0] == "D":
                    v = max(v, self.cur_cum.get(k, v))
                if deps.get(k, 0) < v:
                    deps[k] = v
        for b in reads:
            add(b.w)
        for b in writes:
            if not b.multi:
                add(b.w)
            add(b.r)
        out = []
        wd = self.waited[eng]
        for k, v in deps.items():
            if k[0] == "E" and k[1] == eng and eng in ("tensor", "sync"):
                continue
            if wd.get(k, 0) >= v:
                continue
            wd[k] = v
            out.append((k, v))
        return out

    def _record(self, me, reads, writes):
        k, v = me
        for b in writes:
            if b.multi:
                if b.w.get(k, 0) < v:
                    b.w[k] = v
            else:
                b.w = {k: v}
                b.r = {}
        for b in reads:
            if b.r.get(k, 0) < v:
                b.r[k] = v

    def op(self, eng, fn, reads=(), writes=()):
        waits = self._collect(eng, reads, writes)
        key, val = self._engkey(eng)
        self.ops[eng].append((waits, fn, key, 1))
        self.cnt[eng] += 1
        self._record((key, val), reads, writes)

    def dma(self, queue, fn, reads=(), writes=(), owner=None):
        waits = self._collect(queue, reads, writes)
        if owner is None:
            owner = (list(writes) + list(reads))[0]
        if owner.dsem is None or owner.dcum + 16 > EPOCH * 2:
            key = ("D", id(owner), self.nsem)
            self.sems[key] = self._newsem("d_" + owner.name)
            owner.dsem = key
            owner.dcum = 0
        owner.dcum += 16
        key = owner.dsem
        self.cur_cum[key] = owner.dcum
        self.ops[queue].append((waits, fn, key, 16))
        self._record((key, owner.dcum), reads, writes)

    def raw(self, eng, fn, reads=()):
        waits = self._collect(eng, reads, [])
        self.ops[eng].append((waits, fn, "RAW", 0))

    def barrier(self):
        allk = {}
        for e in ENGS:
            if self.cnt[e] > 0:
                idx = self.cnt[e] - 1
                allk[("E", e, idx // EPOCH)] = (idx % EPOCH) + 1
        for k, v in self.cur_cum.items():
            allk[k] = v
        for e in ENGS:
            wd = self.waited[e]
            ws = []
            for k, v in allk.items():
                if wd.get(k, 0) >= v:
                    continue
                wd[k] = v
                if k[0] == "E" and k[1] == e:
                    continue
                ws.append((k, v))
            if ws:
                self.ops[e].append((ws, None, None, 0))
        for b in self.bufs:
            b.w = {}
            b.r = {}

    def emit(self):
        nc = self.nc
        with nc.Block() as block:
            for e in ENGS:
                ops = self.ops[e]
                if not ops:
                    continue

                def body(eng, ops=ops):
                    for waits, fn, key, inc in ops:
                        for k, v in waits:
                            eng.wait_ge(self.sems[k], v)
                        if fn is None:
                            continue
                        if key == "RAW":
                            fn(eng)
                        else:
                            fn(eng).then_inc(self.sems[key], inc)
                getattr(block, e)(body)
        self.ops = {e: [] for e in ENGS}


def build(NT=32, NITER=16, dbg=False):
    ST = NT * 128
    C = 512
    NSB = (2 * ST + C - 1) // C + NEXP
    nc = bass.Bass("TRN2", target_bir_lowering=False)

    def din(name, shape, dt=F32):
        return nc.dram_tensor(name, list(shape), dt, kind="ExternalInput")

    def dscr(name, shape, dt=F32):
        if dbg:
            return nc.dram_tensor(name, list(shape), dt, kind="ExternalOutput")
        return nc.dram_tensor(name, list(shape), dt)

    x_d = din("x", [ST, D])
    c_d = din("c", [D])
    pos_d = din("pos", [ST], I32)
    wada_d = din("w_ada", [D, 6 * D])
    bada_d = din("b_ada", [6 * D])
    n1g_d = din("norm1_g", [D])
    win_d = din("w_in", [D, 3912])
    vng_d = din("v_norm_g", [1024])
    vnb_d = din("v_norm_b", [1024])
    wsp_d = din("w_sp", [8, 128, 128])
    bsp_d = din("b_sp", [8, 128])
    kvg_d = din("kv_norm_g", [256])
    wuk_d = din("w_uk", [256, 64])
    wuv_d = din("w_uv", [256, 64])
    kig_d = din("kidx_norm_g", [64])
    gna_d = din("gnorm_a_g", [1024])
    gnb_d = din("gnorm_b_g", [1024])
    wout_d = din("w_out", [D, D])
    n2g_d = din("norm2_g", [D])
    wr_d = din("w_r", [D, 36])
    br_d = din("b_r", [36])
    wexp_d = din("wexp", [NEXP, 12, 128, 4096])
    fg_d = din("final_g", [D])
    out_d = nc.dram_tensor("out", [ST, D], F32, kind="ExternalOutput")

    mod_s = dscr("mod_s", [6 * D])
    ya_s = dscr("ya_s", [ST, 1024], BF16)
    qTe_s = dscr("qTe_s", [NT, 128, 1024], BF16)
    qTo_s = dscr("qTo_s", [NT, 128, 1024], BF16)
    qiTe_s = dscr("qiTe_s", [NT, 128, 512], BF16)
    qiTo_s = dscr("qiTo_s", [NT, 128, 512], BF16)
    xmid_s = dscr("xmid_s", [ST, D])
    xn2_s = dscr("xn2_s", [ST, D], BF16)
    xe_s = dscr("xe_s", [NSB * C, D], BF16)
    ye_s = dscr("ye_s", [NSB * C, D])

    with contextlib.ExitStack() as outer:
        S = Sched(nc, outer)
        B = [S.ps(f"B{i}", [128, 512], F32) for i in range(8)]
        DV = {n: S.view("dv_" + n) for n in
              ["in", "mod", "ya", "qTe", "qTo", "qiTe", "qiTo", "xmid", "xe", "ye", "out", "xn2"]}

        def mm(o, l, r, st, sp, R, W, skip=False):
            S.op("tensor", lambda e: e.matmul(o, l, r, start=st, stop=sp, skip_group_check=skip), R, W)

        def tr(o, i, idn, R, W):
            S.op("tensor", lambda e: e.transpose(o, i, idn), R, W)

        def act(o, i, f, R, W, bias=None, scale=None, acc=None):
            kw = {}
            if bias is not None:
                kw["bias"] = bias
            if scale is not None:
                kw["scale"] = scale
            if acc is not None:
                kw["accum_out"] = acc
            S.op("scalar", lambda e: e.activation(out=o, in_=i, func=f, **kw), R, W)

        def ts(o, i, s1, op0, R, W, s2=None, op1=None, eng="vector", acc=None):
            if acc is not None:
                S.op(eng, lambda e: e.tensor_scalar(o, i, s1, s2, op0, op1, accum_out=acc), R, W)
            elif op1 is None:
                S.op(eng, lambda e: e.tensor_scalar(o, i, s1, None, op0), R, W)
            else:
                S.op(eng, lambda e: e.tensor_scalar(o, i, s1, s2, op0, op1), R, W)

        def tt(o, a, b, op, R, W, eng="vector"):
            S.op(eng, lambda e: e.tensor_tensor(out=o, in0=a, in1=b, op=op), R, W)

        def stt(o, a, s, b, op0, op1, R, W):
            S.op("vector", lambda e: e.scalar_tensor_tensor(out=o, in0=a, scalar=s, in1=b, op0=op0, op1=op1), R, W)

        def cp(o, i, R, W, eng="vector"):
            if eng == "scalar":
                S.op("scalar", lambda e: e.copy(o, i), R, W)
            else:
                S.op(eng, lambda e: e.tensor_copy(o, i), R, W)

        def mset(ap, val, W, eng="vector"):
            S.op(eng, lambda e: e.memset(ap, val), [], W)

        def ld(o, i, R, W, q="sync", owner=None):
            S.dma(q, lambda e: e.dma_start(out=o, in_=i), R, W, owner=owner)

        def rsqrt_col(dst, src, scale, R, W, tmp):
            w_ = src.shape[-1]
            ts(tmp[:, 0:w_], src, scale, ALU.mult, R, [tmp], s2=EPS, op1=ALU.add)
            act(tmp[:, 0:w_], tmp[:, 0:w_], AF.Sqrt, [tmp], [tmp])
            S.op("vector", lambda e: e.reciprocal(dst, tmp[:, 0:w_]), [tmp], W)

        ident = S.sb("ident", [128, 128], F32)
        identb = S.sb("identb", [128, 128], BF16)
        onesf = S.sb("onesf", [128, 128], F32)
        onesb = S.sb("onesb", [128, 128], BF16)
        zerob = S.sb("zerob", [128, 512], BF16)
        LTb = S.sb("LTb", [128, 128], BF16)
        pvec = S.sb("pvec", [128, 64], F32)
        gT = S.sb("gT", [128, 58], F32)
        kT2 = S.sb("kT2", [128, ST], BF16)
        kiT2 = S.sb("kiT2", [128, ST], BF16)
        vaug = S.sb("vaug", [128, NT, 65], BF16)
        sgn_all = S.sb("sgn_all", [128, NT, 8], F32)
        dest_all = S.sb("dest_all", [128, NT, 2], I32)
        eid_all = S.sb("eid_all", [128, NT, 2], F32)
        pos_all = S.sb("pos_all", [128, NT, 2], F32)
        base_bc = S.sb("base_bc", [128, 32], F32)
        widx = S.sb("widx", [128, NSB, 12], I32)
        wgt_all = S.sb("wgt_all", [128, NT, 2], F32)
        junk = S.sb("junk", [128, 2048], BF16)
        col = [S.sb(f"col{i}", [128, 16], F32) for i in range(8)]

        mset(onesf[:], 1.0, [onesf], eng="gpsimd")
        mset(onesb[:], 1.0, [onesb], eng="gpsimd")
        mset(zerob[:], 0.0, [zerob], eng="gpsimd")
        S.op("gpsimd", lambda e: e.affine_select(out=ident[:], in_=onesf[:], pattern=[[1, 128]],
                                                 compare_op=ALU.is_equal, fill=0.0, base=0,
                                                 channel_multiplier=-1), [onesf], [ident])
        S.op("gpsimd", lambda e: e.affine_select(out=identb[:], in_=onesf[:], pattern=[[1, 128]],
                                                 compare_op=ALU.is_equal, fill=0.0, base=0,
                                                 channel_multiplier=-1), [onesf], [identb])
        S.op("gpsimd", lambda e: e.affine_select(out=LTb[:], in_=onesf[:], pattern=[[1, 128]],
                                                 compare_op=ALU.is_ge, fill=0.0, base=-1,
                                                 channel_multiplier=-1), [onesf], [LTb])
        mset(vaug[:, :, 64:65], 1.0, [vaug], eng="gpsimd")
        mset(dest_all[:], 0, [dest_all], eng="gpsimd")
        mset(widx[:], 0, [widx], eng="gpsimd")
        r_slots = outer.enter_context(nc.gpsimd.register("r_slots"))
        r_wrows = outer.enter_context(nc.gpsimd.register("r_wrows"))
        S.raw("gpsimd", lambda e: e.reg_mov(r_slots, NSB * C - 1))
        S.raw("gpsimd", lambda e: e.reg_mov(r_wrows, NEXP * 12 * 128 - 1))

        with contextlib.ExitStack() as ph:
            S.stack = ph
            stA = S.sb("stA", [112, 128], F32)
            stB = S.sb("stB", [58, 128], F32)
            siluc = S.sb("siluc", [128, 16], F32)
            cT = S.sb("cT", [128, 16], F32)
            badaT = S.sb("badaT", [128, 96], F32)
            modT = S.sb("modT", [128, 96], F32)
            modR = S.sb("modR", [96, 128], F32)
            wa = [S.sb(f"wa{i}", [128, 16, 512], F32) for i in range(2)]
            modrow = S.sb("modrow", [1, 6 * D], F32)

            ld(stA[0:16, :], c_d.ap().rearrange("(k p) -> k p", p=128), [DV["in"]], [stA])
            ld(stA[16:112, :], bada_d.ap().rearrange("(k p) -> k p", p=128), [DV["in"]], [stA])
            r0 = 0
            for src, nr in [(n1g_d, 16), (n2g_d, 16), (gna_d, 8), (gnb_d, 8), (kvg_d, 2)]:
                ld(stB[r0:r0 + nr, :], src.ap().rearrange("(k p) -> k p", p=128), [DV["in"]], [stB])
                r0 += nr
            ld(stB[50:58, :], bsp_d.ap(), [DV["in"]], [stB])
            tr(B[0][:, 0:112], stA[0:112, :], ident[0:112, 0:112], [stA, ident], [B[0]])
            tr(B[1][:, 0:58], stB[0:58, :], ident[0:58, 0:58], [stB, ident], [B[1]])
            cp(cT[:], B[0][:, 0:16], [B[0]], [cT])
            act(siluc[:], cT[:], AF.Silu, [cT], [siluc])
            cp(badaT[:], B[0][:, 16:112], [B[0]], [badaT])
            cp(gT[:], B[1][:, 0:58], [B[1]], [gT])
            for cb in range(24):
                w = wa[cb % 2]
                ld(w[:], wada_d.ap().rearrange("(k p) c -> p k c", p=128)[:, :, cb * 512:(cb + 1) * 512],
                   [DV["in"]], [w])
                rb = B[2 + cb % 2]
                for k in range(16):
                    mm(rb[0:1, :], siluc[:, k:k + 1], w[:, k, :], k == 0, k == 15, [w, siluc], [rb])
                cp(modrow[0:1, cb * 512:(cb + 1) * 512], rb[0:1, :], [rb], [modrow])
            for j in range(96):
                mm(B[4][:, j:j + 1], modrow[0:1, j * 128:(j + 1) * 128], onesf[0:1, 0:1], True, True,
                   [modrow, onesf], [B[4]])
            tt(modT[:], B[4][:, 0:96], badaT[:], ALU.add, [B[4], badaT], [modT])
            stt(pvec[:, 0:16], modT[:, 16:32], 1.0, gT[:, 0:16], ALU.add, ALU.mult, [modT, gT], [pvec])
            cp(pvec[:, 16:32], modT[:, 0:16], [modT], [pvec])
            stt(pvec[:, 32:48], modT[:, 64:80], 1.0, gT[:, 16:32], ALU.add, ALU.mult, [modT, gT], [pvec])
            cp(pvec[:, 48:64], modT[:, 48:64], [modT], [pvec])
            tr(B[3][0:96, 0:128], modT[:, 0:96], ident[:], [modT, ident], [B[3]])
            cp(modR[:], B[3][0:96, 0:128], [B[3]], [modR])
            ld(mod_s.ap().rearrange("(j p) -> j p", p=128), modR[:], [modR], [DV["mod"]], q="gpsimd")
            S.barrier()
            S.emit()

        def prep_hT(xt, xn, hT, c_ss, c_rs, c_tmp, sc_off, sh_off):
            act(junk[:], xt[:], AF.Square, [xt], [junk, c_ss], acc=c_ss[:, 0:1])
            rsqrt_col(c_rs[:, 0:1], c_ss[:, 0:1], 1.0 / D, [c_ss], [c_rs], c_tmp)
            ts(xn[:], xt[:], c_rs[:, 0:1], ALU.mult, [xt, c_rs], [xn])
            for k in range(16):
                bk = B[k // 8]
                tr(bk[:].bitcast(BF16)[:, (k % 8) * 128:(k % 8 + 1) * 128], xn[:, k * 128:(k + 1) * 128],
                   identb[:], [xn, identb], [bk])
            for k in range(16):
                bk = B[k // 8]
                src = bk[:].bitcast(BF16)[:, (k % 8) * 128:(k % 8 + 1) * 128]
                if k < 8:
                    ts(hT[:, k, :], src, pvec[:, sc_off + k:sc_off + k + 1], ALU.mult, [bk, pvec], [hT.sub[k]],
                       s2=pvec[:, sh_off + k:sh_off + k + 1], op1=ALU.add)
                else:
                    act(hT[:, k, :], src, AF.Identity, [bk, pvec], [hT.sub[k]],
                        bias=pvec[:, sh_off + k:sh_off + k + 1], scale=pvec[:, sc_off + k:sc_off + k + 1])

        def load_w_bf(dst, src_ap_fn, ncols, stg, rowscale=None, colscale=None):
            step = 256
            i = 0
            for c0 in range(0, ncols, step):
                cw = min(step, ncols - c0)
                sg = stg[i % 2]
                ld(sg[:, :, 0:cw], src_ap_fn(c0, cw), [DV["in"]], [sg])
                eng = ["vector", "gpsimd"][i % 2]
                if rowscale is None:
                    if i % 3 == 2:
                        cp(dst[:, :, c0:c0 + cw], sg[:, :, 0:cw], [sg], [dst], eng="scalar")
                    else:
                        cp(dst[:, :, c0:c0 + cw], sg[:, :, 0:cw], [sg], [dst], eng=eng)
                else:
                    for k in range(16):
                        stt(dst[:, k, c0:c0 + cw], sg[:, k, 0:cw], rowscale[:, k:k + 1], colscale[:, c0:c0 + cw],
                            ALU.mult, ALU.mult, [sg] + rowscale_bufs, [dst])
                i += 1

        rowscale_bufs = []

        with contextlib.ExitStack() as ph:
            S.stack = ph
            wbf = S.sb("wbfA", [128, 16, 2048], BF16)
            stg = [S.sb(f"stgA{i}", [128, 16, 256], F32) for i in range(2)]
            xts = [S.sb(f"xtA{i}", [128, D], F32) for i in range(2)]
            xns = [S.sb(f"xnA{i}", [128, D], BF16) for i in range(2)]
            hTs = [S.subs(S.sb(f"hTA{i}", [128, 16, 128], BF16), 16, "hT") for i in range(2)]
            gu = S.sb("gu", [128, 1024], F32)
            gv = S.sb("gv", [128, 1024], F32)
            vn = S.sb("vn", [128, 1024], F32)
            vgb = S.sb("vgb", [128, 1024], BF16)
            Gbc = S.sb("Gbc", [128, 1024], F32)
            Bbc = S.sb("Bbc", [128, 1024], F32)
            wsp = S.sb("wsp", [128, 8, 128], F32)
            WmT = S.sb("WmT", [128, 8, 128], BF16)
            ya = S.sb("ya", [128, 1024], F32)
            yan = S.sb("yan", [128, 1024], BF16)
            stats = S.sb("stats", [128, 8, 6], F32)
            mv = S.sb("mv", [128, 8, 2], F32)

            win_v = win_d.ap().rearrange("(k p) c -> p k c", p=128)
            load_w_bf(wbf, lambda c0, cw: win_v[:, :, c0:c0 + cw], 2048, stg)
            ld(Gbc[:], vng_d.ap().partition_broadcast(128), [DV["in"]], [Gbc])
            ld(Bbc[:], vnb_d.ap().partition_broadcast(128), [DV["in"]], [Bbc])
            ld(wsp[:], wsp_d.ap().rearrange("g i j -> i g j"), [DV["in"]], [wsp])
            for g in range(8):
                bk = B[6 + g // 4]
                tr(bk[:, (g % 4) * 128:(g % 4 + 1) * 128], wsp[:, g, :], ident[:], [wsp, ident], [bk])
            for g in range(8):
                bk = B[6 + g // 4]
                cp(WmT[:, g, :], bk[:, (g % 4) * 128:(g % 4 + 1) * 128], [bk], [WmT])
            mset(WmT[64:128, :, 0:64], 0.0, [WmT])

            def H_a(n):
                xt = xts[n % 2]
                ld(xt[:], x_d.ap()[n * 128:(n + 1) * 128, :], [DV["in"]], [xt])
                prep_hT(xt, xns[n % 2], hTs[n % 2], col[0], col[1], col[2], 0, 16)

            def M_a(n):
                hT = hTs[n % 2]
                for cg in range(4):
                    for k in range(16):
                        mm(B[2 + cg][:], hT[:, k, :], wbf[:, k, cg * 512:(cg + 1) * 512], k == 0, k == 15,
                           [hT.sub[k], wbf], [B[2 + cg]])

            def E_a(n):
                act(gu[:, 0:512], B[2][:], AF.Gelu_apprx_tanh, [B[2]], [gu])
                act(gu[:, 512:1024], B[3][:], AF.Gelu_apprx_tanh, [B[3]], [gu])
                act(gv[:, 0:512], B[4][:], AF.Gelu_apprx_tanh, [B[4]], [gv])
                act(gv[:, 512:1024], B[5][:], AF.Gelu_apprx_tanh, [B[5]], [gv])
                for g in range(8):
                    S.op("vector", (lambda g: lambda e: e.bn_stats(stats[:, g, :], gv[:, g * 128:(g + 1) * 128]))(g),
                         [gv], [stats])
                for g in range(8):
                    S.op("vector", (lambda g: lambda e: e.bn_aggr(mv[:, g, :], stats[:, g, :]))(g), [stats], [mv])
                ts(col[4][:, 0:8], mv[:, :, 1], EPS, ALU.add, [mv], [col[4]])
                act(col[4][:, 0:8], col[4][:, 0:8], AF.Sqrt, [col[4]], [col[4]])
                S.op("vector", lambda e: e.reciprocal(col[3][:, 0:8], col[4][:, 0:8]), [col[4]], [col[3]])
                for g in range(8):
                    ts(vn[:, g * 128:(g + 1) * 128], gv[:, g * 128:(g + 1) * 128], mv[:, g, 0:1], ALU.subtract,
                       [gv, mv, col[3]], [vn], s2=col[3][:, g:g + 1], op1=ALU.mult)
                tt(vn[:], vn[:], Gbc[:], ALU.mult, [vn, Gbc], [vn], eng="gpsimd")
                tt(vgb[:], vn[:], Bbc[:], ALU.add, [vn, Bbc], [vgb])

            def E2_a(n):
                for g in range(8):
                    bk = B[6 + g // 4]
                    mm(bk[:, (g % 4) * 128:(g % 4 + 1) * 128], WmT[:, g, :], vgb[:, g * 128:(g + 1) * 128],
                       True, True, [WmT, vgb], [bk])
                for g in range(8):
                    bk = B[6 + g // 4]
                    stt(ya[:, g * 128:(g + 1) * 128], bk[:, (g % 4) * 128:(g % 4 + 1) * 128], gT[:, 50 + g:51 + g],
                        gu[:, g * 128:(g + 1) * 128], ALU.add, ALU.mult, [bk, gT, gu], [ya])
                act(junk[:, 0:1024], ya[:], AF.Square, [ya], [junk, col[5]], acc=col[5][:, 0:1])
                rsqrt_col(col[6][:, 0:1], col[5][:, 0:1], 1.0 / 1024, [col[5]], [col[6]], col[7])
                ts(yan[:], ya[:], col[6][:, 0:1], ALU.mult, [ya, col[6]], [yan])
                ld(ya_s.ap()[n * 128:(n + 1) * 128, :], yan[:], [yan], [DV["ya"]], q="gpsimd")

            H_a(0)
            for n in range(NT):
                M_a(n)
                if n + 1 < NT:
                    H_a(n + 1)
                if FLAGS.get("reorder", 1):
                    if n >= 1:
                        E2_a(n - 1)
                    E_a(n)
                else:
                    E_a(n)
                    E2_a(n)
            if FLAGS.get("reorder", 1):
                E2_a(NT - 1)
            S.barrier()
            S.emit()

        with contextlib.ExitStack() as ph:
            S.stack = ph
            NCB = 3912 - 2048
            wbf = S.sb("wbfB", [128, 16, NCB], BF16)
            posT = S.sb("posT", [128, NT], F32)
            sin_t = S.sb("sin_t", [128, NT, 32], F32)
            cos_t = S.sb("cos_t", [128, NT, 32], F32)
            wukv = S.sb("wukv", [128, 2, 128], BF16)
            kig_bc = S.sb("kig_bc", [128, 64], F32)
            ph2 = contextlib.ExitStack()
            S.stack = ph2
            stg = [S.sb(f"stgB{i}", [128, 16, 256], F32) for i in range(2)]
            posR = S.sb("posR", [NT, 128], I32)
            posF = S.sb("posF", [NT, 128], F32)
            fr = S.sb("fr", [128, 32], F32)
            ang = S.sb("ang", [128, NT, 32], F32)
            rr = S.sb("rr", [128, NT, 32], F32)
            rq = S.sb("rq", [128, NT, 32], F32)
            rni = S.sb("rni", [128, NT, 32], I32)
            stkv = S.sb("stkv", [128, 2, 128], F32)
            win_v = win_d.ap().rearrange("(k p) c -> p k c", p=128)
            load_w_bf(wbf, lambda c0, cw: win_v[:, :, 2048 + c0:2048 + c0 + cw], NCB, stg)
            ld(posR[:], pos_d.ap().rearrange("(n p) -> n p", p=128), [DV["in"]], [posR])
            cp(posF[:], posR[:], [posR], [posF])
            tr(B[7][:, 0:NT], posF[0:NT, :], ident[0:NT, 0:NT], [posF, ident], [B[7]])
            cp(posT[:], B[7][:, 0:NT], [B[7]], [posT])
            for i in range(32):
                mset(fr[:, i:i + 1], float(np.float32(10000.0) ** np.float32(-i / 32.0)), [fr], eng="gpsimd")
            tt(ang[:], posT[:].unsqueeze(2).broadcast_to([128, NT, 32]),
               fr[:].unsqueeze(1).broadcast_to([128, NT, 32]), ALU.mult, [posT, fr], [ang])
            TWO_PI = 2.0 * math.pi
            C1 = 6.28125
            C2 = TWO_PI - C1
            for (dst, shift) in ((sin_t, 0.0), (cos_t, math.pi / 2)):
                ts(rq[:], ang[:], shift, ALU.add, [ang], [rq])
                ts(rr[:], rq[:], 1.0 / TWO_PI, ALU.mult, [rq], [rr])
                cp(rni[:], rr[:], [rr], [rni])
                cp(rr[:], rni[:], [rni], [rr])
                stt(rq[:], rr[:], -C1, rq[:], ALU.mult, ALU.add, [rr, rq], [rq])
                stt(rq[:], rr[:], -C2, rq[:], ALU.mult, ALU.add, [rr, rq], [rq])
                ts(rr[:], rq[:], math.pi, ALU.is_gt, [rq], [rr])
                stt(rq[:], rr[:], -TWO_PI, rq[:], ALU.mult, ALU.add, [rr, rq], [rq])
                ts(rr[:], rq[:], -math.pi, ALU.is_lt, [rq], [rr])
                stt(rq[:], rr[:], TWO_PI, rq[:], ALU.mult, ALU.add, [rr, rq], [rq])
                ts(rq[:], rq[:], 3.141592, ALU.min, [rq], [rq], s2=-3.141592, op1=ALU.max)
                act(dst[:], rq[:], AF.Sin, [rq], [dst])
            ld(stkv[:, :, 0:64], wuk_d.ap().rearrange("(c p) d -> p c d", p=128), [DV["in"]], [stkv])
            ld(stkv[:, :, 64:128], wuv_d.ap().rearrange("(c p) d -> p c d", p=128), [DV["in"]], [stkv])
            for c2 in range(2):
                ts(wukv[:, c2, :], stkv[:, c2, :], gT[:, 48 + c2:49 + c2], ALU.mult, [stkv, gT], [wukv])
            ld(kig_bc[:], kig_d.ap().partition_broadcast(128), [DV["in"]], [kig_bc])
            S.barrier()
            S.emit()
            ph2.close()
            S.stack = ph
            xts = [S.sb(f"xtB{i}", [128, D], F32) for i in range(2)]
            xns = [S.sb(f"xnB{i}", [128, D], BF16) for i in range(2)]
            hTs = [S.subs(S.sb(f"hTB{i}", [128, 16, 128], BF16), 16, "hT") for i in range(2)]
            qrs = [S.sb(f"qr{i}", [128, 1024], BF16) for i in range(2)]
            qirs = [S.sb(f"qir{i}", [128, 512], BF16) for i in range(2)]
            qf = S.subs(S.sb("qf", [128, 1024], F32), 2)
            c4f = S.sb("c4f", [128, 512], F32)
            c5f = S.sb("c5f", [128, 328], F32)
            t1 = S.sb("t1", [128, 512], F32)
            t2 = S.sb("t2", [128, 512], F32)
            cw_ = S.sb("cw_", [128, 8, 32], F32)
            sw_ = S.sb("sw_", [128, 8, 32], F32)
            wis = S.sb("wis", [128, 8], F32)
            ckvb = S.sb("ckvb", [128, 256], BF16)
            ckvf = S.sb("ckvf", [128, 256], F32)
            kif = S.sb("kif", [128, 64], F32)
            ckvT = S.sb("ckvT", [128, 2, 128], BF16)
            kk = S.sb("kk", [128, 64], F32)
            kk2 = S.sb("kk2", [128, 128], BF16)
            kin = S.sb("kin", [128, 64], F32)
            kir = S.sb("kir", [128, 64], F32)
            kki2 = S.sb("kki2", [128, 128], BF16)
            qTe = S.sb("qTe", [128, 8, 128], BF16)
            qTo = S.sb("qTo", [128, 8, 128], BF16)
            qiTe = S.sb("qiTe", [128, 4, 128], BF16)
            qiTo = S.sb("qiTo", [128, 4, 128], BF16)
            mset(qTe[:], 0.0, [qTe], eng="gpsimd")
            mset(qTo[:], 0.0, [qTo], eng="gpsimd")
            mset(qiTe[:], 0.0, [qiTe], eng="gpsimd")
            mset(qiTo[:], 0.0, [qiTo], eng="gpsimd")
            CSC = (8 ** -0.5) * (64 ** -0.5)

            def rope(o_lo, o_hi, x_lo, x_hi, cs, sn, nh, RB):
                a = t1[:, 0:nh * 32].rearrange("p (h d) -> p h d", d=32)
                b = t2[:, 0:nh * 32].rearrange("p (h d) -> p h d", d=32)
                tt(a, x_lo, cs, ALU.mult, RB, [t1])
                tt(b, x_hi, sn, ALU.mult, RB, [t2])
                tt(o_lo, a, b, ALU.subtract, [t1, t2], RB[-1:], eng="gpsimd")
                tt(a, x_lo, sn, ALU.mult, RB + [t1], [t1])
                tt(b, x_hi, cs, ALU.mult, RB + [t2], [t2])
                tt(o_hi, a, b, ALU.add, [t1, t2], RB[-1:], eng="gpsimd")

            zt = S.sb("zt", [128, D], BF16)
            mset(zt[:], 0.0, [zt], eng="gpsimd")
            zf_total = NSB * C // 512
            zf_done = [0]

            def H_b(n):
                xt = xts[n % 2]
                ld(xt[:], x_d.ap()[n * 128:(n + 1) * 128, :], [DV["in"]], [xt])
                want = (zf_total * (n + 1) + NT - 1) // NT
                while zf_done[0] < min(want, zf_total):
                    r = zf_done[0]
                    ld(xe_s.ap()[r * 512:(r + 1) * 512, :].rearrange("(a p) d -> p a d", p=128),
                       zt[:].unsqueeze(1).broadcast_to([128, 4, D]), [zt], [DV["xe"]], q="sync", owner=zt)
                    zf_done[0] += 1
                prep_hT(xt, xns[n % 2], hTs[n % 2], col[0], col[1], col[2], 0, 16)

            def M_b(n):
                hT = hTs[n % 2]
                widths = [512, 512, 512, NCB - 1536]
                for cg in range(4):
                    for k in range(16):
                        mm(B[2 + cg][:, 0:widths[cg]], hT[:, k, :], wbf[:, k, cg * 512:cg * 512 + widths[cg]],
                           k == 0, k == 15, [hT.sub[k], wbf], [B[2 + cg]])

            def E1_b(n):
                cosb = cos_t[:, n, :].unsqueeze(1)
                sinb = sin_t[:, n, :].unsqueeze(1)
                qr, qir = qrs[n % 2], qirs[n % 2]
                cp(c4f[:], B[4][:], [B[4]], [c4f], eng="scalar")
                cp(c5f[:, 0:328], B[5][:, 0:328], [B[5]], [c5f], eng="scalar")
                cp(qf[:, 0:512], B[2][:], [B[2]], [qf.sub[0]], eng="scalar")
                cp(qf[:, 512:1024], B[3][:], [B[3]], [qf.sub[1]], eng="scalar")
                act(junk[:, 0:256], c4f[:, 0:256], AF.Square, [c4f], [junk, col[3]], acc=col[3][:, 0:1])
                rsqrt_col(col[4][:, 0:1], col[3][:, 0:1], 1.0 / 256, [col[3]], [col[4]], col[5])
                cp(ckvb[:], c4f[:, 0:256], [c4f], [ckvb])
                for c2 in range(2):
                    tr(B[0][:].bitcast(BF16)[:, c2 * 128:(c2 + 1) * 128], ckvb[:, c2 * 128:(c2 + 1) * 128], identb[:],
                       [ckvb, identb], [B[0]])
                cp(ckvT[:], B[0][:].bitcast(BF16)[:, 0:256].rearrange("p (c t) -> p c t", t=128), [B[0]], [ckvT])
                for c2 in range(2):
                    mm(B[1][:, 0:128], ckvT[:, c2, :], wukv[:, c2, :], c2 == 0, c2 == 1, [ckvT, wukv], [B[1]])
                act(junk[:, 0:64], c5f[:, 256:320], AF.Square, [c5f], [junk, col[6]], acc=col[6][:, 0:1])
                rsqrt_col(col[7][:, 0:1], col[6][:, 0:1], 1.0 / 64, [col[6]], [col[7]], col[5])
                stt(kin[:], c5f[:, 256:320], col[7][:, 0:1], kig_bc[:], ALU.mult, ALU.mult,
                    [c5f, col[7], kig_bc], [kin])
                ki3 = kin[:].rearrange("p (h d) -> p h d", d=64)
                kr3 = kir[:].rearrange("p (h d) -> p h d", d=64)
                rope(kr3[:, :, 0:32], kr3[:, :, 32:64], ki3[:, :, 0:32], ki3[:, :, 32:64], cosb, sinb, 1,
                     [kin, cos_t, sin_t, kir])
                cp(kki2[:, 0:64], kir[:], [kir], [kki2])
                cp(kki2[:, 64:128], kir[:], [kir], [kki2])
                tr(B[0][:].bitcast(BF16)[:, 384:512], kki2[:], identb[:], [kki2, identb], [B[0]])
                cp(kiT2[:, n * 128:(n + 1) * 128], B[0][:].bitcast(BF16)[:, 384:512], [B[0]], [kiT2])
                ts(vaug[:, n, 0:64], B[1][:, 64:128], col[4][:, 0:1], ALU.mult, [B[1], col[4]], [vaug])
                kv3 = B[1][:, 0:64].rearrange("p (h d) -> p h d", d=64)
                kk3 = kk[:].rearrange("p (h d) -> p h d", d=64)
                rope(kk3[:, :, 0:32], kk3[:, :, 32:64], kv3[:, :, 0:32], kv3[:, :, 32:64], cosb, sinb, 1,
                     [B[1], cos_t, sin_t, kk])
                ts(kk2[:, 0:64], kk[:], col[4][:, 0:1], ALU.mult, [kk, col[4]], [kk2])
                ts(kk2[:, 64:128], kk[:], col[4][:, 0:1], ALU.mult, [kk, col[4]], [kk2])
                tr(B[0][:].bitcast(BF16)[:, 256:384], kk2[:], identb[:], [kk2, identb], [B[0]])
                cp(kT2[:, n * 128:(n + 1) * 128], B[0][:].bitcast(BF16)[:, 256:384], [B[0]], [kT2])
                ts(wis[:], c5f[:, 320:328], CSC, ALU.mult, [c5f], [wis])
                ts(sgn_all[:, n, :], c5f[:, 320:328], 0.0, ALU.is_ge, [c5f], [sgn_all], s2=2.0, op1=ALU.mult)
                ts(sgn_all[:, n, :], sgn_all[:, n, :], -1.0, ALU.add, [sgn_all], [sgn_all])
                tt(cw_[:], cosb.broadcast_to([128, 8, 32]), wis[:].unsqueeze(2).broadcast_to([128, 8, 32]),
                   ALU.mult, [cos_t, wis], [cw_])
                tt(sw_[:], sinb.broadcast_to([128, 8, 32]), wis[:].unsqueeze(2).broadcast_to([128, 8, 32]),
                   ALU.mult, [sin_t, wis], [sw_])
                for hb in range(2):
                    qv = qf[:, hb * 512:(hb + 1) * 512].rearrange("p (h d) -> p h d", d=64)
                    ov = qr[:, hb * 512:(hb + 1) * 512].rearrange("p (h d) -> p h d", d=64)
                    rope(ov[:, :, 0:32], ov[:, :, 32:64], qv[:, :, 0:32], qv[:, :, 32:64],
                         cosb.broadcast_to([128, 8, 32]), sinb.broadcast_to([128, 8, 32]), 8,
                         [qf.sub[hb], cos_t, sin_t, qr])
                for hb in range(2):
                    src = c4f[:, 256:512] if hb == 0 else c5f[:, 0:256]
                    qv = src.rearrange("p (h d) -> p h d", d=64)
                    ov = qir[:, hb * 256:(hb + 1) * 256].rearrange("p (h d) -> p h d", d=64)
                    rope(ov[:, :, 0:32], ov[:, :, 32:64], qv[:, :, 0:32], qv[:, :, 32:64],
                         cw_[:, hb * 4:(hb + 1) * 4, :], sw_[:, hb * 4:(hb + 1) * 4, :], 4,
                         [c4f if hb == 0 else c5f, cw_, sw_, qir])

            def E2_b(n):
                qr, qir = qrs[n % 2], qirs[n % 2]
                b6 = B[6][:].bitcast(BF16)
                for pr in range(8):
                    tr(b6[:, pr * 128:(pr + 1) * 128], qr[:, pr * 128:(pr + 1) * 128], identb[:], [qr, identb], [B[6]])
                b63 = b6.rearrange("p (a t) -> p a t", t=128)
                cp(qTe[0:64, :, :], b63[0:64, :, :], [B[6]], [qTe])
                cp(qTo[64:128, :, :], b63[64:128, :, :], [B[6]], [qTo])
                b7 = B[7][:].bitcast(BF16)
                for pr in range(4):
                    tr(b7[:, pr * 128:(pr + 1) * 128], qir[:, pr * 128:(pr + 1) * 128], identb[:], [qir, identb], [B[7]])
                b73 = b7[:, 0:512].rearrange("p (a t) -> p a t", t=128)
                cp(qiTe[0:64, :, :], b73[0:64, :, :], [B[7]], [qiTe], eng="scalar")
                cp(qiTo[64:128, :, :], b73[64:128, :, :], [B[7]], [qiTo], eng="scalar")
                ld(qTe_s.ap()[n], qTe[:].rearrange("p a t -> p (a t)"), [qTe], [DV["qTe"]], q="gpsimd")
                ld(qTo_s.ap()[n], qTo[:].rearrange("p a t -> p (a t)"), [qTo], [DV["qTo"]], q="gpsimd")
                ld(qiTe_s.ap()[n], qiTe[:].rearrange("p a t -> p (a t)"), [qiTe], [DV["qiTe"]], q="gpsimd")
                ld(qiTo_s.ap()[n], qiTo[:].rearrange("p a t -> p (a t)"), [qiTo], [DV["qiTo"]], q="gpsimd")

            H_b(0)
            for n in range(NT):
                M_b(n)
                if n + 1 < NT:
                    H_b(n + 1)
                if n >= 1:
                    E2_b(n - 1)
                E1_b(n)
            E2_b(NT - 1)
            S.barrier()
            S.emit()

        with contextlib.ExitStack() as ph:
            S.stack = ph
            woutb = S.sb("woutb", [128, 16, D], BF16)
            with contextlib.ExitStack() as ph2:
                S.stack = ph2
                stg = [S.sb(f"stgO{i}", [128, 16, 256], F32) for i in range(2)]
                gate1_bc = S.sb("gate1_bc", [128, D], F32)
                ld(gate1_bc[:], mod_s.ap()[2 * D:3 * D].partition_broadcast(128), [DV["mod"]], [gate1_bc])
                wout_v = wout_d.ap().rearrange("(k p) c -> p k c", p=128)
                rowscale_bufs.clear()
                rowscale_bufs.extend([gT, gate1_bc])
                load_w_bf(woutb, lambda c0, cw: wout_v[:, :, c0:c0 + cw], D, stg, rowscale=gT[:, 32:48],
                          colscale=gate1_bc)
                S.barrier()
                S.emit()
            S.stack = ph
            score = S.sb("score", [128, ST], F32)
            NMs = [S.sb(f"NM{i}", [128, ST], BF16) for i in range(2)]
            qTe = [S.sb(f"qTeL{i}", [128, 1024], BF16) for i in range(2)]
            qTo = [S.sb(f"qToL{i}", [128, 1024], BF16) for i in range(2)]
            qiTe = [S.sb(f"qiTeL{i}", [128, 512], BF16) for i in range(2)]
            qiTo = [S.sb(f"qiToL{i}", [128, 512], BF16) for i in range(2)]
            pT = [S.sb(f"pT{i}", [128, 512], BF16) for i in range(3)]
            xt = S.sb("xtC", [128, D], F32)
            yanl = S.sb("yanl", [128, 1024], BF16)
            yb = S.sb("yb", [128, 1024], F32)
            ybn = S.sb("ybn", [128, 1024], BF16)
            yT = S.subs(S.sb("yT", [128, 16, 128], BF16), 2, "yT")
            xm = S.sb("xm", [128, D], F32)
            xn2b = S.sb("xn2b", [128, D], BF16)
            h2T = S.subs(S.sb("h2T", [128, 16, 128], F32), 16)
            wr = S.sb("wr", [128, 16, 36], F32)
            bias_bc = S.sb("bias_bc", [128, 36], F32)
            lg = S.sb("lg", [128, 36], F32)
            em = S.sb("em", [128, 32], F32)
            m8 = S.sb("m8", [128, 8], F32)
            i8 = S.sb("i8", [128, 8], U32)
            Ab = S.sb("Ab", [128, 32], BF16)
            oh0 = S.sb("oh0", [128, 32], F32)
            oh1 = S.sb("oh1", [128, 32], F32)
            posf = S.sb("posf", [128, 32], F32)
            tmp32 = S.sb("tmp32", [128, 32], F32)
            sm = S.sb("sm", [128, 32], F32)
            lo = S.sb("lo", [128, 1], F32)
            w0c = S.sb("w0c", [128, 1], F32)
            mid = S.sb("mid", [128, 1], F32)
            cnt = S.sb("cnt", [128, 1], F32)
            gei = S.sb("gei", [128, 1], U32)
            thr = S.sb("thr", [128, 1], F32)
            rden = S.sb("rden", [128, 16], F32)
            destf = S.sb("destf", [128, 2], F32)

            ld(wr[:], wr_d.ap().rearrange("(k p) c -> p k c", p=128), [DV["in"]], [wr])
            ld(bias_bc[:], br_d.ap().partition_broadcast(128), [DV["in"]], [bias_bc])
            mset(base_bc[:], 0.0, [base_bc])

            def stage_A(n):
                Sk = (n + 1) * 128
                qe, qo, qie, qio = qTe[n % 2], qTo[n % 2], qiTe[n % 2], qiTo[n % 2]
                NM = NMs[n % 2]
                ld(qie[:], qiTe_s.ap()[n], [DV["qiTe"]], [qie])
                ld(qio[:], qiTo_s.ap()[n], [DV["qiTo"]], [qio])
                ld(qe[:], qTe_s.ap()[n], [DV["qTe"]], [qe])
                ld(qo[:], qTo_s.ap()[n], [DV["qTo"]], [qo])
                bi = 0
                for ks in range(0, Sk, 512):
                    ke = min(Sk, ks + 512)
                    for h in range(8):
                        bk = B[bi % 2]
                        bi += 1
                        src = (qie if h % 2 == 0 else qio)[:, (h // 2) * 128:(h // 2 + 1) * 128]
                        mm(bk[:, 0:ke - ks], src, kiT2[:, ks:ke], True, True, [qie, qio, kiT2], [bk])
                        sg = sgn_all[:, n, h:h + 1]
                        act(bk[:, 0:ke - ks], bk[:, 0:ke - ks], AF.Relu, [bk, sgn_all], [bk], scale=sg)
                        if h == 0:
                            ts(score[:, ks:ke], bk[:, 0:ke - ks], sg, ALU.mult, [bk, sgn_all], [score])
                        else:
                            stt(score[:, ks:ke], bk[:, 0:ke - ks], sg, score[:, ks:ke], ALU.mult, ALU.add,
                                [bk, sgn_all, score], [score])
                mset(score[0:64, Sk - 64:Sk], -1.0e30, [score])
                if n < 2:
                    mset(thr[:], -1.0e29, [thr])
                else:
                    S.op("vector", (lambda Sk: lambda e: e.tensor_reduce(out=lo[:], in_=score[:, 0:Sk - 64], axis=AX.X,
                                                                         op=ALU.min))(Sk), [score], [lo])
                    S.op("vector", (lambda Sk: lambda e: e.tensor_reduce(out=w0c[:], in_=score[:, 0:Sk], axis=AX.X,
                                                                         op=ALU.max))(Sk), [score], [w0c])
                    stt(w0c[:], w0c[:], 1.0e-4, lo[:], ALU.add, ALU.subtract, [w0c, lo], [w0c])
                    for it in range(NITER):
                        stt(mid[:], w0c[:], 2.0 ** -(it + 1), lo[:], ALU.mult, ALU.add, [w0c, lo], [mid])
                        ts(NM[:, 0:Sk], score[:, 0:Sk], mid[:, 0:1], ALU.is_ge, [score, mid], [NM, cnt],
                           s2=0.0, op1=ALU.add, acc=cnt[:, 0:1])
                        ts(gei[:], cnt[:], 255.5, ALU.is_ge, [cnt], [gei])
                        S.op("vector", lambda e: e.copy_predicated(lo[:], gei[:], mid[:]), [gei, mid, lo], [lo])
                    cp(thr[:], lo[:], [lo], [thr])
                ts(NM[:, 0:Sk], score[:, 0:Sk], thr[:, 0:1], ALU.is_lt, [score, thr], [NM], s2=-30000.0, op1=ALU.mult)

            def stage_B(n):
                Sk = (n + 1) * 128
                qe, qo = qTe[n % 2], qTo[n % 2]
                NM = NMs[n % 2]
                ld(xt[:], x_d.ap()[n * 128:(n + 1) * 128, :], [DV["in"]], [xt])
                ld(yanl[:], ya_s.ap()[n * 128:(n + 1) * 128, :], [DV["ya"]], [yanl])
                for b3 in range(3):
                    mm(B[4 + b3][:], zerob[:, 0:128], zerob[:], True, False, [zerob], [B[4 + b3]], skip=True)
                units = [(kb, j) for kb in range(n + 1) for j in range(4)]

                def QK(u):
                    kb, j = units[u]
                    bk = B[2 + u % 2]
                    pt = pT[u % 3]
                    qsrc = (qe if j < 2 else qo)[:, (j % 2) * 512:(j % 2 + 1) * 512]
                    mm(bk[:], kT2[:, kb * 128:(kb + 1) * 128], qsrc, True, False, [kT2, qe, qo], [bk])
                    mm(bk[:], NM[:, kb * 128:(kb + 1) * 128],
                       identb[:].unsqueeze(1).broadcast_to([128, 4, 128]), False, True, [NM, identb], [bk])
                    act(pt[:], bk[:], AF.Exp, [bk], [pt], scale=0.125)

                def PV(u):
                    kb, j = units[u]
                    pt = pT[u % 3]
                    for hh in range(4):
                        pair = (j % 2) * 4 + hh
                        head = pair * 2 + (0 if j < 2 else 1)
                        ob = B[4 + head // 7]
                        off = (head % 7) * 65
                        mm(ob[:, off:off + 65], pt[:, hh * 128:(hh + 1) * 128], vaug[:, kb, :], False,
                           kb == n, [pt, vaug], [ob], skip=True)

                QK(0)
                for u in range(len(units)):
                    if u + 1 < len(units):
                        QK(u + 1)
                    PV(u)
                for b3 in range(3):
                    nh = 7 if b3 < 2 else 2
                    ov = B[4 + b3][:, 0:nh * 65].rearrange("p (h d) -> p h d", d=65)
                    S.op("vector", (lambda ov, b3, nh: lambda e: e.reciprocal(
                        rden[:, b3 * 7:b3 * 7 + nh].unsqueeze(2), ov[:, :, 64:65]))(ov, b3, nh), [B[4 + b3]], [rden])
                    tt(yb[:, b3 * 448:b3 * 448 + nh * 64].rearrange("p (h d) -> p h d", d=64), ov[:, :, 0:64],
                       rden[:, b3 * 7:b3 * 7 + nh].unsqueeze(2).broadcast_to([128, nh, 64]), ALU.mult,
                       [B[4 + b3], rden], [yb])
                act(junk[:, 0:1024], yb[:], AF.Square, [yb], [junk, col[0]], acc=col[0][:, 0:1])
                rsqrt_col(col[1][:, 0:1], col[0][:, 0:1], 1.0 / 1024, [col[0]], [col[1]], col[2])
                ts(ybn[:], yb[:], col[1][:, 0:1], ALU.mult, [yb, col[1]], [ybn])
                for k in range(16):
                    bk = B[2 + k // 8]
                    srcy = yanl[:, k * 128:(k + 1) * 128] if k < 8 else ybn[:, (k - 8) * 128:(k - 7) * 128]
                    tr(bk[:].bitcast(BF16)[:, (k % 8) * 128:(k % 8 + 1) * 128], srcy, identb[:],
                       [yanl, ybn, identb], [bk])
                cp(yT[:, 0:8, :], B[2][:].bitcast(BF16).rearrange("p (a t) -> p a t", t=128), [B[2]], [yT.sub[0]])
                cp(yT[:, 8:16, :], B[3][:].bitcast(BF16).rearrange("p (a t) -> p a t", t=128), [B[3]], [yT.sub[1]],
                   eng="scalar")
                for db in range(4):
                    bk = B[(7, 4, 5, 6)[db]]
                    for k in range(16):
                        mm(bk[:], yT[:, k, :], woutb[:, k, db * 512:(db + 1) * 512], k == 0, k == 15,
                           [yT.sub[k // 8], woutb], [bk])
                    tt(xm[:, db * 512:(db + 1) * 512], bk[:], xt[:, db * 512:(db + 1) * 512], ALU.add, [bk, xt], [xm])
                ld(xmid_s.ap()[n * 128:(n + 1) * 128, :], xm[:], [xm], [DV["xmid"]], q="gpsimd")
                act(junk[:], xm[:], AF.Square, [xm], [junk, col[3]], acc=col[3][:, 0:1])
                rsqrt_col(col[4][:, 0:1], col[3][:, 0:1], 1.0 / D, [col[3]], [col[4]], col[5])
                ts(xt[:], xm[:], col[4][:, 0:1], ALU.mult, [xm, col[4]], [xt])
                act(xn2b[:], xm[:], AF.Copy, [xm, col[4]], [xn2b], scale=col[4][:, 0:1])
                for g4 in range(4):
                    bk = B[2 + g4 % 2]
                    for k in range(g4 * 4, g4 * 4 + 4):
                        tr(bk[:, (k % 4) * 128:(k % 4 + 1) * 128], xt[:, k * 128:(k + 1) * 128], ident[:],
                           [xt, ident], [bk])
                    for k in range(g4 * 4, g4 * 4 + 4):
                        if g4 % 2 == 0:
                            ts(h2T[:, k, :], bk[:, (k % 4) * 128:(k % 4 + 1) * 128], pvec[:, 32 + k:33 + k], ALU.mult,
                               [bk, pvec], [h2T.sub[k]], s2=pvec[:, 48 + k:49 + k], op1=ALU.add)
                        else:
                            act(h2T[:, k, :], bk[:, (k % 4) * 128:(k % 4 + 1) * 128], AF.Identity, [bk, pvec],
                                [h2T.sub[k]], bias=pvec[:, 48 + k:49 + k], scale=pvec[:, 32 + k:33 + k])
                for k in range(16):
                    mm(B[7][:, 0:36], h2T[:, k, :], wr[:, k, :], k == 0, k == 15, [h2T.sub[k], wr], [B[7]])
                tt(lg[:], B[7][:, 0:36], bias_bc[:], ALU.add, [B[7], bias_bc], [lg])
                S.op("vector", lambda e: e.tensor_reduce(out=sm[:, 0:1], in_=lg[:, 0:4], axis=AX.X, op=ALU.max), [lg], [sm])
                ts(sm[:, 1:2], sm[:, 0:1], -1.0, ALU.mult, [sm], [sm])
                act(sm[:, 4:8], lg[:, 0:4], AF.Exp, [lg, sm], [sm, col[6]], bias=sm[:, 1:2], scale=1.0,
                    acc=col[6][:, 0:1])
                S.op("vector", lambda e: e.reciprocal(sm[:, 2:3], col[6][:, 0:1]), [col[6]], [sm])
                ts(sm[:, 8:12], lg[:, 0:4], sm[:, 0:1], ALU.is_ge, [lg, sm], [sm], s2=1.0e9, op1=ALU.mult)
                ts(sm[:, 8:12], sm[:, 8:12], -1.0e9, ALU.add, [sm], [sm])
                tt(em[:].rearrange("p (g j) -> p g j", j=8), lg[:, 4:36].rearrange("p (g j) -> p g j", j=8),
                   sm[:, 8:12].unsqueeze(2).broadcast_to([128, 4, 8]), ALU.add, [lg, sm], [em])
                S.op("vector", lambda e: e.max(m8[:], em[:]), [em], [m8])
                S.op("vector", lambda e: e.max_index(i8[:], m8[:], em[:]), [m8, em], [i8])
                tt(sm[:, 12:13], m8[:, 1:2], m8[:, 0:1], ALU.subtract, [m8], [sm])
                act(sm[:, 13:14], sm[:, 12:13], AF.Exp, [sm], [sm])
                ts(sm[:, 13:14], sm[:, 13:14], 1.0, ALU.add, [sm], [sm])
                S.op("vector", lambda e: e.reciprocal(sm[:, 14:15], sm[:, 13:14]), [sm], [sm])
                tt(wgt_all[:, n, 0:1], sm[:, 14:15], sm[:, 2:3], ALU.mult, [sm], [wgt_all])
                tt(wgt_all[:, n, 1:2], sm[:, 2:3], wgt_all[:, n, 0:1], ALU.subtract, [sm, wgt_all], [wgt_all])
                ts(Ab[:], em[:], m8[:, 1:2], ALU.is_ge, [em, m8], [Ab])
                ts(oh0[:], em[:], m8[:, 0:1], ALU.is_ge, [em, m8], [oh0])
                tt(oh1[:], Ab[:], oh0[:], ALU.subtract, [Ab, oh0], [oh1])
                mm(B[4][:, 0:32], LTb[:], Ab[:], True, True, [LTb, Ab], [B[4]])
                tt(posf[:], B[4][:, 0:32], base_bc[:], ALU.add, [B[4], base_bc], [posf])
                mm(B[4][:, 64:96], onesb[:], Ab[:], True, True, [onesb, Ab], [B[4]])
                tt(base_bc[:], B[4][:, 64:96], base_bc[:], ALU.add, [B[4], base_bc, posf], [base_bc])
                cp(eid_all[:, n, :], i8[:, 0:2], [i8], [eid_all])
                for j, oh in enumerate((oh0, oh1)):
                    tt(tmp32[:], posf[:], oh[:], ALU.mult, [posf, oh], [tmp32])
                    S.op("vector", (lambda n, j: lambda e: e.tensor_reduce(out=pos_all[:, n, j:j + 1], in_=tmp32[:],
                                                                            axis=AX.X, op=ALU.add))(n, j),
                         [tmp32], [pos_all])
                ld(xn2_s.ap()[n * 128:(n + 1) * 128, :], xn2b[:], [xn2b], [DV["xn2"]], q="gpsimd")

            stage_A(0)
            for n in range(NT):
                if n + 1 < NT:
                    stage_A(n + 1)
                stage_B(n)
            S.barrier()
            S.emit()

        with contextlib.ExitStack() as ph:
            S.stack = ph
            pa = S.sb("pa", [128, 32], F32)
            pb = S.sb("pb", [128, 32], F32)
            pi_ = S.sb("pi_", [128, 32], I32)
            padded = S.sb("padded", [128, 32], F32)
            pstart = S.sb("pstart", [128, 32], F32)
            iotI = S.sb("iotI", [128, NSB], I32)
            iotF = S.sb("iotF", [128, NSB], F32)
            cmp3 = S.sb("cmp3", [128, NSB, 32], F32)
            bef = S.sb("bef", [128, NSB], F32)
            iw12 = S.sb("iw12", [128, 12], I32)
            iw12f = S.sb("iw12f", [128, 12], F32)
            widxf = S.sb("widxf", [128, NSB, 12], F32)
            ohd = S.sb("ohd", [128, 32], F32)
            dcol = S.sb("dcol", [128, 2], F32)
            xr2 = [S.sb(f"xr2_{i}", [128, D], BF16) for i in range(2)]
            ts(pa[:], base_bc[:], float(C - 1), ALU.add, [base_bc], [pa], s2=1.0 / C, op1=ALU.mult)
            cp(pi_[:], pa[:], [pa], [pi_])
            cp(pb[:], pi_[:], [pi_], [pb])
            tt(pa[:], pb[:], pa[:], ALU.is_gt, [pb, pa], [pa])
            tt(pb[:], pb[:], pa[:], ALU.subtract, [pb, pa], [pb])
            ts(padded[:], pb[:], float(C), ALU.mult, [pb], [padded])
            cp(pa[:], padded[:], [padded], [pa])
            src_, dst_ = pa, pb
            for sh in (1, 2, 4, 8, 16):
                cp(dst_[:, 0:sh], src_[:, 0:sh], [src_], [dst_])
                tt(dst_[:, sh:32], src_[:, sh:32], src_[:, 0:32 - sh], ALU.add, [src_], [dst_])
                src_, dst_ = dst_, src_
            pend = src_
            tt(pstart[:], pend[:], padded[:], ALU.subtract, [pend, padded], [pstart])
            S.op("gpsimd", lambda e: e.iota(iotI[:], [[C, NSB]], base=0, channel_multiplier=0), [], [iotI])
            cp(iotF[:], iotI[:], [iotI], [iotF])
            tt(cmp3[:], pend[:].unsqueeze(1).broadcast_to([128, NSB, 32]),
               iotF[:].unsqueeze(2).broadcast_to([128, NSB, 32]), ALU.is_le, [pend, iotF], [cmp3])
            S.op("vector", lambda e: e.tensor_reduce(out=bef[:], in_=cmp3[:], axis=AX.X, op=ALU.add), [cmp3], [bef])
            ts(bef[:], bef[:], 31.0, ALU.min, [bef], [bef], s2=1536.0, op1=ALU.mult)
            S.op("gpsimd", lambda e: e.iota(iw12[:], [[128, 12]], base=0, channel_multiplier=1), [], [iw12])
            cp(iw12f[:], iw12[:], [iw12], [iw12f])
            tt(widxf[:], bef[:].unsqueeze(2).broadcast_to([128, NSB, 12]),
               iw12f[:].unsqueeze(1).broadcast_to([128, NSB, 12]), ALU.add, [bef, iw12f], [widxf])
            cp(widx[:], widxf[:], [widxf], [widx])
            S.op("gpsimd", lambda e: e.iota(pi_[:], [[1, 32]], base=0, channel_multiplier=0), [pi_], [pi_])
            cp(pa[:], pi_[:], [pi_], [pa])
            for n in range(NT):
                for j in range(2):
                    ts(ohd[:], pa[:], eid_all[:, n, j:j + 1], ALU.is_equal, [pa, eid_all], [ohd])
                    tt(ohd[:], ohd[:], pstart[:], ALU.mult, [ohd, pstart], [ohd])
                    S.op("vector", (lambda j: lambda e: e.tensor_reduce(out=dcol[:, j:j + 1], in_=ohd[:], axis=AX.X,
                                                                         op=ALU.add))(j), [ohd], [dcol])
                tt(dcol[:], dcol[:], pos_all[:, n, :], ALU.add, [dcol, pos_all], [dcol])
                cp(dest_all[:, n, :], dcol[:], [dcol], [dest_all])
                xr = xr2[n % 2]
                ld(xr[:], xn2_s.ap()[n * 128:(n + 1) * 128, :], [DV["xn2"]], [xr])
                for j in range(2):
                    S.dma("gpsimd", (lambda n, j, xr: lambda e: e.indirect_dma_start(
                        out=xe_s.ap(), out_offset=bass.IndirectOffsetOnAxis(ap=dest_all[:, n, j:j + 1], axis=0),
                        in_=xr[:], in_offset=None, bounds_check=r_slots, oob_is_err=False))(n, j, xr),
                        [xr, dest_all, DV["xe"]], [DV["xe"]], owner=xr)
            S.barrier()
            S.emit()

        with contextlib.ExitStack() as ph:
            S.stack = ph
            NB = C // 128
            stg = [S.sb(f"stgE{i}", [128, 4096], F32) for i in range(4)]
            wpb = [S.subs(S.sb(f"wpb{i}", [128, 4096], BF16), 2, "p3") for i in range(3)]
            xer1 = [S.sb(f"xer_{b}", [128, D], BF16) for b in range(NB)]
            xer = [xer1, xer1]
            xeT = S.subs(S.sb("xeT", [128, 16, C], BF16), 16, "p3")
            gTt = S.sb("gTt", [128, 8, C], BF16)
            sa = [S.sb(f"sa{i}", [128, C], F32) for i in range(2)]
            yo = [S.subs(S.sb(f"yo{i}", [128, D], F32), 4) for i in range(NB)]
            wexp_rows = wexp_d.ap().rearrange("e q p c -> (e q p) c")
            pieces = [(j, p) for j in range(NSB) for p in range(12)]
            rot = [0]

            def nbank():
                bk = B[2 + rot[0] % 6]
                rot[0] += 1
                return bk

            def emit_gather(i):
                j, piece = pieces[i]
                sg = stg[i % 4]
                S.dma("gpsimd", (lambda sg, j, piece: lambda e: e.indirect_dma_start(
                    out=sg[:], out_offset=None, in_=wexp_rows,
                    in_offset=bass.IndirectOffsetOnAxis(ap=widx[:, j, piece:piece + 1], axis=0),
                    bounds_check=r_wrows, oob_is_err=False))(sg, j, piece), [DV["in"], widx], [sg], owner=sg)

            def emit_cast(i):
                sg = stg[i % 4]
                wb = wpb[i % 3]
                cp(wb[:, 0:2048], sg[:, 0:2048], [sg], [wb.sub[0]], eng="vector")
                cp(wb[:, 2048:4096], sg[:, 2048:4096], [sg], [wb.sub[1]], eng="scalar")

            def load_xe(j):
                for blk in range(NB):
                    xr = xer[j % 2][blk]
                    ld(xr[:], xe_s.ap()[j * C + blk * 128:j * C + (blk + 1) * 128, :], [DV["xe"]], [xr])

            def prologue_part(j, part):
                for kp in (2 * part, 2 * part + 1):
                    bk = B[kp % 2]
                    bv = bk[:].bitcast(BF16)
                    for kk in range(2):
                        k = kp * 2 + kk
                        for blk in range(NB):
                            xr = xer[j % 2][blk]
                            tr(bv[:, (kk * NB + blk) * 128:(kk * NB + blk + 1) * 128], xr[:, k * 128:(k + 1) * 128],
                               identb[:], [xr, identb], [bk])
                    for kk in range(2):
                        k = kp * 2 + kk
                        src = bv[:, kk * C:(kk + 1) * C]
                        if kp % 2 == 0:
                            ts(xeT[:, k, :], src, pvec[:, 32 + k:33 + k], ALU.mult, [bk, pvec], [xeT.sub[k]],
                               s2=pvec[:, 48 + k:49 + k], op1=ALU.add)
                        else:
                            act(xeT[:, k, :], src, AF.Identity, [bk, pvec], [xeT.sub[k]],
                                bias=pvec[:, 48 + k:49 + k], scale=pvec[:, 32 + k:33 + k])

            yoi = [0]

            def compute(i):
                j, piece = pieces[i]
                wb = wpb[i % 3]
                if piece < 8:
                    f = piece
                    w13 = wb[:].rearrange("p (t k c) -> p t k c", t=2, k=16)
                    ba, bb = nbank(), nbank()
                    for k in range(16):
                        mm(ba[:, 0:C], w13[:, 0, k, :], xeT[:, k, :], k == 0, k == 15, [wb.sub[0], xeT.sub[k]], [ba])
                    for k in range(16):
                        mm(bb[:, 0:C], w13[:, 1, k, :], xeT[:, k, :], k == 0, k == 15, [wb.sub[1], xeT.sub[k]], [bb])
                    s_ = sa[f % 2]
                    act(s_[:], ba[:, 0:C], AF.Silu, [ba], [s_])
                    tt(gTt[:, f, :], bb[:, 0:C], s_[:], ALU.mult, [bb, s_], [gTt])
                else:
                    db = piece - 8
                    w2v = wb[:].rearrange("p (k c) -> p k c", k=8)
                    for blk in range(NB):
                        bk = nbank()
                        for fc in range(8):
                            mm(bk[:], gTt[:, fc, blk * 128:(blk + 1) * 128], w2v[:, fc, :], fc == 0, fc == 7,
                               [gTt, wb.sub[fc // 4]], [bk])
                        y_ = yo[blk]
                        if blk % 2 == 0:
                            cp(y_[:, db * 512:(db + 1) * 512], bk[:], [bk], [y_.sub[db]])
                        else:
                            cp(y_[:, db * 512:(db + 1) * 512], bk[:], [bk], [y_.sub[db]], eng="scalar")
                        if db == 3:
                            ld(ye_s.ap()[j * C + blk * 128:j * C + (blk + 1) * 128, :], y_[:],
                               list(y_.sub), [DV["ye"]], q="sync", owner=y_)

            load_xe(0)
            emit_gather(0)
            emit_gather(1)
            emit_gather(2)
            emit_cast(0)
            for part in range(4):
                prologue_part(0, part)
            if NSB > 1:
                load_xe(1)
            for i, (j, piece) in enumerate(pieces):
                if piece == 0 and j >= 1 and j + 1 < NSB:
                    load_xe(j + 1)
                if i + 3 < len(pieces):
                    emit_gather(i + 3)
                if i + 1 < len(pieces):
                    emit_cast(i + 1)
                compute(i)
                if piece >= 8 and j + 1 < NSB:
                    prologue_part(j + 1, piece - 8)
            S.barrier()
            S.emit()

        with contextlib.ExitStack() as ph:
            S.stack = ph
            gate2_bc = S.sb("gate2_bc", [128, D], F32)
            fg_bc = S.sb("fg_bc", [128, D], F32)
            y0 = [S.sb(f"y0_{i}", [128, D], F32) for i in range(2)]
            y1 = [S.sb(f"y1_{i}", [128, D], F32) for i in range(2)]
            xmt = [S.sb(f"xmt{i}", [128, D], F32) for i in range(2)]
            accs = [S.sb(f"acc{i}", [128, D], F32) for i in range(2)]
            ot = [S.sb(f"ot{i}", [128, D], F32) for i in range(2)]
            ld(gate2_bc[:], mod_s.ap()[5 * D:6 * D].partition_broadcast(128), [DV["mod"]], [gate2_bc])
            ld(fg_bc[:], fg_d.ap().partition_broadcast(128), [DV["in"]], [fg_bc])
            for n in range(NT):
                a0, a1, xq, o_ = y0[n % 2], y1[n % 2], xmt[n % 2], ot[n % 2]
                acc = accs[n % 2]
                for j, dst in enumerate((a0, a1)):
                    S.dma("gpsimd", (lambda n, j, dst: lambda e: e.indirect_dma_start(
                        out=dst[:], out_offset=None, in_=ye_s.ap(),
                        in_offset=bass.IndirectOffsetOnAxis(ap=dest_all[:, n, j:j + 1], axis=0),
                        bounds_check=r_slots, oob_is_err=False))(n, j, dst),
                        [DV["ye"], dest_all], [dst], owner=dst)
                ld(xq[:], xmid_s.ap()[n * 128:(n + 1) * 128, :], [DV["xmid"]], [xq])
                act(acc[:], a0[:], AF.Copy, [a0, wgt_all], [acc], scale=wgt_all[:, n, 0:1])
                stt(acc[:], a1[:], wgt_all[:, n, 1:2], acc[:], ALU.mult, ALU.add, [a1, wgt_all, acc], [acc])
                tt(acc[:], acc[:], gate2_bc[:], ALU.mult, [acc, gate2_bc], [acc], eng="gpsimd")
                tt(acc[:], acc[:], xq[:], ALU.add, [acc, xq], [acc])
                act(junk[:], acc[:], AF.Square, [acc], [junk, col[0]], acc=col[0][:, 0:1])
                rsqrt_col(col[1][:, 0:1], col[0][:, 0:1], 1.0 / D, [col[0]], [col[1]], col[2])
                stt(o_[:], acc[:], col[1][:, 0:1], fg_bc[:], ALU.mult, ALU.mult, [acc, col[1], fg_bc], [o_])
                ld(out_d.ap()[n * 128:(n + 1) * 128, :], o_[:], [o_], [DV["out"]], q="sync")
            S.barrier()
            S.emit()
    return nc


_CACHE = {}


def _prep_weights(inp):
    f = lambda a: np.ascontiguousarray(np.asarray(a, dtype=np.float32))
    w1 = np.asarray(inp["w1"], dtype=np.float32)[0]
    w3 = np.asarray(inp["w3"], dtype=np.float32)[0]
    w2 = np.asarray(inp["w2"], dtype=np.float32)[0]
    wexp = np.empty((NEXP, 12, 128, 4096), dtype=np.float32)
    a1 = w1.reshape(NEXP, 16, 128, 8, 128).transpose(0, 3, 2, 1, 4)
    a3 = w3.reshape(NEXP, 16, 128, 8, 128).transpose(0, 3, 2, 1, 4)
    v = wexp[:, 0:8].reshape(NEXP, 8, 128, 2, 16, 128)
    v[:, :, :, 0] = a1
    v[:, :, :, 1] = a3
    a2 = w2.reshape(NEXP, 8, 128, 4, 512).transpose(0, 3, 2, 1, 4)
    wexp[:, 8:12] = a2.reshape(NEXP, 4, 128, 4096)
    shared = {
        "w_ada": f(inp["w_ada"][0]), "b_ada": f(inp["b_ada"][0]), "norm1_g": f(inp["norm1_g"][0]),
        "w_in": f(inp["w_in"][0]), "v_norm_g": f(inp["v_norm_g"][0]).reshape(-1),
        "v_norm_b": f(inp["v_norm_b"][0]).reshape(-1), "w_sp": f(inp["w_sp"][0]), "b_sp": f(inp["b_sp"][0]),
        "kv_norm_g": f(inp["kv_norm_g"][0]), "w_uk": f(inp["w_uk"][0]), "w_uv": f(inp["w_uv"][0]),
        "kidx_norm_g": f(inp["kidx_norm_g"][0]), "gnorm_a_g": f(inp["gnorm_a_g"][0]),
        "gnorm_b_g": f(inp["gnorm_b_g"][0]), "w_out": f(inp["w_out"][0]), "norm2_g": f(inp["norm2_g"][0]),
        "w_r": np.ascontiguousarray(np.concatenate([np.asarray(inp["w_group"][0]), np.asarray(inp["w_expert"][0])],
                                                   axis=1).astype(np.float32)),
        "b_r": np.ascontiguousarray(np.concatenate([np.asarray(inp["b_group"][0]), np.asarray(inp["b_expert"][0])],
                                                   axis=0).astype(np.float32)),
        "wexp": wexp, "final_g": f(inp["final_g"]),
    }
    return shared


def kernel(**inputs):
    x = np.asarray(inputs["x"], dtype=np.float32)
    c = np.asarray(inputs["c"], dtype=np.float32)
    pos = np.asarray(inputs["positions"], dtype=np.int32)
    nb, seq, _ = x.shape
    NT = seq // 128
    key = (NT,)
    if key not in _CACHE:
        _CACHE[key] = build(NT=NT)
    nc = _CACHE[key]
    shared = _prep_weights(inputs)
    in_maps = []
    for b in range(nb):
        m = dict(shared)
        m["x"] = np.ascontiguousarray(x[b])
        m["c"] = np.ascontiguousarray(c[b])
        m["pos"] = np.ascontiguousarray(pos[b])
        in_maps.append(m)
    res = run_bass_kernel_spmd(nc, in_maps, core_ids=list(range(nb)))
    return np.stack([np.asarray(r["out"], dtype=np.float32) for r in res.results], axis=0)
```
